# Optimizing a Trainium2 kernel written in Bass

```python
import math
import jax, jax.numpy as jnp
from jax import lax
import numpy as np

D_MODEL = 2048
BATCH = 4
SEQ = 2048
DEPTH = 4

EPS = 1e-6
D_FF = 5632
MLA_HEADS = 8
MLA_Q_LORA = 512
MLA_KV_LORA = 512
MLA_NOPE = 128
MLA_ROPE = 64
MLA_V = 128
ROPE_BASE = 10000.0
NSA_HEADS = 8
NSA_KV_HEADS = 2
NSA_GROUP = NSA_HEADS // NSA_KV_HEADS
NSA_DK = 192
NSA_DV = 128
CMP_LEN = 32
CMP_STRIDE = 16
CMP_HIDDEN = 256
SLC_LEN = 64
SLC_TOPN = 16
WINDOW = 512
FORCED_SCORE = 1e6
REL_BUCKETS = 32
REL_MAX_DIST = 128
Q_BLOCK = 128
SLC_Q_BLOCK = 64
NEG = -1e30

IN_SPLITS = [MLA_Q_LORA, MLA_KV_LORA, MLA_ROPE,
             NSA_HEADS * NSA_DK,
             NSA_KV_HEADS * NSA_DK, NSA_KV_HEADS * NSA_DV,
             NSA_KV_HEADS * NSA_DK, NSA_KV_HEADS * NSA_DV,
             NSA_KV_HEADS * NSA_DK, NSA_KV_HEADS * NSA_DV,
             NSA_HEADS * 3, 2 * D_MODEL]
IN_OFFSETS = [int(v) for v in np.cumsum(IN_SPLITS)[:-1]]
D_IN = int(sum(IN_SPLITS))

kernel_name = "hybrid_mla_nsa_macaron_block"


def rmsnorm(x, g):
    x32 = x.astype(jnp.float32)
    y = x32 * lax.rsqrt(jnp.mean(x32 * x32, axis=-1, keepdims=True) + EPS)
    return (y * g.astype(jnp.float32)).astype(x.dtype)


def swiglu_half_step(x, pre_g, post_g, w_gate, w_up, w_down):
    u = rmsnorm(x, pre_g)
    y = (jax.nn.silu(u @ w_gate) * (u @ w_up)) @ w_down
    return x + 0.5 * rmsnorm(y, post_g)


def rope(x, positions):
    d = x.shape[-1]
    half = d // 2
    inv = ROPE_BASE ** (-jnp.arange(half, dtype=jnp.float32) * 2.0 / d)
    ang = positions.astype(jnp.float32)[..., None] * inv
    cos = jnp.cos(ang)[:, :, None, :]
    sin = jnp.sin(ang)[:, :, None, :]
    x32 = x.astype(jnp.float32)
    x1, x2 = x32[..., :half], x32[..., half:]
    return jnp.concatenate([x1 * cos - x2 * sin, x1 * sin + x2 * cos], axis=-1).astype(x.dtype)


def rel_bucket(dist):
    n = jnp.maximum(dist, 0)
    max_exact = REL_BUCKETS // 2
    large = max_exact + (jnp.log(jnp.maximum(n, 1).astype(jnp.float32) / max_exact)
                         / math.log(REL_MAX_DIST / max_exact)
                         * (REL_BUCKETS - max_exact)).astype(jnp.int32)
    large = jnp.minimum(large, REL_BUCKETS - 1)
    return jnp.where(n < max_exact, n, large).astype(jnp.int32)


def mla_mixer(c_q, c_kv, k_rope, positions, q_norm_g, w_q_up, kv_norm_g, w_uk, w_uv):
    B, S, _ = c_q.shape
    H = MLA_HEADS
    q = (rmsnorm(c_q, q_norm_g) @ w_q_up).reshape(B, S, H, MLA_NOPE + MLA_ROPE)
    q = jnp.concatenate([q[..., :MLA_NOPE], rope(q[..., MLA_NOPE:], positions)], axis=-1)
    ckv = rmsnorm(c_kv, kv_norm_g)
    k_nope = (ckv @ w_uk).reshape(B, S, H, MLA_NOPE)
    v = (ckv @ w_uv).reshape(B, S, H, MLA_V)
    k_pe = rope(k_rope[:, :, None, :], positions)
    k = jnp.concatenate([k_nope, jnp.broadcast_to(k_pe, (B, S, H, MLA_ROPE))], axis=-1)
    scale = (MLA_NOPE + MLA_ROPE) ** -0.5
    n_blk = S // Q_BLOCK
    q_blocks = q.reshape(B, n_blk, Q_BLOCK, H, -1).transpose(1, 0, 2, 3, 4)
    kpos = jnp.arange(S)

    def block(args):
        qi, i = args
        tq = i * Q_BLOCK + jnp.arange(Q_BLOCK)
        s = jnp.einsum('bqhd,bkhd->bhqk', qi, k).astype(jnp.float32) * scale
        s = jnp.where(kpos[None, :] <= tq[:, None], s, NEG)
        p = jax.nn.softmax(s, axis=-1).astype(v.dtype)
        return jnp.einsum('bhqk,bkhd->bqhd', p, v)

    out = lax.map(block, (q_blocks, jnp.arange(n_blk, dtype=jnp.int32)))
    return out.transpose(1, 0, 2, 3, 4).reshape(B, S, H * MLA_V)


def nsa_mixer(q, k_c, v_c, k_s, v_s, k_w, v_w, gate_logits, positions, rel_bias,
              pe_k, w1_k, w2_k, pe_v, w1_v, w2_v):
    B, S, _ = q.shape
    G, J = NSA_KV_HEADS, NSA_GROUP
    scale = NSA_DK ** -0.5
    q = q.reshape(B, S, G, J, NSA_DK)
    k_c = k_c.reshape(B, S, G, NSA_DK)
    v_c = v_c.reshape(B, S, G, NSA_DV)
    k_s = k_s.reshape(B, S, G, NSA_DK)
    v_s = v_s.reshape(B, S, G, NSA_DV)
    k_w = k_w.reshape(B, S, G, NSA_DK)
    v_w = v_w.reshape(B, S, G, NSA_DV)
    t = jnp.arange(S)

    n_cmp = (S - CMP_LEN) // CMP_STRIDE + 1
    idx = np.arange(n_cmp)[:, None] * CMP_STRIDE + np.arange(CMP_LEN)[None, :]

    def compress(z, pe, w1, w2):
        zb = z[:, idx] + pe[None, None, :, None, :]
        zb = zb.transpose(0, 1, 3, 2, 4).reshape(B, n_cmp, G, -1)
        return jax.nn.silu(zb @ w1) @ w2

    kc = compress(k_c, pe_k, w1_k, w2_k)
    vc = compress(v_c, pe_v, w1_v, w2_v)
    ends = jnp.asarray(idx[:, -1])
    mask_c = ends[None, :] <= t[:, None]
    dist_c = positions[:, :, None] - positions[:, ends][:, None, :]
    bias_c = rel_bias[rel_bucket(dist_c)].astype(jnp.float32)
    bias_c = bias_c.reshape(B, S, n_cmp, G, J).transpose(0, 3, 4, 1, 2)
    s_c = jnp.einsum('bsgjd,bngd->bgjsn', q, kc).astype(jnp.float32) * scale + bias_c
    p_c = jnp.where(mask_c, jax.nn.softmax(jnp.where(mask_c, s_c, NEG), axis=-1), 0.0)
    o_cmp = jnp.einsum('bgjsn,bngd->bsgjd', p_c.astype(vc.dtype), vc)

    n_slc = S // SLC_LEN
    cs = np.arange(n_cmp) * CMP_STRIDE
    ce = cs + CMP_LEN - 1
    bs = np.arange(n_slc) * SLC_LEN
    be = bs + SLC_LEN - 1
    overlap = ((cs[:, None] <= be[None, :]) & (ce[:, None] >= bs[None, :])).astype(np.float32)
    imp = jnp.einsum('bgjsn,nm->bgsm', p_c, jnp.asarray(overlap))
    cur = t // SLC_LEN
    jb = jnp.arange(n_slc)
    valid = jb[None, :] <= cur[:, None]
    forced = valid & ((jb[None, :] == 0) | (jb[None, :] >= cur[:, None] - 1))
    score = jnp.where(forced, FORCED_SCORE, jnp.where(valid, imp, -1.0))
    topn = min(SLC_TOPN, n_slc)
    vals, sel = lax.top_k(score, topn)
    sel_ok = vals >= 0.0

    k_blk = k_s.reshape(B, n_slc, SLC_LEN, G, NSA_DK).transpose(0, 3, 1, 2, 4)
    v_blk = v_s.reshape(B, n_slc, SLC_LEN, G, NSA_DV).transpose(0, 3, 1, 2, 4)
    nq = S // SLC_Q_BLOCK
    q_ch = q.reshape(B, nq, SLC_Q_BLOCK, G, J, NSA_DK).transpose(1, 0, 2, 3, 4, 5)
    sel_ch = sel.reshape(B, G, nq, SLC_Q_BLOCK, topn).transpose(2, 0, 1, 3, 4)
    ok_ch = sel_ok.reshape(B, G, nq, SLC_Q_BLOCK, topn).transpose(2, 0, 1, 3, 4)
    table_g = rel_bias.reshape(REL_BUCKETS, G, J)
    g_idx = jnp.arange(G).reshape(1, G, 1, 1, 1)
    gather = jax.vmap(jax.vmap(lambda blocks, ix: blocks[ix]))
    pos_gather = jax.vmap(lambda p, k: p[k])

    def slc_block(args):
        qi, si, oki, i = args
        tq = i * SLC_Q_BLOCK + jnp.arange(SLC_Q_BLOCK)
        ks = gather(k_blk, si)
        vs = gather(v_blk, si)
        kpos = si[..., None] * SLC_LEN + jnp.arange(SLC_LEN)
        mask = oki[..., None] & (kpos <= tq[None, None, :, None, None])
        qpos = lax.dynamic_slice_in_dim(positions, i * SLC_Q_BLOCK, SLC_Q_BLOCK, axis=1)
        kp = pos_gather(positions, kpos)
        bias = table_g[rel_bucket(qpos[:, None, :, None, None] - kp), g_idx]
        bias = jnp.moveaxis(bias, -1, 2).astype(jnp.float32)
        s = jnp.einsum('bqgjd,bgqnrd->bgjqnr', qi, ks).astype(jnp.float32) * scale + bias
        s = jnp.where(mask[:, :, None], s, NEG)
        shp = s.shape
        p = jax.nn.softmax(s.reshape(shp[:4] + (-1,)), axis=-1).reshape(shp)
        return jnp.einsum('bgjqnr,bgqnrd->bqgjd', p.astype(vs.dtype), vs)

    o_slc = lax.map(slc_block, (q_ch, sel_ch, ok_ch, jnp.arange(nq, dtype=jnp.int32)))
    o_slc = o_slc.transpose(1, 0, 2, 3, 4, 5).reshape(B, S, G, J, NSA_DV)

    span = WINDOW + Q_BLOCK
    kp_w = jnp.pad(k_w, ((0, 0), (WINDOW, 0), (0, 0), (0, 0)))
    vp_w = jnp.pad(v_w, ((0, 0), (WINDOW, 0), (0, 0), (0, 0)))
    pos_p = jnp.pad(positions, ((0, 0), (WINDOW, 0)))
    nb = S // Q_BLOCK
    q_wb = q.reshape(B, nb, Q_BLOCK, G, J, NSA_DK).transpose(1, 0, 2, 3, 4, 5)

    def win_block(args):
        qi, i = args
        s0 = i * Q_BLOCK
        kw = lax.dynamic_slice_in_dim(kp_w, s0, span, axis=1)
        vw = lax.dynamic_slice_in_dim(vp_w, s0, span, axis=1)
        pw = lax.dynamic_slice_in_dim(pos_p, s0, span, axis=1)
        kidx = s0 - WINDOW + jnp.arange(span)
        tq = s0 + jnp.arange(Q_BLOCK)
        diff = tq[:, None] - kidx[None, :]
        mask = (diff >= 0) & (diff < WINDOW) & (kidx >= 0)[None, :]
        qpos = lax.dynamic_slice_in_dim(positions, s0, Q_BLOCK, axis=1)
        bias = rel_bias[rel_bucket(qpos[:, :, None] - pw[:, None, :])].astype(jnp.float32)
        bias = bias.reshape(B, Q_BLOCK, span, G, J).transpose(0, 3, 4, 1, 2)
        s = jnp.einsum('bqgjd,bkgd->bgjqk', qi, kw).astype(jnp.float32) * scale + bias
        p = jax.nn.softmax(jnp.where(mask, s, NEG), axis=-1)
        return jnp.einsum('bgjqk,bkgd->bqgjd', p.astype(vw.dtype), vw)

    o_win = lax.map(win_block, (q_wb, jnp.arange(nb, dtype=jnp.int32)))
    o_win = o_win.transpose(1, 0, 2, 3, 4, 5).reshape(B, S, G, J, NSA_DV)

    g = jax.nn.sigmoid(gate_logits.astype(jnp.float32)).reshape(B, S, G, J, 3).astype(q.dtype)
    o = g[..., 0:1] * o_cmp + g[..., 1:2] * o_slc + g[..., 2:3] * o_win
    return o.reshape(B, S, NSA_HEADS * NSA_DV)


def setup_inputs(seed: int = 0) -> dict:
    key = jax.random.key(seed)
    ks = iter(jax.random.split(key, 40))
    f32 = jnp.float32

    def dense(shape, fan_in):
        return jax.random.normal(next(ks), shape, f32) * (fan_in ** -0.5)

    def gain(n):
        return 1.0 + 0.05 * jax.random.normal(next(ks), (DEPTH, n), f32)

    x = jax.random.normal(next(ks), (BATCH, SEQ, D_MODEL), f32)
    offs = jax.random.randint(next(ks), (BATCH, 1), 0, 4096, dtype=jnp.int32)
    positions = offs + jnp.arange(SEQ, dtype=jnp.int32)[None, :]
    rel_bias = 0.5 * jax.random.normal(next(ks), (REL_BUCKETS, NSA_HEADS), f32)
    inp = {"x": x, "positions": positions, "rel_bias": rel_bias}
    inp["ffn1_pre_g"] = gain(D_MODEL)
    inp["ffn1_post_g"] = gain(D_MODEL)
    inp["ffn1_w_gate"] = dense((DEPTH, D_MODEL, D_FF), D_MODEL)
    inp["ffn1_w_up"] = dense((DEPTH, D_MODEL, D_FF), D_MODEL)
    inp["ffn1_w_down"] = dense((DEPTH, D_FF, D_MODEL), D_FF)
    inp["mix_pre_g"] = gain(D_MODEL)
    inp["mix_post_g"] = gain(D_MODEL)
    inp["w_in"] = dense((DEPTH, D_MODEL, D_IN), D_MODEL)
    inp["mla_q_norm_g"] = gain(MLA_Q_LORA)
    inp["mla_w_q_up"] = dense((DEPTH, MLA_Q_LORA, MLA_HEADS * (MLA_NOPE + MLA_ROPE)), MLA_Q_LORA)
    inp["mla_kv_norm_g"] = gain(MLA_KV_LORA)
    inp["mla_w_uk"] = dense((DEPTH, MLA_KV_LORA, MLA_HEADS * MLA_NOPE), MLA_KV_LORA)
    inp["mla_w_uv"] = dense((DEPTH, MLA_KV_LORA, MLA_HEADS * MLA_V), MLA_KV_LORA)
    inp["cmp_pe_k"] = 0.1 * jax.random.normal(next(ks), (DEPTH, CMP_LEN, NSA_DK), f32)
    inp["cmp_w1_k"] = dense((DEPTH, CMP_LEN * NSA_DK, CMP_HIDDEN), CMP_LEN * NSA_DK)
    inp["cmp_w2_k"] = dense((DEPTH, CMP_HIDDEN, NSA_DK), CMP_HIDDEN)
    inp["cmp_pe_v"] = 0.1 * jax.random.normal(next(ks), (DEPTH, CMP_LEN, NSA_DV), f32)
    inp["cmp_w1_v"] = dense((DEPTH, CMP_LEN * NSA_DV, CMP_HIDDEN), CMP_LEN * NSA_DV)
    inp["cmp_w2_v"] = dense((DEPTH, CMP_HIDDEN, NSA_DV), CMP_HIDDEN)
    inp["w_branch_mla"] = dense((DEPTH, MLA_HEADS * MLA_V, D_MODEL), MLA_HEADS * MLA_V)
    inp["w_branch_nsa"] = dense((DEPTH, NSA_HEADS * NSA_DV, D_MODEL), NSA_HEADS * NSA_DV)
    inp["w_out"] = dense((DEPTH, D_MODEL, D_MODEL), D_MODEL)
    inp["ffn2_pre_g"] = gain(D_MODEL)
    inp["ffn2_post_g"] = gain(D_MODEL)
    inp["ffn2_w_gate"] = dense((DEPTH, D_MODEL, D_FF), D_MODEL)
    inp["ffn2_w_up"] = dense((DEPTH, D_MODEL, D_FF), D_MODEL)
    inp["ffn2_w_down"] = dense((DEPTH, D_FF, D_MODEL), D_FF)
    return inp


def reference(x, positions, rel_bias,
              ffn1_pre_g, ffn1_post_g, ffn1_w_gate, ffn1_w_up, ffn1_w_down,
              mix_pre_g, mix_post_g, w_in,
              mla_q_norm_g, mla_w_q_up, mla_kv_norm_g, mla_w_uk, mla_w_uv,
              cmp_pe_k, cmp_w1_k, cmp_w2_k, cmp_pe_v, cmp_w1_v, cmp_w2_v,
              w_branch_mla, w_branch_nsa, w_out,
              ffn2_pre_g, ffn2_post_g, ffn2_w_gate, ffn2_w_up, ffn2_w_down):
    for l in range(DEPTH):
        h = swiglu_half_step(x, ffn1_pre_g[l], ffn1_post_g[l], ffn1_w_gate[l], ffn1_w_up[l], ffn1_w_down[l])
        u = rmsnorm(h, mix_pre_g[l])
        z = u @ w_in[l]
        (c_q, c_kv, k_rope, nsa_q, k_c, v_c, k_s, v_s, k_w, v_w,
         nsa_g, merge_g) = jnp.split(z, IN_OFFSETS, axis=-1)
        a = mla_mixer(c_q, c_kv, k_rope, positions, mla_q_norm_g[l], mla_w_q_up[l],
                      mla_kv_norm_g[l], mla_w_uk[l], mla_w_uv[l])
        b = nsa_mixer(nsa_q, k_c, v_c, k_s, v_s, k_w, v_w, nsa_g, positions, rel_bias,
                      cmp_pe_k[l], cmp_w1_k[l], cmp_w2_k[l], cmp_pe_v[l], cmp_w1_v[l], cmp_w2_v[l])
        gates = jax.nn.sigmoid(merge_g.astype(jnp.float32)).astype(h.dtype)
        m = gates[..., :D_MODEL] * (a @ w_branch_mla[l]) + gates[..., D_MODEL:] * (b @ w_branch_nsa[l])
        h = h + rmsnorm(m @ w_out[l], mix_post_g[l])
        x = swiglu_half_step(h, ffn2_pre_g[l], ffn2_post_g[l], ffn2_w_gate[l], ffn2_w_up[l], ffn2_w_down[l])
    return x
```

```python
import math
import numpy as np
from contextlib import ExitStack
import concourse.bass as bass
import concourse.mybir as mybir
from concourse.bass_utils import run_bass_kernel_spmd

F32 = mybir.dt.float32
BF16 = mybir.dt.bfloat16
I32 = mybir.dt.int32
AF = mybir.ActivationFunctionType
ALU = mybir.AluOpType

D = 2048
FF = 5632
SEQ = 2048
DEPTH = 4
EPS = 1e-6
NEGM = -30000.0
GL = 4096
GOFF = 2048
DEAD = [False]
MARKS = []


def mark(tk, label):
    MARKS.append((label, tk.cnt['pe']))

_SBN = [0]


def _sbt(nc, name, shape, dt):
    _SBN[0] += 1
    return nc.sbuf_tensor("%s_u%d" % (name, _SBN[0]), shape, dt)


class Buf:
    __slots__ = ("name", "lw", "rd", "dsem")

    def __init__(self, name, dsem):
        self.name = name
        self.lw = None
        self.rd = {}
        self.dsem = dsem


class TK:
    ENG = ("pe", "act", "dve", "pool", "sp")

    def __init__(self, nc, es, n_dma_sems=48):
        self.nc = nc
        self.E = {"pe": nc.tensor, "act": nc.scalar, "dve": nc.vector,
                  "pool": nc.gpsimd, "sp": nc.sync}
        self.sem = {k: es.enter_context(nc.semaphore("s_" + k)) for k in ("pe", "act", "dve", "pool")}
        self.cnt = {k: 0 for k in self.sem}
        self.dsems = [es.enter_context(nc.semaphore("d%d" % i)) for i in range(n_dma_sems)]
        self.dtot = [0] * n_dma_sems
        self.seen = {k: {} for k in self.ENG}
        self._rr = 0
        self.nbuf = 0

    def buf(self, name=None):
        self.nbuf += 1
        b = Buf(name or ("b%d" % self.nbuf), self._rr)
        self._rr = (self._rr + 1) % len(self.dsems)
        return b

    def bufs(self, n):
        return [self.buf() for _ in range(n)]

    def _wait(self, eng, need):
        seen = self.seen[eng]
        for k2, val in need.items():
            kind, key = k2
            if kind == "dma":
                val = self.dtot[key]
            if seen.get(k2, 0) >= val:
                continue
            sem = self.sem[key] if kind == "eng" else self.dsems[key]
            self.E[eng].wait_ge(sem, val)
            seen[k2] = val

    @staticmethod
    def _add(need, ev):
        if ev is None:
            return
        k2 = (ev[0], ev[1])
        if need.get(k2, 0) < ev[2]:
            need[k2] = ev[2]

    def _deps(self, reads, writes):
        need = {}
        for b in reads:
            self._add(need, b.lw)
        for b in writes:
            self._add(need, b.lw)
            for k2, v in b.rd.items():
                if need.get(k2, 0) < v:
                    need[k2] = v
        return need

    def _record(self, ev, reads, writes):
        k2 = (ev[0], ev[1])
        for b in reads:
            if b.rd.get(k2, 0) < ev[2]:
                b.rd[k2] = ev[2]
        for b in writes:
            b.lw = ev
            b.rd = {}

    def op(self, eng, fn, reads=(), writes=(), pe_acc=False):
        if DEAD[0]:
            return None
        need = self._deps(reads, writes)
        if pe_acc:
            need.pop(("eng", "pe"), None)
        self._wait(eng, need)
        ins = fn(self.E[eng])
        self.cnt[eng] += 1
        ev = ("eng", eng, self.cnt[eng])
        ins.then_inc(self.sem[eng], 1)
        self._record(ev, reads, writes)
        return ev

    def dma(self, q, out, in_, reads=(), writes=(), join=False, anchor=None):
        if DEAD[0]:
            return None
        anchor = anchor or (list(writes) + list(reads))[0]
        si = anchor.dsem
        need = self._deps(reads, writes)
        if join:
            need.pop(("dma", si), None)
        self._wait(q, need)
        ins = self.E[q].dma_start(out=out, in_=in_)
        ins.then_inc(self.dsems[si], 16)
        self.dtot[si] += 16
        ev = ("dma", si, self.dtot[si])
        self._record(ev, reads, writes)
        return ev

    def barrier(self):
        need = {("eng", k): v for k, v in self.cnt.items() if v > 0}
        for i, v in enumerate(self.dtot):
            if v > 0:
                need[("dma", i)] = v
        for e in self.ENG:
            self._wait(e, dict(need))


class Ctx:
    def __init__(self, nc, es):
        self.nc = nc
        self.es = es
        self.tk = TK(nc, es)
        tk = self.tk
        self.ps = [es.enter_context(nc.psum_tensor("ps%d" % i, [128, 512], F32)) for i in range(8)]
        self.ps_b = tk.bufs(8)
        self.ones = es.enter_context(_sbt(nc, "ones_bf", [128, 128], BF16))
        self.ones_b = tk.buf()
        tk.op("dve", lambda e: e.memset(self.ones[:], 1.0), writes=[self.ones_b])
        self.stg = [es.enter_context(_sbt(nc, "wstg%d" % i, [128, 4096], F32)) for i in range(2)]
        self.stg_b = tk.bufs(2)
        self.slab = [es.enter_context(_sbt(nc, "wslab%d" % i, [128, 4096], BF16)) for i in range(2)]
        self.slab_b = tk.bufs(2)
        self.wi = 0
        self.cast_rr = 0

    def fetch(self, W, r0, nk, c0, M, pk=128, rstride=None):
        tk = self.tk
        assert nk * M <= 4096
        rstride = rstride or pk
        N = W.shape[1]
        j = self.wi % 2
        self.wi += 1
        stg, sb = self.stg[j], self.stg_b[j]
        src = bass.AP(W.tensor, W.offset + r0 * N + c0, [[N, pk], [rstride * N, nk], [1, M]])
        dst = stg[0:pk, 0:nk * M].rearrange("p (kc m) -> p kc m", m=M)
        step = max(1, 2048 // max(M, 1))
        first = True
        for k0 in range(0, nk, step):
            k1 = min(nk, k0 + step)
            tk.dma("sp", dst[:, k0:k1, :], src[:, k0:k1, :], writes=[sb], join=not first)
            first = False
        slab, lb = self.slab[j], self.slab_b[j]
        eng = ("pool", "dve", "act")[self.cast_rr % 3]
        self.cast_rr += 1
        if eng == "act":
            tk.op("act", lambda e: e.copy(out=slab[0:pk, 0:nk * M], in_=stg[0:pk, 0:nk * M]), reads=[sb], writes=[lb])
        else:
            tk.op(eng, lambda e: e.tensor_copy(out=slab[0:pk, 0:nk * M], in_=stg[0:pk, 0:nk * M]), reads=[sb], writes=[lb])
        return slab[0:pk, 0:nk * M].rearrange("p (kc m) -> p kc m", m=M), lb


def gemm_fm(cx, W, nk, slabs, inT, in_bufs, T, ps_ids, epi, r0=0):
    tk = cx.tk
    pend = cx.fetch(W, r0, nk, slabs[0][0], sum(slabs[0][1]))
    ci = 0
    pi = 0
    for si, (c0, ms) in enumerate(slabs):
        nxt = cx.fetch(W, r0, nk, slabs[si + 1][0], sum(slabs[si + 1][1])) if si + 1 < len(slabs) else None
        view, wb = pend
        off = 0
        for m in ms:
            for t in range(T // 512):
                pid = ps_ids[pi % len(ps_ids)]
                pi += 1
                ps, pb = cx.ps[pid], cx.ps_b[pid]
                for k in range(nk):
                    tk.op("pe", lambda e: e.matmul(ps[0:m, :], lhsT=view[:, k, off:off + m],
                                                   rhs=inT[:, k, t * 512:(t + 1) * 512],
                                                   start=(k == 0), stop=(k == nk - 1)),
                          reads=[wb] + list(in_bufs), writes=[pb], pe_acc=(k > 0))
                epi(ci, m, t, ps, pb)
            off += m
            ci += 1
        pend = nxt


def col_slabs(c0, n, width=256):
    out = []
    c = c0
    end = c0 + n
    while c < end:
        w = min(width, end - c)
        ms = [min(128, w - i) for i in range(0, w, 128)]
        out.append((c, ms))
        c += w
    return out


def ssq_rstd(cx, es, chunk_src, nchunk, T, Kdim, rstd, rstd_b, ps_ids, pk=128):
    tk, nc = cx.tk, cx.nc
    sq = [es.enter_context(_sbt(nc, "sq%d_%d" % (i, tk.nbuf), [128, T], BF16)) for i in range(2)]
    sq_b = tk.bufs(2)
    nt = T // 512
    for k in range(nchunk):
        ap, b = chunk_src(k)
        s, sb_ = sq[k % 2], sq_b[k % 2]
        tk.op("act", lambda e: e.activation(out=s[0:pk, :], in_=ap, func=AF.Square), reads=[b], writes=[sb_])
        for t in range(nt):
            pid = ps_ids[t]
            tk.op("pe", lambda e: e.matmul(cx.ps[pid][:, :], lhsT=cx.ones[0:pk, :], rhs=s[0:pk, t * 512:(t + 1) * 512],
                                           start=(k == 0), stop=(k == nchunk - 1)),
                  reads=[sb_, cx.ones_b], writes=[cx.ps_b[pid]], pe_acc=(k > 0))
    for t in range(nt):
        pid = ps_ids[t]
        sl = slice(t * 512, (t + 1) * 512)
        tk.op("act", lambda e: e.activation(out=rstd[:, sl], in_=cx.ps[pid][:, :], func=AF.Sqrt,
                                            scale=1.0 / Kdim, bias=cx.eps_ap),
              reads=[cx.ps_b[pid]], writes=[rstd_b])
    tk.op("dve", lambda e: e.reciprocal(out=rstd[:, :], in_=rstd[:, :]), reads=[rstd_b], writes=[rstd_b])


def norm_from_dram(cx, src_d, g_sb, g_b, uT, u_bufs, T, K, ps_ids):
    tk, nc = cx.tk, cx.nc
    nk = K // 128
    with ExitStack() as es:
        xs = [es.enter_context(_sbt(nc, "nx%d_%d" % (i, tk.nbuf), [128, T], F32)) for i in range(2)]
        xs_b = tk.bufs(2)
        rstd = es.enter_context(_sbt(nc, "nrstd_%d" % tk.nbuf, [128, T], F32))
        rstd_b = tk.buf()

        def src(k):
            tk.dma("sp", xs[k % 2][:, :], src_d[k * 128:(k + 1) * 128, :], writes=[xs_b[k % 2]])
            return xs[k % 2][:, :], xs_b[k % 2]
        ssq_rstd(cx, es, src, nk, T, K, rstd, rstd_b, ps_ids)
        for k in range(nk):
            ap, b = src(k)
            tk.op("dve", lambda e: e.scalar_tensor_tensor(out=uT[:, k, :], in0=ap, scalar=g_sb[:, k:k + 1],
                                                          in1=rstd[:, :], op0=ALU.mult, op1=ALU.mult),
                  reads=[b, rstd_b, g_b], writes=[u_bufs[k]])
        tk.barrier()


def resid_tail(cx, yT, y_bufs, x_d, gf_sb, gf_b, out_d, T, ps_ids):
    tk, nc = cx.tk, cx.nc
    with ExitStack() as es:
        rstd = es.enter_context(_sbt(nc, "trstd_%d" % tk.nbuf, [128, T], F32))
        rstd_b = tk.buf()
        ssq_rstd(cx, es, lambda k: (yT[:, k, :], y_bufs[k]), 16, T, D, rstd, rstd_b, ps_ids)
        xs = [es.enter_context(_sbt(nc, "tx%d_%d" % (i, tk.nbuf), [128, T], F32)) for i in range(2)]
        xs_b = tk.bufs(2)
        for k in range(16):
            x, xb = xs[k % 2], xs_b[k % 2]
            tk.dma("sp", x[:, :], x_d[k * 128:(k + 1) * 128, :], writes=[xb])
            tk.op("dve", lambda e: e.scalar_tensor_tensor(out=yT[:, k, :], in0=yT[:, k, :], scalar=gf_sb[:, k:k + 1],
                                                          in1=rstd[:, :], op0=ALU.mult, op1=ALU.mult),
                  reads=[rstd_b, gf_b], writes=[y_bufs[k]])
            tk.op("pool" if k % 3 == 0 else "dve", lambda e: e.tensor_tensor(out=x[:, :], in0=x[:, :], in1=yT[:, k, :], op=ALU.add),
                  reads=[y_bufs[k]], writes=[xb])
            tk.dma("sp", out_d[k * 128:(k + 1) * 128, :], x[:, :], reads=[xb])
        tk.barrier()


def stage_ffn(cx, x_d, out_d, gains, gcol_pre, gcol_post, Wg, Wu, Wd, T):
    tk, nc = cx.tk, cx.nc
    mark(tk, 'ffn')
    g_sb, g_b = gains
    NG = 4
    CPG = 11
    with ExitStack() as es:
        yT = es.enter_context(_sbt(nc, "ffn_y_%d" % tk.nbuf, [128, 16, T], F32))
        y_b = tk.bufs(16)
        uT = es.enter_context(_sbt(nc, "ffn_u_%d" % tk.nbuf, [128, 16, T], BF16))
        u_b = tk.bufs(16)
        hT = es.enter_context(_sbt(nc, "ffn_h_%d" % tk.nbuf, [128, CPG, T], BF16))
        h_b = tk.bufs(CPG)
        sg = [es.enter_context(_sbt(nc, "ffn_sg%d_%d" % (i, tk.nbuf), [128, 512], F32)) for i in range(2)]
        sg_b = tk.bufs(2)
        norm_from_dram(cx, x_d, g_sb[:, gcol_pre:gcol_pre + 16], g_b, uT, u_b, T, D, [0, 1, 2, 3])
        nt = T // 512
        cnt = [0]
        for grp in range(NG):
            f0 = grp * CPG * 128
            specs = []
            for c in range(0, CPG * 128, 256):
                w = min(256, CPG * 128 - c)
                specs.append(("g", f0 + c, w))
                specs.append(("u", f0 + c, w))
            pend = cx.fetch(Wg if specs[0][0] == "g" else Wu, 0, 16, specs[0][1], specs[0][2])
            for si, (kind, c0, w) in enumerate(specs):
                nxt = None
                if si + 1 < len(specs):
                    k2, c2, w2 = specs[si + 1]
                    nxt = cx.fetch(Wg if k2 == "g" else Wu, 0, 16, c2, w2)
                view, wb = pend
                nch = w // 128
                for ci in range(nch):
                    n_loc = (c0 - f0) // 128 + ci
                    for t in range(nt):
                        pid = (0 if kind == "g" else 2) + t + 4 * (n_loc % 2)
                        ps, pb = cx.ps[pid], cx.ps_b[pid]
                        for k in range(16):
                            tk.op("pe", lambda e: e.matmul(ps[:, :], lhsT=view[:, k, ci * 128:(ci + 1) * 128],
                                                           rhs=uT[:, k, t * 512:(t + 1) * 512],
                                                           start=(k == 0), stop=(k == 15)),
                                  reads=[wb, u_b[k]], writes=[pb], pe_acc=(k > 0))
                        if kind == "u":
                            gid = t + 4 * (n_loc % 2)
                            s, sb_ = sg[cnt[0] % 2], sg_b[cnt[0] % 2]
                            cnt[0] += 1
                            tk.op("act", lambda e: e.activation(out=s[:, :], in_=cx.ps[gid][:, :], func=AF.Silu),
                                  reads=[cx.ps_b[gid]], writes=[sb_])
                            tk.op("dve", lambda e: e.tensor_tensor(out=hT[:, n_loc, t * 512:(t + 1) * 512], in0=s[:, :],
                                                                   in1=ps[:, :], op=ALU.mult),
                                  reads=[sb_, pb], writes=[h_b[n_loc]])
                pend = nxt
            dsl = col_slabs(0, D, 256)

            def epi(ci, m, t, ps, pb, grp=grp):
                sl = slice(t * 512, (t + 1) * 512)
                if grp == 0:
                    tk.op("act", lambda e: e.copy(out=yT[:, ci, sl], in_=ps[:, :]), reads=[pb], writes=[y_b[ci]])
                else:
                    tk.op("dve", lambda e: e.tensor_tensor(out=yT[:, ci, sl], in0=ps[:, :], in1=yT[:, ci, sl], op=ALU.add),
                          reads=[pb], writes=[y_b[ci]])
            gemm_fm(cx, Wd, CPG, dsl, hT, h_b, T, [0, 1, 2, 3, 4, 5, 6, 7], epi, r0=f0)
        resid_tail(cx, yT, y_b, x_d, g_sb[:, gcol_post:gcol_post + 16], g_b, out_d, T, [0, 1, 2, 3])


def load_gains(cx, es, g_d, ncol, half_cols=()):
    tk, nc = cx.tk, cx.nc
    g_sb = es.enter_context(_sbt(nc, "gains_sb", [128, ncol], F32))
    g_b = tk.buf()
    tk.dma("sp", g_sb[:, :], g_d[:, :], writes=[g_b])
    for (c0, c1) in half_cols:
        tk.op("dve", lambda e: e.tensor_scalar(out=g_sb[:, c0:c1], in0=g_sb[:, c0:c1], scalar1=0.5, scalar2=None,
                                               op0=ALU.mult), reads=[g_b], writes=[g_b])
    eps = es.enter_context(_sbt(nc, "eps_t", [128, 1], F32))
    tk.op("dve", lambda e: e.memset(eps[:], EPS), writes=[g_b])
    cx.eps_ap = eps[:, 0:1]
    return g_sb, g_b


def build_ffn_prog(T=1024):
    nc = bass.Bass("TRN2", target_bir_lowering=False)
    x_d = nc.dram_tensor("xT", [D, T], F32, kind="ExternalInput").ap()
    g_d = nc.dram_tensor("gains", [128, 32], F32, kind="ExternalInput").ap()
    Wg = nc.dram_tensor("wg", [D, FF], F32, kind="ExternalInput").ap()
    Wu = nc.dram_tensor("wu", [D, FF], F32, kind="ExternalInput").ap()
    Wd = nc.dram_tensor("wd", [FF, D], F32, kind="ExternalInput").ap()
    o_d = nc.dram_tensor("outT", [D, T], F32, kind="ExternalOutput").ap()
    with ExitStack() as es:
        cx = Ctx(nc, es)
        gains = load_gains(cx, es, g_d, 32, half_cols=[(16, 32)])
        stage_ffn(cx, x_d, o_d, gains, 0, 16, Wg, Wu, Wd, T)
        cx.tk.barrier()
    return nc


def garr(g):
    return np.ascontiguousarray(np.asarray(g, dtype=np.float32).reshape(-1, 128).T)


def rel_thresholds():
    n = np.arange(0, 256)
    large = 16 + (np.log(np.maximum(n, 1).astype(np.float32) / np.float32(16)) / np.float32(math.log(128 / 16))
                  * np.float32(16)).astype(np.int32)
    large = np.minimum(large, 31)
    bucket = np.where(n < 16, n, large)
    return [int(np.argmax(bucket >= b)) for b in range(1, 32)]


def stage_p0(tk, nc, rb_d, pos_d, rc_d, heads):
    pass


def p0_tables(tk, nc, rb_d, pos_d, rc_d, head_dsts, cos_d, sin_d):
    thr = rel_thresholds()
    with ExitStack() as es:
        sb = lambda n, s, d=F32: es.enter_context(_sbt(nc, n, s, d))
        rb = sb("rb_sb", [128, 32]); rb_b = tk.buf()
        dt = sb("dtab", [128, 32]); dt_b = tk.buf()
        dg = sb("dgrid", [128, 128]); dg_b = tk.buf()
        tk.op("pool", lambda e: e.iota(dg[:, :], [[1, 128]], base=0, channel_multiplier=0,
                                       allow_small_or_imprecise_dtypes=True), writes=[dg_b])
        band = sb("band", [128, 128]); band_b = tk.buf()
        tmp = sb("btmp", [128, 128]); tmp_b = tk.buf()
        G = sb("gtab", [128, GL]); G_b = tk.buf()
        for (hrow, gc_dst, gw_dst) in head_dsts:
            tk.dma("sp", rb[:, :], rb_d[hrow:hrow + 1, :].to_broadcast([128, 32]), writes=[rb_b])
            tk.op("dve", lambda e: e.tensor_tensor(out=dt[:, 1:32], in0=rb[:, 1:32], in1=rb[:, 0:31], op=ALU.subtract),
                  reads=[rb_b], writes=[dt_b])
            tk.op("dve", lambda e: e.tensor_scalar(out=band[:, :], in0=dg[:, :], scalar1=0.0, scalar2=rb[:, 0:1],
                                                   op0=ALU.mult, op1=ALU.add), reads=[dg_b, rb_b], writes=[band_b])
            for b in range(1, 32):
                tk.op("dve", lambda e: e.tensor_scalar(out=tmp[:, :], in0=dg[:, :], scalar1=float(thr[b - 1]),
                                                       scalar2=dt[:, b:b + 1], op0=ALU.is_ge, op1=ALU.mult),
                      reads=[dg_b, dt_b], writes=[tmp_b])
                tk.op("dve", lambda e: e.tensor_tensor(out=band[:, :], in0=band[:, :], in1=tmp[:, :], op=ALU.add),
                      reads=[tmp_b], writes=[band_b])
            for kind, dst in (("c", gc_dst), ("w", gw_dst)):
                hi = GL if kind == "c" else GOFF + 512
                tk.op("pool", lambda e: e.memset(G[:, :], NEGM), writes=[G_b])
                tk.op("dve", lambda e: e.tensor_copy(out=G[:, GOFF:GOFF + 128], in_=band[:, :]), reads=[band_b], writes=[G_b])
                tk.op("dve", lambda e: e.tensor_scalar(out=G[:, GOFF + 128:hi], in0=G[:, GOFF + 128:hi], scalar1=0.0,
                                                       scalar2=rb[:, 31:32], op0=ALU.mult, op1=ALU.add),
                      reads=[rb_b], writes=[G_b])
                tk.dma("sp", dst, G[:, :], reads=[G_b])
        rc = sb("rc_sb", [64, 2]); rc_b = tk.buf()
        tk.dma("sp", rc[:, :], rc_d[:, :], writes=[rc_b])
        pi_ = sb("pos_i", [64, SEQ], I32); pi_b = tk.buf()
        tk.dma("sp", pi_[:, :], pos_d[0:1, :].to_broadcast([64, SEQ]), writes=[pi_b])
        ang = sb("ang", [64, SEQ]); ang_b = tk.buf()
        tk.op("dve", lambda e: e.tensor_copy(out=ang[:, :], in_=pi_[:, :]), reads=[pi_b], writes=[ang_b])
        tk.op("dve", lambda e: e.tensor_scalar(out=ang[:, :], in0=ang[:, :], scalar1=rc[:, 0:1], scalar2=None,
                                               op0=ALU.mult), reads=[rc_b], writes=[ang_b])
        kf = sb("kf", [64, SEQ]); kf_b = tk.buf()
        ki = sb("ki", [64, SEQ], I32); ki_b = tk.buf()
        r = sb("rr", [64, SEQ]); r_b = tk.buf()
        res_t = sb("rope_res", [64, SEQ]); res_b = tk.buf()
        C1 = 6.28125
        C2 = 2.0 * math.pi - C1
        for which, shift, dst in (("sin", 0.0, sin_d), ("cos", math.pi / 2, cos_d)):
            tk.op("dve", lambda e: e.tensor_scalar(out=kf[:, :], in0=ang[:, :], scalar1=shift, scalar2=1.0 / (2 * math.pi),
                                                   op0=ALU.add, op1=ALU.mult), reads=[ang_b], writes=[kf_b])
            tk.op("dve", lambda e: e.tensor_copy(out=ki[:, :], in_=kf[:, :]), reads=[kf_b], writes=[ki_b])
            tk.op("dve", lambda e: e.tensor_copy(out=kf[:, :], in_=ki[:, :]), reads=[ki_b], writes=[kf_b])
            tk.op("dve", lambda e: e.scalar_tensor_tensor(out=r[:, :], in0=kf[:, :], scalar=-C1, in1=ang[:, :],
                                                          op0=ALU.mult, op1=ALU.add), reads=[kf_b, ang_b], writes=[r_b])
            tk.op("dve", lambda e: e.tensor_scalar(out=r[:, :], in0=r[:, :], scalar1=shift, scalar2=None, op0=ALU.add),
                  writes=[r_b])
            tk.op("dve", lambda e: e.scalar_tensor_tensor(out=r[:, :], in0=kf[:, :], scalar=-C2, in1=r[:, :],
                                                          op0=ALU.mult, op1=ALU.add), reads=[kf_b], writes=[r_b])
            tk.op("dve", lambda e: e.tensor_scalar(out=r[:, :], in0=r[:, :], scalar1=3.1415925, scalar2=-3.1415925,
                                                   op0=ALU.min, op1=ALU.max), writes=[r_b])
            tk.op("act", lambda e: e.activation(out=res_t[:, :], in_=r[:, :], func=AF.Sin), reads=[r_b], writes=[res_b])
            if which == "sin":
                tk.op("dve", lambda e: e.tensor_scalar(out=res_t[:, :], in0=res_t[:, :], scalar1=rc[:, 1:2], scalar2=None,
                                                       op0=ALU.mult), reads=[rc_b], writes=[res_b])
            tk.dma("sp", dst[:, :], res_t[:, :], reads=[res_b])
        tk.barrier()


def build_p0_prog():
    nc = bass.Bass("TRN2", target_bir_lowering=False)
    rb_d = nc.dram_tensor("rb", [1, 32], F32, kind="ExternalInput").ap()
    pos_d = nc.dram_tensor("pos", [1, SEQ], I32, kind="ExternalInput").ap()
    rc_d = nc.dram_tensor("ropec", [64, 2], F32, kind="ExternalInput").ap()
    gc_d = nc.dram_tensor("gc", [128, GL], F32, kind="ExternalOutput").ap()
    gw_d = nc.dram_tensor("gw", [128, GL], F32, kind="ExternalOutput").ap()
    cos_d = nc.dram_tensor("cos2", [64, SEQ], F32, kind="ExternalOutput").ap()
    sin_d = nc.dram_tensor("sins", [64, SEQ], F32, kind="ExternalOutput").ap()
    with ExitStack() as es:
        tk = TK(nc, es)
        p0_tables(tk, nc, rb_d, pos_d, rc_d, [(0, gc_d[:, :], gw_d[:, :])], cos_d, sin_d)
    return nc


def rope_consts():
    inv = (10000.0 ** (-np.arange(32, dtype=np.float32) * 2.0 / 64)).astype(np.float32)
    rc = np.zeros((64, 2), np.float32)
    rc[:, 0] = np.concatenate([inv, inv])
    rc[:, 1] = np.concatenate([-np.ones(32), np.ones(32)])
    return rc


CQ, CKV, KR, NQ, KC, VC, KS, KW, VS, VW, GT, NZ = 0, 512, 1024, 1088, 1856, 2048, 2176, 2368, 2560, 2688, 2816, 2828
DEBUG_STOP = 99
DEBUG_FLAGS = set()


class StopBuild(Exception):
    pass


def dbg(level):
    if DEBUG_STOP <= level:
        DEAD[0] = True
S = SEQ
NT = S // 512
MLA_SCALE = 192 ** -0.5
NSA_SCALE = 192 ** -0.5


def run_pipelined(items, phA, phB, depth=2):
    n = len(items)
    for i in range(n + depth):
        if i < n:
            phA(items[i])
        if i - depth >= 0:
            phB(items[i - depth])


def glob_col(c, hh):
    if hh is None:
        return c
    segs = [(CQ, 0), (CKV, 512), (KR, 1024), (NQ, 1088 + hh * 768), (KC, 2624 + hh * 192), (VC, 3008 + hh * 128),
            (KS, 3264 + hh * 192), (KW, 3904 + hh * 192), (VS, 3648 + hh * 128), (VW, 4288 + hh * 128), (GT, 4544 + hh * 12)]
    base = None
    for lo, g in segs:
        if c >= lo:
            base = (lo, g)
    return base[1] + (c - base[0])


def wq_c0(hh):
    return 0 if hh is None else hh * 768


def wkv_c0(hh):
    return 0 if hh is None else hh * 512


def orow(hh):
    return 0 if hh is None else hh * 512


def win_cols(hh):
    g = hh
    r = lambda a, n: list(range(a, a + n))
    cols = r(0, 512) + r(512, 512) + r(1024, 64) + r(1088 + g * 768, 768)
    cols += r(2624 + g * 192, 192) + r(3008 + g * 128, 128) + r(3264 + g * 192, 192) + r(3904 + g * 192, 192)
    cols += r(3648 + g * 128, 128) + r(4288 + g * 128, 128) + r(4544 + g * 12, 12)
    assert len(cols) == NZ
    return np.array(cols)


def build_attn_prog():
    nc = bass.Bass("TRN2", target_bir_lowering=False)
    din = lambda n, s, d=F32: nc.dram_tensor(n, s, d, kind="ExternalInput").ap()
    hT_d = din("hT", [D, S])
    g_d = din("gains", [128, 24])
    win_d = din("win", [D, NZ])
    wq_d = din("wqup", [512, 768])
    wuk_d = din("wuk", [512, 512])
    wuv_d = din("wuv", [512, 512])
    pek_d = din("pekT", [192, 32])
    w1k_d = din("w1k", [6144, 256])
    w2k_d = din("w2k", [256, 192])
    pev_d = din("pevT", [128, 32])
    w1v_d = din("w1v", [4096, 256])
    w2v_d = din("w2v", [256, 128])
    gc_d = nc.dram_tensor("gc", [4, 128, GL], F32, kind="ExternalInput")
    gw_d = nc.dram_tensor("gw", [4, 128, GL], F32, kind="ExternalInput")
    cos_d = din("cos2", [64, S])
    sin_d = din("sins", [64, S])
    ov_d = din("ov1", [128, 33])
    sa_d = din("scoreA", [128, 16, 32])
    sbb_d = din("scoreB", [128, 16, 32])
    ex_d = din("expand", [32, 16 * 128])
    sel_d = din("gsel", [12, 12 * 128])
    id_d = din("ident", [128, 128])
    aT_d = nc.dram_tensor("aT", [512, S], BF16, kind="ExternalOutput").ap()
    bT_d = nc.dram_tensor("bT", [512, S], BF16, kind="ExternalOutput").ap()
    z32_d = nc.dram_tensor("z32", [NZ, S], F32, kind="Internal").ap()
    z16_d = nc.dram_tensor("z16", [NZ, S], BF16, kind="Internal").ap()
    vtok_d = nc.dram_tensor("vtok", [S, 256], BF16, kind="Internal").ap()

    with ExitStack() as es0:
        cx = Ctx(nc, es0)
        tk = cx.tk
        DEAD[0] = False
        _attn_body(cx, es0, nc, locals())
        DEAD[0] = False
        tk.barrier()
    return nc


def _attn_body(cx, es0, nc, L):
    globals_ = L
    (hT_d, g_d, win_d, wq_d, wuk_d, wuv_d, pek_d, w1k_d, w2k_d, pev_d, w1v_d, w2v_d, gc_d, gw_d, cos_d, sin_d, ov_d, sa_d, sbb_d,
     ex_d, sel_d, id_d, aT_d, bT_d, z32_d, z16_d, vtok_d) = [L[k] for k in (
        'hT_d', 'g_d', 'win_d', 'wq_d', 'wuk_d', 'wuv_d', 'pek_d', 'w1k_d', 'w2k_d', 'pev_d', 'w1v_d', 'w2v_d', 'gc_d', 'gw_d',
        'cos_d', 'sin_d', 'ov_d', 'sa_d', 'sbb_d', 'ex_d', 'sel_d', 'id_d', 'aT_d', 'bT_d', 'z32_d', 'z16_d', 'vtok_d')]
    tk = cx.tk
    hh = L.get('hh', None)
    gm, gq, gkv = L.get('gcols', (0, 16, 20))
    if True:
        gains = L['gains'] if 'gains' in L else load_gains(cx, es0, g_d, 24)
        g_sb, g_b = gains
        z32_b, z16_b, vtok_b = tk.buf(), tk.buf(), tk.buf()

        mark(tk, 'attn.s1')
        with ExitStack() as es:
            uT = es.enter_context(_sbt(nc, "uT", [128, 16, S], BF16))
            u_b = tk.bufs(16)
            norm_from_dram(cx, hT_d, g_sb[:, gm:gm + 16], g_b, uT, u_b, S, D, [0, 1, 2, 3])
            st32 = [es.enter_context(_sbt(nc, "st32_%d" % i, [128, 512], F32)) for i in range(2)]
            st16 = [es.enter_context(_sbt(nc, "st16_%d" % i, [128, 512], BF16)) for i in range(2)]
            st32_b, st16_b = tk.bufs(2), tk.bufs(2)
            chunks = []
            for c in range(0, 1024, 128):
                chunks.append((c, 128))
            chunks.append((KR, 64))
            for h in range(4):
                chunks += [(NQ + h * 192, 128), (NQ + h * 192 + 128, 64)]
            chunks += [(KC, 128), (KC + 128, 64), (VC, 128), (KS, 128), (KS + 128, 64), (KW, 128), (KW + 128, 64), (GT, 12)]
            slabs = []
            lastc = None
            for (c, m) in chunks:
                gcl = glob_col(c, hh)
                if slabs and lastc == c and slabs[-1][0] + sum(slabs[-1][1]) == gcl and sum(slabs[-1][1]) + m <= 256:
                    slabs[-1][1].append(m)
                else:
                    slabs.append((gcl, [m]))
                lastc = c + m
            cnt = [0]

            def epi(ci, m, t, ps, pb):
                c0 = chunks[ci][0]
                i = cnt[0] % 2
                cnt[0] += 1
                sl = slice(t * 512, (t + 1) * 512)
                eng = "act" if cnt[0] % 2 else "dve"
                if c0 == GT:
                    tk.op("act", lambda e: e.activation(out=st32[i][0:m, :], in_=ps[0:m, :], func=AF.Sigmoid),
                          reads=[pb], writes=[st32_b[i]])
                    tk.dma("sp", z32_d[c0:c0 + m, sl], st32[i][0:m, :], reads=[st32_b[i]], writes=[z32_b], join=True, anchor=st32_b[i])
                elif c0 < NQ or KC <= c0 < KS:
                    if eng == "act":
                        tk.op("act", lambda e: e.copy(out=st32[i][0:m, :], in_=ps[0:m, :]), reads=[pb], writes=[st32_b[i]])
                    else:
                        tk.op("dve", lambda e: e.tensor_copy(out=st32[i][0:m, :], in_=ps[0:m, :]), reads=[pb], writes=[st32_b[i]])
                    tk.dma("sp", z32_d[c0:c0 + m, sl], st32[i][0:m, :], reads=[st32_b[i]], writes=[z32_b], join=True, anchor=st32_b[i])
                else:
                    if eng == "act":
                        tk.op("act", lambda e: e.copy(out=st16[i][0:m, :], in_=ps[0:m, :]), reads=[pb], writes=[st16_b[i]])
                    else:
                        tk.op("dve", lambda e: e.tensor_copy(out=st16[i][0:m, :], in_=ps[0:m, :]), reads=[pb], writes=[st16_b[i]])
                    tk.dma("sp", z16_d[c0:c0 + m, sl], st16[i][0:m, :], reads=[st16_b[i]], writes=[z16_b], join=True, anchor=st16_b[i])
            gemm_fm(cx, win_d, 16, slabs, uT, u_b, S, [0, 1, 2, 3, 4, 5, 6, 7], epi)
            wv0, wvb0 = cx.fetch(win_d, 0, 16, glob_col(VS, hh), 128)
            wv1, wvb1 = cx.fetch(win_d, 0, 16, glob_col(VW, hh), 128)
            for tt in range(16):
                pid = tt % 4
                ps, pb = cx.ps[pid], cx.ps_b[pid]
                for (wv, wvb, co) in ((wv0, wvb0, 0), (wv1, wvb1, 128)):
                    for k in range(16):
                        tk.op("pe", lambda e: e.matmul(ps[:, co:co + 128], lhsT=uT[:, k, tt * 128:(tt + 1) * 128], rhs=wv[:, k, :],
                                                       start=(k == 0), stop=(k == 15)),
                              reads=[wvb, u_b[k]], writes=[pb], pe_acc=(k > 0 or co > 0))
                i = tt % 2
                tk.op("act", lambda e: e.copy(out=st16[i][:, 0:256], in_=ps[:, 0:256]), reads=[pb], writes=[st16_b[i]])
                tk.dma("sp", vtok_d[tt * 128:(tt + 1) * 128, :], st16[i][:, 0:256], reads=[st16_b[i]], writes=[vtok_b], join=True, anchor=st16_b[i])
            tk.barrier()

        mark(tk, 'mla.proj')
        with ExitStack() as es:
          dbg(1)
          if True:
              sbt = lambda n, s, d: es.enter_context(_sbt(nc, n, s, d))
              qa = sbt("m_qa", [128, 4, S], BF16); qa_b = tk.buf()
              qrr = sbt("m_qrr", [64, 4, S], BF16); qrr_b = tk.buf()
              ka = sbt("m_ka", [128, 4, S], BF16); ka_b = tk.buf()
              krr = sbt("m_krr", [64, S], BF16); krr_b = tk.buf()
              vt = sbt("m_v", [128, 16, 512], BF16); vt_b = tk.buf()
              cos_t = sbt("m_cos", [64, S], F32); sin_t = sbt("m_sin", [64, S], F32); rp_b = tk.buf()
              tk.dma("sp", cos_t[:, :], cos_d[:, :], writes=[rp_b])
              tk.dma("sp", sin_t[:, :], sin_d[:, :], writes=[rp_b], join=True)
              rt = [sbt("m_rt%d" % i, [64, 512], F32) for i in range(2)]
              rs = [sbt("m_rs%d" % i, [64, 512], F32) for i in range(2)]
              rt_b, rs_b = tk.bufs(2), tk.bufs(2)
              raw = [sbt("m_raw%d" % i, [64, 512], F32) for i in range(2)]; raw_b = tk.bufs(2)
              sinx = sbt("m_sinx", [64, S], F32)
              tk.op("dve", lambda e: e.tensor_scalar(out=sinx[:, :], in0=sin_t[:, :], scalar1=-1.0, scalar2=None, op0=ALU.mult),
                    reads=[rp_b], writes=[rp_b])
              rcnt = [0]

              def rope_epi(ps, pb, m_dst, dst_b, t):
                  if 'norope' in DEBUG_FLAGS:
                      tk.op("act", lambda e: e.copy(out=m_dst, in_=ps[0:64, :]), reads=[pb], writes=[dst_b])
                      return
                  i = rcnt[0] % 2
                  rcnt[0] += 1
                  sl = slice(t * 512, (t + 1) * 512)
                  tk.op("act", lambda e: e.copy(out=raw[i][:, :], in_=ps[0:64, :]), reads=[pb], writes=[raw_b[i]])
                  rope_math(raw[i][:, :], raw_b[i], i, sl, m_dst, dst_b)

              def rope_math(src, src_b, i, sl, m_dst, dst_b):
                  a, s_ = rt[i], rs[i]
                  tk.op("dve", lambda e: e.tensor_tensor(out=s_[0:32, :], in0=src[32:64, :], in1=sinx[32:64, sl], op=ALU.mult),
                        reads=[src_b, rp_b], writes=[rs_b[i]])
                  tk.op("dve", lambda e: e.tensor_tensor(out=s_[32:64, :], in0=src[0:32, :], in1=sinx[0:32, sl], op=ALU.mult),
                        reads=[src_b, rp_b], writes=[rs_b[i]])
                  tk.op("dve", lambda e: e.tensor_tensor(out=a[:, :], in0=src, in1=cos_t[:, sl], op=ALU.mult),
                        reads=[src_b, rp_b], writes=[rt_b[i]])
                  tk.op("dve", lambda e: e.tensor_tensor(out=m_dst, in0=a[:, :], in1=s_[:, :], op=ALU.add),
                        reads=[rt_b[i], rs_b[i]], writes=[dst_b])

              for which in ("q", "kv"):
                  with ExitStack() as es2:
                      cn = es2.enter_context(_sbt(nc, "m_cn" + which, [128, 4, S], BF16)); cn_b = tk.bufs(4)
                      base = CQ if which == "q" else CKV
                      gcol = gq if which == "q" else gkv
                      dbg(1.21)
                      norm_from_dram(cx, z32_d[base:base + 512, :], g_sb[:, gcol:gcol + 4], g_b, cn, cn_b, S, 512, [0, 1, 2, 3])
                      dbg(1.22)
                      if which == "q":
                          slabs = [(wq_c0(hh) + h * 192, [128, 64]) for h in range(4)]

                          def epi(ci, m, t, ps, pb):
                              h = ci // 2
                              sl = slice(t * 512, (t + 1) * 512)
                              if ci % 2 == 0:
                                  tk.op("act", lambda e: e.copy(out=qa[:, h, sl], in_=ps[:, :]), reads=[pb], writes=[qa_b])
                              else:
                                  rope_epi(ps, pb, qrr[:, h, sl], qrr_b, t)
                          gemm_fm(cx, wq_d, 4, slabs, cn, cn_b, S, [4, 5, 6, 7], epi)
                      else:
                          def epi(ci, m, t, ps, pb):
                              sl = slice(t * 512, (t + 1) * 512)
                              tk.op("act", lambda e: e.copy(out=ka[:, ci, sl], in_=ps[:, :]), reads=[pb], writes=[ka_b])
                          gemm_fm(cx, wuk_d, 4, col_slabs(wkv_c0(hh), 512, 256), cn, cn_b, S, [4, 5, 6, 7], epi)
                          wv, wvb = cx.fetch(wuv_d, 0, 4, wkv_c0(hh), 512)
                          for tt in range(16):
                              pid = 4 + tt % 4
                              ps, pb = cx.ps[pid], cx.ps_b[pid]
                              for k in range(4):
                                  tk.op("pe", lambda e: e.matmul(ps[:, :], lhsT=cn[:, k, tt * 128:(tt + 1) * 128], rhs=wv[:, k, :],
                                                                 start=(k == 0), stop=(k == 3)),
                                        reads=[wvb, cn_b[k]], writes=[pb], pe_acc=(k > 0))
                              tk.op("dve", lambda e: e.tensor_copy(out=vt[:, tt, :], in_=ps[:, :]), reads=[pb], writes=[vt_b])
                      tk.barrier()
              dbg(1.3)
              with ExitStack() as es2:
                  kr32 = es2.enter_context(_sbt(nc, "m_kr32", [64, S], F32)); kr32_b = tk.buf()
                  tk.dma("sp", kr32[:, :], z32_d[KR:KR + 64, :], reads=[z32_b], writes=[kr32_b])
                  for t in range(NT):
                      sl = slice(t * 512, (t + 1) * 512)
                      i = rcnt[0] % 2
                      rcnt[0] += 1
                      rope_math(kr32[:, sl], kr32_b, i, sl, krr[:, sl], krr_b)
                  tk.barrier()
              dbg(1.5)
              cm = sbt("m_cm", [128, 4, 512], F32); cm_b = tk.buf()
              tk.op("pool", lambda e: e.memset(cm[:, :, :], 0.0), writes=[cm_b])
              for di in range(4):
                  tk.op("pool", lambda e: e.affine_select(out=cm[:, di, :], in_=cm[:, di, :], pattern=[[1, 512]],
                                                          compare_op=ALU.is_ge, fill=NEGM, base=-128 * di,
                                                          channel_multiplier=-1), writes=[cm_b])
              dbg(1.7)
              mark(tk, 'mla.attn')
              NB = 4
              pt = [sbt("m_p%d" % i, [128, 512], BF16) for i in range(NB)]; pt_b = tk.bufs(NB)
              tm = [sbt("m_tm%d" % i, [128, 512], F32) for i in range(NB)]; tm_b = tk.bufs(NB)
              rl = sbt("m_rl", [128, 512], F32); rl_b = tk.buf()
              ot = [sbt("m_ot%d" % i, [128, 512], BF16) for i in range(2)]; ot_b = tk.bufs(2)
              items = []
              hq = 0
              for h in range(4):
                  for qt in range(NT):
                      nkt = 4 * qt + 4
                      for kt in range(nkt):
                          items.append((h, qt, kt, nkt, hq, len(items)))
                      hq += 1

              def phA(it):
                  h, qt, kt, nkt, g, n = it
                  qsl = slice(qt * 512, (qt + 1) * 512)
                  ksl = slice(kt * 128, (kt + 1) * 128)
                  i = n % NB
                  ps, pb = cx.ps[i], cx.ps_b[i]
                  tk.op("pe", lambda e: e.matmul(ps[:, :], lhsT=ka[:, h, ksl], rhs=qa[:, h, qsl], start=True, stop=False),
                        reads=[ka_b, qa_b], writes=[pb])
                  tk.op("pe", lambda e: e.matmul(ps[:, :], lhsT=krr[:, ksl], rhs=qrr[:, h, qsl], start=False, stop=True),
                        reads=[krr_b, qrr_b], writes=[pb], pe_acc=True)
                  di = kt - 4 * qt
                  if di >= 0:
                      tk.op("dve", lambda e: e.tensor_tensor(out=tm[i][:, :], in0=ps[:, :], in1=cm[:, di, :], op=ALU.add),
                            reads=[pb, cm_b], writes=[tm_b[i]])
                      tk.op("act", lambda e: e.activation(out=pt[i][:, :], in_=tm[i][:, :], func=AF.Exp, scale=MLA_SCALE),
                            reads=[tm_b[i]], writes=[pt_b[i]])
                  else:
                      tk.op("act", lambda e: e.activation(out=pt[i][:, :], in_=ps[:, :], func=AF.Exp, scale=MLA_SCALE),
                            reads=[pb], writes=[pt_b[i]])

              def phB(it):
                  h, qt, kt, nkt, g, n = it
                  qsl = slice(qt * 512, (qt + 1) * 512)
                  i = n % NB
                  O_id, L_id = 4 + (g % 2) * 2, 5 + (g % 2) * 2
                  tk.op("pe", lambda e: e.matmul(cx.ps[O_id][:, :], lhsT=vt[:, kt, h * 128:(h + 1) * 128], rhs=pt[i][:, :],
                                                 start=(kt == 0), stop=(kt == nkt - 1)),
                        reads=[vt_b, pt_b[i]], writes=[cx.ps_b[O_id]], pe_acc=(kt > 0))
                  tk.op("pe", lambda e: e.matmul(cx.ps[L_id][:, :], lhsT=cx.ones[:, :], rhs=pt[i][:, :],
                                                 start=(kt == 0), stop=(kt == nkt - 1)),
                        reads=[cx.ones_b, pt_b[i]], writes=[cx.ps_b[L_id]], pe_acc=(kt > 0))
                  if kt == nkt - 1:
                      oi = g % 2
                      tk.op("dve", lambda e: e.reciprocal(out=rl[:, :], in_=cx.ps[L_id][:, :]), reads=[cx.ps_b[L_id]], writes=[rl_b])
                      tk.op("dve", lambda e: e.tensor_tensor(out=ot[oi][:, :], in0=cx.ps[O_id][:, :], in1=rl[:, :], op=ALU.mult),
                            reads=[cx.ps_b[O_id], rl_b], writes=[ot_b[oi]])
                      tk.dma("sp", aT_d[orow(hh) + h * 128:orow(hh) + (h + 1) * 128, qsl], ot[oi][:, :], reads=[ot_b[oi]])
              run_pipelined(items, phA, phB, depth=2)
              tk.barrier()

        dbg(2)
        if True:
            nsa_stage(cx, es0, nc, gains, z32_d, z16_d, vtok_d, (z32_b, z16_b, vtok_b), pek_d, w1k_d, w2k_d, pev_d, w1v_d, w2v_d,
                  gc_d, gw_d, ov_d, sa_d, sbb_d, ex_d, sel_d, id_d, bT_d, hh)


def nsa_stage(cx, es0, nc, gains, z32_d, z16_d, vtok_d, zbufs, pek_d, w1k_d, w2k_d, pev_d, w1v_d, w2v_d,
              gc_d, gw_d, ov_d, sa_d, sbb_d, ex_d, sel_d, id_d, bT_d, hh=None):
    tk = cx.tk
    hb = 0 if hh is None else hh * 4
    with ExitStack() as es:
        sbt = lambda n, s, d: es.enter_context(_sbt(nc, n, s, d))
        kcTa = sbt("n_kcTa", [128, 128], BF16); kcTb = sbt("n_kcTb", [64, 128], BF16); vc = sbt("n_vc", [128, 128], BF16)
        kc_b = tk.buf()
        mark(tk, 'nsa.load')
        nqa = sbt("n_qa", [128, 4, S], BF16); nqb = sbt("n_qb", [64, 4, S], BF16)
        ksa = sbt("n_ksa", [128, S], BF16); ksb = sbt("n_ksb", [64, S], BF16)
        kwa = sbt("n_kwa", [128, S], BF16); kwb = sbt("n_kwb", [64, S], BF16)
        vsw = sbt("n_vsw", [128, 16, 256], BF16)
        gsig = sbt("n_gsig", [12, S], F32)
        ld_b = tk.buf()
        first = [True]

        def ld(dst, src):
            tk.dma("act", dst, src, writes=[ld_b], join=not first[0])
            first[0] = False
        for h in range(4):
            ld(nqa[:, h, :], z16_d[NQ + h * 192:NQ + h * 192 + 128, :])
            ld(nqb[:, h, :], z16_d[NQ + h * 192 + 128:NQ + h * 192 + 192, :])
        ld(ksa[:, :], z16_d[KS:KS + 128, :]); ld(ksb[:, :], z16_d[KS + 128:KS + 192, :])
        ld(kwa[:, :], z16_d[KW:KW + 128, :]); ld(kwb[:, :], z16_d[KW + 128:KW + 192, :])
        ld(vsw[:, :, :], vtok_d.rearrange("(t p) c -> p t c", p=128))
        ld(gsig[:, :], z32_d[GT:GT + 12, :])
        ov1 = sbt("n_ov1", [128, 33], BF16); expd = sbt("n_exp", [32, 16, 128], BF16)
        scA = sbt("n_scA", [128, 16, 32], F32); scB = sbt("n_scB", [128, 16, 32], F32)
        gsel = sbt("n_gsel", [12, 12, 128], F32); ident = sbt("n_ident", [128, 128], F32)
        ld(scA[:, :, :], sa_d[:, :, :]); ld(scB[:, :, :], sbb_d[:, :, :])
        ld(gsel[:, :, :], sel_d.rearrange("r (a p) -> r a p", p=128)); ld(ident[:, :], id_d[:, :])
        mark(tk, 'nsa.compress')
        with ExitStack() as es2:
            sb2 = lambda n, s, d: es2.enter_context(_sbt(nc, n, s, d))
            src32 = {"ka": sb2("c_ka", [128, S], F32), "kb": sb2("c_kb", [64, S], F32), "v": sb2("c_v", [128, S], F32)}
            src_b = tk.buf()
            tk.dma("sp", src32["ka"][:, :], z32_d[KC:KC + 128, :], writes=[src_b])
            tk.dma("sp", src32["kb"][:, :], z32_d[KC + 128:KC + 192, :], writes=[src_b], join=True)
            tk.dma("sp", src32["v"][:, :], z32_d[VC:VC + 128, :], writes=[src_b], join=True)
            pe = {"ka": sb2("c_pea", [128, 32], F32), "kb": sb2("c_peb", [64, 32], F32), "v": sb2("c_pev", [128, 32], F32)}
            pe_b = tk.buf()
            tk.dma("sp", pe["ka"][:, :], pek_d[0:128, :], writes=[pe_b])
            tk.dma("sp", pe["kb"][:, :], pek_d[128:192, :], writes=[pe_b], join=True)
            tk.dma("sp", pe["v"][:, :], pev_d[:, :], writes=[pe_b], join=True)
            zl = {"ka": sb2("c_zla", [128, 32, 127], BF16), "kb": sb2("c_zlb", [64, 32, 127], BF16),
                  "v": sb2("c_zlv", [128, 32, 127], BF16)}
            zlb = {key: tk.bufs(32) for key in ("ka", "kb", "v")}
            for key, pk in (("ka", 128), ("kb", 64), ("v", 128)):
                v3 = src32[key][:, :].rearrange("p (n s) -> p n s", s=16)
                for l in range(32):
                    a, r = l // 16, l % 16
                    tk.op("dve", lambda e: e.tensor_scalar(out=zl[key][:, l, :], in0=v3[:, a:a + 127, r],
                                                           scalar1=pe[key][:, l:l + 1], scalar2=None, op0=ALU.add),
                          reads=[src_b, pe_b], writes=[zlb[key][l]])
            hid = {"k": sb2("c_hk", [128, 2, 128], BF16), "v": sb2("c_hv", [128, 2, 128], BF16)}
            hid_b = tk.buf()
            for hc in range(2):
                sa_, sab = cx.fetch(w1k_d, 0, 32, hc * 128, 128, pk=128, rstride=192)
                sb_, sbb = cx.fetch(w1k_d, 128, 32, hc * 128, 128, pk=64, rstride=192)
                ps, pb = cx.ps[hc], cx.ps_b[hc]
                for l in range(32):
                    tk.op("pe", lambda e: e.matmul(ps[:, 0:127], lhsT=sa_[:, l, :], rhs=zl["ka"][:, l, :], start=(l == 0), stop=False),
                          reads=[sab, zlb["ka"][l]], writes=[pb], pe_acc=(l > 0))
                    tk.op("pe", lambda e: e.matmul(ps[:, 0:127], lhsT=sb_[:, l, :], rhs=zl["kb"][:, l, :], start=False, stop=(l == 31)),
                          reads=[sbb, zlb["kb"][l]], writes=[pb], pe_acc=True)
                tk.op("act", lambda e: e.activation(out=hid["k"][:, hc, 0:127], in_=ps[:, 0:127], func=AF.Silu),
                      reads=[pb], writes=[hid_b])
            for hc in range(2):
                sv_, svb = cx.fetch(w1v_d, 0, 32, hc * 128, 128, pk=128, rstride=128)
                ps, pb = cx.ps[2 + hc], cx.ps_b[2 + hc]
                for l in range(32):
                    tk.op("pe", lambda e: e.matmul(ps[:, 0:127], lhsT=sv_[:, l, :], rhs=zl["v"][:, l, :], start=(l == 0), stop=(l == 31)),
                          reads=[svb, zlb["v"][l]], writes=[pb], pe_acc=(l > 0))
                tk.op("act", lambda e: e.activation(out=hid["v"][:, hc, 0:127], in_=ps[:, 0:127], func=AF.Silu),
                      reads=[pb], writes=[hid_b])
            w2k, w2kb = cx.fetch(w2k_d, 0, 2, 0, 192)
            w2v, w2vb = cx.fetch(w2v_d, 0, 2, 0, 128)
            for hc in range(2):
                tk.op("pe", lambda e: e.matmul(cx.ps[4][:, 0:127], lhsT=w2k[:, hc, 0:128], rhs=hid["k"][:, hc, 0:127],
                                               start=(hc == 0), stop=(hc == 1)), reads=[w2kb, hid_b], writes=[cx.ps_b[4]], pe_acc=(hc > 0))
            for hc in range(2):
                tk.op("pe", lambda e: e.matmul(cx.ps[5][0:64, 0:127], lhsT=w2k[:, hc, 128:192], rhs=hid["k"][:, hc, 0:127],
                                               start=(hc == 0), stop=(hc == 1)), reads=[w2kb, hid_b], writes=[cx.ps_b[5]], pe_acc=(hc > 0))
            for hc in range(2):
                tk.op("pe", lambda e: e.matmul(cx.ps[6][0:127, 0:128], lhsT=hid["v"][:, hc, 0:127], rhs=w2v[:, hc, :],
                                               start=(hc == 0), stop=(hc == 1)), reads=[w2vb, hid_b], writes=[cx.ps_b[6]], pe_acc=(hc > 0))
            tk.op("dve", lambda e: e.tensor_copy(out=kcTa[:, 0:127], in_=cx.ps[4][:, 0:127]), reads=[cx.ps_b[4]], writes=[kc_b])
            tk.op("dve", lambda e: e.tensor_copy(out=kcTb[:, 0:127], in_=cx.ps[5][0:64, 0:127]), reads=[cx.ps_b[5]], writes=[kc_b])
            tk.op("dve", lambda e: e.tensor_copy(out=vc[0:127, :], in_=cx.ps[6][0:127, 0:128]), reads=[cx.ps_b[6]], writes=[kc_b])
            tk.barrier()

        acc = sbt("n_acc", [128, 4, S], F32); acc_b = tk.buf()
        negT = sbt("n_negT", [32, S], BF16); negT_b = tk.buf()
        with ExitStack() as es2:
            t_ov = es2.enter_context(_sbt(nc, "n_tov", [128, 33], F32))
            t_ex = es2.enter_context(_sbt(nc, "n_tex", [32, 16 * 128], F32))
            ld(t_ov[:, :], ov_d[:, :]); ld(t_ex[:, :], ex_d[:, :])
            tk.op("dve", lambda e: e.tensor_copy(out=ov1[:, :], in_=t_ov[:, :]), reads=[ld_b], writes=[ld_b])
            tk.op("dve", lambda e: e.tensor_copy(out=expd[:, :, :].rearrange("p a b -> p (a b)"), in_=t_ex[:, :]), reads=[ld_b], writes=[ld_b])
            tk.barrier()

        NBN = 3
        tm = [sbt("n_tm%d" % i, [128, 512], F32) for i in range(NBN)]; tm_b = tk.bufs(NBN)
        pt = [sbt("n_pt%d" % i, [128, 512], BF16) for i in range(NBN)]; pt_b = tk.bufs(NBN)
        rl = sbt("n_rl", [128, 512], F32); rl_b = tk.buf()
        rlg = sbt("n_rlg", [128, 512], F32); rlg_b = tk.buf()
        tmp = sbt("n_tmp", [128, 512], F32); tmp_b = tk.buf()
        cc = [0]

        def combine(br, j, qt, O_id, L_id, G_id=None):
            qsl = slice(qt * 512, (qt + 1) * 512)
            if G_id is None:
                G_id = 6 + cc[0] % 2
                cc[0] += 1
            tk.op("pe", lambda e: e.matmul(cx.ps[G_id][:, :], lhsT=gsel[:, j * 3 + br, :], rhs=gsig[:, qsl], start=True, stop=True),
                  reads=[ld_b], writes=[cx.ps_b[G_id]])
            tk.op("dve", lambda e: e.tensor_scalar(out=rl[:, :], in0=cx.ps[L_id][:, :], scalar1=1e-30, scalar2=None, op0=ALU.max),
                  reads=[cx.ps_b[L_id]], writes=[rl_b])
            tk.op("dve", lambda e: e.reciprocal(out=rl[:, :], in_=rl[:, :]), writes=[rl_b])
            tk.op("dve", lambda e: e.tensor_tensor(out=rlg[:, :], in0=cx.ps[G_id][:, :], in1=rl[:, :], op=ALU.mult),
                  reads=[cx.ps_b[G_id], rl_b], writes=[rlg_b])
            if br == 0:
                tk.op("dve", lambda e: e.tensor_tensor(out=acc[:, j, qsl], in0=cx.ps[O_id][:, :], in1=rlg[:, :], op=ALU.mult),
                      reads=[cx.ps_b[O_id], rlg_b], writes=[acc_b])
            else:
                tk.op("dve", lambda e: e.tensor_tensor(out=tmp[:, :], in0=cx.ps[O_id][:, :], in1=rlg[:, :], op=ALU.mult),
                      reads=[cx.ps_b[O_id], rlg_b], writes=[tmp_b])
                tk.op("pool", lambda e: e.tensor_tensor(out=acc[:, j, qsl], in0=acc[:, j, qsl], in1=tmp[:, :], op=ALU.add),
                      reads=[tmp_b], writes=[acc_b])

        mark(tk, 'nsa.cmp')
        with ExitStack() as es2:
            eT = es2.enter_context(_sbt(nc, "n_eT", [128, 4, S], BF16)); eT_b = tk.buf()
            with ExitStack() as es3:
                bmc = es3.enter_context(_sbt(nc, "n_bmc", [128, S], F32)); bmc_b = tk.buf()
                pc = 0
                for j in range(4):
                    tk.dma("sp", bmc[0:127, :], bass.AP(gc_d, (hb + j) * 128 * GL + 2017, [[GL - 16, 127], [1, S]]), writes=[bmc_b])
                    for qt in range(NT):
                        qsl = slice(qt * 512, (qt + 1) * 512)
                        sid, i = pc % 2, pc % 2
                        O_id, L_id = 2 + pc % 2, 4 + pc % 2
                        pc += 1
                        ps, pb = cx.ps[sid], cx.ps_b[sid]
                        tk.op("pe", lambda e: e.matmul(ps[0:127, :], lhsT=kcTa[:, 0:127], rhs=nqa[:, j, qsl], start=True, stop=False),
                              reads=[kc_b, ld_b], writes=[pb])
                        tk.op("pe", lambda e: e.matmul(ps[0:127, :], lhsT=kcTb[:, 0:127], rhs=nqb[:, j, qsl], start=False, stop=True),
                              reads=[kc_b, ld_b], writes=[pb], pe_acc=True)
                        tk.op("dve", lambda e: e.scalar_tensor_tensor(out=tm[i][0:127, :], in0=ps[0:127, :], scalar=NSA_SCALE,
                                                                      in1=bmc[0:127, qsl], op0=ALU.mult, op1=ALU.add),
                              reads=[pb, bmc_b], writes=[tm_b[i]])
                        tk.op("act", lambda e: e.activation(out=eT[0:127, j, qsl], in_=tm[i][0:127, :], func=AF.Exp),
                              reads=[tm_b[i]], writes=[eT_b])
                        tk.op("pe", lambda e: e.matmul(cx.ps[O_id][:, :], lhsT=vc[0:127, :], rhs=eT[0:127, j, qsl], start=True, stop=True),
                              reads=[kc_b, eT_b], writes=[cx.ps_b[O_id]])
                        tk.op("pe", lambda e: e.matmul(cx.ps[L_id][:, :], lhsT=cx.ones[0:127, :], rhs=eT[0:127, j, qsl], start=True, stop=True),
                              reads=[cx.ones_b, eT_b], writes=[cx.ps_b[L_id]])
                        combine(0, j, qt, O_id, L_id)
                tk.barrier()
            mark(tk, 'nsa.topk')
            NQQ = 16
            mk = lambda nm, shp: [es2.enter_context(_sbt(nc, "%s%d" % (nm, q), shp, F32)) for q in range(NQQ)]
            l4, imp, sc2, m8, thr, neg = mk("n_l4", [128, 4]), mk("n_imp", [128, 32]), mk("n_sc2", [128, 32]), mk("n_m8", [128, 16]), \
                mk("n_thr", [128, 1]), mk("n_neg", [128, 32])
            tb = tk.bufs(NQQ)
            psI = lambda qq: (cx.ps[qq // 3], cx.ps_b[qq // 3], (qq % 3) * 132)

            def s_mm(qq):
                ps, pb, c0 = psI(qq)
                for j in range(4):
                    tk.op("pe", lambda e: e.matmul(ps[:, c0 + j * 33:c0 + (j + 1) * 33], lhsT=eT[0:127, j, qq * 128:(qq + 1) * 128],
                                                   rhs=ov1[0:127, :], start=True, stop=True), reads=[eT_b, ld_b], writes=[pb], pe_acc=True)

            def s_l4(qq):
                ps, pb, c0 = psI(qq)
                lv = ps[:, c0:c0 + 132].rearrange("p (j c) -> p j c", c=33)[:, :, 32]
                tk.op("dve", lambda e: e.tensor_scalar(out=l4[qq][:, :], in0=lv, scalar1=1e-30, scalar2=None, op0=ALU.max),
                      reads=[pb], writes=[tb[qq]])

            def s_rc(qq):
                tk.op("dve", lambda e: e.reciprocal(out=l4[qq][:, :], in_=l4[qq][:, :]), writes=[tb[qq]])

            def s_imp(j):
                def f(qq):
                    ps, pb, c0 = psI(qq)
                    if j == 0:
                        tk.op("dve", lambda e: e.tensor_scalar(out=imp[qq][:, :], in0=ps[:, c0:c0 + 32], scalar1=l4[qq][:, 0:1], scalar2=None,
                                                               op0=ALU.mult), reads=[pb], writes=[tb[qq]])
                    else:
                        tk.op("dve", lambda e: e.scalar_tensor_tensor(out=imp[qq][:, :], in0=ps[:, c0 + j * 33:c0 + j * 33 + 32],
                                                                      scalar=l4[qq][:, j:j + 1], in1=imp[qq][:, :], op0=ALU.mult, op1=ALU.add),
                              reads=[pb], writes=[tb[qq]])
                return f

            def s_sa(qq):
                tk.op("dve", lambda e: e.tensor_tensor(out=imp[qq][:, :], in0=imp[qq][:, :], in1=scA[:, qq, :], op=ALU.mult), reads=[ld_b], writes=[tb[qq]])

            def s_sb(qq):
                tk.op("dve", lambda e: e.tensor_tensor(out=imp[qq][:, :], in0=imp[qq][:, :], in1=scB[:, qq, :], op=ALU.add), reads=[ld_b], writes=[tb[qq]])

            def s_m1(qq):
                tk.op("dve", lambda e: e.max(out=m8[qq][:, 0:8], in_=imp[qq][:, :]), writes=[tb[qq]])

            def s_mr(qq):
                tk.op("dve", lambda e: e.match_replace(out=sc2[qq][:, :], in_to_replace=m8[qq][:, 0:8], in_values=imp[qq][:, :], imm_value=-1e30),
                      writes=[tb[qq]])

            def s_m2(qq):
                tk.op("dve", lambda e: e.max(out=m8[qq][:, 8:16], in_=sc2[qq][:, :]), writes=[tb[qq]])

            def s_th(qq):
                tk.op("dve", lambda e: e.tensor_scalar(out=thr[qq][:, :], in0=m8[qq][:, 15:16], scalar1=0.0, scalar2=None, op0=ALU.max), writes=[tb[qq]])

            def s_ng(qq):
                tk.op("dve", lambda e: e.tensor_scalar(out=neg[qq][:, :], in0=imp[qq][:, :], scalar1=thr[qq][:, 0:1], scalar2=None, op0=ALU.is_ge),
                      writes=[tb[qq]])

            def s_n2(qq):
                tk.op("dve", lambda e: e.tensor_scalar(out=neg[qq][:, :], in0=neg[qq][:, :], scalar1=-NEGM, scalar2=NEGM, op0=ALU.mult, op1=ALU.add),
                      writes=[tb[qq]])

            def s_tr(qq):
                tp, tpb = cx.ps[6 + qq % 2], cx.ps_b[6 + qq % 2]
                tk.op("pe", lambda e: e.transpose(out=tp[0:32, 0:128], in_=neg[qq][:, :], identity=ident[:, :]), reads=[tb[qq], ld_b], writes=[tpb])
                tk.op("act", lambda e: e.copy(out=negT[:, qq * 128:(qq + 1) * 128], in_=tp[0:32, 0:128]), reads=[tpb], writes=[negT_b])
            for step in (s_mm, s_l4, s_rc, s_imp(0), s_imp(1), s_imp(2), s_imp(3), s_sa, s_sb, s_m1, s_mr, s_m2, s_th, s_ng, s_n2, s_tr):
                for qq in range(NQQ):
                    step(qq)
            tk.barrier()

        for br in (1, 2):
            mark(tk, 'nsa.br%d' % br)
            with ExitStack() as es2:
                W_ = 1152 if br == 1 else 1408
                bm = [es2.enter_context(_sbt(nc, "n_bm%d_%d" % (br, i), [128, W_], F32)) for i in range(2)]
                bm_b = tk.bufs(2)
                g_tab = gc_d if br == 1 else gw_d
                ka_, kb_ = (ksa, ksb) if br == 1 else (kwa, kwb)
                voff = 0 if br == 1 else 128
                items = []
                g = 0
                for j in range(4):
                    for qt in range(NT):
                        kts = list(range(0, 4 * qt + 4)) if br == 1 else list(range(max(0, 4 * qt - 4), 4 * qt + 4))
                        for n_, kt in enumerate(kts):
                            items.append((j, qt, kt, n_, len(kts), g, len(items)))
                        g += 1

                def phA(it):
                    j, qt, kt, n_, nk_, g, n = it
                    if qt == 0 and n_ == 0:
                        tk.dma("sp", bm[j % 2][:, :], bass.AP(g_tab, (hb + j) * 128 * GL + 1664, [[GL - 1, 128], [1, W_]]), writes=[bm_b[j % 2]])
                    qsl = slice(qt * 512, (qt + 1) * 512)
                    ksl = slice(kt * 128, (kt + 1) * 128)
                    i = n % NBN
                    ps, pb = cx.ps[i], cx.ps_b[i]
                    tk.op("pe", lambda e: e.matmul(ps[:, :], lhsT=ka_[:, ksl], rhs=nqa[:, j, qsl], start=True, stop=False),
                          reads=[ld_b], writes=[pb])
                    tk.op("pe", lambda e: e.matmul(ps[:, :], lhsT=kb_[:, ksl], rhs=nqb[:, j, qsl], start=False, stop=(br == 2)),
                          reads=[ld_b], writes=[pb], pe_acc=True)
                    if br == 1:
                        tk.op("pe", lambda e: e.matmul(ps[:, :], lhsT=expd[:, kt, :], rhs=negT[:, qsl], start=False, stop=True),
                              reads=[ld_b, negT_b], writes=[pb], pe_acc=True)
                    delta = qt * 512 - kt * 128
                    off = min(delta, 256) + 384 if br == 1 else delta + 384
                    tk.op("dve", lambda e: e.scalar_tensor_tensor(out=tm[i][:, :], in0=ps[:, :], scalar=NSA_SCALE,
                                                                  in1=bm[j % 2][:, off:off + 512], op0=ALU.mult, op1=ALU.add),
                          reads=[pb, bm_b[j % 2]], writes=[tm_b[i]])
                    tk.op("act", lambda e: e.activation(out=pt[i][:, :], in_=tm[i][:, :], func=AF.Exp),
                          reads=[tm_b[i]], writes=[pt_b[i]])

                def phB(it):
                    j, qt, kt, n_, nk_, g, n = it
                    i = n % NBN
                    O_id, L_id = 3 + g % 2, 5 + g % 2
                    tk.op("pe", lambda e: e.matmul(cx.ps[O_id][:, :], lhsT=vsw[:, kt, voff:voff + 128], rhs=pt[i][:, :],
                                                   start=(n_ == 0), stop=(n_ == nk_ - 1)),
                          reads=[ld_b, pt_b[i]], writes=[cx.ps_b[O_id]], pe_acc=(n_ > 0))
                    tk.op("pe", lambda e: e.matmul(cx.ps[L_id][:, :], lhsT=cx.ones[:, :], rhs=pt[i][:, :],
                                                   start=(n_ == 0), stop=(n_ == nk_ - 1)),
                          reads=[cx.ones_b, pt_b[i]], writes=[cx.ps_b[L_id]], pe_acc=(n_ > 0))
                    if n_ == nk_ - 1:
                        combine(br, j, qt, O_id, L_id, G_id=7)
                run_pipelined(items, phA, phB, depth=2)
                tk.barrier()
        mark(tk, 'nsa.out')
        ob = [sbt("n_ob%d" % i, [128, S], BF16) for i in range(2)]; ob_b = tk.bufs(2)
        for j in range(4):
            tk.op("act", lambda e: e.copy(out=ob[j % 2][:, :], in_=acc[:, j, :]), reads=[acc_b], writes=[ob_b[j % 2]])
            tk.dma("sp", bT_d[orow(hh) + j * 128:orow(hh) + (j + 1) * 128, :], ob[j % 2][:, :], reads=[ob_b[j % 2]])
        tk.barrier()


def attn_consts():
    n_cmp, n_slc = 127, 32
    cs = np.arange(n_cmp) * 16
    ce = cs + 31
    bs = np.arange(n_slc) * 64
    be = bs + 63
    ov = ((cs[:, None] <= be[None, :]) & (ce[:, None] >= bs[None, :])).astype(np.float32)
    ov1 = np.zeros((128, 33), np.float32)
    ov1[:127, :32] = ov
    ov1[:127, 32] = 1.0
    t = np.arange(SEQ)
    cur = t // 64
    jb = np.arange(n_slc)
    valid = jb[None, :] <= cur[:, None]
    forced = valid & ((jb[None, :] == 0) | (jb[None, :] >= cur[:, None] - 1))
    A = (valid & ~forced).astype(np.float32)
    B = np.where(forced, 1e6, np.where(valid, 0.0, -1.0)).astype(np.float32)
    A = np.ascontiguousarray(A.reshape(16, 128, 32).transpose(1, 0, 2))
    B = np.ascontiguousarray(B.reshape(16, 128, 32).transpose(1, 0, 2))
    ex = np.zeros((32, 16, 128), np.float32)
    for kt in range(16):
        ex[2 * kt, kt, :64] = 1.0
        ex[2 * kt + 1, kt, 64:] = 1.0
    gsel = np.zeros((12, 12, 128), np.float32)
    for r in range(12):
        gsel[r, r, :] = 1.0
    return {"ov1": ov1, "scoreA": A, "scoreB": B, "expand": ex.reshape(32, 2048), "gsel": gsel.reshape(12, 12 * 128),
            "ident": np.eye(128, dtype=np.float32)}


def stream(cx, specs, consume):
    pend = cx.fetch(*specs[0])
    for i in range(len(specs)):
        nxt = cx.fetch(*specs[i + 1]) if i + 1 < len(specs) else None
        consume(i, pend[0], pend[1])
        pend = nxt


def stage_merge(cx, h_d, aT_d, bT_d, out_d, gains, gcol_pre, gcol_post, wmg, wa, wb, wo, T, cbase=0):
    tk, nc = cx.tk, cx.nc
    mark(tk, 'merge')
    g_sb, g_b = gains
    nt = T // 512
    with ExitStack() as es:
        mT = es.enter_context(_sbt(nc, "mg_m", [128, 16, T], BF16)); m_b = tk.bufs(16)
        with ExitStack() as es2:
            uT = es2.enter_context(_sbt(nc, "mg_u", [128, 16, T], BF16)); u_b = tk.bufs(16)
            aS = es2.enter_context(_sbt(nc, "mg_a", [128, 8, T], BF16)); bS = es2.enter_context(_sbt(nc, "mg_b", [128, 8, T], BF16))
            ab_b = tk.buf()
            tk.dma("sp", aS[:, :, :], aT_d.rearrange("(k p) t -> p k t", p=128), writes=[ab_b])
            tk.dma("sp", bS[:, :, :], bT_d.rearrange("(k p) t -> p k t", p=128), writes=[ab_b], join=True)
            norm_from_dram(cx, h_d, g_sb[:, gcol_pre:gcol_pre + 16], g_b, uT, u_b, T, D, [0, 1, 2, 3])
            sg = [es2.enter_context(_sbt(nc, "mg_sg%d" % i, [128, 512], F32)) for i in range(2)]; sg_b = tk.bufs(2)
            t1 = [es2.enter_context(_sbt(nc, "mg_t%d" % i, [128, 512], F32)) for i in range(2)]; t1_b = tk.bufs(2)
            specs = []
            for o in range(16):
                specs += [(wmg, 0, 16, cbase + o * 128, 128), (wmg, 0, 16, cbase + 2048 + o * 128, 128), (wa, 0, 8, o * 128, 128), (wb, 0, 8, o * 128, 128)]

            def consume(i, view, wbuf):
                o, kind = i // 4, i % 4
                nk = 16 if kind < 2 else 8
                src, src_bufs = (uT, u_b) if kind < 2 else ((aS, [ab_b] * 8) if kind == 2 else (bS, [ab_b] * 8))
                for t in range(nt):
                    pid = kind * 2 + t
                    ps, pb = cx.ps[pid], cx.ps_b[pid]
                    for k in range(nk):
                        tk.op("pe", lambda e: e.matmul(ps[:, :], lhsT=view[:, k, :], rhs=src[:, k, t * 512:(t + 1) * 512],
                                                       start=(k == 0), stop=(k == nk - 1)),
                              reads=[wbuf, src_bufs[k]], writes=[pb], pe_acc=(k > 0))
                if kind == 3:
                    for t in range(nt):
                        sl = slice(t * 512, (t + 1) * 512)
                        for br in range(2):
                            gid, pid = br * 2 + t, 4 + br * 2 + t
                            tk.op("act", lambda e: e.activation(out=sg[br][:, :], in_=cx.ps[gid][:, :], func=AF.Sigmoid),
                                  reads=[cx.ps_b[gid]], writes=[sg_b[br]])
                            tk.op("dve", lambda e: e.tensor_tensor(out=t1[br][:, :], in0=cx.ps[pid][:, :], in1=sg[br][:, :], op=ALU.mult),
                                  reads=[cx.ps_b[pid], sg_b[br]], writes=[t1_b[br]])
                        tk.op("pool", lambda e: e.tensor_tensor(out=mT[:, o, sl], in0=t1[0][:, :], in1=t1[1][:, :], op=ALU.add),
                              reads=[t1_b[0], t1_b[1]], writes=[m_b[o]])
            stream(cx, specs, consume)
            tk.barrier()
        yT = es.enter_context(_sbt(nc, "mg_y", [128, 16, T], F32)); y_b = tk.bufs(16)

        def epi(ci, m, t, ps, pb):
            sl = slice(t * 512, (t + 1) * 512)
            if (ci + t) % 2:
                tk.op("act", lambda e: e.copy(out=yT[:, ci, sl], in_=ps[:, :]), reads=[pb], writes=[y_b[ci]])
            else:
                tk.op("dve", lambda e: e.tensor_copy(out=yT[:, ci, sl], in_=ps[:, :]), reads=[pb], writes=[y_b[ci]])
        gemm_fm(cx, wo, 16, col_slabs(0, D, 256), mT, m_b, T, [0, 1, 2, 3, 4, 5, 6, 7], epi)
        resid_tail(cx, yT, y_b, h_d, g_sb[:, gcol_post:gcol_post + 16], g_b, out_d, T, [0, 1, 2, 3])


def build_ca_prog(with_next, T=1024):
    nc = bass.Bass("TRN2", target_bir_lowering=False)
    din = lambda n, s, d=F32: nc.dram_tensor(n, s, d, kind="ExternalInput").ap()
    h_d = din("hT", [D, T])
    aT_d = din("aT", [1024, T], BF16)
    bT_d = din("bT", [1024, T], BF16)
    g_d = din("gains", [128, 96])
    wmg = din("wmg", [D, 4096]); wa = din("wa", [1024, D]); wb = din("wb", [1024, D]); wo = din("wo", [D, D])
    w2g = din("w2g", [D, FF]); w2u = din("w2u", [D, FF]); w2d = din("w2d", [FF, D])
    if with_next:
        w1g = din("w1g", [D, FF]); w1u = din("w1u", [D, FF]); w1d = din("w1d", [FF, D])
        hn_d = nc.dram_tensor("hnT", [D, T], F32, kind="ExternalOutput").ap()
    x_d = nc.dram_tensor("xT", [D, T], F32, kind="ExternalOutput").ap()
    h2_d = nc.dram_tensor("h2T", [D, T], F32, kind="Internal").ap()
    with ExitStack() as es:
        cx = Ctx(nc, es)
        gains = load_gains(cx, es, g_d, 96, half_cols=[(48, 64), (80, 96)])
        stage_merge(cx, h_d, aT_d, bT_d, h2_d, gains, 0, 16, wmg, wa, wb, wo, T)
        stage_ffn(cx, h2_d, x_d, gains, 32, 48, w2g, w2u, w2d, T)
        if with_next:
            stage_ffn(cx, x_d, hn_d, gains, 64, 80, w1g, w1u, w1d, T)
        cx.tk.barrier()
    return nc


N_LAUNCH_CORES = 4
GPL = 104


def build_fused_prog():
    nc = bass.Bass("TRN2", target_bir_lowering=False)
    din = lambda n, s, d=F32: nc.dram_tensor(n, s, d, kind="ExternalInput").ap()
    x_d = din("xT", [D, S])
    pos_d = din("pos", [1, S], I32)
    rb_d = din("rbT", [8, 32])
    rc_d = din("ropec", [64, 2])
    g_d = din("gains", [128, GPL * DEPTH])
    W = {}
    for nm, shp in (("f1g", [D, FF]), ("f1u", [D, FF]), ("f1d", [FF, D]), ("f2g", [D, FF]), ("f2u", [D, FF]), ("f2d", [FF, D]),
                    ("win", [D, 8664]), ("wq", [512, 1536]), ("wuk", [512, 1024]), ("wuv", [512, 1024]),
                    ("pekT", [192, 32]), ("w1k", [6144, 256]), ("w2k", [256, 192]), ("pevT", [128, 32]), ("w1v", [4096, 256]),
                    ("w2v", [256, 128]), ("wa", [1024, D]), ("wb", [1024, D]), ("wo", [D, D])):
        W[nm] = (din(nm, [DEPTH * shp[0], shp[1]]), shp[0])
    wl = lambda nm, l: W[nm][0][l * W[nm][1]:(l + 1) * W[nm][1], :]
    ov_d = din("ov1", [128, 33]); sa_d = din("scoreA", [128, 16, 32]); sbb_d = din("scoreB", [128, 16, 32])
    ex_d = din("expand", [32, 16 * 128]); sel_d = din("gsel", [12, 12 * 128]); id_d = din("ident", [128, 128])
    out_d = nc.dram_tensor("outT", [D, S], F32, kind="ExternalOutput").ap()
    di = lambda n, s, d=F32: nc.dram_tensor(n, s, d, kind="Internal")
    hA = di("hA", [D, S]).ap(); hB = di("hB", [D, S]).ap(); xb = [di("xb0", [D, S]).ap(), di("xb1", [D, S]).ap()]
    aT = di("aTi", [1024, S], BF16).ap(); bT = di("bTi", [1024, S], BF16).ap()
    z32 = di("z32", [NZ, S]).ap(); z16 = di("z16", [NZ, S], BF16).ap(); vtok = di("vtok", [S, 256], BF16).ap()
    gc_all = di("gc_all", [8, 128, GL]); gw_all = di("gw_all", [8, 128, GL])
    cos_i = di("cos_i", [64, S]).ap(); sin_i = di("sin_i", [64, S]).ap()
    T = 1024
    with ExitStack() as es:
        cx = Ctx(nc, es)
        halves = []
        for l in range(DEPTH):
            halves += [(l * GPL + 16, l * GPL + 32), (l * GPL + 80, l * GPL + 96)]
        gains = load_gains(cx, es, g_d, GPL * DEPTH, half_cols=halves)
        p0_tables(cx.tk, nc, rb_d, pos_d, rc_d,
                  [(h, bass.AP(gc_all, h * 128 * GL, [[GL, 128], [1, GL]]), bass.AP(gw_all, h * 128 * GL, [[GL, 128], [1, GL]]))
                   for h in range(8)], cos_i, sin_i)
        cur = x_d
        for l in range(DEPTH):
            gb = l * GPL
            for half in range(2):
                tsl = slice(half * T, (half + 1) * T)
                stage_ffn(cx, cur[:, tsl], hA[:, tsl], gains, gb + 0, gb + 16, wl("f1g", l), wl("f1u", l), wl("f1d", l), T)
            for hh in range(2):
                L = {"hT_d": hA, "g_d": None, "win_d": wl("win", l), "wq_d": wl("wq", l), "wuk_d": wl("wuk", l), "wuv_d": wl("wuv", l),
                     "pek_d": wl("pekT", l), "w1k_d": wl("w1k", l), "w2k_d": wl("w2k", l), "pev_d": wl("pevT", l),
                     "w1v_d": wl("w1v", l), "w2v_d": wl("w2v", l), "gc_d": gc_all, "gw_d": gw_all, "cos_d": cos_i, "sin_d": sin_i,
                     "ov_d": ov_d, "sa_d": sa_d, "sbb_d": sbb_d, "ex_d": ex_d, "sel_d": sel_d, "id_d": id_d, "aT_d": aT, "bT_d": bT,
                     "z32_d": z32, "z16_d": z16, "vtok_d": vtok, "hh": hh, "gcols": (gb + 32, gb + 96, gb + 100), "gains": gains}
                _attn_body(cx, es, nc, L)
                cx.tk.barrier()
            for half in range(2):
                tsl = slice(half * T, (half + 1) * T)
                stage_merge(cx, hA[:, tsl], aT[:, tsl], bT[:, tsl], hB[:, tsl], gains, gb + 32, gb + 48,
                            wl("win", l), wl("wa", l), wl("wb", l), wl("wo", l), T, cbase=4568)
            dst = out_d if l == DEPTH - 1 else xb[l % 2]
            for half in range(2):
                tsl = slice(half * T, (half + 1) * T)
                stage_ffn(cx, hB[:, tsl], dst[:, tsl], gains, gb + 64, gb + 80, wl("f2g", l), wl("f2u", l), wl("f2d", l), T)
            cur = dst
        cx.tk.barrier()
    return nc


_PROGS = {}


def kernel(x, positions, rel_bias,
           ffn1_pre_g, ffn1_post_g, ffn1_w_gate, ffn1_w_up, ffn1_w_down,
           mix_pre_g, mix_post_g, w_in,
           mla_q_norm_g, mla_w_q_up, mla_kv_norm_g, mla_w_uk, mla_w_uv,
           cmp_pe_k, cmp_w1_k, cmp_w2_k, cmp_pe_v, cmp_w1_v, cmp_w2_v,
           w_branch_mla, w_branch_nsa, w_out,
           ffn2_pre_g, ffn2_post_g, ffn2_w_gate, ffn2_w_up, ffn2_w_down):
    f = lambda a: np.ascontiguousarray(np.asarray(a, dtype=np.float32))
    st = lambda a: f(a).reshape(-1, np.asarray(a).shape[-1])
    x = f(x)
    positions = np.asarray(positions).astype(np.int32)
    gl = []
    for l in range(DEPTH):
        gl += [garr(ffn1_pre_g[l]), garr(ffn1_post_g[l]), garr(mix_pre_g[l]), garr(mix_post_g[l]), garr(ffn2_pre_g[l]),
               garr(ffn2_post_g[l]), garr(mla_q_norm_g[l]), garr(mla_kv_norm_g[l])]
    common = {"rbT": np.ascontiguousarray(f(rel_bias).T), "ropec": rope_consts(), "gains": np.concatenate(gl, axis=1),
              "f1g": st(ffn1_w_gate), "f1u": st(ffn1_w_up), "f1d": st(ffn1_w_down),
              "f2g": st(ffn2_w_gate), "f2u": st(ffn2_w_up), "f2d": st(ffn2_w_down),
              "win": st(w_in), "wq": st(mla_w_q_up), "wuk": st(mla_w_uk), "wuv": st(mla_w_uv),
              "pekT": np.ascontiguousarray(f(cmp_pe_k).transpose(0, 2, 1)).reshape(-1, 32), "w1k": st(cmp_w1_k), "w2k": st(cmp_w2_k),
              "pevT": np.ascontiguousarray(f(cmp_pe_v).transpose(0, 2, 1)).reshape(-1, 32), "w1v": st(cmp_w1_v), "w2v": st(cmp_w2_v),
              "wa": st(w_branch_mla), "wb": st(w_branch_nsa), "wo": st(w_out)}
    common.update(attn_consts())
    if "fused" not in _PROGS:
        _PROGS["fused"] = build_fused_prog()
    active = {0: 0, 1: 1, 2: 2, 3: 3} if N_LAUNCH_CORES == 4 else {0: 0, 1: 1, 4: 2, 5: 3}
    zeros = None
    maps = []
    for c in range(N_LAUNCH_CORES):
        if c in active:
            b = active[c]
            m = dict(common)
            m["xT"] = np.ascontiguousarray(x[b].T)
            m["pos"] = np.ascontiguousarray(positions[b][None, :])
        else:
            if zeros is None:
                zeros = {k: np.zeros_like(v) for k, v in common.items()}
                zeros["xT"] = np.zeros((D, S), np.float32)
                zeros["pos"] = np.zeros((1, S), np.int32)
            m = zeros
        maps.append(m)
    res = run_bass_kernel_spmd(_PROGS["fused"], maps, core_ids=list(range(N_LAUNCH_CORES))).results
    inv = {b: c for c, b in active.items()}
    out = np.stack([np.asarray(res[inv[b]]["outT"]).T for b in range(4)], axis=0)
    return np.ascontiguousarray(out.astype(np.float32))
```

```python
import math
import numpy as np
from contextlib import ExitStack
import concourse.bass as bass
import concourse.mybir as mybir
from concourse.bass_utils import run_bass_kernel_spmd

F32 = mybir.dt.float32
BF16 = mybir.dt.bfloat16
I32 = mybir.dt.int32
AF = mybir.ActivationFunctionType
ALU = mybir.AluOpType

D = 2048
FF = 5632
SEQ = 2048
DEPTH = 4
EPS = 1e-6
NEGM = -30000.0
GL = 4096
GOFF = 2048
DEAD = [False]
MARKS = []


def mark(tk, label):
    MARKS.append((label, tk.cnt['pe']))

_SBN = [0]


def _sbt(nc, name, shape, dt):
    _SBN[0] += 1
    return nc.sbuf_tensor("%s_u%d" % (name, _SBN[0]), shape, dt)


class Buf:
    __slots__ = ("name", "lw", "rd", "dsem")

    def __init__(self, name, dsem):
        self.name = name
        self.lw = None
        self.rd = {}
        self.dsem = dsem


class TK:
    ENG = ("pe", "act", "dve", "pool", "sp")

    def __init__(self, nc, es, n_dma_sems=48):
        self.nc = nc
        self.E = {"pe": nc.tensor, "act": nc.scalar, "dve": nc.vector,
                  "pool": nc.gpsimd, "sp": nc.sync}
        self.sem = {k: es.enter_context(nc.semaphore("s_" + k)) for k in ("pe", "act", "dve", "pool")}
        self.cnt = {k: 0 for k in self.sem}
        self.dsems = [es.enter_context(nc.semaphore("d%d" % i)) for i in range(n_dma_sems)]
        self.dtot = [0] * n_dma_sems
        self.seen = {k: {} for k in self.ENG}
        self._rr = 0
        self.nbuf = 0

    def buf(self, name=None):
        self.nbuf += 1
        b = Buf(name or ("b%d" % self.nbuf), self._rr)
        self._rr = (self._rr + 1) % len(self.dsems)
        return b

    def bufs(self, n):
        return [self.buf() for _ in range(n)]

    def _wait(self, eng, need):
        seen = self.seen[eng]
        for k2, val in need.items():
            kind, key = k2
            if kind == "dma":
                val = self.dtot[key]
            if seen.get(k2, 0) >= val:
                continue
            sem = self.sem[key] if kind == "eng" else self.dsems[key]
            self.E[eng].wait_ge(sem, val)
            seen[k2] = val

    @staticmethod
    def _add(need, ev):
        if ev is None:
            return
        k2 = (ev[0], ev[1])
        if need.get(k2, 0) < ev[2]:
            need[k2] = ev[2]

    def _deps(self, reads, writes):
        need = {}
        for b in reads:
            self._add(need, b.lw)
        for b in writes:
            self._add(need, b.lw)
            for k2, v in b.rd.items():
                if need.get(k2, 0) < v:
                    need[k2] = v
        return need

    def _record(self, ev, reads, writes):
        k2 = (ev[0], ev[1])
        for b in reads:
            if b.rd.get(k2, 0) < ev[2]:
                b.rd[k2] = ev[2]
        for b in writes:
            b.lw = ev
            b.rd = {}

    def op(self, eng, fn, reads=(), writes=(), pe_acc=False):
        if DEAD[0]:
            return None
        need = self._deps(reads, writes)
        if pe_acc:
            need.pop(("eng", "pe"), None)
        self._wait(eng, need)
        ins = fn(self.E[eng])
        self.cnt[eng] += 1
        ev = ("eng", eng, self.cnt[eng])
        ins.then_inc(self.sem[eng], 1)
        self._record(ev, reads, writes)
        return ev

    def dma(self, q, out, in_, reads=(), writes=(), join=False, anchor=None):
        if DEAD[0]:
            return None
        anchor = anchor or (list(writes) + list(reads))[0]
        si = anchor.dsem
        need = self._deps(reads, writes)
        if join:
            need.pop(("dma", si), None)
        self._wait(q, need)
        ins = self.E[q].dma_start(out=out, in_=in_)
        ins.then_inc(self.dsems[si], 16)
        self.dtot[si] += 16
        ev = ("dma", si, self.dtot[si])
        self._record(ev, reads, writes)
        return ev

    def barrier(self):
        need = {("eng", k): v for k, v in self.cnt.items() if v > 0}
        for i, v in enumerate(self.dtot):
            if v > 0:
                need[("dma", i)] = v
        for e in self.ENG:
            self._wait(e, dict(need))


class Ctx:
    def __init__(self, nc, es):
        self.nc = nc
        self.es = es
        self.tk = TK(nc, es)
        tk = self.tk
        self.ps = [es.enter_context(nc.psum_tensor("ps%d" % i, [128, 512], F32)) for i in range(8)]
        self.ps_b = tk.bufs(8)
        self.ones = es.enter_context(_sbt(nc, "ones_bf", [128, 128], BF16))
        self.ones_b = tk.buf()
        tk.op("dve", lambda e: e.memset(self.ones[:], 1.0), writes=[self.ones_b])
        self.stg = [es.enter_context(_sbt(nc, "wstg%d" % i, [128, 4096], F32)) for i in range(2)]
        self.stg_b = tk.bufs(2)
        self.slab = [es.enter_context(_sbt(nc, "wslab%d" % i, [128, 4096], BF16)) for i in range(2)]
        self.slab_b = tk.bufs(2)
        self.wi = 0
        self.cast_rr = 0

    def fetch(self, W, r0, nk, c0, M, pk=128, rstride=None):
        tk = self.tk
        assert nk * M <= 4096
        rstride = rstride or pk
        N = W.shape[1]
        j = self.wi % 2
        self.wi += 1
        stg, sb = self.stg[j], self.stg_b[j]
        src = bass.AP(W.tensor, W.offset + r0 * N + c0, [[N, pk], [rstride * N, nk], [1, M]])
        dst = stg[0:pk, 0:nk * M].rearrange("p (kc m) -> p kc m", m=M)
        step = max(1, 2048 // max(M, 1))
        first = True
        for k0 in range(0, nk, step):
            k1 = min(nk, k0 + step)
            tk.dma("sp", dst[:, k0:k1, :], src[:, k0:k1, :], writes=[sb], join=not first)
            first = False
        slab, lb = self.slab[j], self.slab_b[j]
        eng = ("pool", "dve", "act")[self.cast_rr % 3]
        self.cast_rr += 1
        if eng == "act":
            tk.op("act", lambda e: e.copy(out=slab[0:pk, 0:nk * M], in_=stg[0:pk, 0:nk * M]), reads=[sb], writes=[lb])
        else:
            tk.op(eng, lambda e: e.tensor_copy(out=slab[0:pk, 0:nk * M], in_=stg[0:pk, 0:nk * M]), reads=[sb], writes=[lb])
        return slab[0:pk, 0:nk * M].rearrange("p (kc m) -> p kc m", m=M), lb


def gemm_fm(cx, W, nk, slabs, inT, in_bufs, T, ps_ids, epi, r0=0):
    tk = cx.tk
    pend = cx.fetch(W, r0, nk, slabs[0][0], sum(slabs[0][1]))
    ci = 0
    pi = 0
    for si, (c0, ms) in enumerate(slabs):
        nxt = cx.fetch(W, r0, nk, slabs[si + 1][0], sum(slabs[si + 1][1])) if si + 1 < len(slabs) else None
        view, wb = pend
        off = 0
        for m in ms:
            for t in range(T // 512):
                pid = ps_ids[pi % len(ps_ids)]
                pi += 1
                ps, pb = cx.ps[pid], cx.ps_b[pid]
                for k in range(nk):
                    tk.op("pe", lambda e: e.matmul(ps[0:m, :], lhsT=view[:, k, off:off + m],
                                                   rhs=inT[:, k, t * 512:(t + 1) * 512],
                                                   start=(k == 0), stop=(k == nk - 1)),
                          reads=[wb] + list(in_bufs), writes=[pb], pe_acc=(k > 0))
                epi(ci, m, t, ps, pb)
            off += m
            ci += 1
        pend = nxt


def col_slabs(c0, n, width=256):
    out = []
    c = c0
    end = c0 + n
    while c < end:
        w = min(width, end - c)
        ms = [min(128, w - i) for i in range(0, w, 128)]
        out.append((c, ms))
        c += w
    return out


def ssq_rstd(cx, es, chunk_src, nchunk, T, Kdim, rstd, rstd_b, ps_ids, pk=128):
    tk, nc = cx.tk, cx.nc
    sq = [es.enter_context(_sbt(nc, "sq%d_%d" % (i, tk.nbuf), [128, T], BF16)) for i in range(2)]
    sq_b = tk.bufs(2)
    nt = T // 512
    for k in range(nchunk):
        ap, b = chunk_src(k)
        s, sb_ = sq[k % 2], sq_b[k % 2]
        tk.op("act", lambda e: e.activation(out=s[0:pk, :], in_=ap, func=AF.Square), reads=[b], writes=[sb_])
        for t in range(nt):
            pid = ps_ids[t]
            tk.op("pe", lambda e: e.matmul(cx.ps[pid][:, :], lhsT=cx.ones[0:pk, :], rhs=s[0:pk, t * 512:(t + 1) * 512],
                                           start=(k == 0), stop=(k == nchunk - 1)),
                  reads=[sb_, cx.ones_b], writes=[cx.ps_b[pid]], pe_acc=(k > 0))
    for t in range(nt):
        pid = ps_ids[t]
        sl = slice(t * 512, (t + 1) * 512)
        tk.op("act", lambda e: e.activation(out=rstd[:, sl], in_=cx.ps[pid][:, :], func=AF.Sqrt,
                                            scale=1.0 / Kdim, bias=cx.eps_ap),
              reads=[cx.ps_b[pid]], writes=[rstd_b])
    tk.op("dve", lambda e: e.reciprocal(out=rstd[:, :], in_=rstd[:, :]), reads=[rstd_b], writes=[rstd_b])


def norm_from_dram(cx, src_d, g_sb, g_b, uT, u_bufs, T, K, ps_ids):
    tk, nc = cx.tk, cx.nc
    nk = K // 128
    with ExitStack() as es:
        xs = [es.enter_context(_sbt(nc, "nx%d_%d" % (i, tk.nbuf), [128, T], F32)) for i in range(2)]
        xs_b = tk.bufs(2)
        rstd = es.enter_context(_sbt(nc, "nrstd_%d" % tk.nbuf, [128, T], F32))
        rstd_b = tk.buf()

        def src(k):
            tk.dma("sp", xs[k % 2][:, :], src_d[k * 128:(k + 1) * 128, :], writes=[xs_b[k % 2]])
            return xs[k % 2][:, :], xs_b[k % 2]
        ssq_rstd(cx, es, src, nk, T, K, rstd, rstd_b, ps_ids)
        for k in range(nk):
            ap, b = src(k)
            tk.op("dve", lambda e: e.scalar_tensor_tensor(out=uT[:, k, :], in0=ap, scalar=g_sb[:, k:k + 1],
                                                          in1=rstd[:, :], op0=ALU.mult, op1=ALU.mult),
                  reads=[b, rstd_b, g_b], writes=[u_bufs[k]])
        tk.barrier()


def resid_tail(cx, yT, y_bufs, x_d, gf_sb, gf_b, out_d, T, ps_ids):
    tk, nc = cx.tk, cx.nc
    with ExitStack() as es:
        rstd = es.enter_context(_sbt(nc, "trstd_%d" % tk.nbuf, [128, T], F32))
        rstd_b = tk.buf()
        ssq_rstd(cx, es, lambda k: (yT[:, k, :], y_bufs[k]), 16, T, D, rstd, rstd_b, ps_ids)
        xs = [es.enter_context(_sbt(nc, "tx%d_%d" % (i, tk.nbuf), [128, T], F32)) for i in range(2)]
        xs_b = tk.bufs(2)
        for k in range(16):
            x, xb = xs[k % 2], xs_b[k % 2]
            tk.dma("sp", x[:, :], x_d[k * 128:(k + 1) * 128, :], writes=[xb])
            tk.op("dve", lambda e: e.scalar_tensor_tensor(out=yT[:, k, :], in0=yT[:, k, :], scalar=gf_sb[:, k:k + 1],
                                                          in1=rstd[:, :], op0=ALU.mult, op1=ALU.mult),
                  reads=[rstd_b, gf_b], writes=[y_bufs[k]])
            tk.op("pool" if k % 3 == 0 else "dve", lambda e: e.tensor_tensor(out=x[:, :], in0=x[:, :], in1=yT[:, k, :], op=ALU.add),
                  reads=[y_bufs[k]], writes=[xb])
            tk.dma("sp", out_d[k * 128:(k + 1) * 128, :], x[:, :], reads=[xb])
        tk.barrier()


def stage_ffn(cx, x_d, out_d, gains, gcol_pre, gcol_post, Wg, Wu, Wd, T):
    tk, nc = cx.tk, cx.nc
    mark(tk, 'ffn')
    g_sb, g_b = gains
    NG = 4
    CPG = 11
    with ExitStack() as es:
        yT = es.enter_context(_sbt(nc, "ffn_y_%d" % tk.nbuf, [128, 16, T], F32))
        y_b = tk.bufs(16)
        uT = es.enter_context(_sbt(nc, "ffn_u_%d" % tk.nbuf, [128, 16, T], BF16))
        u_b = tk.bufs(16)
        hT = es.enter_context(_sbt(nc, "ffn_h_%d" % tk.nbuf, [128, CPG, T], BF16))
        h_b = tk.bufs(CPG)
        sg = [es.enter_context(_sbt(nc, "ffn_sg%d_%d" % (i, tk.nbuf), [128, 512], F32)) for i in range(2)]
        sg_b = tk.bufs(2)
        norm_from_dram(cx, x_d, g_sb[:, gcol_pre:gcol_pre + 16], g_b, uT, u_b, T, D, [0, 1, 2, 3])
        nt = T // 512
        cnt = [0]
        for grp in range(NG):
            f0 = grp * CPG * 128
            specs = []
            for c in range(0, CPG * 128, 256):
                w = min(256, CPG * 128 - c)
                specs.append(("g", f0 + c, w))
                specs.append(("u", f0 + c, w))
            pend = cx.fetch(Wg if specs[0][0] == "g" else Wu, 0, 16, specs[0][1], specs[0][2])
            for si, (kind, c0, w) in enumerate(specs):
                nxt = None
                if si + 1 < len(specs):
                    k2, c2, w2 = specs[si + 1]
                    nxt = cx.fetch(Wg if k2 == "g" else Wu, 0, 16, c2, w2)
                view, wb = pend
                nch = w // 128
                for ci in range(nch):
                    n_loc = (c0 - f0) // 128 + ci
                    for t in range(nt):
                        pid = (0 if kind == "g" else 2) + t + 4 * (n_loc % 2)
                        ps, pb = cx.ps[pid], cx.ps_b[pid]
                        for k in range(16):
                            tk.op("pe", lambda e: e.matmul(ps[:, :], lhsT=view[:, k, ci * 128:(ci + 1) * 128],
                                                           rhs=uT[:, k, t * 512:(t + 1) * 512],
                                                           start=(k == 0), stop=(k == 15)),
                                  reads=[wb, u_b[k]], writes=[pb], pe_acc=(k > 0))
                        if kind == "u":
                            gid = t + 4 * (n_loc % 2)
                            s, sb_ = sg[cnt[0] % 2], sg_b[cnt[0] % 2]
                            cnt[0] += 1
                            tk.op("act", lambda e: e.activation(out=s[:, :], in_=cx.ps[gid][:, :], func=AF.Silu),
                                  reads=[cx.ps_b[gid]], writes=[sb_])
                            tk.op("dve", lambda e: e.tensor_tensor(out=hT[:, n_loc, t * 512:(t + 1) * 512], in0=s[:, :],
                                                                   in1=ps[:, :], op=ALU.mult),
                                  reads=[sb_, pb], writes=[h_b[n_loc]])
                pend = nxt
            dsl = col_slabs(0, D, 256)

            def epi(ci, m, t, ps, pb, grp=grp):
                sl = slice(t * 512, (t + 1) * 512)
                if grp == 0:
                    tk.op("act", lambda e: e.copy(out=yT[:, ci, sl], in_=ps[:, :]), reads=[pb], writes=[y_b[ci]])
                else:
                    tk.op("dve", lambda e: e.tensor_tensor(out=yT[:, ci, sl], in0=ps[:, :], in1=yT[:, ci, sl], op=ALU.add),
                          reads=[pb], writes=[y_b[ci]])
            gemm_fm(cx, Wd, CPG, dsl, hT, h_b, T, [0, 1, 2, 3, 4, 5, 6, 7], epi, r0=f0)
        resid_tail(cx, yT, y_b, x_d, g_sb[:, gcol_post:gcol_post + 16], g_b, out_d, T, [0, 1, 2, 3])


def load_gains(cx, es, g_d, ncol, half_cols=()):
    tk, nc = cx.tk, cx.nc
    g_sb = es.enter_context(_sbt(nc, "gains_sb", [128, ncol], F32))
    g_b = tk.buf()
    tk.dma("sp", g_sb[:, :], g_d[:, :], writes=[g_b])
    for (c0, c1) in half_cols:
        tk.op("dve", lambda e: e.tensor_scalar(out=g_sb[:, c0:c1], in0=g_sb[:, c0:c1], scalar1=0.5, scalar2=None,
                                               op0=ALU.mult), reads=[g_b], writes=[g_b])
    eps = es.enter_context(_sbt(nc, "eps_t", [128, 1], F32))
    tk.op("dve", lambda e: e.memset(eps[:], EPS), writes=[g_b])
    cx.eps_ap = eps[:, 0:1]
    return g_sb, g_b


def build_ffn_prog(T=1024):
    nc = bass.Bass("TRN2", target_bir_lowering=False)
    x_d = nc.dram_tensor("xT", [D, T], F32, kind="ExternalInput").ap()
    g_d = nc.dram_tensor("gains", [128, 32], F32, kind="ExternalInput").ap()
    Wg = nc.dram_tensor("wg", [D, FF], F32, kind="ExternalInput").ap()
    Wu = nc.dram_tensor("wu", [D, FF], F32, kind="ExternalInput").ap()
    Wd = nc.dram_tensor("wd", [FF, D], F32, kind="ExternalInput").ap()
    o_d = nc.dram_tensor("outT", [D, T], F32, kind="ExternalOutput").ap()
    with ExitStack() as es:
        cx = Ctx(nc, es)
        gains = load_gains(cx, es, g_d, 32, half_cols=[(16, 32)])
        stage_ffn(cx, x_d, o_d, gains, 0, 16, Wg, Wu, Wd, T)
        cx.tk.barrier()
    return nc


def garr(g):
    return np.ascontiguousarray(np.asarray(g, dtype=np.float32).reshape(-1, 128).T)


def rel_thresholds():
    n = np.arange(0, 256)
    large = 16 + (np.log(np.maximum(n, 1).astype(np.float32) / np.float32(16)) / np.float32(math.log(128 / 16))
                  * np.float32(16)).astype(np.int32)
    large = np.minimum(large, 31)
    bucket = np.where(n < 16, n, large)
    return [int(np.argmax(bucket >= b)) for b in range(1, 32)]


def stage_p0(tk, nc, rb_d, pos_d, rc_d, heads):
    pass


def p0_tables(tk, nc, rb_d, pos_d, rc_d, head_dsts, cos_d, sin_d):
    thr = rel_thresholds()
    with ExitStack() as es:
        sb = lambda n, s, d=F32: es.enter_context(_sbt(nc, n, s, d))
        rb = sb("rb_sb", [128, 32]); rb_b = tk.buf()
        dt = sb("dtab", [128, 32]); dt_b = tk.buf()
        dg = sb("dgrid", [128, 128]); dg_b = tk.buf()
        tk.op("pool", lambda e: e.iota(dg[:, :], [[1, 128]], base=0, channel_multiplier=0,
                                       allow_small_or_imprecise_dtypes=True), writes=[dg_b])
        band = sb("band", [128, 128]); band_b = tk.buf()
        tmp = sb("btmp", [128, 128]); tmp_b = tk.buf()
        G = sb("gtab", [128, GL]); G_b = tk.buf()
        for (hrow, gc_dst, gw_dst) in head_dsts:
            tk.dma("sp", rb[:, :], rb_d[hrow:hrow + 1, :].to_broadcast([128, 32]), writes=[rb_b])
            tk.op("dve", lambda e: e.tensor_tensor(out=dt[:, 1:32], in0=rb[:, 1:32], in1=rb[:, 0:31], op=ALU.subtract),
                  reads=[rb_b], writes=[dt_b])
            tk.op("dve", lambda e: e.tensor_scalar(out=band[:, :], in0=dg[:, :], scalar1=0.0, scalar2=rb[:, 0:1],
                                                   op0=ALU.mult, op1=ALU.add), reads=[dg_b, rb_b], writes=[band_b])
            for b in range(1, 32):
                tk.op("dve", lambda e: e.tensor_scalar(out=tmp[:, :], in0=dg[:, :], scalar1=float(thr[b - 1]),
                                                       scalar2=dt[:, b:b + 1], op0=ALU.is_ge, op1=ALU.mult),
                      reads=[dg_b, dt_b], writes=[tmp_b])
                tk.op("dve", lambda e: e.tensor_tensor(out=band[:, :], in0=band[:, :], in1=tmp[:, :], op=ALU.add),
                      reads=[tmp_b], writes=[band_b])
            for kind, dst in (("c", gc_dst), ("w", gw_dst)):
                hi = GL if kind == "c" else GOFF + 512
                tk.op("pool", lambda e: e.memset(G[:, :], NEGM), writes=[G_b])
                tk.op("dve", lambda e: e.tensor_copy(out=G[:, GOFF:GOFF + 128], in_=band[:, :]), reads=[band_b], writes=[G_b])
                tk.op("dve", lambda e: e.tensor_scalar(out=G[:, GOFF + 128:hi], in0=G[:, GOFF + 128:hi], scalar1=0.0,
                                                       scalar2=rb[:, 31:32], op0=ALU.mult, op1=ALU.add),
                      reads=[rb_b], writes=[G_b])
                tk.dma("sp", dst, G[:, :], reads=[G_b])
        rc = sb("rc_sb", [64, 2]); rc_b = tk.buf()
        tk.dma("sp", rc[:, :], rc_d[:, :], writes=[rc_b])
        pi_ = sb("pos_i", [64, SEQ], I32); pi_b = tk.buf()
        tk.dma("sp", pi_[:, :], pos_d[0:1, :].to_broadcast([64, SEQ]), writes=[pi_b])
        ang = sb("ang", [64, SEQ]); ang_b = tk.buf()
        tk.op("dve", lambda e: e.tensor_copy(out=ang[:, :], in_=pi_[:, :]), reads=[pi_b], writes=[ang_b])
        tk.op("dve", lambda e: e.tensor_scalar(out=ang[:, :], in0=ang[:, :], scalar1=rc[:, 0:1], scalar2=None,
                                               op0=ALU.mult), reads=[rc_b], writes=[ang_b])
        kf = sb("kf", [64, SEQ]); kf_b = tk.buf()
        ki = sb("ki", [64, SEQ], I32); ki_b = tk.buf()
        r = sb("rr", [64, SEQ]); r_b = tk.buf()
        res_t = sb("rope_res", [64, SEQ]); res_b = tk.buf()
        C1 = 6.28125
        C2 = 2.0 * math.pi - C1
        for which, shift, dst in (("sin", 0.0, sin_d), ("cos", math.pi / 2, cos_d)):
            tk.op("dve", lambda e: e.tensor_scalar(out=kf[:, :], in0=ang[:, :], scalar1=shift, scalar2=1.0 / (2 * math.pi),
                                                   op0=ALU.add, op1=ALU.mult), reads=[ang_b], writes=[kf_b])
            tk.op("dve", lambda e: e.tensor_copy(out=ki[:, :], in_=kf[:, :]), reads=[kf_b], writes=[ki_b])
            tk.op("dve", lambda e: e.tensor_copy(out=kf[:, :], in_=ki[:, :]), reads=[ki_b], writes=[kf_b])
            tk.op("dve", lambda e: e.scalar_tensor_tensor(out=r[:, :], in0=kf[:, :], scalar=-C1, in1=ang[:, :],
                                                          op0=ALU.mult, op1=ALU.add), reads=[kf_b, ang_b], writes=[r_b])
            tk.op("dve", lambda e: e.tensor_scalar(out=r[:, :], in0=r[:, :], scalar1=shift, scalar2=None, op0=ALU.add),
                  writes=[r_b])
            tk.op("dve", lambda e: e.scalar_tensor_tensor(out=r[:, :], in0=kf[:, :], scalar=-C2, in1=r[:, :],
                                                          op0=ALU.mult, op1=ALU.add), reads=[kf_b], writes=[r_b])
            tk.op("dve", lambda e: e.tensor_scalar(out=r[:, :], in0=r[:, :], scalar1=3.1415925, scalar2=-3.1415925,
                                                   op0=ALU.min, op1=ALU.max), writes=[r_b])
            tk.op("act", lambda e: e.activation(out=res_t[:, :], in_=r[:, :], func=AF.Sin), reads=[r_b], writes=[res_b])
            if which == "sin":
                tk.op("dve", lambda e: e.tensor_scalar(out=res_t[:, :], in0=res_t[:, :], scalar1=rc[:, 1:2], scalar2=None,
                                                       op0=ALU.mult), reads=[rc_b], writes=[res_b])
            tk.dma("sp", dst[:, :], res_t[:, :], reads=[res_b])
        tk.barrier()


def build_p0_prog():
    nc = bass.Bass("TRN2", target_bir_lowering=False)
    rb_d = nc.dram_tensor("rb", [1, 32], F32, kind="ExternalInput").ap()
    pos_d = nc.dram_tensor("pos", [1, SEQ], I32, kind="ExternalInput").ap()
    rc_d = nc.dram_tensor("ropec", [64, 2], F32, kind="ExternalInput").ap()
    gc_d = nc.dram_tensor("gc", [128, GL], F32, kind="ExternalOutput").ap()
    gw_d = nc.dram_tensor("gw", [128, GL], F32, kind="ExternalOutput").ap()
    cos_d = nc.dram_tensor("cos2", [64, SEQ], F32, kind="ExternalOutput").ap()
    sin_d = nc.dram_tensor("sins", [64, SEQ], F32, kind="ExternalOutput").ap()
    with ExitStack() as es:
        tk = TK(nc, es)
        p0_tables(tk, nc, rb_d, pos_d, rc_d, [(0, gc_d[:, :], gw_d[:, :])], cos_d, sin_d)
    return nc


def rope_consts():
    inv = (10000.0 ** (-np.arange(32, dtype=np.float32) * 2.0 / 64)).astype(np.float32)
    rc = np.zeros((64, 2), np.float32)
    rc[:, 0] = np.concatenate([inv, inv])
    rc[:, 1] = np.concatenate([-np.ones(32), np.ones(32)])
    return rc


CQ, CKV, KR, NQ, KC, VC, KS, KW, VS, VW, GT, NZ = 0, 512, 1024, 1088, 1856, 2048, 2176, 2368, 2560, 2688, 2816, 2828
DEBUG_STOP = 99
DEBUG_FLAGS = set()


class StopBuild(Exception):
    pass


def dbg(level):
    if DEBUG_STOP <= level:
        DEAD[0] = True
S = SEQ
NT = S // 512
MLA_SCALE = 192 ** -0.5
NSA_SCALE = 192 ** -0.5


def run_pipelined(items, phA, phB, depth=2):
    n = len(items)
    for i in range(n + depth):
        if i < n:
            phA(items[i])
        if i - depth >= 0:
            phB(items[i - depth])


def glob_col(c, hh):
    if hh is None:
        return c
    segs = [(CQ, 0), (CKV, 512), (KR, 1024), (NQ, 1088 + hh * 768), (KC, 2624 + hh * 192), (VC, 3008 + hh * 128),
            (KS, 3264 + hh * 192), (KW, 3904 + hh * 192), (VS, 3648 + hh * 128), (VW, 4288 + hh * 128), (GT, 4544 + hh * 12)]
    base = None
    for lo, g in segs:
        if c >= lo:
            base = (lo, g)
    return base[1] + (c - base[0])


def wq_c0(hh):
    return 0 if hh is None else hh * 768


def wkv_c0(hh):
    return 0 if hh is None else hh * 512


def orow(hh):
    return 0 if hh is None else hh * 512


def win_cols(hh):
    g = hh
    r = lambda a, n: list(range(a, a + n))
    cols = r(0, 512) + r(512, 512) + r(1024, 64) + r(1088 + g * 768, 768)
    cols += r(2624 + g * 192, 192) + r(3008 + g * 128, 128) + r(3264 + g * 192, 192) + r(3904 + g * 192, 192)
    cols += r(3648 + g * 128, 128) + r(4288 + g * 128, 128) + r(4544 + g * 12, 12)
    assert len(cols) == NZ
    return np.array(cols)


def build_attn_prog():
    nc = bass.Bass("TRN2", target_bir_lowering=False)
    din = lambda n, s, d=F32: nc.dram_tensor(n, s, d, kind="ExternalInput").ap()
    hT_d = din("hT", [D, S])
    g_d = din("gains", [128, 24])
    win_d = din("win", [D, NZ])
    wq_d = din("wqup", [512, 768])
    wuk_d = din("wuk", [512, 512])
    wuv_d = din("wuv", [512, 512])
    pek_d = din("pekT", [192, 32])
    w1k_d = din("w1k", [6144, 256])
    w2k_d = din("w2k", [256, 192])
    pev_d = din("pevT", [128, 32])
    w1v_d = din("w1v", [4096, 256])
    w2v_d = din("w2v", [256, 128])
    gc_d = nc.dram_tensor("gc", [4, 128, GL], F32, kind="ExternalInput")
    gw_d = nc.dram_tensor("gw", [4, 128, GL], F32, kind="ExternalInput")
    cos_d = din("cos2", [64, S])
    sin_d = din("sins", [64, S])
    ov_d = din("ov1", [128, 33])
    sa_d = din("scoreA", [128, 16, 32])
    sbb_d = din("scoreB", [128, 16, 32])
    ex_d = din("expand", [32, 16 * 128])
    sel_d = din("gsel", [12, 12 * 128])
    id_d = din("ident", [128, 128])
    aT_d = nc.dram_tensor("aT", [512, S], BF16, kind="ExternalOutput").ap()
    bT_d = nc.dram_tensor("bT", [512, S], BF16, kind="ExternalOutput").ap()
    z32_d = nc.dram_tensor("z32", [NZ, S], F32, kind="Internal").ap()
    z16_d = nc.dram_tensor("z16", [NZ, S], BF16, kind="Internal").ap()
    vtok_d = nc.dram_tensor("vtok", [S, 256], BF16, kind="Internal").ap()

    with ExitStack() as es0:
        cx = Ctx(nc, es0)
        tk = cx.tk
        DEAD[0] = False
        _attn_body(cx, es0, nc, locals())
        DEAD[0] = False
        tk.barrier()
    return nc


def _attn_body(cx, es0, nc, L):
    globals_ = L
    (hT_d, g_d, win_d, wq_d, wuk_d, wuv_d, pek_d, w1k_d, w2k_d, pev_d, w1v_d, w2v_d, gc_d, gw_d, cos_d, sin_d, ov_d, sa_d, sbb_d,
     ex_d, sel_d, id_d, aT_d, bT_d, z32_d, z16_d, vtok_d) = [L[k] for k in (
        'hT_d', 'g_d', 'win_d', 'wq_d', 'wuk_d', 'wuv_d', 'pek_d', 'w1k_d', 'w2k_d', 'pev_d', 'w1v_d', 'w2v_d', 'gc_d', 'gw_d',
        'cos_d', 'sin_d', 'ov_d', 'sa_d', 'sbb_d', 'ex_d', 'sel_d', 'id_d', 'aT_d', 'bT_d', 'z32_d', 'z16_d', 'vtok_d')]
    tk = cx.tk
    hh = L.get('hh', None)
    gm, gq, gkv = L.get('gcols', (0, 16, 20))
    if True:
        gains = L['gains'] if 'gains' in L else load_gains(cx, es0, g_d, 24)
        g_sb, g_b = gains
        z32_b, z16_b, vtok_b = tk.buf(), tk.buf(), tk.buf()

        mark(tk, 'attn.s1')
        with ExitStack() as es:
            uT = es.enter_context(_sbt(nc, "uT", [128, 16, S], BF16))
            u_b = tk.bufs(16)
            norm_from_dram(cx, hT_d, g_sb[:, gm:gm + 16], g_b, uT, u_b, S, D, [0, 1, 2, 3])
            st32 = [es.enter_context(_sbt(nc, "st32_%d" % i, [128, 512], F32)) for i in range(2)]
            st16 = [es.enter_context(_sbt(nc, "st16_%d" % i, [128, 512], BF16)) for i in range(2)]
            st32_b, st16_b = tk.bufs(2), tk.bufs(2)
            chunks = []
            for c in range(0, 1024, 128):
                chunks.append((c, 128))
            chunks.append((KR, 64))
            for h in range(4):
                chunks += [(NQ + h * 192, 128), (NQ + h * 192 + 128, 64)]
            chunks += [(KC, 128), (KC + 128, 64), (VC, 128), (KS, 128), (KS + 128, 64), (KW, 128), (KW + 128, 64), (GT, 12)]
            slabs = []
            lastc = None
            for (c, m) in chunks:
                gcl = glob_col(c, hh)
                if slabs and lastc == c and slabs[-1][0] + sum(slabs[-1][1]) == gcl and sum(slabs[-1][1]) + m <= 256:
                    slabs[-1][1].append(m)
                else:
                    slabs.append((gcl, [m]))
                lastc = c + m
            cnt = [0]

            def epi(ci, m, t, ps, pb):
                c0 = chunks[ci][0]
                i = cnt[0] % 2
                cnt[0] += 1
                sl = slice(t * 512, (t + 1) * 512)
                eng = "act" if cnt[0] % 2 else "dve"
                if c0 == GT:
                    tk.op("act", lambda e: e.activation(out=st32[i][0:m, :], in_=ps[0:m, :], func=AF.Sigmoid),
                          reads=[pb], writes=[st32_b[i]])
                    tk.dma("sp", z32_d[c0:c0 + m, sl], st32[i][0:m, :], reads=[st32_b[i]], writes=[z32_b], join=True, anchor=st32_b[i])
                elif c0 < NQ or KC <= c0 < KS:
                    if eng == "act":
                        tk.op("act", lambda e: e.copy(out=st32[i][0:m, :], in_=ps[0:m, :]), reads=[pb], writes=[st32_b[i]])
                    else:
                        tk.op("dve", lambda e: e.tensor_copy(out=st32[i][0:m, :], in_=ps[0:m, :]), reads=[pb], writes=[st32_b[i]])
                    tk.dma("sp", z32_d[c0:c0 + m, sl], st32[i][0:m, :], reads=[st32_b[i]], writes=[z32_b], join=True, anchor=st32_b[i])
                else:
                    if eng == "act":
                        tk.op("act", lambda e: e.copy(out=st16[i][0:m, :], in_=ps[0:m, :]), reads=[pb], writes=[st16_b[i]])
                    else:
                        tk.op("dve", lambda e: e.tensor_copy(out=st16[i][0:m, :], in_=ps[0:m, :]), reads=[pb], writes=[st16_b[i]])
                    tk.dma("sp", z16_d[c0:c0 + m, sl], st16[i][0:m, :], reads=[st16_b[i]], writes=[z16_b], join=True, anchor=st16_b[i])
            gemm_fm(cx, win_d, 16, slabs, uT, u_b, S, [0, 1, 2, 3, 4, 5, 6, 7], epi)
            wv0, wvb0 = cx.fetch(win_d, 0, 16, glob_col(VS, hh), 128)
            wv1, wvb1 = cx.fetch(win_d, 0, 16, glob_col(VW, hh), 128)
            for tt in range(16):
                pid = tt % 4
                ps, pb = cx.ps[pid], cx.ps_b[pid]
                for (wv, wvb, co) in ((wv0, wvb0, 0), (wv1, wvb1, 128)):
                    for k in range(16):
                        tk.op("pe", lambda e: e.matmul(ps[:, co:co + 128], lhsT=uT[:, k, tt * 128:(tt + 1) * 128], rhs=wv[:, k, :],
                                                       start=(k == 0), stop=(k == 15)),
                              reads=[wvb, u_b[k]], writes=[pb], pe_acc=(k > 0 or co > 0))
                i = tt % 2
                tk.op("act", lambda e: e.copy(out=st16[i][:, 0:256], in_=ps[:, 0:256]), reads=[pb], writes=[st16_b[i]])
                tk.dma("sp", vtok_d[tt * 128:(tt + 1) * 128, :], st16[i][:, 0:256], reads=[st16_b[i]], writes=[vtok_b], join=True, anchor=st16_b[i])
            tk.barrier()

        mark(tk, 'mla.proj')
        with ExitStack() as es:
          dbg(1)
          if True:
              sbt = lambda n, s, d: es.enter_context(_sbt(nc, n, s, d))
              qa = sbt("m_qa", [128, 4, S], BF16); qa_b = tk.buf()
              qrr = sbt("m_qrr", [64, 4, S], BF16); qrr_b = tk.buf()
              ka = sbt("m_ka", [128, 4, S], BF16); ka_b = tk.buf()
              krr = sbt("m_krr", [64, S], BF16); krr_b = tk.buf()
              vt = sbt("m_v", [128, 16, 512], BF16); vt_b = tk.buf()
              cos_t = sbt("m_cos", [64, S], F32); sin_t = sbt("m_sin", [64, S], F32); rp_b = tk.buf()
              tk.dma("sp", cos_t[:, :], cos_d[:, :], writes=[rp_b])
              tk.dma("sp", sin_t[:, :], sin_d[:, :], writes=[rp_b], join=True)
              rt = [sbt("m_rt%d" % i, [64, 512], F32) for i in range(2)]
              rs = [sbt("m_rs%d" % i, [64, 512], F32) for i in range(2)]
              rt_b, rs_b = tk.bufs(2), tk.bufs(2)
              raw = [sbt("m_raw%d" % i, [64, 512], F32) for i in range(2)]; raw_b = tk.bufs(2)
              sinx = sbt("m_sinx", [64, S], F32)
              tk.op("dve", lambda e: e.tensor_scalar(out=sinx[:, :], in0=sin_t[:, :], scalar1=-1.0, scalar2=None, op0=ALU.mult),
                    reads=[rp_b], writes=[rp_b])
              rcnt = [0]

              def rope_epi(ps, pb, m_dst, dst_b, t):
                  if 'norope' in DEBUG_FLAGS:
                      tk.op("act", lambda e: e.copy(out=m_dst, in_=ps[0:64, :]), reads=[pb], writes=[dst_b])
                      return
                  i = rcnt[0] % 2
                  rcnt[0] += 1
                  sl = slice(t * 512, (t + 1) * 512)
                  tk.op("act", lambda e: e.copy(out=raw[i][:, :], in_=ps[0:64, :]), reads=[pb], writes=[raw_b[i]])
                  rope_math(raw[i][:, :], raw_b[i], i, sl, m_dst, dst_b)

              def rope_math(src, src_b, i, sl, m_dst, dst_b):
                  a, s_ = rt[i], rs[i]
                  tk.op("dve", lambda e: e.tensor_tensor(out=s_[0:32, :], in0=src[32:64, :], in1=sinx[32:64, sl], op=ALU.mult),
                        reads=[src_b, rp_b], writes=[rs_b[i]])
                  tk.op("dve", lambda e: e.tensor_tensor(out=s_[32:64, :], in0=src[0:32, :], in1=sinx[0:32, sl], op=ALU.mult),
                        reads=[src_b, rp_b], writes=[rs_b[i]])
                  tk.op("dve", lambda e: e.tensor_tensor(out=a[:, :], in0=src, in1=cos_t[:, sl], op=ALU.mult),
                        reads=[src_b, rp_b], writes=[rt_b[i]])
                  tk.op("dve", lambda e: e.tensor_tensor(out=m_dst, in0=a[:, :], in1=s_[:, :], op=ALU.add),
                        reads=[rt_b[i], rs_b[i]], writes=[dst_b])

              for which in ("q", "kv"):
                  with ExitStack() as es2:
                      cn = es2.enter_context(_sbt(nc, "m_cn" + which, [128, 4, S], BF16)); cn_b = tk.bufs(4)
                      base = CQ if which == "q" else CKV
                      gcol = gq if which == "q" else gkv
                      dbg(1.21)
                      norm_from_dram(cx, z32_d[base:base + 512, :], g_sb[:, gcol:gcol + 4], g_b, cn, cn_b, S, 512, [0, 1, 2, 3])
                      dbg(1.22)
                      if which == "q":
                          slabs = [(wq_c0(hh) + h * 192, [128, 64]) for h in range(4)]

                          def epi(ci, m, t, ps, pb):
                              h = ci // 2
                              sl = slice(t * 512, (t + 1) * 512)
                              if ci % 2 == 0:
                                  tk.op("act", lambda e: e.copy(out=qa[:, h, sl], in_=ps[:, :]), reads=[pb], writes=[qa_b])
                              else:
                                  rope_epi(ps, pb, qrr[:, h, sl], qrr_b, t)
                          gemm_fm(cx, wq_d, 4, slabs, cn, cn_b, S, [4, 5, 6, 7], epi)
                      else:
                          def epi(ci, m, t, ps, pb):
                              sl = slice(t * 512, (t + 1) * 512)
                              tk.op("act", lambda e: e.copy(out=ka[:, ci, sl], in_=ps[:, :]), reads=[pb], writes=[ka_b])
                          gemm_fm(cx, wuk_d, 4, col_slabs(wkv_c0(hh), 512, 256), cn, cn_b, S, [4, 5, 6, 7], epi)
                          wv, wvb = cx.fetch(wuv_d, 0, 4, wkv_c0(hh), 512)
                          for tt in range(16):
                              pid = 4 + tt % 4
                              ps, pb = cx.ps[pid], cx.ps_b[pid]
                              for k in range(4):
                                  tk.op("pe", lambda e: e.matmul(ps[:, :], lhsT=cn[:, k, tt * 128:(tt + 1) * 128], rhs=wv[:, k, :],
                                                                 start=(k == 0), stop=(k == 3)),
                                        reads=[wvb, cn_b[k]], writes=[pb], pe_acc=(k > 0))
                              tk.op("dve", lambda e: e.tensor_copy(out=vt[:, tt, :], in_=ps[:, :]), reads=[pb], writes=[vt_b])
                      tk.barrier()
              dbg(1.3)
              with ExitStack() as es2:
                  kr32 = es2.enter_context(_sbt(nc, "m_kr32", [64, S], F32)); kr32_b = tk.buf()
                  tk.dma("sp", kr32[:, :], z32_d[KR:KR + 64, :], reads=[z32_b], writes=[kr32_b])
                  for t in range(NT):
                      sl = slice(t * 512, (t + 1) * 512)
                      i = rcnt[0] % 2
                      rcnt[0] += 1
                      rope_math(kr32[:, sl], kr32_b, i, sl, krr[:, sl], krr_b)
                  tk.barrier()
              dbg(1.5)
              cm = sbt("m_cm", [128, 4, 512], F32); cm_b = tk.buf()
              tk.op("pool", lambda e: e.memset(cm[:, :, :], 0.0), writes=[cm_b])
              for di in range(4):
                  tk.op("pool", lambda e: e.affine_select(out=cm[:, di, :], in_=cm[:, di, :], pattern=[[1, 512]],
                                                          compare_op=ALU.is_ge, fill=NEGM, base=-128 * di,
                                                          channel_multiplier=-1), writes=[cm_b])
              dbg(1.7)
              mark(tk, 'mla.attn')
              NB = 4
              pt = [sbt("m_p%d" % i, [128, 512], BF16) for i in range(NB)]; pt_b = tk.bufs(NB)
              tm = [sbt("m_tm%d" % i, [128, 512], F32) for i in range(NB)]; tm_b = tk.bufs(NB)
              rl = sbt("m_rl", [128, 512], F32); rl_b = tk.buf()
              ot = [sbt("m_ot%d" % i, [128, 512], BF16) for i in range(2)]; ot_b = tk.bufs(2)
              items = []
              hq = 0
              for h in range(4):
                  for qt in range(NT):
                      nkt = 4 * qt + 4
                      for kt in range(nkt):
                          items.append((h, qt, kt, nkt, hq, len(items)))
                      hq += 1

              def phA(it):
                  h, qt, kt, nkt, g, n = it
                  qsl = slice(qt * 512, (qt + 1) * 512)
                  ksl = slice(kt * 128, (kt + 1) * 128)
                  i = n % NB
                  ps, pb = cx.ps[i], cx.ps_b[i]
                  tk.op("pe", lambda e: e.matmul(ps[:, :], lhsT=ka[:, h, ksl], rhs=qa[:, h, qsl], start=True, stop=False),
                        reads=[ka_b, qa_b], writes=[pb])
                  tk.op("pe", lambda e: e.matmul(ps[:, :], lhsT=krr[:, ksl], rhs=qrr[:, h, qsl], start=False, stop=True),
                        reads=[krr_b, qrr_b], writes=[pb], pe_acc=True)
                  di = kt - 4 * qt
                  if di >= 0:
                      tk.op("dve", lambda e: e.tensor_tensor(out=tm[i][:, :], in0=ps[:, :], in1=cm[:, di, :], op=ALU.add),
                            reads=[pb, cm_b], writes=[tm_b[i]])
                      tk.op("act", lambda e: e.activation(out=pt[i][:, :], in_=tm[i][:, :], func=AF.Exp, scale=MLA_SCALE),
                            reads=[tm_b[i]], writes=[pt_b[i]])
                  else:
                      tk.op("act", lambda e: e.activation(out=pt[i][:, :], in_=ps[:, :], func=AF.Exp, scale=MLA_SCALE),
                            reads=[pb], writes=[pt_b[i]])

              def phB(it):
                  h, qt, kt, nkt, g, n = it
                  qsl = slice(qt * 512, (qt + 1) * 512)
                  i = n % NB
                  O_id, L_id = 4 + (g % 2) * 2, 5 + (g % 2) * 2
                  tk.op("pe", lambda e: e.matmul(cx.ps[O_id][:, :], lhsT=vt[:, kt, h * 128:(h + 1) * 128], rhs=pt[i][:, :],
                                                 start=(kt == 0), stop=(kt == nkt - 1)),
                        reads=[vt_b, pt_b[i]], writes=[cx.ps_b[O_id]], pe_acc=(kt > 0))
                  tk.op("pe", lambda e: e.matmul(cx.ps[L_id][:, :], lhsT=cx.ones[:, :], rhs=pt[i][:, :],
                                                 start=(kt == 0), stop=(kt == nkt - 1)),
                        reads=[cx.ones_b, pt_b[i]], writes=[cx.ps_b[L_id]], pe_acc=(kt > 0))
                  if kt == nkt - 1:
                      oi = g % 2
                      tk.op("dve", lambda e: e.reciprocal(out=rl[:, :], in_=cx.ps[L_id][:, :]), reads=[cx.ps_b[L_id]], writes=[rl_b])
                      tk.op("dve", lambda e: e.tensor_tensor(out=ot[oi][:, :], in0=cx.ps[O_id][:, :], in1=rl[:, :], op=ALU.mult),
                            reads=[cx.ps_b[O_id], rl_b], writes=[ot_b[oi]])
                      tk.dma("sp", aT_d[orow(hh) + h * 128:orow(hh) + (h + 1) * 128, qsl], ot[oi][:, :], reads=[ot_b[oi]])
              run_pipelined(items, phA, phB, depth=2)
              tk.barrier()

        dbg(2)
        if True:
            nsa_stage(cx, es0, nc, gains, z32_d, z16_d, vtok_d, (z32_b, z16_b, vtok_b), pek_d, w1k_d, w2k_d, pev_d, w1v_d, w2v_d,
                  gc_d, gw_d, ov_d, sa_d, sbb_d, ex_d, sel_d, id_d, bT_d, hh)


def nsa_stage(cx, es0, nc, gains, z32_d, z16_d, vtok_d, zbufs, pek_d, w1k_d, w2k_d, pev_d, w1v_d, w2v_d,
              gc_d, gw_d, ov_d, sa_d, sbb_d, ex_d, sel_d, id_d, bT_d, hh=None):
    tk = cx.tk
    hb = 0 if hh is None else hh * 4
    with ExitStack() as es:
        sbt = lambda n, s, d: es.enter_context(_sbt(nc, n, s, d))
        kcTa = sbt("n_kcTa", [128, 128], BF16); kcTb = sbt("n_kcTb", [64, 128], BF16); vc = sbt("n_vc", [128, 128], BF16)
        kc_b = tk.buf()
        mark(tk, 'nsa.load')
        nqa = sbt("n_qa", [128, 4, S], BF16); nqb = sbt("n_qb", [64, 4, S], BF16)
        ksa = sbt("n_ksa", [128, S], BF16); ksb = sbt("n_ksb", [64, S], BF16)
        kwa = sbt("n_kwa", [128, S], BF16); kwb = sbt("n_kwb", [64, S], BF16)
        vsw = sbt("n_vsw", [128, 16, 256], BF16)
        gsig = sbt("n_gsig", [12, S], F32)
        ld_b = tk.buf()
        first = [True]

        def ld(dst, src):
            tk.dma("act", dst, src, writes=[ld_b], join=not first[0])
            first[0] = False
        for h in range(4):
            ld(nqa[:, h, :], z16_d[NQ + h * 192:NQ + h * 192 + 128, :])
            ld(nqb[:, h, :], z16_d[NQ + h * 192 + 128:NQ + h * 192 + 192, :])
        ld(ksa[:, :], z16_d[KS:KS + 128, :]); ld(ksb[:, :], z16_d[KS + 128:KS + 192, :])
        ld(kwa[:, :], z16_d[KW:KW + 128, :]); ld(kwb[:, :], z16_d[KW + 128:KW + 192, :])
        ld(vsw[:, :, :], vtok_d.rearrange("(t p) c -> p t c", p=128))
        ld(gsig[:, :], z32_d[GT:GT + 12, :])
        ov1 = sbt("n_ov1", [128, 33], BF16); expd = sbt("n_exp", [32, 16, 128], BF16)
        scA = sbt("n_scA", [128, 16, 32], F32); scB = sbt("n_scB", [128, 16, 32], F32)
        gsel = sbt("n_gsel", [12, 12, 128], F32); ident = sbt("n_ident", [128, 128], F32)
        ld(scA[:, :, :], sa_d[:, :, :]); ld(scB[:, :, :], sbb_d[:, :, :])
        ld(gsel[:, :, :], sel_d.rearrange("r (a p) -> r a p", p=128)); ld(ident[:, :], id_d[:, :])
        mark(tk, 'nsa.compress')
        with ExitStack() as es2:
            sb2 = lambda n, s, d: es2.enter_context(_sbt(nc, n, s, d))
            src32 = {"ka": sb2("c_ka", [128, S], F32), "kb": sb2("c_kb", [64, S], F32), "v": sb2("c_v", [128, S], F32)}
            src_b = tk.buf()
            tk.dma("sp", src32["ka"][:, :], z32_d[KC:KC + 128, :], writes=[src_b])
            tk.dma("sp", src32["kb"][:, :], z32_d[KC + 128:KC + 192, :], writes=[src_b], join=True)
            tk.dma("sp", src32["v"][:, :], z32_d[VC:VC + 128, :], writes=[src_b], join=True)
            pe = {"ka": sb2("c_pea", [128, 32], F32), "kb": sb2("c_peb", [64, 32], F32), "v": sb2("c_pev", [128, 32], F32)}
            pe_b = tk.buf()
            tk.dma("sp", pe["ka"][:, :], pek_d[0:128, :], writes=[pe_b])
            tk.dma("sp", pe["kb"][:, :], pek_d[128:192, :], writes=[pe_b], join=True)
            tk.dma("sp", pe["v"][:, :], pev_d[:, :], writes=[pe_b], join=True)
            zl = {"ka": sb2("c_zla", [128, 32, 127], BF16), "kb": sb2("c_zlb", [64, 32, 127], BF16),
                  "v": sb2("c_zlv", [128, 32, 127], BF16)}
            zlb = {key: tk.bufs(32) for key in ("ka", "kb", "v")}
            for key, pk in (("ka", 128), ("kb", 64), ("v", 128)):
                v3 = src32[key][:, :].rearrange("p (n s) -> p n s", s=16)
                for l in range(32):
                    a, r = l // 16, l % 16
                    tk.op("dve", lambda e: e.tensor_scalar(out=zl[key][:, l, :], in0=v3[:, a:a + 127, r],
                                                           scalar1=pe[key][:, l:l + 1], scalar2=None, op0=ALU.add),
                          reads=[src_b, pe_b], writes=[zlb[key][l]])
            hid = {"k": sb2("c_hk", [128, 2, 128], BF16), "v": sb2("c_hv", [128, 2, 128], BF16)}
            hid_b = tk.buf()
            for hc in range(2):
                sa_, sab = cx.fetch(w1k_d, 0, 32, hc * 128, 128, pk=128, rstride=192)
                sb_, sbb = cx.fetch(w1k_d, 128, 32, hc * 128, 128, pk=64, rstride=192)
                ps, pb = cx.ps[hc], cx.ps_b[hc]
                for l in range(32):
                    tk.op("pe", lambda e: e.matmul(ps[:, 0:127], lhsT=sa_[:, l, :], rhs=zl["ka"][:, l, :], start=(l == 0), stop=False),
                          reads=[sab, zlb["ka"][l]], writes=[pb], pe_acc=(l > 0))
                    tk.op("pe", lambda e: e.matmul(ps[:, 0:127], lhsT=sb_[:, l, :], rhs=zl["kb"][:, l, :], start=False, stop=(l == 31)),
                          reads=[sbb, zlb["kb"][l]], writes=[pb], pe_acc=True)
                tk.op("act", lambda e: e.activation(out=hid["k"][:, hc, 0:127], in_=ps[:, 0:127], func=AF.Silu),
                      reads=[pb], writes=[hid_b])
            for hc in range(2):
                sv_, svb = cx.fetch(w1v_d, 0, 32, hc * 128, 128, pk=128, rstride=128)
                ps, pb = cx.ps[2 + hc], cx.ps_b[2 + hc]
                for l in range(32):
                    tk.op("pe", lambda e: e.matmul(ps[:, 0:127], lhsT=sv_[:, l, :], rhs=zl["v"][:, l, :], start=(l == 0), stop=(l == 31)),
                          reads=[svb, zlb["v"][l]], writes=[pb], pe_acc=(l > 0))
                tk.op("act", lambda e: e.activation(out=hid["v"][:, hc, 0:127], in_=ps[:, 0:127], func=AF.Silu),
                      reads=[pb], writes=[hid_b])
            w2k, w2kb = cx.fetch(w2k_d, 0, 2, 0, 192)
            w2v, w2vb = cx.fetch(w2v_d, 0, 2, 0, 128)
            for hc in range(2):
                tk.op("pe", lambda e: e.matmul(cx.ps[4][:, 0:127], lhsT=w2k[:, hc, 0:128], rhs=hid["k"][:, hc, 0:127],
                                               start=(hc == 0), stop=(hc == 1)), reads=[w2kb, hid_b], writes=[cx.ps_b[4]], pe_acc=(hc > 0))
            for hc in range(2):
                tk.op("pe", lambda e: e.matmul(cx.ps[5][0:64, 0:127], lhsT=w2k[:, hc, 128:192], rhs=hid["k"][:, hc, 0:127],
                                               start=(hc == 0), stop=(hc == 1)), reads=[w2kb, hid_b], writes=[cx.ps_b[5]], pe_acc=(hc > 0))
            for hc in range(2):
                tk.op("pe", lambda e: e.matmul(cx.ps[6][0:127, 0:128], lhsT=hid["v"][:, hc, 0:127], rhs=w2v[:, hc, :],
                                               start=(hc == 0), stop=(hc == 1)), reads=[w2vb, hid_b], writes=[cx.ps_b[6]], pe_acc=(hc > 0))
            tk.op("dve", lambda e: e.tensor_copy(out=kcTa[:, 0:127], in_=cx.ps[4][:, 0:127]), reads=[cx.ps_b[4]], writes=[kc_b])
            tk.op("dve", lambda e: e.tensor_copy(out=kcTb[:, 0:127], in_=cx.ps[5][0:64, 0:127]), reads=[cx.ps_b[5]], writes=[kc_b])
            tk.op("dve", lambda e: e.tensor_copy(out=vc[0:127, :], in_=cx.ps[6][0:127, 0:128]), reads=[cx.ps_b[6]], writes=[kc_b])
            tk.barrier()

        acc = sbt("n_acc", [128, 4, S], F32); acc_b = tk.buf()
        negT = sbt("n_negT", [32, S], BF16); negT_b = tk.buf()
        with ExitStack() as es2:
            t_ov = es2.enter_context(_sbt(nc, "n_tov", [128, 33], F32))
            t_ex = es2.enter_context(_sbt(nc, "n_tex", [32, 16 * 128], F32))
            ld(t_ov[:, :], ov_d[:, :]); ld(t_ex[:, :], ex_d[:, :])
            tk.op("dve", lambda e: e.tensor_copy(out=ov1[:, :], in_=t_ov[:, :]), reads=[ld_b], writes=[ld_b])
            tk.op("dve", lambda e: e.tensor_copy(out=expd[:, :, :].rearrange("p a b -> p (a b)"), in_=t_ex[:, :]), reads=[ld_b], writes=[ld_b])
            tk.barrier()

        NBN = 3
        tm = [sbt("n_tm%d" % i, [128, 512], F32) for i in range(NBN)]; tm_b = tk.bufs(NBN)
        pt = [sbt("n_pt%d" % i, [128, 512], BF16) for i in range(NBN)]; pt_b = tk.bufs(NBN)
        rl = sbt("n_rl", [128, 512], F32); rl_b = tk.buf()
        rlg = sbt("n_rlg", [128, 512], F32); rlg_b = tk.buf()
        tmp = sbt("n_tmp", [128, 512], F32); tmp_b = tk.buf()
        cc = [0]

        def combine(br, j, qt, O_id, L_id, G_id=None):
            qsl = slice(qt * 512, (qt + 1) * 512)
            if G_id is None:
                G_id = 6 + cc[0] % 2
                cc[0] += 1
            tk.op("pe", lambda e: e.matmul(cx.ps[G_id][:, :], lhsT=gsel[:, j * 3 + br, :], rhs=gsig[:, qsl], start=True, stop=True),
                  reads=[ld_b], writes=[cx.ps_b[G_id]])
            tk.op("dve", lambda e: e.tensor_scalar(out=rl[:, :], in0=cx.ps[L_id][:, :], scalar1=1e-30, scalar2=None, op0=ALU.max),
                  reads=[cx.ps_b[L_id]], writes=[rl_b])
            tk.op("dve", lambda e: e.reciprocal(out=rl[:, :], in_=rl[:, :]), writes=[rl_b])
            tk.op("dve", lambda e: e.tensor_tensor(out=rlg[:, :], in0=cx.ps[G_id][:, :], in1=rl[:, :], op=ALU.mult),
                  reads=[cx.ps_b[G_id], rl_b], writes=[rlg_b])
            if br == 0:
                tk.op("dve", lambda e: e.tensor_tensor(out=acc[:, j, qsl], in0=cx.ps[O_id][:, :], in1=rlg[:, :], op=ALU.mult),
                      reads=[cx.ps_b[O_id], rlg_b], writes=[acc_b])
            else:
                tk.op("dve", lambda e: e.tensor_tensor(out=tmp[:, :], in0=cx.ps[O_id][:, :], in1=rlg[:, :], op=ALU.mult),
                      reads=[cx.ps_b[O_id], rlg_b], writes=[tmp_b])
                tk.op("pool", lambda e: e.tensor_tensor(out=acc[:, j, qsl], in0=acc[:, j, qsl], in1=tmp[:, :], op=ALU.add),
                      reads=[tmp_b], writes=[acc_b])

        mark(tk, 'nsa.cmp')
        with ExitStack() as es2:
            eT = es2.enter_context(_sbt(nc, "n_eT", [128, 4, S], BF16)); eT_b = tk.buf()
            with ExitStack() as es3:
                bmc = [es3.enter_context(_sbt(nc, "n_bmc%d" % i, [128, S // 2], F32)) for i in range(2)]; bmc_b = tk.bufs(2)
                eTb = [[tk.buf() for _ in range(NT)] for _ in range(4)]
                items = [(j, qt, j * NT + qt) for j in range(4) for qt in range(NT)]

                def strip(u):
                    j, qh = u // 2, u % 2
                    tk.dma("sp", bmc[u % 2][0:127, :], bass.AP(gc_d, (hb + j) * 128 * GL + 2017 + qh * (S // 2), [[GL - 16, 127], [1, S // 2]]),
                           writes=[bmc_b[u % 2]])
                strip(0)

                def cA(it):
                    j, qt, n = it
                    u = j * 2 + qt // 2
                    if qt % 2 == 0 and u + 1 < 8:
                        strip(u + 1)
                    qsl = slice(qt * 512, (qt + 1) * 512)
                    sid, i = n % 2, n % NBN
                    ps, pb = cx.ps[sid], cx.ps_b[sid]
                    tk.op("pe", lambda e: e.matmul(ps[0:127, :], lhsT=kcTa[:, 0:127], rhs=nqa[:, j, qsl], start=True, stop=False),
                          reads=[kc_b, ld_b], writes=[pb])
                    tk.op("pe", lambda e: e.matmul(ps[0:127, :], lhsT=kcTb[:, 0:127], rhs=nqb[:, j, qsl], start=False, stop=True),
                          reads=[kc_b, ld_b], writes=[pb], pe_acc=True)
                    tk.op("dve", lambda e: e.scalar_tensor_tensor(out=tm[i][0:127, :], in0=ps[0:127, :], scalar=NSA_SCALE,
                                                                  in1=bmc[u % 2][0:127, (qt % 2) * 512:(qt % 2 + 1) * 512], op0=ALU.mult, op1=ALU.add),
                          reads=[pb, bmc_b[u % 2]], writes=[tm_b[i]])
                    tk.op("act", lambda e: e.activation(out=eT[0:127, j, qsl], in_=tm[i][0:127, :], func=AF.Exp),
                          reads=[tm_b[i]], writes=[eTb[j][qt]])

                def cB(it):
                    j, qt, n = it
                    qsl = slice(qt * 512, (qt + 1) * 512)
                    O_id, L_id = 2 + n % 2, 4 + n % 2
                    tk.op("pe", lambda e: e.matmul(cx.ps[O_id][:, :], lhsT=vc[0:127, :], rhs=eT[0:127, j, qsl], start=True, stop=True),
                          reads=[kc_b, eTb[j][qt]], writes=[cx.ps_b[O_id]])
                    tk.op("pe", lambda e: e.matmul(cx.ps[L_id][:, :], lhsT=cx.ones[0:127, :], rhs=eT[0:127, j, qsl], start=True, stop=True),
                          reads=[cx.ones_b, eTb[j][qt]], writes=[cx.ps_b[L_id]])
                    combine(0, j, qt, O_id, L_id)
                run_pipelined(items, cA, cB, depth=1)
                tk.barrier()
            mark(tk, 'nsa.topk')
            NQQ = 16
            mk = lambda nm, shp: [es2.enter_context(_sbt(nc, "%s%d" % (nm, q), shp, F32)) for q in range(NQQ)]
            l4, imp, sc2, m8, thr, neg = mk("n_l4", [128, 4]), mk("n_imp", [128, 32]), mk("n_sc2", [128, 32]), mk("n_m8", [128, 16]), \
                mk("n_thr", [128, 1]), mk("n_neg", [128, 32])
            tb = tk.bufs(NQQ)
            psI = lambda qq: (cx.ps[qq // 3], cx.ps_b[qq // 3], (qq % 3) * 132)

            def s_mm(qq):
                ps, pb, c0 = psI(qq)
                for j in range(4):
                    tk.op("pe", lambda e: e.matmul(ps[:, c0 + j * 33:c0 + (j + 1) * 33], lhsT=eT[0:127, j, qq * 128:(qq + 1) * 128],
                                                   rhs=ov1[0:127, :], start=True, stop=True), reads=[eTb[j][qq // 4], ld_b], writes=[pb], pe_acc=True)

            def s_l4(qq):
                ps, pb, c0 = psI(qq)
                lv = ps[:, c0:c0 + 132].rearrange("p (j c) -> p j c", c=33)[:, :, 32]
                tk.op("dve", lambda e: e.tensor_scalar(out=l4[qq][:, :], in0=lv, scalar1=1e-30, scalar2=None, op0=ALU.max),
                      reads=[pb], writes=[tb[qq]])

            def s_rc(qq):
                tk.op("dve", lambda e: e.reciprocal(out=l4[qq][:, :], in_=l4[qq][:, :]), writes=[tb[qq]])

            def s_imp(j):
                def f(qq):
                    ps, pb, c0 = psI(qq)
                    if j == 0:
                        tk.op("dve", lambda e: e.tensor_scalar(out=imp[qq][:, :], in0=ps[:, c0:c0 + 32], scalar1=l4[qq][:, 0:1], scalar2=None,
                                                               op0=ALU.mult), reads=[pb], writes=[tb[qq]])
                    else:
                        tk.op("dve", lambda e: e.scalar_tensor_tensor(out=imp[qq][:, :], in0=ps[:, c0 + j * 33:c0 + j * 33 + 32],
                                                                      scalar=l4[qq][:, j:j + 1], in1=imp[qq][:, :], op0=ALU.mult, op1=ALU.add),
                              reads=[pb], writes=[tb[qq]])
                return f

            def s_sa(qq):
                tk.op("dve", lambda e: e.tensor_tensor(out=imp[qq][:, :], in0=imp[qq][:, :], in1=scA[:, qq, :], op=ALU.mult), reads=[ld_b], writes=[tb[qq]])

            def s_sb(qq):
                tk.op("dve", lambda e: e.tensor_tensor(out=imp[qq][:, :], in0=imp[qq][:, :], in1=scB[:, qq, :], op=ALU.add), reads=[ld_b], writes=[tb[qq]])

            def s_m1(qq):
                tk.op("dve", lambda e: e.max(out=m8[qq][:, 0:8], in_=imp[qq][:, :]), writes=[tb[qq]])

            def s_mr(qq):
                tk.op("dve", lambda e: e.match_replace(out=sc2[qq][:, :], in_to_replace=m8[qq][:, 0:8], in_values=imp[qq][:, :], imm_value=-1e30),
                      writes=[tb[qq]])

            def s_m2(qq):
                tk.op("dve", lambda e: e.max(out=m8[qq][:, 8:16], in_=sc2[qq][:, :]), writes=[tb[qq]])

            def s_th(qq):
                tk.op("dve", lambda e: e.tensor_scalar(out=thr[qq][:, :], in0=m8[qq][:, 15:16], scalar1=0.0, scalar2=None, op0=ALU.max), writes=[tb[qq]])

            def s_ng(qq):
                tk.op("dve", lambda e: e.tensor_scalar(out=neg[qq][:, :], in0=imp[qq][:, :], scalar1=thr[qq][:, 0:1], scalar2=None, op0=ALU.is_ge),
                      writes=[tb[qq]])

            def s_n2(qq):
                tk.op("dve", lambda e: e.tensor_scalar(out=neg[qq][:, :], in0=neg[qq][:, :], scalar1=-NEGM, scalar2=NEGM, op0=ALU.mult, op1=ALU.add),
                      writes=[tb[qq]])

            def s_tr(qq):
                tp, tpb = cx.ps[6 + qq % 2], cx.ps_b[6 + qq % 2]
                tk.op("pe", lambda e: e.transpose(out=tp[0:32, 0:128], in_=neg[qq][:, :], identity=ident[:, :]), reads=[tb[qq], ld_b], writes=[tpb])
                tk.op("act", lambda e: e.copy(out=negT[:, qq * 128:(qq + 1) * 128], in_=tp[0:32, 0:128]), reads=[tpb], writes=[negT_b])
            for step in (s_mm, s_l4, s_rc, s_imp(0), s_imp(1), s_imp(2), s_imp(3), s_sa, s_sb, s_m1, s_mr, s_m2, s_th, s_ng, s_n2, s_tr):
                for qq in range(NQQ):
                    step(qq)
            tk.barrier()

        for br in (1, 2):
            mark(tk, 'nsa.br%d' % br)
            with ExitStack() as es2:
                W_ = 1152 if br == 1 else 1408
                bm = [es2.enter_context(_sbt(nc, "n_bm%d_%d" % (br, i), [128, W_], F32)) for i in range(2)]
                bm_b = tk.bufs(2)
                g_tab = gc_d if br == 1 else gw_d
                ka_, kb_ = (ksa, ksb) if br == 1 else (kwa, kwb)
                voff = 0 if br == 1 else 128
                items = []
                g = 0
                for j in range(4):
                    for qt in range(NT):
                        kts = list(range(0, 4 * qt + 4)) if br == 1 else list(range(max(0, 4 * qt - 4), 4 * qt + 4))
                        for n_, kt in enumerate(kts):
                            items.append((j, qt, kt, n_, len(kts), g, len(items)))
                        g += 1

                def phA(it):
                    j, qt, kt, n_, nk_, g, n = it
                    if qt == 0 and n_ == 0:
                        tk.dma("sp", bm[j % 2][:, :], bass.AP(g_tab, (hb + j) * 128 * GL + 1664, [[GL - 1, 128], [1, W_]]), writes=[bm_b[j % 2]])
                    qsl = slice(qt * 512, (qt + 1) * 512)
                    ksl = slice(kt * 128, (kt + 1) * 128)
                    i = n % NBN
                    ps, pb = cx.ps[i], cx.ps_b[i]
                    tk.op("pe", lambda e: e.matmul(ps[:, :], lhsT=ka_[:, ksl], rhs=nqa[:, j, qsl], start=True, stop=False),
                          reads=[ld_b], writes=[pb])
                    tk.op("pe", lambda e: e.matmul(ps[:, :], lhsT=kb_[:, ksl], rhs=nqb[:, j, qsl], start=False, stop=(br == 2)),
                          reads=[ld_b], writes=[pb], pe_acc=True)
                    if br == 1:
                        tk.op("pe", lambda e: e.matmul(ps[:, :], lhsT=expd[:, kt, :], rhs=negT[:, qsl], start=False, stop=True),
                              reads=[ld_b, negT_b], writes=[pb], pe_acc=True)
                    delta = qt * 512 - kt * 128
                    off = min(delta, 256) + 384 if br == 1 else delta + 384
                    tk.op("dve", lambda e: e.scalar_tensor_tensor(out=tm[i][:, :], in0=ps[:, :], scalar=NSA_SCALE,
                                                                  in1=bm[j % 2][:, off:off + 512], op0=ALU.mult, op1=ALU.add),
                          reads=[pb, bm_b[j % 2]], writes=[tm_b[i]])
                    tk.op("act", lambda e: e.activation(out=pt[i][:, :], in_=tm[i][:, :], func=AF.Exp),
                          reads=[tm_b[i]], writes=[pt_b[i]])

                def phB(it):
                    j, qt, kt, n_, nk_, g, n = it
                    i = n % NBN
                    O_id, L_id = 3 + g % 2, 5 + g % 2
                    tk.op("pe", lambda e: e.matmul(cx.ps[O_id][:, :], lhsT=vsw[:, kt, voff:voff + 128], rhs=pt[i][:, :],
                                                   start=(n_ == 0), stop=(n_ == nk_ - 1)),
                          reads=[ld_b, pt_b[i]], writes=[cx.ps_b[O_id]], pe_acc=(n_ > 0))
                    tk.op("pe", lambda e: e.matmul(cx.ps[L_id][:, :], lhsT=cx.ones[:, :], rhs=pt[i][:, :],
                                                   start=(n_ == 0), stop=(n_ == nk_ - 1)),
                          reads=[cx.ones_b, pt_b[i]], writes=[cx.ps_b[L_id]], pe_acc=(n_ > 0))
                    if n_ == nk_ - 1:
                        combine(br, j, qt, O_id, L_id, G_id=7)
                run_pipelined(items, phA, phB, depth=2)
                tk.barrier()
        mark(tk, 'nsa.out')
        ob = [sbt("n_ob%d" % i, [128, S], BF16) for i in range(2)]; ob_b = tk.bufs(2)
        for j in range(4):
            tk.op("act", lambda e: e.copy(out=ob[j % 2][:, :], in_=acc[:, j, :]), reads=[acc_b], writes=[ob_b[j % 2]])
            tk.dma("sp", bT_d[orow(hh) + j * 128:orow(hh) + (j + 1) * 128, :], ob[j % 2][:, :], reads=[ob_b[j % 2]])
        tk.barrier()


def attn_consts():
    n_cmp, n_slc = 127, 32
    cs = np.arange(n_cmp) * 16
    ce = cs + 31
    bs = np.arange(n_slc) * 64
    be = bs + 63
    ov = ((cs[:, None] <= be[None, :]) & (ce[:, None] >= bs[None, :])).astype(np.float32)
    ov1 = np.zeros((128, 33), np.float32)
    ov1[:127, :32] = ov
    ov1[:127, 32] = 1.0
    t = np.arange(SEQ)
    cur = t // 64
    jb = np.arange(n_slc)
    valid = jb[None, :] <= cur[:, None]
    forced = valid & ((jb[None, :] == 0) | (jb[None, :] >= cur[:, None] - 1))
    A = (valid & ~forced).astype(np.float32)
    B = np.where(forced, 1e6, np.where(valid, 0.0, -1.0)).astype(np.float32)
    A = np.ascontiguousarray(A.reshape(16, 128, 32).transpose(1, 0, 2))
    B = np.ascontiguousarray(B.reshape(16, 128, 32).transpose(1, 0, 2))
    ex = np.zeros((32, 16, 128), np.float32)
    for kt in range(16):
        ex[2 * kt, kt, :64] = 1.0
        ex[2 * kt + 1, kt, 64:] = 1.0
    gsel = np.zeros((12, 12, 128), np.float32)
    for r in range(12):
        gsel[r, r, :] = 1.0
    return {"ov1": ov1, "scoreA": A, "scoreB": B, "expand": ex.reshape(32, 2048), "gsel": gsel.reshape(12, 12 * 128),
            "ident": np.eye(128, dtype=np.float32)}


def stream(cx, specs, consume):
    pend = cx.fetch(*specs[0])
    for i in range(len(specs)):
        nxt = cx.fetch(*specs[i + 1]) if i + 1 < len(specs) else None
        consume(i, pend[0], pend[1])
        pend = nxt


def stage_merge(cx, h_d, aT_d, bT_d, out_d, gains, gcol_pre, gcol_post, wmg, wa, wb, wo, T, cbase=0):
    tk, nc = cx.tk, cx.nc
    mark(tk, 'merge')
    g_sb, g_b = gains
    nt = T // 512
    with ExitStack() as es:
        mT = es.enter_context(_sbt(nc, "mg_m", [128, 16, T], BF16)); m_b = tk.bufs(16)
        with ExitStack() as es2:
            uT = es2.enter_context(_sbt(nc, "mg_u", [128, 16, T], BF16)); u_b = tk.bufs(16)
            aS = es2.enter_context(_sbt(nc, "mg_a", [128, 8, T], BF16)); bS = es2.enter_context(_sbt(nc, "mg_b", [128, 8, T], BF16))
            ab_b = tk.buf()
            tk.dma("sp", aS[:, :, :], aT_d.rearrange("(k p) t -> p k t", p=128), writes=[ab_b])
            tk.dma("sp", bS[:, :, :], bT_d.rearrange("(k p) t -> p k t", p=128), writes=[ab_b], join=True)
            norm_from_dram(cx, h_d, g_sb[:, gcol_pre:gcol_pre + 16], g_b, uT, u_b, T, D, [0, 1, 2, 3])
            sg = [es2.enter_context(_sbt(nc, "mg_sg%d" % i, [128, 512], F32)) for i in range(2)]; sg_b = tk.bufs(2)
            t1 = [es2.enter_context(_sbt(nc, "mg_t%d" % i, [128, 512], F32)) for i in range(2)]; t1_b = tk.bufs(2)
            specs = []
            for o in range(16):
                specs += [(wmg, 0, 16, cbase + o * 128, 128), (wmg, 0, 16, cbase + 2048 + o * 128, 128), (wa, 0, 8, o * 128, 128), (wb, 0, 8, o * 128, 128)]

            def consume(i, view, wbuf):
                o, kind = i // 4, i % 4
                nk = 16 if kind < 2 else 8
                src, src_bufs = (uT, u_b) if kind < 2 else ((aS, [ab_b] * 8) if kind == 2 else (bS, [ab_b] * 8))
                for t in range(nt):
                    pid = kind * 2 + t
                    ps, pb = cx.ps[pid], cx.ps_b[pid]
                    for k in range(nk):
                        tk.op("pe", lambda e: e.matmul(ps[:, :], lhsT=view[:, k, :], rhs=src[:, k, t * 512:(t + 1) * 512],
                                                       start=(k == 0), stop=(k == nk - 1)),
                              reads=[wbuf, src_bufs[k]], writes=[pb], pe_acc=(k > 0))
                if kind == 3:
                    for t in range(nt):
                        sl = slice(t * 512, (t + 1) * 512)
                        for br in range(2):
                            gid, pid = br * 2 + t, 4 + br * 2 + t
                            tk.op("act", lambda e: e.activation(out=sg[br][:, :], in_=cx.ps[gid][:, :], func=AF.Sigmoid),
                                  reads=[cx.ps_b[gid]], writes=[sg_b[br]])
                            tk.op("dve", lambda e: e.tensor_tensor(out=t1[br][:, :], in0=cx.ps[pid][:, :], in1=sg[br][:, :], op=ALU.mult),
                                  reads=[cx.ps_b[pid], sg_b[br]], writes=[t1_b[br]])
                        tk.op("pool", lambda e: e.tensor_tensor(out=mT[:, o, sl], in0=t1[0][:, :], in1=t1[1][:, :], op=ALU.add),
                              reads=[t1_b[0], t1_b[1]], writes=[m_b[o]])
            stream(cx, specs, consume)
            tk.barrier()
        yT = es.enter_context(_sbt(nc, "mg_y", [128, 16, T], F32)); y_b = tk.bufs(16)

        def epi(ci, m, t, ps, pb):
            sl = slice(t * 512, (t + 1) * 512)
            if (ci + t) % 2:
                tk.op("act", lambda e: e.copy(out=yT[:, ci, sl], in_=ps[:, :]), reads=[pb], writes=[y_b[ci]])
            else:
                tk.op("dve", lambda e: e.tensor_copy(out=yT[:, ci, sl], in_=ps[:, :]), reads=[pb], writes=[y_b[ci]])
        gemm_fm(cx, wo, 16, col_slabs(0, D, 256), mT, m_b, T, [0, 1, 2, 3, 4, 5, 6, 7], epi)
        resid_tail(cx, yT, y_b, h_d, g_sb[:, gcol_post:gcol_post + 16], g_b, out_d, T, [0, 1, 2, 3])


def build_ca_prog(with_next, T=1024):
    nc = bass.Bass("TRN2", target_bir_lowering=False)
    din = lambda n, s, d=F32: nc.dram_tensor(n, s, d, kind="ExternalInput").ap()
    h_d = din("hT", [D, T])
    aT_d = din("aT", [1024, T], BF16)
    bT_d = din("bT", [1024, T], BF16)
    g_d = din("gains", [128, 96])
    wmg = din("wmg", [D, 4096]); wa = din("wa", [1024, D]); wb = din("wb", [1024, D]); wo = din("wo", [D, D])
    w2g = din("w2g", [D, FF]); w2u = din("w2u", [D, FF]); w2d = din("w2d", [FF, D])
    if with_next:
        w1g = din("w1g", [D, FF]); w1u = din("w1u", [D, FF]); w1d = din("w1d", [FF, D])
        hn_d = nc.dram_tensor("hnT", [D, T], F32, kind="ExternalOutput").ap()
    x_d = nc.dram_tensor("xT", [D, T], F32, kind="ExternalOutput").ap()
    h2_d = nc.dram_tensor("h2T", [D, T], F32, kind="Internal").ap()
    with ExitStack() as es:
        cx = Ctx(nc, es)
        gains = load_gains(cx, es, g_d, 96, half_cols=[(48, 64), (80, 96)])
        stage_merge(cx, h_d, aT_d, bT_d, h2_d, gains, 0, 16, wmg, wa, wb, wo, T)
        stage_ffn(cx, h2_d, x_d, gains, 32, 48, w2g, w2u, w2d, T)
        if with_next:
            stage_ffn(cx, x_d, hn_d, gains, 64, 80, w1g, w1u, w1d, T)
        cx.tk.barrier()
    return nc


N_LAUNCH_CORES = 4
GPL = 104


def build_fused_prog():
    nc = bass.Bass("TRN2", target_bir_lowering=False)
    din = lambda n, s, d=F32: nc.dram_tensor(n, s, d, kind="ExternalInput").ap()
    x_d = din("xT", [D, S])
    pos_d = din("pos", [1, S], I32)
    rb_d = din("rbT", [8, 32])
    rc_d = din("ropec", [64, 2])
    g_d = din("gains", [128, GPL * DEPTH])
    W = {}
    for nm, shp in (("f1g", [D, FF]), ("f1u", [D, FF]), ("f1d", [FF, D]), ("f2g", [D, FF]), ("f2u", [D, FF]), ("f2d", [FF, D]),
                    ("win", [D, 8664]), ("wq", [512, 1536]), ("wuk", [512, 1024]), ("wuv", [512, 1024]),
                    ("pekT", [192, 32]), ("w1k", [6144, 256]), ("w2k", [256, 192]), ("pevT", [128, 32]), ("w1v", [4096, 256]),
                    ("w2v", [256, 128]), ("wa", [1024, D]), ("wb", [1024, D]), ("wo", [D, D])):
        W[nm] = (din(nm, [DEPTH * shp[0], shp[1]]), shp[0])
    wl = lambda nm, l: W[nm][0][l * W[nm][1]:(l + 1) * W[nm][1], :]
    ov_d = din("ov1", [128, 33]); sa_d = din("scoreA", [128, 16, 32]); sbb_d = din("scoreB", [128, 16, 32])
    ex_d = din("expand", [32, 16 * 128]); sel_d = din("gsel", [12, 12 * 128]); id_d = din("ident", [128, 128])
    out_d = nc.dram_tensor("outT", [D, S], F32, kind="ExternalOutput").ap()
    di = lambda n, s, d=F32: nc.dram_tensor(n, s, d, kind="Internal")
    hA = di("hA", [D, S]).ap(); hB = di("hB", [D, S]).ap(); xb = [di("xb0", [D, S]).ap(), di("xb1", [D, S]).ap()]
    aT = di("aTi", [1024, S], BF16).ap(); bT = di("bTi", [1024, S], BF16).ap()
    z32 = di("z32", [NZ, S]).ap(); z16 = di("z16", [NZ, S], BF16).ap(); vtok = di("vtok", [S, 256], BF16).ap()
    gc_all = di("gc_all", [8, 128, GL]); gw_all = di("gw_all", [8, 128, GL])
    cos_i = di("cos_i", [64, S]).ap(); sin_i = di("sin_i", [64, S]).ap()
    T = 1024
    with ExitStack() as es:
        cx = Ctx(nc, es)
        halves = []
        for l in range(DEPTH):
            halves += [(l * GPL + 16, l * GPL + 32), (l * GPL + 80, l * GPL + 96)]
        gains = load_gains(cx, es, g_d, GPL * DEPTH, half_cols=halves)
        p0_tables(cx.tk, nc, rb_d, pos_d, rc_d,
                  [(h, bass.AP(gc_all, h * 128 * GL, [[GL, 128], [1, GL]]), bass.AP(gw_all, h * 128 * GL, [[GL, 128], [1, GL]]))
                   for h in range(8)], cos_i, sin_i)
        cur = x_d
        for l in range(DEPTH):
            gb = l * GPL
            for half in range(2):
                tsl = slice(half * T, (half + 1) * T)
                stage_ffn(cx, cur[:, tsl], hA[:, tsl], gains, gb + 0, gb + 16, wl("f1g", l), wl("f1u", l), wl("f1d", l), T)
            for hh in range(2):
                L = {"hT_d": hA, "g_d": None, "win_d": wl("win", l), "wq_d": wl("wq", l), "wuk_d": wl("wuk", l), "wuv_d": wl("wuv", l),
                     "pek_d": wl("pekT", l), "w1k_d": wl("w1k", l), "w2k_d": wl("w2k", l), "pev_d": wl("pevT", l),
                     "w1v_d": wl("w1v", l), "w2v_d": wl("w2v", l), "gc_d": gc_all, "gw_d": gw_all, "cos_d": cos_i, "sin_d": sin_i,
                     "ov_d": ov_d, "sa_d": sa_d, "sbb_d": sbb_d, "ex_d": ex_d, "sel_d": sel_d, "id_d": id_d, "aT_d": aT, "bT_d": bT,
                     "z32_d": z32, "z16_d": z16, "vtok_d": vtok, "hh": hh, "gcols": (gb + 32, gb + 96, gb + 100), "gains": gains}
                _attn_body(cx, es, nc, L)
                cx.tk.barrier()
            for half in range(2):
                tsl = slice(half * T, (half + 1) * T)
                stage_merge(cx, hA[:, tsl], aT[:, tsl], bT[:, tsl], hB[:, tsl], gains, gb + 32, gb + 48,
                            wl("win", l), wl("wa", l), wl("wb", l), wl("wo", l), T, cbase=4568)
            dst = out_d if l == DEPTH - 1 else xb[l % 2]
            for half in range(2):
                tsl = slice(half * T, (half + 1) * T)
                stage_ffn(cx, hB[:, tsl], dst[:, tsl], gains, gb + 64, gb + 80, wl("f2g", l), wl("f2u", l), wl("f2d", l), T)
            cur = dst
        cx.tk.barrier()
    return nc


_PROGS = {}


def kernel(x, positions, rel_bias,
           ffn1_pre_g, ffn1_post_g, ffn1_w_gate, ffn1_w_up, ffn1_w_down,
           mix_pre_g, mix_post_g, w_in,
           mla_q_norm_g, mla_w_q_up, mla_kv_norm_g, mla_w_uk, mla_w_uv,
           cmp_pe_k, cmp_w1_k, cmp_w2_k, cmp_pe_v, cmp_w1_v, cmp_w2_v,
           w_branch_mla, w_branch_nsa, w_out,
           ffn2_pre_g, ffn2_post_g, ffn2_w_gate, ffn2_w_up, ffn2_w_down):
    f = lambda a: np.ascontiguousarray(np.asarray(a, dtype=np.float32))
    st = lambda a: f(a).reshape(-1, np.asarray(a).shape[-1])
    x = f(x)
    positions = np.asarray(positions).astype(np.int32)
    gl = []
    for l in range(DEPTH):
        gl += [garr(ffn1_pre_g[l]), garr(ffn1_post_g[l]), garr(mix_pre_g[l]), garr(mix_post_g[l]), garr(ffn2_pre_g[l]),
               garr(ffn2_post_g[l]), garr(mla_q_norm_g[l]), garr(mla_kv_norm_g[l])]
    common = {"rbT": np.ascontiguousarray(f(rel_bias).T), "ropec": rope_consts(), "gains": np.concatenate(gl, axis=1),
              "f1g": st(ffn1_w_gate), "f1u": st(ffn1_w_up), "f1d": st(ffn1_w_down),
              "f2g": st(ffn2_w_gate), "f2u": st(ffn2_w_up), "f2d": st(ffn2_w_down),
              "win": st(w_in), "wq": st(mla_w_q_up), "wuk": st(mla_w_uk), "wuv": st(mla_w_uv),
              "pekT": np.ascontiguousarray(f(cmp_pe_k).transpose(0, 2, 1)).reshape(-1, 32), "w1k": st(cmp_w1_k), "w2k": st(cmp_w2_k),
              "pevT": np.ascontiguousarray(f(cmp_pe_v).transpose(0, 2, 1)).reshape(-1, 32), "w1v": st(cmp_w1_v), "w2v": st(cmp_w2_v),
              "wa": st(w_branch_mla), "wb": st(w_branch_nsa), "wo": st(w_out)}
    common.update(attn_consts())
    if "fused" not in _PROGS:
        _PROGS["fused"] = build_fused_prog()
    active = {0: 0, 1: 1, 2: 2, 3: 3} if N_LAUNCH_CORES == 4 else {0: 0, 1: 1, 4: 2, 5: 3}
    zeros = None
    maps = []
    for c in range(N_LAUNCH_CORES):
        if c in active:
            b = active[c]
            m = dict(common)
            m["xT"] = np.ascontiguousarray(x[b].T)
            m["pos"] = np.ascontiguousarray(positions[b][None, :])
        else:
            if zeros is None:
                zeros = {k: np.zeros_like(v) for k, v in common.items()}
                zeros["xT"] = np.zeros((D, S), np.float32)
                zeros["pos"] = np.zeros((1, S), np.int32)
            m = zeros
        maps.append(m)
    res = run_bass_kernel_spmd(_PROGS["fused"], maps, core_ids=list(range(N_LAUNCH_CORES))).results
    inv = {b: c for c, b in active.items()}
    out = np.stack([np.asarray(res[inv[b]]["outT"]).T for b in range(4)], axis=0)
    return np.ascontiguousarray(out.astype(np.float32))
```

```python
import math
import numpy as np
from contextlib import ExitStack
import concourse.bass as bass
import concourse.mybir as mybir
from concourse.bass_utils import run_bass_kernel_spmd

F32 = mybir.dt.float32
BF16 = mybir.dt.bfloat16
I32 = mybir.dt.int32
AF = mybir.ActivationFunctionType
ALU = mybir.AluOpType

D = 2048
FF = 5632
SEQ = 2048
DEPTH = 4
EPS = 1e-6
NEGM = -30000.0
GL = 4096
GOFF = 2048
DEAD = [False]
MARKS = []


def mark(tk, label):
    MARKS.append((label, tk.cnt['pe']))

_SBN = [0]


def _sbt(nc, name, shape, dt):
    _SBN[0] += 1
    return nc.sbuf_tensor("%s_u%d" % (name, _SBN[0]), shape, dt)


class Buf:
    __slots__ = ("name", "lw", "rd", "dsem")

    def __init__(self, name, dsem):
        self.name = name
        self.lw = None
        self.rd = {}
        self.dsem = dsem


class TK:
    ENG = ("pe", "act", "dve", "pool", "sp")

    def __init__(self, nc, es, n_dma_sems=48):
        self.nc = nc
        self.E = {"pe": nc.tensor, "act": nc.scalar, "dve": nc.vector,
                  "pool": nc.gpsimd, "sp": nc.sync}
        self.sem = {k: es.enter_context(nc.semaphore("s_" + k)) for k in ("pe", "act", "dve", "pool")}
        self.cnt = {k: 0 for k in self.sem}
        self.dsems = [es.enter_context(nc.semaphore("d%d" % i)) for i in range(n_dma_sems)]
        self.dtot = [0] * n_dma_sems
        self.seen = {k: {} for k in self.ENG}
        self._rr = 0
        self.nbuf = 0

    def buf(self, name=None):
        self.nbuf += 1
        b = Buf(name or ("b%d" % self.nbuf), self._rr)
        self._rr = (self._rr + 1) % len(self.dsems)
        return b

    def bufs(self, n):
        return [self.buf() for _ in range(n)]

    def _wait(self, eng, need):
        seen = self.seen[eng]
        for k2, val in need.items():
            kind, key = k2
            if kind == "dma":
                val = self.dtot[key]
            if seen.get(k2, 0) >= val:
                continue
            sem = self.sem[key] if kind == "eng" else self.dsems[key]
            self.E[eng].wait_ge(sem, val)
            seen[k2] = val

    @staticmethod
    def _add(need, ev):
        if ev is None:
            return
        k2 = (ev[0], ev[1])
        if need.get(k2, 0) < ev[2]:
            need[k2] = ev[2]

    def _deps(self, reads, writes):
        need = {}
        for b in reads:
            self._add(need, b.lw)
        for b in writes:
            self._add(need, b.lw)
            for k2, v in b.rd.items():
                if need.get(k2, 0) < v:
                    need[k2] = v
        return need

    def _record(self, ev, reads, writes):
        k2 = (ev[0], ev[1])
        for b in reads:
            if b.rd.get(k2, 0) < ev[2]:
                b.rd[k2] = ev[2]
        for b in writes:
            b.lw = ev
            b.rd = {}

    def op(self, eng, fn, reads=(), writes=(), pe_acc=False):
        if DEAD[0]:
            return None
        need = self._deps(reads, writes)
        if pe_acc:
            need.pop(("eng", "pe"), None)
        self._wait(eng, need)
        ins = fn(self.E[eng])
        self.cnt[eng] += 1
        ev = ("eng", eng, self.cnt[eng])
        ins.then_inc(self.sem[eng], 1)
        self._record(ev, reads, writes)
        return ev

    def dma(self, q, out, in_, reads=(), writes=(), join=False, anchor=None):
        if DEAD[0]:
            return None
        anchor = anchor or (list(writes) + list(reads))[0]
        si = anchor.dsem
        need = self._deps(reads, writes)
        if join:
            need.pop(("dma", si), None)
        self._wait(q, need)
        ins = self.E[q].dma_start(out=out, in_=in_)
        ins.then_inc(self.dsems[si], 16)
        self.dtot[si] += 16
        ev = ("dma", si, self.dtot[si])
        self._record(ev, reads, writes)
        return ev

    def barrier(self):
        need = {("eng", k): v for k, v in self.cnt.items() if v > 0}
        for i, v in enumerate(self.dtot):
            if v > 0:
                need[("dma", i)] = v
        for e in self.ENG:
            self._wait(e, dict(need))


class Ctx:
    def __init__(self, nc, es):
        self.nc = nc
        self.es = es
        self.tk = TK(nc, es)
        tk = self.tk
        self.ps = [es.enter_context(nc.psum_tensor("ps%d" % i, [128, 512], F32)) for i in range(8)]
        self.ps_b = tk.bufs(8)
        self.ones = es.enter_context(_sbt(nc, "ones_bf", [128, 128], BF16))
        self.ones_b = tk.buf()
        tk.op("dve", lambda e: e.memset(self.ones[:], 1.0), writes=[self.ones_b])
        self.stg = [es.enter_context(_sbt(nc, "wstg%d" % i, [128, 4096], F32)) for i in range(2)]
        self.stg_b = tk.bufs(2)
        self.slab = [es.enter_context(_sbt(nc, "wslab%d" % i, [128, 4096], BF16)) for i in range(2)]
        self.slab_b = tk.bufs(2)
        self.wi = 0
        self.cast_rr = 0

    def fetch(self, W, r0, nk, c0, M, pk=128, rstride=None):
        tk = self.tk
        assert nk * M <= 4096
        rstride = rstride or pk
        N = W.shape[1]
        j = self.wi % 2
        self.wi += 1
        stg, sb = self.stg[j], self.stg_b[j]
        src = bass.AP(W.tensor, W.offset + r0 * N + c0, [[N, pk], [rstride * N, nk], [1, M]])
        dst = stg[0:pk, 0:nk * M].rearrange("p (kc m) -> p kc m", m=M)
        step = max(1, 2048 // max(M, 1))
        first = True
        for k0 in range(0, nk, step):
            k1 = min(nk, k0 + step)
            tk.dma("sp", dst[:, k0:k1, :], src[:, k0:k1, :], writes=[sb], join=not first)
            first = False
        slab, lb = self.slab[j], self.slab_b[j]
        eng = ("pool", "dve", "act")[self.cast_rr % 3]
        self.cast_rr += 1
        if eng == "act":
            tk.op("act", lambda e: e.copy(out=slab[0:pk, 0:nk * M], in_=stg[0:pk, 0:nk * M]), reads=[sb], writes=[lb])
        else:
            tk.op(eng, lambda e: e.tensor_copy(out=slab[0:pk, 0:nk * M], in_=stg[0:pk, 0:nk * M]), reads=[sb], writes=[lb])
        return slab[0:pk, 0:nk * M].rearrange("p (kc m) -> p kc m", m=M), lb


def gemm_fm(cx, W, nk, slabs, inT, in_bufs, T, ps_ids, epi, r0=0):
    tk = cx.tk
    pend = cx.fetch(W, r0, nk, slabs[0][0], sum(slabs[0][1]))
    ci = 0
    pi = 0
    for si, (c0, ms) in enumerate(slabs):
        nxt = cx.fetch(W, r0, nk, slabs[si + 1][0], sum(slabs[si + 1][1])) if si + 1 < len(slabs) else None
        view, wb = pend
        off = 0
        for m in ms:
            for t in range(T // 512):
                pid = ps_ids[pi % len(ps_ids)]
                pi += 1
                ps, pb = cx.ps[pid], cx.ps_b[pid]
                for k in range(nk):
                    tk.op("pe", lambda e: e.matmul(ps[0:m, :], lhsT=view[:, k, off:off + m],
                                                   rhs=inT[:, k, t * 512:(t + 1) * 512],
                                                   start=(k == 0), stop=(k == nk - 1)),
                          reads=[wb] + list(in_bufs), writes=[pb], pe_acc=(k > 0))
                epi(ci, m, t, ps, pb)
            off += m
            ci += 1
        pend = nxt


def col_slabs(c0, n, width=256):
    out = []
    c = c0
    end = c0 + n
    while c < end:
        w = min(width, end - c)
        ms = [min(128, w - i) for i in range(0, w, 128)]
        out.append((c, ms))
        c += w
    return out


def ssq_rstd(cx, es, chunk_src, nchunk, T, Kdim, rstd, rstd_b, ps_ids, pk=128):
    tk, nc = cx.tk, cx.nc
    sq = [es.enter_context(_sbt(nc, "sq%d_%d" % (i, tk.nbuf), [128, T], BF16)) for i in range(2)]
    sq_b = tk.bufs(2)
    nt = T // 512
    for k in range(nchunk):
        ap, b = chunk_src(k)
        s, sb_ = sq[k % 2], sq_b[k % 2]
        tk.op("act", lambda e: e.activation(out=s[0:pk, :], in_=ap, func=AF.Square), reads=[b], writes=[sb_])
        for t in range(nt):
            pid = ps_ids[t]
            tk.op("pe", lambda e: e.matmul(cx.ps[pid][:, :], lhsT=cx.ones[0:pk, :], rhs=s[0:pk, t * 512:(t + 1) * 512],
                                           start=(k == 0), stop=(k == nchunk - 1)),
                  reads=[sb_, cx.ones_b], writes=[cx.ps_b[pid]], pe_acc=(k > 0))
    for t in range(nt):
        pid = ps_ids[t]
        sl = slice(t * 512, (t + 1) * 512)
        tk.op("act", lambda e: e.activation(out=rstd[:, sl], in_=cx.ps[pid][:, :], func=AF.Sqrt,
                                            scale=1.0 / Kdim, bias=cx.eps_ap),
              reads=[cx.ps_b[pid]], writes=[rstd_b])
    tk.op("dve", lambda e: e.reciprocal(out=rstd[:, :], in_=rstd[:, :]), reads=[rstd_b], writes=[rstd_b])


def norm_from_dram(cx, src_d, g_sb, g_b, uT, u_bufs, T, K, ps_ids):
    tk, nc = cx.tk, cx.nc
    nk = K // 128
    with ExitStack() as es:
        NXB = 4 if T <= 1024 else 2
        xs = [es.enter_context(_sbt(nc, "nx%d_%d" % (i, tk.nbuf), [128, T], F32)) for i in range(NXB)]
        xs_b = tk.bufs(NXB)
        rstd = es.enter_context(_sbt(nc, "nrstd_%d" % tk.nbuf, [128, T], F32))
        rstd_b = tk.buf()

        def src(k):
            tk.dma("sp", xs[k % NXB][:, :], src_d[k * 128:(k + 1) * 128, :], writes=[xs_b[k % NXB]])
            return xs[k % NXB][:, :], xs_b[k % NXB]
        ssq_rstd(cx, es, src, nk, T, K, rstd, rstd_b, ps_ids)
        for k in range(nk):
            ap, b = src(k)
            tk.op("dve", lambda e: e.scalar_tensor_tensor(out=uT[:, k, :], in0=ap, scalar=g_sb[:, k:k + 1],
                                                          in1=rstd[:, :], op0=ALU.mult, op1=ALU.mult),
                  reads=[b, rstd_b, g_b], writes=[u_bufs[k]])
        tk.barrier()


def resid_tail(cx, yT, y_bufs, x_d, gf_sb, gf_b, out_d, T, ps_ids):
    tk, nc = cx.tk, cx.nc
    with ExitStack() as es:
        rstd = es.enter_context(_sbt(nc, "trstd_%d" % tk.nbuf, [128, T], F32))
        rstd_b = tk.buf()
        ssq_rstd(cx, es, lambda k: (yT[:, k, :], y_bufs[k]), 16, T, D, rstd, rstd_b, ps_ids)
        xs = [es.enter_context(_sbt(nc, "tx%d_%d" % (i, tk.nbuf), [128, T], F32)) for i in range(4)]
        xs_b = tk.bufs(4)
        for k in range(16):
            x, xb = xs[k % 4], xs_b[k % 4]
            tk.dma("sp", x[:, :], x_d[k * 128:(k + 1) * 128, :], writes=[xb])
            tk.op("dve", lambda e: e.scalar_tensor_tensor(out=yT[:, k, :], in0=yT[:, k, :], scalar=gf_sb[:, k:k + 1],
                                                          in1=rstd[:, :], op0=ALU.mult, op1=ALU.mult),
                  reads=[rstd_b, gf_b], writes=[y_bufs[k]])
            tk.op("pool" if k % 3 == 0 else "dve", lambda e: e.tensor_tensor(out=x[:, :], in0=x[:, :], in1=yT[:, k, :], op=ALU.add),
                  reads=[y_bufs[k]], writes=[xb])
            tk.dma("sp", out_d[k * 128:(k + 1) * 128, :], x[:, :], reads=[xb])
        tk.barrier()


def stage_ffn(cx, x_d, out_d, gains, gcol_pre, gcol_post, Wg, Wu, Wd, T):
    tk, nc = cx.tk, cx.nc
    mark(tk, 'ffn')
    g_sb, g_b = gains
    NG = 4
    CPG = 11
    nt = T // 512
    with ExitStack() as es:
        yT = es.enter_context(_sbt(nc, "ffn_y_%d" % tk.nbuf, [128, 16, T], F32))
        y_b = tk.bufs(16)
        uT = es.enter_context(_sbt(nc, "ffn_u_%d" % tk.nbuf, [128, 16, T], BF16))
        u_b = tk.bufs(16)
        hT = es.enter_context(_sbt(nc, "ffn_h_%d" % tk.nbuf, [128, CPG, T], BF16))
        h_b = tk.bufs(CPG)
        sg = [es.enter_context(_sbt(nc, "ffn_sg%d_%d" % (i, tk.nbuf), [128, 512], F32)) for i in range(2)]
        sg_b = tk.bufs(2)
        specs, tags = [], []
        for grp in range(NG):
            f0 = grp * CPG * 128
            for c in range(0, CPG * 128, 256):
                w = min(256, CPG * 128 - c)
                specs.append((Wg, 0, 16, f0 + c, w)); tags.append(("g", grp, c, w))
                specs.append((Wu, 0, 16, f0 + c, w)); tags.append(("u", grp, c, w))
            for (c0, ms) in col_slabs(0, D, 256):
                specs.append((Wd, f0, CPG, c0, sum(ms))); tags.append(("d", grp, c0, ms))
        pend = cx.fetch(*specs[0])
        norm_from_dram(cx, x_d, g_sb[:, gcol_pre:gcol_pre + 16], g_b, uT, u_b, T, D, [0, 1, 2, 3])
        cnt = [0]
        dpi = [0]
        for si in range(len(specs)):
            nxt = cx.fetch(*specs[si + 1]) if si + 1 < len(specs) else None
            view, wb = pend
            kind, grp, c, w = tags[si]
            if kind in ("g", "u"):
                nch = w // 128
                for ci in range(nch):
                    n_loc = c // 128 + ci
                    for t in range(nt):
                        pid = (0 if kind == "g" else 2) + t + 4 * (n_loc % 2)
                        ps, pb = cx.ps[pid], cx.ps_b[pid]
                        for k in range(16):
                            tk.op("pe", lambda e: e.matmul(ps[:, :], lhsT=view[:, k, ci * 128:(ci + 1) * 128],
                                                           rhs=uT[:, k, t * 512:(t + 1) * 512],
                                                           start=(k == 0), stop=(k == 15)),
                                  reads=[wb, u_b[k]], writes=[pb], pe_acc=(k > 0))
                        if kind == "u":
                            gid = t + 4 * (n_loc % 2)
                            s_, sb_ = sg[cnt[0] % 2], sg_b[cnt[0] % 2]
                            cnt[0] += 1
                            tk.op("act", lambda e: e.activation(out=s_[:, :], in_=cx.ps[gid][:, :], func=AF.Silu),
                                  reads=[cx.ps_b[gid]], writes=[sb_])
                            tk.op("dve", lambda e: e.tensor_tensor(out=hT[:, n_loc, t * 512:(t + 1) * 512], in0=s_[:, :],
                                                                   in1=ps[:, :], op=ALU.mult),
                                  reads=[sb_, pb], writes=[h_b[n_loc]])
            else:
                ms = w
                off = 0
                for m in ms:
                    ci = (c + off) // 128
                    for t in range(nt):
                        pid = dpi[0] % 8
                        dpi[0] += 1
                        ps, pb = cx.ps[pid], cx.ps_b[pid]
                        for k in range(CPG):
                            tk.op("pe", lambda e: e.matmul(ps[0:m, :], lhsT=view[:, k, off:off + m], rhs=hT[:, k, t * 512:(t + 1) * 512],
                                                           start=(k == 0), stop=(k == CPG - 1)),
                                  reads=[wb, h_b[k]], writes=[pb], pe_acc=(k > 0))
                        sl = slice(t * 512, (t + 1) * 512)
                        if grp == 0:
                            tk.op("act", lambda e: e.copy(out=yT[:, ci, sl], in_=ps[:, :]), reads=[pb], writes=[y_b[ci]])
                        else:
                            tk.op("dve", lambda e: e.tensor_tensor(out=yT[:, ci, sl], in0=ps[:, :], in1=yT[:, ci, sl], op=ALU.add),
                                  reads=[pb], writes=[y_b[ci]])
                    off += m
            pend = nxt
        resid_tail(cx, yT, y_b, x_d, g_sb[:, gcol_post:gcol_post + 16], g_b, out_d, T, [0, 1, 2, 3])


def load_gains(cx, es, g_d, ncol, half_cols=()):
    tk, nc = cx.tk, cx.nc
    g_sb = es.enter_context(_sbt(nc, "gains_sb", [128, ncol], F32))
    g_b = tk.buf()
    tk.dma("sp", g_sb[:, :], g_d[:, :], writes=[g_b])
    for (c0, c1) in half_cols:
        tk.op("dve", lambda e: e.tensor_scalar(out=g_sb[:, c0:c1], in0=g_sb[:, c0:c1], scalar1=0.5, scalar2=None,
                                               op0=ALU.mult), reads=[g_b], writes=[g_b])
    eps = es.enter_context(_sbt(nc, "eps_t", [128, 1], F32))
    tk.op("dve", lambda e: e.memset(eps[:], EPS), writes=[g_b])
    cx.eps_ap = eps[:, 0:1]
    return g_sb, g_b


def build_ffn_prog(T=1024):
    nc = bass.Bass("TRN2", target_bir_lowering=False)
    x_d = nc.dram_tensor("xT", [D, T], F32, kind="ExternalInput").ap()
    g_d = nc.dram_tensor("gains", [128, 32], F32, kind="ExternalInput").ap()
    Wg = nc.dram_tensor("wg", [D, FF], F32, kind="ExternalInput").ap()
    Wu = nc.dram_tensor("wu", [D, FF], F32, kind="ExternalInput").ap()
    Wd = nc.dram_tensor("wd", [FF, D], F32, kind="ExternalInput").ap()
    o_d = nc.dram_tensor("outT", [D, T], F32, kind="ExternalOutput").ap()
    with ExitStack() as es:
        cx = Ctx(nc, es)
        gains = load_gains(cx, es, g_d, 32, half_cols=[(16, 32)])
        stage_ffn(cx, x_d, o_d, gains, 0, 16, Wg, Wu, Wd, T)
        cx.tk.barrier()
    return nc


def garr(g):
    return np.ascontiguousarray(np.asarray(g, dtype=np.float32).reshape(-1, 128).T)


def rel_thresholds():
    n = np.arange(0, 256)
    large = 16 + (np.log(np.maximum(n, 1).astype(np.float32) / np.float32(16)) / np.float32(math.log(128 / 16))
                  * np.float32(16)).astype(np.int32)
    large = np.minimum(large, 31)
    bucket = np.where(n < 16, n, large)
    return [int(np.argmax(bucket >= b)) for b in range(1, 32)]


def stage_p0(tk, nc, rb_d, pos_d, rc_d, heads):
    pass


def p0_tables(tk, nc, rb_d, pos_d, rc_d, head_dsts, cos_d, sin_d):
    thr = rel_thresholds()
    with ExitStack() as es:
        sb = lambda n, s, d=F32: es.enter_context(_sbt(nc, n, s, d))
        rb = sb("rb_sb", [128, 32]); rb_b = tk.buf()
        dt = sb("dtab", [128, 32]); dt_b = tk.buf()
        dg = sb("dgrid", [128, 128]); dg_b = tk.buf()
        tk.op("pool", lambda e: e.iota(dg[:, :], [[1, 128]], base=0, channel_multiplier=0,
                                       allow_small_or_imprecise_dtypes=True), writes=[dg_b])
        band = sb("band", [128, 128]); band_b = tk.buf()
        tmp = sb("btmp", [128, 128]); tmp_b = tk.buf()
        G = sb("gtab", [128, GL]); G_b = tk.buf()
        for (hrow, gc_dst, gw_dst) in head_dsts:
            tk.dma("sp", rb[:, :], rb_d[hrow:hrow + 1, :].to_broadcast([128, 32]), writes=[rb_b])
            tk.op("dve", lambda e: e.tensor_tensor(out=dt[:, 1:32], in0=rb[:, 1:32], in1=rb[:, 0:31], op=ALU.subtract),
                  reads=[rb_b], writes=[dt_b])
            tk.op("dve", lambda e: e.tensor_scalar(out=band[:, :], in0=dg[:, :], scalar1=0.0, scalar2=rb[:, 0:1],
                                                   op0=ALU.mult, op1=ALU.add), reads=[dg_b, rb_b], writes=[band_b])
            for b in range(1, 32):
                tk.op("dve", lambda e: e.tensor_scalar(out=tmp[:, :], in0=dg[:, :], scalar1=float(thr[b - 1]),
                                                       scalar2=dt[:, b:b + 1], op0=ALU.is_ge, op1=ALU.mult),
                      reads=[dg_b, dt_b], writes=[tmp_b])
                tk.op("dve", lambda e: e.tensor_tensor(out=band[:, :], in0=band[:, :], in1=tmp[:, :], op=ALU.add),
                      reads=[tmp_b], writes=[band_b])
            for kind, dst in (("c", gc_dst), ("w", gw_dst)):
                hi = GL if kind == "c" else GOFF + 512
                tk.op("pool", lambda e: e.memset(G[:, :], NEGM), writes=[G_b])
                tk.op("dve", lambda e: e.tensor_copy(out=G[:, GOFF:GOFF + 128], in_=band[:, :]), reads=[band_b], writes=[G_b])
                tk.op("dve", lambda e: e.tensor_scalar(out=G[:, GOFF + 128:hi], in0=G[:, GOFF + 128:hi], scalar1=0.0,
                                                       scalar2=rb[:, 31:32], op0=ALU.mult, op1=ALU.add),
                      reads=[rb_b], writes=[G_b])
                tk.dma("sp", dst, G[:, :], reads=[G_b])
        rc = sb("rc_sb", [64, 2]); rc_b = tk.buf()
        tk.dma("sp", rc[:, :], rc_d[:, :], writes=[rc_b])
        pi_ = sb("pos_i", [64, SEQ], I32); pi_b = tk.buf()
        tk.dma("sp", pi_[:, :], pos_d[0:1, :].to_broadcast([64, SEQ]), writes=[pi_b])
        ang = sb("ang", [64, SEQ]); ang_b = tk.buf()
        tk.op("dve", lambda e: e.tensor_copy(out=ang[:, :], in_=pi_[:, :]), reads=[pi_b], writes=[ang_b])
        tk.op("dve", lambda e: e.tensor_scalar(out=ang[:, :], in0=ang[:, :], scalar1=rc[:, 0:1], scalar2=None,
                                               op0=ALU.mult), reads=[rc_b], writes=[ang_b])
        kf = sb("kf", [64, SEQ]); kf_b = tk.buf()
        ki = sb("ki", [64, SEQ], I32); ki_b = tk.buf()
        r = sb("rr", [64, SEQ]); r_b = tk.buf()
        res_t = sb("rope_res", [64, SEQ]); res_b = tk.buf()
        C1 = 6.28125
        C2 = 2.0 * math.pi - C1
        for which, shift, dst in (("sin", 0.0, sin_d), ("cos", math.pi / 2, cos_d)):
            tk.op("dve", lambda e: e.tensor_scalar(out=kf[:, :], in0=ang[:, :], scalar1=shift, scalar2=1.0 / (2 * math.pi),
                                                   op0=ALU.add, op1=ALU.mult), reads=[ang_b], writes=[kf_b])
            tk.op("dve", lambda e: e.tensor_copy(out=ki[:, :], in_=kf[:, :]), reads=[kf_b], writes=[ki_b])
            tk.op("dve", lambda e: e.tensor_copy(out=kf[:, :], in_=ki[:, :]), reads=[ki_b], writes=[kf_b])
            tk.op("dve", lambda e: e.scalar_tensor_tensor(out=r[:, :], in0=kf[:, :], scalar=-C1, in1=ang[:, :],
                                                          op0=ALU.mult, op1=ALU.add), reads=[kf_b, ang_b], writes=[r_b])
            tk.op("dve", lambda e: e.tensor_scalar(out=r[:, :], in0=r[:, :], scalar1=shift, scalar2=None, op0=ALU.add),
                  writes=[r_b])
            tk.op("dve", lambda e: e.scalar_tensor_tensor(out=r[:, :], in0=kf[:, :], scalar=-C2, in1=r[:, :],
                                                          op0=ALU.mult, op1=ALU.add), reads=[kf_b], writes=[r_b])
            tk.op("dve", lambda e: e.tensor_scalar(out=r[:, :], in0=r[:, :], scalar1=3.1415925, scalar2=-3.1415925,
                                                   op0=ALU.min, op1=ALU.max), writes=[r_b])
            tk.op("act", lambda e: e.activation(out=res_t[:, :], in_=r[:, :], func=AF.Sin), reads=[r_b], writes=[res_b])
            if which == "sin":
                tk.op("dve", lambda e: e.tensor_scalar(out=res_t[:, :], in0=res_t[:, :], scalar1=rc[:, 1:2], scalar2=None,
                                                       op0=ALU.mult), reads=[rc_b], writes=[res_b])
            tk.dma("sp", dst[:, :], res_t[:, :], reads=[res_b])
        tk.barrier()


def build_p0_prog():
    nc = bass.Bass("TRN2", target_bir_lowering=False)
    rb_d = nc.dram_tensor("rb", [1, 32], F32, kind="ExternalInput").ap()
    pos_d = nc.dram_tensor("pos", [1, SEQ], I32, kind="ExternalInput").ap()
    rc_d = nc.dram_tensor("ropec", [64, 2], F32, kind="ExternalInput").ap()
    gc_d = nc.dram_tensor("gc", [128, GL], F32, kind="ExternalOutput").ap()
    gw_d = nc.dram_tensor("gw", [128, GL], F32, kind="ExternalOutput").ap()
    cos_d = nc.dram_tensor("cos2", [64, SEQ], F32, kind="ExternalOutput").ap()
    sin_d = nc.dram_tensor("sins", [64, SEQ], F32, kind="ExternalOutput").ap()
    with ExitStack() as es:
        tk = TK(nc, es)
        p0_tables(tk, nc, rb_d, pos_d, rc_d, [(0, gc_d[:, :], gw_d[:, :])], cos_d, sin_d)
    return nc


def rope_consts():
    inv = (10000.0 ** (-np.arange(32, dtype=np.float32) * 2.0 / 64)).astype(np.float32)
    rc = np.zeros((64, 2), np.float32)
    rc[:, 0] = np.concatenate([inv, inv])
    rc[:, 1] = np.concatenate([-np.ones(32), np.ones(32)])
    return rc


CQ, CKV, KR, NQ, KC, VC, KS, KW, VS, VW, GT, NZ = 0, 512, 1024, 1088, 1856, 2048, 2176, 2368, 2560, 2688, 2816, 2828
DEBUG_STOP = 99
DEBUG_FLAGS = set()


class StopBuild(Exception):
    pass


def dbg(level):
    if DEBUG_STOP <= level:
        DEAD[0] = True
S = SEQ
NT = S // 512
MLA_SCALE = 192 ** -0.5
NSA_SCALE = 192 ** -0.5


def run_pipelined(items, phA, phB, depth=2):
    n = len(items)
    for i in range(n + depth):
        if i < n:
            phA(items[i])
        if i - depth >= 0:
            phB(items[i - depth])


def glob_col(c, hh):
    if hh is None:
        return c
    segs = [(CQ, 0), (CKV, 512), (KR, 1024), (NQ, 1088 + hh * 768), (KC, 2624 + hh * 192), (VC, 3008 + hh * 128),
            (KS, 3264 + hh * 192), (KW, 3904 + hh * 192), (VS, 3648 + hh * 128), (VW, 4288 + hh * 128), (GT, 4544 + hh * 12)]
    base = None
    for lo, g in segs:
        if c >= lo:
            base = (lo, g)
    return base[1] + (c - base[0])


def wq_c0(hh):
    return 0 if hh is None else hh * 768


def wkv_c0(hh):
    return 0 if hh is None else hh * 512


def orow(hh):
    return 0 if hh is None else hh * 512


def win_cols(hh):
    g = hh
    r = lambda a, n: list(range(a, a + n))
    cols = r(0, 512) + r(512, 512) + r(1024, 64) + r(1088 + g * 768, 768)
    cols += r(2624 + g * 192, 192) + r(3008 + g * 128, 128) + r(3264 + g * 192, 192) + r(3904 + g * 192, 192)
    cols += r(3648 + g * 128, 128) + r(4288 + g * 128, 128) + r(4544 + g * 12, 12)
    assert len(cols) == NZ
    return np.array(cols)


def build_attn_prog():
    nc = bass.Bass("TRN2", target_bir_lowering=False)
    din = lambda n, s, d=F32: nc.dram_tensor(n, s, d, kind="ExternalInput").ap()
    hT_d = din("hT", [D, S])
    g_d = din("gains", [128, 24])
    win_d = din("win", [D, NZ])
    wq_d = din("wqup", [512, 768])
    wuk_d = din("wuk", [512, 512])
    wuv_d = din("wuv", [512, 512])
    pek_d = din("pekT", [192, 32])
    w1k_d = din("w1k", [6144, 256])
    w2k_d = din("w2k", [256, 192])
    pev_d = din("pevT", [128, 32])
    w1v_d = din("w1v", [4096, 256])
    w2v_d = din("w2v", [256, 128])
    gc_d = nc.dram_tensor("gc", [4, 128, GL], F32, kind="ExternalInput")
    gw_d = nc.dram_tensor("gw", [4, 128, GL], F32, kind="ExternalInput")
    cos_d = din("cos2", [64, S])
    sin_d = din("sins", [64, S])
    ov_d = din("ov1", [128, 33])
    sa_d = din("scoreA", [128, 16, 32])
    sbb_d = din("scoreB", [128, 16, 32])
    ex_d = din("expand", [32, 16 * 128])
    sel_d = din("gsel", [12, 12 * 128])
    id_d = din("ident", [128, 128])
    aT_d = nc.dram_tensor("aT", [512, S], BF16, kind="ExternalOutput").ap()
    bT_d = nc.dram_tensor("bT", [512, S], BF16, kind="ExternalOutput").ap()
    z32_d = nc.dram_tensor("z32", [NZ, S], F32, kind="Internal").ap()
    z16_d = nc.dram_tensor("z16", [NZ, S], BF16, kind="Internal").ap()
    vtok_d = nc.dram_tensor("vtok", [S, 256], BF16, kind="Internal").ap()

    with ExitStack() as es0:
        cx = Ctx(nc, es0)
        tk = cx.tk
        DEAD[0] = False
        _attn_body(cx, es0, nc, locals())
        DEAD[0] = False
        tk.barrier()
    return nc


def _attn_body(cx, es0, nc, L):
    globals_ = L
    (hT_d, g_d, win_d, wq_d, wuk_d, wuv_d, pek_d, w1k_d, w2k_d, pev_d, w1v_d, w2v_d, gc_d, gw_d, cos_d, sin_d, ov_d, sa_d, sbb_d,
     ex_d, sel_d, id_d, aT_d, bT_d, z32_d, z16_d, vtok_d) = [L[k] for k in (
        'hT_d', 'g_d', 'win_d', 'wq_d', 'wuk_d', 'wuv_d', 'pek_d', 'w1k_d', 'w2k_d', 'pev_d', 'w1v_d', 'w2v_d', 'gc_d', 'gw_d',
        'cos_d', 'sin_d', 'ov_d', 'sa_d', 'sbb_d', 'ex_d', 'sel_d', 'id_d', 'aT_d', 'bT_d', 'z32_d', 'z16_d', 'vtok_d')]
    tk = cx.tk
    hh = L.get('hh', None)
    gm, gq, gkv = L.get('gcols', (0, 16, 20))
    if True:
        gains = L['gains'] if 'gains' in L else load_gains(cx, es0, g_d, 24)
        g_sb, g_b = gains
        z32_b, z16_b, vtok_b = tk.buf(), tk.buf(), tk.buf()

        mark(tk, 'attn.s1')
        with ExitStack() as es:
            uT = es.enter_context(_sbt(nc, "uT", [128, 16, S], BF16))
            u_b = tk.bufs(16)
            norm_from_dram(cx, hT_d, g_sb[:, gm:gm + 16], g_b, uT, u_b, S, D, [0, 1, 2, 3])
            st32 = [es.enter_context(_sbt(nc, "st32_%d" % i, [128, 512], F32)) for i in range(2)]
            st16 = [es.enter_context(_sbt(nc, "st16_%d" % i, [128, 512], BF16)) for i in range(2)]
            st32_b, st16_b = tk.bufs(2), tk.bufs(2)
            chunks = []
            for c in range(0, 1024, 128):
                chunks.append((c, 128))
            chunks.append((KR, 64))
            for h in range(4):
                chunks += [(NQ + h * 192, 128), (NQ + h * 192 + 128, 64)]
            chunks += [(KC, 128), (KC + 128, 64), (VC, 128), (KS, 128), (KS + 128, 64), (KW, 128), (KW + 128, 64), (GT, 12)]
            slabs = []
            lastc = None
            for (c, m) in chunks:
                gcl = glob_col(c, hh)
                if slabs and lastc == c and slabs[-1][0] + sum(slabs[-1][1]) == gcl and sum(slabs[-1][1]) + m <= 256:
                    slabs[-1][1].append(m)
                else:
                    slabs.append((gcl, [m]))
                lastc = c + m
            cnt = [0]

            def epi(ci, m, t, ps, pb):
                c0 = chunks[ci][0]
                i = cnt[0] % 2
                cnt[0] += 1
                sl = slice(t * 512, (t + 1) * 512)
                eng = "act" if cnt[0] % 2 else "dve"
                if c0 == GT:
                    tk.op("act", lambda e: e.activation(out=st32[i][0:m, :], in_=ps[0:m, :], func=AF.Sigmoid),
                          reads=[pb], writes=[st32_b[i]])
                    tk.dma("sp", z32_d[c0:c0 + m, sl], st32[i][0:m, :], reads=[st32_b[i]], writes=[z32_b], join=True, anchor=st32_b[i])
                elif c0 < NQ or KC <= c0 < KS:
                    if eng == "act":
                        tk.op("act", lambda e: e.copy(out=st32[i][0:m, :], in_=ps[0:m, :]), reads=[pb], writes=[st32_b[i]])
                    else:
                        tk.op("dve", lambda e: e.tensor_copy(out=st32[i][0:m, :], in_=ps[0:m, :]), reads=[pb], writes=[st32_b[i]])
                    tk.dma("sp", z32_d[c0:c0 + m, sl], st32[i][0:m, :], reads=[st32_b[i]], writes=[z32_b], join=True, anchor=st32_b[i])
                else:
                    if eng == "act":
                        tk.op("act", lambda e: e.copy(out=st16[i][0:m, :], in_=ps[0:m, :]), reads=[pb], writes=[st16_b[i]])
                    else:
                        tk.op("dve", lambda e: e.tensor_copy(out=st16[i][0:m, :], in_=ps[0:m, :]), reads=[pb], writes=[st16_b[i]])
                    tk.dma("sp", z16_d[c0:c0 + m, sl], st16[i][0:m, :], reads=[st16_b[i]], writes=[z16_b], join=True, anchor=st16_b[i])
            gemm_fm(cx, win_d, 16, slabs, uT, u_b, S, [0, 1, 2, 3, 4, 5, 6, 7], epi)
            wv0, wvb0 = cx.fetch(win_d, 0, 16, glob_col(VS, hh), 128)
            wv1, wvb1 = cx.fetch(win_d, 0, 16, glob_col(VW, hh), 128)
            for tt in range(16):
                pid = tt % 4
                ps, pb = cx.ps[pid], cx.ps_b[pid]
                for (wv, wvb, co) in ((wv0, wvb0, 0), (wv1, wvb1, 128)):
                    for k in range(16):
                        tk.op("pe", lambda e: e.matmul(ps[:, co:co + 128], lhsT=uT[:, k, tt * 128:(tt + 1) * 128], rhs=wv[:, k, :],
                                                       start=(k == 0), stop=(k == 15)),
                              reads=[wvb, u_b[k]], writes=[pb], pe_acc=(k > 0 or co > 0))
                i = tt % 2
                tk.op("act", lambda e: e.copy(out=st16[i][:, 0:256], in_=ps[:, 0:256]), reads=[pb], writes=[st16_b[i]])
                tk.dma("sp", vtok_d[tt * 128:(tt + 1) * 128, :], st16[i][:, 0:256], reads=[st16_b[i]], writes=[vtok_b], join=True, anchor=st16_b[i])
            tk.barrier()

        mark(tk, 'mla.proj')
        with ExitStack() as es:
          dbg(1)
          if True:
              sbt = lambda n, s, d: es.enter_context(_sbt(nc, n, s, d))
              qa = sbt("m_qa", [128, 4, S], BF16); qa_b = tk.buf()
              qrr = sbt("m_qrr", [64, 4, S], BF16); qrr_b = tk.buf()
              ka = sbt("m_ka", [128, 4, S], BF16); ka_b = tk.buf()
              krr = sbt("m_krr", [64, S], BF16); krr_b = tk.buf()
              vt = sbt("m_v", [128, 16, 512], BF16); vt_b = tk.buf()
              cos_t = sbt("m_cos", [64, S], F32); sin_t = sbt("m_sin", [64, S], F32); rp_b = tk.buf()
              tk.dma("sp", cos_t[:, :], cos_d[:, :], writes=[rp_b])
              tk.dma("sp", sin_t[:, :], sin_d[:, :], writes=[rp_b], join=True)
              rt = [sbt("m_rt%d" % i, [64, 512], F32) for i in range(2)]
              rs = [sbt("m_rs%d" % i, [64, 512], F32) for i in range(2)]
              rt_b, rs_b = tk.bufs(2), tk.bufs(2)
              raw = [sbt("m_raw%d" % i, [64, 512], F32) for i in range(2)]; raw_b = tk.bufs(2)
              sinx = sbt("m_sinx", [64, S], F32)
              tk.op("dve", lambda e: e.tensor_scalar(out=sinx[:, :], in0=sin_t[:, :], scalar1=-1.0, scalar2=None, op0=ALU.mult),
                    reads=[rp_b], writes=[rp_b])
              rcnt = [0]

              def rope_epi(ps, pb, m_dst, dst_b, t):
                  if 'norope' in DEBUG_FLAGS:
                      tk.op("act", lambda e: e.copy(out=m_dst, in_=ps[0:64, :]), reads=[pb], writes=[dst_b])
                      return
                  i = rcnt[0] % 2
                  rcnt[0] += 1
                  sl = slice(t * 512, (t + 1) * 512)
                  tk.op("act", lambda e: e.copy(out=raw[i][:, :], in_=ps[0:64, :]), reads=[pb], writes=[raw_b[i]])
                  rope_math(raw[i][:, :], raw_b[i], i, sl, m_dst, dst_b)

              def rope_math(src, src_b, i, sl, m_dst, dst_b):
                  a, s_ = rt[i], rs[i]
                  tk.op("dve", lambda e: e.tensor_tensor(out=s_[0:32, :], in0=src[32:64, :], in1=sinx[32:64, sl], op=ALU.mult),
                        reads=[src_b, rp_b], writes=[rs_b[i]])
                  tk.op("dve", lambda e: e.tensor_tensor(out=s_[32:64, :], in0=src[0:32, :], in1=sinx[0:32, sl], op=ALU.mult),
                        reads=[src_b, rp_b], writes=[rs_b[i]])
                  tk.op("dve", lambda e: e.tensor_tensor(out=a[:, :], in0=src, in1=cos_t[:, sl], op=ALU.mult),
                        reads=[src_b, rp_b], writes=[rt_b[i]])
                  tk.op("dve", lambda e: e.tensor_tensor(out=m_dst, in0=a[:, :], in1=s_[:, :], op=ALU.add),
                        reads=[rt_b[i], rs_b[i]], writes=[dst_b])

              for which in ("q", "kv"):
                  with ExitStack() as es2:
                      cn = es2.enter_context(_sbt(nc, "m_cn" + which, [128, 4, S], BF16)); cn_b = tk.bufs(4)
                      base = CQ if which == "q" else CKV
                      gcol = gq if which == "q" else gkv
                      dbg(1.21)
                      norm_from_dram(cx, z32_d[base:base + 512, :], g_sb[:, gcol:gcol + 4], g_b, cn, cn_b, S, 512, [0, 1, 2, 3])
                      dbg(1.22)
                      if which == "q":
                          slabs = [(wq_c0(hh) + h * 192, [128, 64]) for h in range(4)]

                          def epi(ci, m, t, ps, pb):
                              h = ci // 2
                              sl = slice(t * 512, (t + 1) * 512)
                              if ci % 2 == 0:
                                  tk.op("act", lambda e: e.copy(out=qa[:, h, sl], in_=ps[:, :]), reads=[pb], writes=[qa_b])
                              else:
                                  rope_epi(ps, pb, qrr[:, h, sl], qrr_b, t)
                          gemm_fm(cx, wq_d, 4, slabs, cn, cn_b, S, [4, 5, 6, 7], epi)
                      else:
                          def epi(ci, m, t, ps, pb):
                              sl = slice(t * 512, (t + 1) * 512)
                              tk.op("act", lambda e: e.copy(out=ka[:, ci, sl], in_=ps[:, :]), reads=[pb], writes=[ka_b])
                          gemm_fm(cx, wuk_d, 4, col_slabs(wkv_c0(hh), 512, 256), cn, cn_b, S, [4, 5, 6, 7], epi)
                          wv, wvb = cx.fetch(wuv_d, 0, 4, wkv_c0(hh), 512)
                          for tt in range(16):
                              pid = 4 + tt % 4
                              ps, pb = cx.ps[pid], cx.ps_b[pid]
                              for k in range(4):
                                  tk.op("pe", lambda e: e.matmul(ps[:, :], lhsT=cn[:, k, tt * 128:(tt + 1) * 128], rhs=wv[:, k, :],
                                                                 start=(k == 0), stop=(k == 3)),
                                        reads=[wvb, cn_b[k]], writes=[pb], pe_acc=(k > 0))
                              tk.op("dve", lambda e: e.tensor_copy(out=vt[:, tt, :], in_=ps[:, :]), reads=[pb], writes=[vt_b])
                      tk.barrier()
              dbg(1.3)
              with ExitStack() as es2:
                  kr32 = es2.enter_context(_sbt(nc, "m_kr32", [64, S], F32)); kr32_b = tk.buf()
                  tk.dma("sp", kr32[:, :], z32_d[KR:KR + 64, :], reads=[z32_b], writes=[kr32_b])
                  for t in range(NT):
                      sl = slice(t * 512, (t + 1) * 512)
                      i = rcnt[0] % 2
                      rcnt[0] += 1
                      rope_math(kr32[:, sl], kr32_b, i, sl, krr[:, sl], krr_b)
                  tk.barrier()
              dbg(1.5)
              cm = sbt("m_cm", [128, 4, 512], F32); cm_b = tk.buf()
              tk.op("pool", lambda e: e.memset(cm[:, :, :], 0.0), writes=[cm_b])
              for di in range(4):
                  tk.op("pool", lambda e: e.affine_select(out=cm[:, di, :], in_=cm[:, di, :], pattern=[[1, 512]],
                                                          compare_op=ALU.is_ge, fill=NEGM, base=-128 * di,
                                                          channel_multiplier=-1), writes=[cm_b])
              dbg(1.7)
              mark(tk, 'mla.attn')
              NB = 4
              pt = [sbt("m_p%d" % i, [128, 512], BF16) for i in range(NB)]; pt_b = tk.bufs(NB)
              tm = [sbt("m_tm%d" % i, [128, 512], F32) for i in range(NB)]; tm_b = tk.bufs(NB)
              rl = sbt("m_rl", [128, 512], F32); rl_b = tk.buf()
              ot = [sbt("m_ot%d" % i, [128, 512], BF16) for i in range(2)]; ot_b = tk.bufs(2)
              items = []
              hq = 0
              for h in range(4):
                  for qt in range(NT):
                      nkt = 4 * qt + 4
                      for kt in range(nkt):
                          items.append((h, qt, kt, nkt, hq, len(items)))
                      hq += 1

              def phA(it):
                  h, qt, kt, nkt, g, n = it
                  qsl = slice(qt * 512, (qt + 1) * 512)
                  ksl = slice(kt * 128, (kt + 1) * 128)
                  i = n % NB
                  ps, pb = cx.ps[i], cx.ps_b[i]
                  tk.op("pe", lambda e: e.matmul(ps[:, :], lhsT=ka[:, h, ksl], rhs=qa[:, h, qsl], start=True, stop=False),
                        reads=[ka_b, qa_b], writes=[pb])
                  tk.op("pe", lambda e: e.matmul(ps[:, :], lhsT=krr[:, ksl], rhs=qrr[:, h, qsl], start=False, stop=True),
                        reads=[krr_b, qrr_b], writes=[pb], pe_acc=True)
                  di = kt - 4 * qt
                  if di >= 0:
                      tk.op("dve", lambda e: e.tensor_tensor(out=tm[i][:, :], in0=ps[:, :], in1=cm[:, di, :], op=ALU.add),
                            reads=[pb, cm_b], writes=[tm_b[i]])
                      tk.op("act", lambda e: e.activation(out=pt[i][:, :], in_=tm[i][:, :], func=AF.Exp, scale=MLA_SCALE),
                            reads=[tm_b[i]], writes=[pt_b[i]])
                  else:
                      tk.op("act", lambda e: e.activation(out=pt[i][:, :], in_=ps[:, :], func=AF.Exp, scale=MLA_SCALE),
                            reads=[pb], writes=[pt_b[i]])

              def phB(it):
                  h, qt, kt, nkt, g, n = it
                  qsl = slice(qt * 512, (qt + 1) * 512)
                  i = n % NB
                  O_id, L_id = 4 + (g % 2) * 2, 5 + (g % 2) * 2
                  tk.op("pe", lambda e: e.matmul(cx.ps[O_id][:, :], lhsT=vt[:, kt, h * 128:(h + 1) * 128], rhs=pt[i][:, :],
                                                 start=(kt == 0), stop=(kt == nkt - 1)),
                        reads=[vt_b, pt_b[i]], writes=[cx.ps_b[O_id]], pe_acc=(kt > 0))
                  tk.op("pe", lambda e: e.matmul(cx.ps[L_id][:, :], lhsT=cx.ones[:, :], rhs=pt[i][:, :],
                                                 start=(kt == 0), stop=(kt == nkt - 1)),
                        reads=[cx.ones_b, pt_b[i]], writes=[cx.ps_b[L_id]], pe_acc=(kt > 0))
                  if kt == nkt - 1:
                      oi = g % 2
                      tk.op("dve", lambda e: e.reciprocal(out=rl[:, :], in_=cx.ps[L_id][:, :]), reads=[cx.ps_b[L_id]], writes=[rl_b])
                      tk.op("dve", lambda e: e.tensor_tensor(out=ot[oi][:, :], in0=cx.ps[O_id][:, :], in1=rl[:, :], op=ALU.mult),
                            reads=[cx.ps_b[O_id], rl_b], writes=[ot_b[oi]])
                      tk.dma("sp", aT_d[orow(hh) + h * 128:orow(hh) + (h + 1) * 128, qsl], ot[oi][:, :], reads=[ot_b[oi]])
              run_pipelined(items, phA, phB, depth=2)
              tk.barrier()

        dbg(2)
        if True:
            nsa_stage(cx, es0, nc, gains, z32_d, z16_d, vtok_d, (z32_b, z16_b, vtok_b), pek_d, w1k_d, w2k_d, pev_d, w1v_d, w2v_d,
                  gc_d, gw_d, ov_d, sa_d, sbb_d, ex_d, sel_d, id_d, bT_d, hh)


def nsa_stage(cx, es0, nc, gains, z32_d, z16_d, vtok_d, zbufs, pek_d, w1k_d, w2k_d, pev_d, w1v_d, w2v_d,
              gc_d, gw_d, ov_d, sa_d, sbb_d, ex_d, sel_d, id_d, bT_d, hh=None):
    tk = cx.tk
    hb = 0 if hh is None else hh * 4
    with ExitStack() as es:
        sbt = lambda n, s, d: es.enter_context(_sbt(nc, n, s, d))
        kcTa = sbt("n_kcTa", [128, 128], BF16); kcTb = sbt("n_kcTb", [64, 128], BF16); vc = sbt("n_vc", [128, 128], BF16)
        kc_b = tk.buf()
        mark(tk, 'nsa.load')
        nqa = sbt("n_qa", [128, 4, S], BF16); nqb = sbt("n_qb", [64, 4, S], BF16)
        ksa = sbt("n_ksa", [128, S], BF16); ksb = sbt("n_ksb", [64, S], BF16)
        kwa = sbt("n_kwa", [128, S], BF16); kwb = sbt("n_kwb", [64, S], BF16)
        vsw = sbt("n_vsw", [128, 16, 256], BF16)
        gsig = sbt("n_gsig", [12, S], F32)
        ld_b = tk.buf()
        first = [True]

        def ld(dst, src):
            tk.dma("act", dst, src, writes=[ld_b], join=not first[0])
            first[0] = False
        for h in range(4):
            ld(nqa[:, h, :], z16_d[NQ + h * 192:NQ + h * 192 + 128, :])
            ld(nqb[:, h, :], z16_d[NQ + h * 192 + 128:NQ + h * 192 + 192, :])
        ld(ksa[:, :], z16_d[KS:KS + 128, :]); ld(ksb[:, :], z16_d[KS + 128:KS + 192, :])
        ld(kwa[:, :], z16_d[KW:KW + 128, :]); ld(kwb[:, :], z16_d[KW + 128:KW + 192, :])
        ld(vsw[:, :, :], vtok_d.rearrange("(t p) c -> p t c", p=128))
        ld(gsig[:, :], z32_d[GT:GT + 12, :])
        ov1 = sbt("n_ov1", [128, 33], BF16); expd = sbt("n_exp", [32, 16, 128], BF16)
        scA = sbt("n_scA", [128, 16, 32], F32); scB = sbt("n_scB", [128, 16, 32], F32)
        gsel = sbt("n_gsel", [12, 12, 128], F32); ident = sbt("n_ident", [128, 128], F32)
        ld(scA[:, :, :], sa_d[:, :, :]); ld(scB[:, :, :], sbb_d[:, :, :])
        ld(gsel[:, :, :], sel_d.rearrange("r (a p) -> r a p", p=128)); ld(ident[:, :], id_d[:, :])
        mark(tk, 'nsa.compress')
        with ExitStack() as es2:
            sb2 = lambda n, s, d: es2.enter_context(_sbt(nc, n, s, d))
            src32 = {"ka": sb2("c_ka", [128, S], F32), "kb": sb2("c_kb", [64, S], F32), "v": sb2("c_v", [128, S], F32)}
            src_b = tk.buf()
            tk.dma("sp", src32["ka"][:, :], z32_d[KC:KC + 128, :], writes=[src_b])
            tk.dma("sp", src32["kb"][:, :], z32_d[KC + 128:KC + 192, :], writes=[src_b], join=True)
            tk.dma("sp", src32["v"][:, :], z32_d[VC:VC + 128, :], writes=[src_b], join=True)
            pe = {"ka": sb2("c_pea", [128, 32], F32), "kb": sb2("c_peb", [64, 32], F32), "v": sb2("c_pev", [128, 32], F32)}
            pe_b = tk.buf()
            tk.dma("sp", pe["ka"][:, :], pek_d[0:128, :], writes=[pe_b])
            tk.dma("sp", pe["kb"][:, :], pek_d[128:192, :], writes=[pe_b], join=True)
            tk.dma("sp", pe["v"][:, :], pev_d[:, :], writes=[pe_b], join=True)
            zl = {"ka": sb2("c_zla", [128, 32, 127], BF16), "kb": sb2("c_zlb", [64, 32, 127], BF16),
                  "v": sb2("c_zlv", [128, 32, 127], BF16)}
            zlb = {key: tk.bufs(32) for key in ("ka", "kb", "v")}
            for key, pk in (("ka", 128), ("kb", 64), ("v", 128)):
                v3 = src32[key][:, :].rearrange("p (n s) -> p n s", s=16)
                for l in range(32):
                    a, r = l // 16, l % 16
                    tk.op("dve", lambda e: e.tensor_scalar(out=zl[key][:, l, :], in0=v3[:, a:a + 127, r],
                                                           scalar1=pe[key][:, l:l + 1], scalar2=None, op0=ALU.add),
                          reads=[src_b, pe_b], writes=[zlb[key][l]])
            hid = {"k": sb2("c_hk", [128, 2, 128], BF16), "v": sb2("c_hv", [128, 2, 128], BF16)}
            hid_b = tk.buf()
            for hc in range(2):
                sa_, sab = cx.fetch(w1k_d, 0, 32, hc * 128, 128, pk=128, rstride=192)
                sb_, sbb = cx.fetch(w1k_d, 128, 32, hc * 128, 128, pk=64, rstride=192)
                ps, pb = cx.ps[hc], cx.ps_b[hc]
                for l in range(32):
                    tk.op("pe", lambda e: e.matmul(ps[:, 0:127], lhsT=sa_[:, l, :], rhs=zl["ka"][:, l, :], start=(l == 0), stop=False),
                          reads=[sab, zlb["ka"][l]], writes=[pb], pe_acc=(l > 0))
                    tk.op("pe", lambda e: e.matmul(ps[:, 0:127], lhsT=sb_[:, l, :], rhs=zl["kb"][:, l, :], start=False, stop=(l == 31)),
                          reads=[sbb, zlb["kb"][l]], writes=[pb], pe_acc=True)
                tk.op("act", lambda e: e.activation(out=hid["k"][:, hc, 0:127], in_=ps[:, 0:127], func=AF.Silu),
                      reads=[pb], writes=[hid_b])
            for hc in range(2):
                sv_, svb = cx.fetch(w1v_d, 0, 32, hc * 128, 128, pk=128, rstride=128)
                ps, pb = cx.ps[2 + hc], cx.ps_b[2 + hc]
                for l in range(32):
                    tk.op("pe", lambda e: e.matmul(ps[:, 0:127], lhsT=sv_[:, l, :], rhs=zl["v"][:, l, :], start=(l == 0), stop=(l == 31)),
                          reads=[svb, zlb["v"][l]], writes=[pb], pe_acc=(l > 0))
                tk.op("act", lambda e: e.activation(out=hid["v"][:, hc, 0:127], in_=ps[:, 0:127], func=AF.Silu),
                      reads=[pb], writes=[hid_b])
            w2k, w2kb = cx.fetch(w2k_d, 0, 2, 0, 192)
            w2v, w2vb = cx.fetch(w2v_d, 0, 2, 0, 128)
            for hc in range(2):
                tk.op("pe", lambda e: e.matmul(cx.ps[4][:, 0:127], lhsT=w2k[:, hc, 0:128], rhs=hid["k"][:, hc, 0:127],
                                               start=(hc == 0), stop=(hc == 1)), reads=[w2kb, hid_b], writes=[cx.ps_b[4]], pe_acc=(hc > 0))
            for hc in range(2):
                tk.op("pe", lambda e: e.matmul(cx.ps[5][0:64, 0:127], lhsT=w2k[:, hc, 128:192], rhs=hid["k"][:, hc, 0:127],
                                               start=(hc == 0), stop=(hc == 1)), reads=[w2kb, hid_b], writes=[cx.ps_b[5]], pe_acc=(hc > 0))
            for hc in range(2):
                tk.op("pe", lambda e: e.matmul(cx.ps[6][0:127, 0:128], lhsT=hid["v"][:, hc, 0:127], rhs=w2v[:, hc, :],
                                               start=(hc == 0), stop=(hc == 1)), reads=[w2vb, hid_b], writes=[cx.ps_b[6]], pe_acc=(hc > 0))
            tk.op("dve", lambda e: e.tensor_copy(out=kcTa[:, 0:127], in_=cx.ps[4][:, 0:127]), reads=[cx.ps_b[4]], writes=[kc_b])
            tk.op("dve", lambda e: e.tensor_copy(out=kcTb[:, 0:127], in_=cx.ps[5][0:64, 0:127]), reads=[cx.ps_b[5]], writes=[kc_b])
            tk.op("dve", lambda e: e.tensor_copy(out=vc[0:127, :], in_=cx.ps[6][0:127, 0:128]), reads=[cx.ps_b[6]], writes=[kc_b])
            tk.barrier()

        acc = sbt("n_acc", [128, 4, S], F32); acc_b = tk.buf()
        negT = sbt("n_negT", [32, S], BF16); negT_b = tk.buf()
        with ExitStack() as es2:
            t_ov = es2.enter_context(_sbt(nc, "n_tov", [128, 33], F32))
            t_ex = es2.enter_context(_sbt(nc, "n_tex", [32, 16 * 128], F32))
            ld(t_ov[:, :], ov_d[:, :]); ld(t_ex[:, :], ex_d[:, :])
            tk.op("dve", lambda e: e.tensor_copy(out=ov1[:, :], in_=t_ov[:, :]), reads=[ld_b], writes=[ld_b])
            tk.op("dve", lambda e: e.tensor_copy(out=expd[:, :, :].rearrange("p a b -> p (a b)"), in_=t_ex[:, :]), reads=[ld_b], writes=[ld_b])
            tk.barrier()

        NBN = 3
        tm = [sbt("n_tm%d" % i, [128, 512], F32) for i in range(NBN)]; tm_b = tk.bufs(NBN)
        pt = [sbt("n_pt%d" % i, [128, 512], BF16) for i in range(NBN)]; pt_b = tk.bufs(NBN)
        rl = sbt("n_rl", [128, 512], F32); rl_b = tk.buf()
        rlg = sbt("n_rlg", [128, 512], F32); rlg_b = tk.buf()
        tmp = sbt("n_tmp", [128, 512], F32); tmp_b = tk.buf()
        cc = [0]

        def combine(br, j, qt, O_id, L_id, G_id=None):
            qsl = slice(qt * 512, (qt + 1) * 512)
            if G_id is None:
                G_id = 6 + cc[0] % 2
                cc[0] += 1
            tk.op("pe", lambda e: e.matmul(cx.ps[G_id][:, :], lhsT=gsel[:, j * 3 + br, :], rhs=gsig[:, qsl], start=True, stop=True),
                  reads=[ld_b], writes=[cx.ps_b[G_id]])
            tk.op("dve", lambda e: e.tensor_scalar(out=rl[:, :], in0=cx.ps[L_id][:, :], scalar1=1e-30, scalar2=None, op0=ALU.max),
                  reads=[cx.ps_b[L_id]], writes=[rl_b])
            tk.op("dve", lambda e: e.reciprocal(out=rl[:, :], in_=rl[:, :]), writes=[rl_b])
            tk.op("dve", lambda e: e.tensor_tensor(out=rlg[:, :], in0=cx.ps[G_id][:, :], in1=rl[:, :], op=ALU.mult),
                  reads=[cx.ps_b[G_id], rl_b], writes=[rlg_b])
            if br == 0:
                tk.op("dve", lambda e: e.tensor_tensor(out=acc[:, j, qsl], in0=cx.ps[O_id][:, :], in1=rlg[:, :], op=ALU.mult),
                      reads=[cx.ps_b[O_id], rlg_b], writes=[acc_b])
            else:
                tk.op("dve", lambda e: e.tensor_tensor(out=tmp[:, :], in0=cx.ps[O_id][:, :], in1=rlg[:, :], op=ALU.mult),
                      reads=[cx.ps_b[O_id], rlg_b], writes=[tmp_b])
                tk.op("pool", lambda e: e.tensor_tensor(out=acc[:, j, qsl], in0=acc[:, j, qsl], in1=tmp[:, :], op=ALU.add),
                      reads=[tmp_b], writes=[acc_b])

        mark(tk, 'nsa.cmp')
        with ExitStack() as es2:
            eT = es2.enter_context(_sbt(nc, "n_eT", [128, 4, S], BF16)); eT_b = tk.buf()
            with ExitStack() as es3:
                bmc = [es3.enter_context(_sbt(nc, "n_bmc%d" % i, [128, S // 2], F32)) for i in range(2)]; bmc_b = tk.bufs(2)
                eTb = [[tk.buf() for _ in range(NT)] for _ in range(4)]
                items = [(j, qt, j * NT + qt) for j in range(4) for qt in range(NT)]

                def strip(u):
                    j, qh = u // 2, u % 2
                    tk.dma("sp", bmc[u % 2][0:127, :], bass.AP(gc_d, (hb + j) * 128 * GL + 2017 + qh * (S // 2), [[GL - 16, 127], [1, S // 2]]),
                           writes=[bmc_b[u % 2]])
                strip(0)

                def cA(it):
                    j, qt, n = it
                    u = j * 2 + qt // 2
                    if qt % 2 == 0 and u + 1 < 8:
                        strip(u + 1)
                    qsl = slice(qt * 512, (qt + 1) * 512)
                    sid, i = n % 2, n % NBN
                    ps, pb = cx.ps[sid], cx.ps_b[sid]
                    tk.op("pe", lambda e: e.matmul(ps[0:127, :], lhsT=kcTa[:, 0:127], rhs=nqa[:, j, qsl], start=True, stop=False),
                          reads=[kc_b, ld_b], writes=[pb])
                    tk.op("pe", lambda e: e.matmul(ps[0:127, :], lhsT=kcTb[:, 0:127], rhs=nqb[:, j, qsl], start=False, stop=True),
                          reads=[kc_b, ld_b], writes=[pb], pe_acc=True)
                    tk.op("dve", lambda e: e.scalar_tensor_tensor(out=tm[i][0:127, :], in0=ps[0:127, :], scalar=NSA_SCALE,
                                                                  in1=bmc[u % 2][0:127, (qt % 2) * 512:(qt % 2 + 1) * 512], op0=ALU.mult, op1=ALU.add),
                          reads=[pb, bmc_b[u % 2]], writes=[tm_b[i]])
                    tk.op("act", lambda e: e.activation(out=eT[0:127, j, qsl], in_=tm[i][0:127, :], func=AF.Exp),
                          reads=[tm_b[i]], writes=[eTb[j][qt]])

                def cB(it):
                    j, qt, n = it
                    qsl = slice(qt * 512, (qt + 1) * 512)
                    O_id, L_id = 2 + n % 2, 4 + n % 2
                    tk.op("pe", lambda e: e.matmul(cx.ps[O_id][:, :], lhsT=vc[0:127, :], rhs=eT[0:127, j, qsl], start=True, stop=True),
                          reads=[kc_b, eTb[j][qt]], writes=[cx.ps_b[O_id]])
                    tk.op("pe", lambda e: e.matmul(cx.ps[L_id][:, :], lhsT=cx.ones[0:127, :], rhs=eT[0:127, j, qsl], start=True, stop=True),
                          reads=[cx.ones_b, eTb[j][qt]], writes=[cx.ps_b[L_id]])
                    combine(0, j, qt, O_id, L_id)
                run_pipelined(items, cA, cB, depth=1)
                tk.barrier()
            mark(tk, 'nsa.topk')
            NQQ = 16
            mk = lambda nm, shp: [es2.enter_context(_sbt(nc, "%s%d" % (nm, q), shp, F32)) for q in range(NQQ)]
            l4, imp, sc2, m8, thr, neg = mk("n_l4", [128, 4]), mk("n_imp", [128, 32]), mk("n_sc2", [128, 32]), mk("n_m8", [128, 16]), \
                mk("n_thr", [128, 1]), mk("n_neg", [128, 32])
            tb = tk.bufs(NQQ)
            psI = lambda qq: (cx.ps[qq // 3], cx.ps_b[qq // 3], (qq % 3) * 132)

            def s_mm(qq):
                ps, pb, c0 = psI(qq)
                for j in range(4):
                    tk.op("pe", lambda e: e.matmul(ps[:, c0 + j * 33:c0 + (j + 1) * 33], lhsT=eT[0:127, j, qq * 128:(qq + 1) * 128],
                                                   rhs=ov1[0:127, :], start=True, stop=True), reads=[eTb[j][qq // 4], ld_b], writes=[pb], pe_acc=True)

            def s_l4(qq):
                ps, pb, c0 = psI(qq)
                lv = ps[:, c0:c0 + 132].rearrange("p (j c) -> p j c", c=33)[:, :, 32]
                tk.op("dve", lambda e: e.tensor_scalar(out=l4[qq][:, :], in0=lv, scalar1=1e-30, scalar2=None, op0=ALU.max),
                      reads=[pb], writes=[tb[qq]])

            def s_rc(qq):
                tk.op("dve", lambda e: e.reciprocal(out=l4[qq][:, :], in_=l4[qq][:, :]), writes=[tb[qq]])

            def s_imp(j):
                def f(qq):
                    ps, pb, c0 = psI(qq)
                    if j == 0:
                        tk.op("dve", lambda e: e.tensor_scalar(out=imp[qq][:, :], in0=ps[:, c0:c0 + 32], scalar1=l4[qq][:, 0:1], scalar2=None,
                                                               op0=ALU.mult), reads=[pb], writes=[tb[qq]])
                    else:
                        tk.op("dve", lambda e: e.scalar_tensor_tensor(out=imp[qq][:, :], in0=ps[:, c0 + j * 33:c0 + j * 33 + 32],
                                                                      scalar=l4[qq][:, j:j + 1], in1=imp[qq][:, :], op0=ALU.mult, op1=ALU.add),
                              reads=[pb], writes=[tb[qq]])
                return f

            def s_sa(qq):
                tk.op("dve", lambda e: e.tensor_tensor(out=imp[qq][:, :], in0=imp[qq][:, :], in1=scA[:, qq, :], op=ALU.mult), reads=[ld_b], writes=[tb[qq]])

            def s_sb(qq):
                tk.op("dve", lambda e: e.tensor_tensor(out=imp[qq][:, :], in0=imp[qq][:, :], in1=scB[:, qq, :], op=ALU.add), reads=[ld_b], writes=[tb[qq]])

            def s_m1(qq):
                tk.op("dve", lambda e: e.max(out=m8[qq][:, 0:8], in_=imp[qq][:, :]), writes=[tb[qq]])

            def s_mr(qq):
                tk.op("dve", lambda e: e.match_replace(out=sc2[qq][:, :], in_to_replace=m8[qq][:, 0:8], in_values=imp[qq][:, :], imm_value=-1e30),
                      writes=[tb[qq]])

            def s_m2(qq):
                tk.op("dve", lambda e: e.max(out=m8[qq][:, 8:16], in_=sc2[qq][:, :]), writes=[tb[qq]])

            def s_th(qq):
                tk.op("dve", lambda e: e.tensor_scalar(out=thr[qq][:, :], in0=m8[qq][:, 15:16], scalar1=0.0, scalar2=None, op0=ALU.max), writes=[tb[qq]])

            def s_ng(qq):
                tk.op("dve", lambda e: e.tensor_scalar(out=neg[qq][:, :], in0=imp[qq][:, :], scalar1=thr[qq][:, 0:1], scalar2=None, op0=ALU.is_ge),
                      writes=[tb[qq]])

            def s_n2(qq):
                tk.op("dve", lambda e: e.tensor_scalar(out=neg[qq][:, :], in0=neg[qq][:, :], scalar1=-NEGM, scalar2=NEGM, op0=ALU.mult, op1=ALU.add),
                      writes=[tb[qq]])

            def s_tr(qq):
                tp, tpb = cx.ps[6 + qq % 2], cx.ps_b[6 + qq % 2]
                tk.op("pe", lambda e: e.transpose(out=tp[0:32, 0:128], in_=neg[qq][:, :], identity=ident[:, :]), reads=[tb[qq], ld_b], writes=[tpb])
                tk.op("act", lambda e: e.copy(out=negT[:, qq * 128:(qq + 1) * 128], in_=tp[0:32, 0:128]), reads=[tpb], writes=[negT_b])
            for step in (s_mm, s_l4, s_rc, s_imp(0), s_imp(1), s_imp(2), s_imp(3), s_sa, s_sb, s_m1, s_mr, s_m2, s_th, s_ng, s_n2, s_tr):
                for qq in range(NQQ):
                    step(qq)
            tk.barrier()

        for br in (1, 2):
            mark(tk, 'nsa.br%d' % br)
            with ExitStack() as es2:
                W_ = 1152 if br == 1 else 1408
                bm = [es2.enter_context(_sbt(nc, "n_bm%d_%d" % (br, i), [128, W_], F32)) for i in range(2)]
                bm_b = tk.bufs(2)
                g_tab = gc_d if br == 1 else gw_d
                ka_, kb_ = (ksa, ksb) if br == 1 else (kwa, kwb)
                voff = 0 if br == 1 else 128
                items = []
                g = 0
                for j in range(4):
                    for qt in range(NT):
                        kts = list(range(0, 4 * qt + 4)) if br == 1 else list(range(max(0, 4 * qt - 4), 4 * qt + 4))
                        for n_, kt in enumerate(kts):
                            items.append((j, qt, kt, n_, len(kts), g, len(items)))
                        g += 1

                def phA(it):
                    j, qt, kt, n_, nk_, g, n = it
                    if qt == 0 and n_ == 0:
                        tk.dma("sp", bm[j % 2][:, :], bass.AP(g_tab, (hb + j) * 128 * GL + 1664, [[GL - 1, 128], [1, W_]]), writes=[bm_b[j % 2]])
                    qsl = slice(qt * 512, (qt + 1) * 512)
                    ksl = slice(kt * 128, (kt + 1) * 128)
                    i = n % NBN
                    ps, pb = cx.ps[i], cx.ps_b[i]
                    tk.op("pe", lambda e: e.matmul(ps[:, :], lhsT=ka_[:, ksl], rhs=nqa[:, j, qsl], start=True, stop=False),
                          reads=[ld_b], writes=[pb])
                    tk.op("pe", lambda e: e.matmul(ps[:, :], lhsT=kb_[:, ksl], rhs=nqb[:, j, qsl], start=False, stop=(br == 2)),
                          reads=[ld_b], writes=[pb], pe_acc=True)
                    if br == 1:
                        tk.op("pe", lambda e: e.matmul(ps[:, :], lhsT=expd[:, kt, :], rhs=negT[:, qsl], start=False, stop=True),
                              reads=[ld_b, negT_b], writes=[pb], pe_acc=True)
                    delta = qt * 512 - kt * 128
                    off = min(delta, 256) + 384 if br == 1 else delta + 384
                    tk.op("dve", lambda e: e.scalar_tensor_tensor(out=tm[i][:, :], in0=ps[:, :], scalar=NSA_SCALE,
                                                                  in1=bm[j % 2][:, off:off + 512], op0=ALU.mult, op1=ALU.add),
                          reads=[pb, bm_b[j % 2]], writes=[tm_b[i]])
                    tk.op("act", lambda e: e.activation(out=pt[i][:, :], in_=tm[i][:, :], func=AF.Exp),
                          reads=[tm_b[i]], writes=[pt_b[i]])

                def phB(it):
                    j, qt, kt, n_, nk_, g, n = it
                    i = n % NBN
                    O_id, L_id = 3 + g % 2, 5 + g % 2
                    tk.op("pe", lambda e: e.matmul(cx.ps[O_id][:, :], lhsT=vsw[:, kt, voff:voff + 128], rhs=pt[i][:, :],
                                                   start=(n_ == 0), stop=(n_ == nk_ - 1)),
                          reads=[ld_b, pt_b[i]], writes=[cx.ps_b[O_id]], pe_acc=(n_ > 0))
                    tk.op("pe", lambda e: e.matmul(cx.ps[L_id][:, :], lhsT=cx.ones[:, :], rhs=pt[i][:, :],
                                                   start=(n_ == 0), stop=(n_ == nk_ - 1)),
                          reads=[cx.ones_b, pt_b[i]], writes=[cx.ps_b[L_id]], pe_acc=(n_ > 0))
                    if n_ == nk_ - 1:
                        combine(br, j, qt, O_id, L_id, G_id=7)
                run_pipelined(items, phA, phB, depth=2)
                tk.barrier()
        mark(tk, 'nsa.out')
        ob = [sbt("n_ob%d" % i, [128, S], BF16) for i in range(2)]; ob_b = tk.bufs(2)
        for j in range(4):
            tk.op("act", lambda e: e.copy(out=ob[j % 2][:, :], in_=acc[:, j, :]), reads=[acc_b], writes=[ob_b[j % 2]])
            tk.dma("sp", bT_d[orow(hh) + j * 128:orow(hh) + (j + 1) * 128, :], ob[j % 2][:, :], reads=[ob_b[j % 2]])
        tk.barrier()


def attn_consts():
    n_cmp, n_slc = 127, 32
    cs = np.arange(n_cmp) * 16
    ce = cs + 31
    bs = np.arange(n_slc) * 64
    be = bs + 63
    ov = ((cs[:, None] <= be[None, :]) & (ce[:, None] >= bs[None, :])).astype(np.float32)
    ov1 = np.zeros((128, 33), np.float32)
    ov1[:127, :32] = ov
    ov1[:127, 32] = 1.0
    t = np.arange(SEQ)
    cur = t // 64
    jb = np.arange(n_slc)
    valid = jb[None, :] <= cur[:, None]
    forced = valid & ((jb[None, :] == 0) | (jb[None, :] >= cur[:, None] - 1))
    A = (valid & ~forced).astype(np.float32)
    B = np.where(forced, 1e6, np.where(valid, 0.0, -1.0)).astype(np.float32)
    A = np.ascontiguousarray(A.reshape(16, 128, 32).transpose(1, 0, 2))
    B = np.ascontiguousarray(B.reshape(16, 128, 32).transpose(1, 0, 2))
    ex = np.zeros((32, 16, 128), np.float32)
    for kt in range(16):
        ex[2 * kt, kt, :64] = 1.0
        ex[2 * kt + 1, kt, 64:] = 1.0
    gsel = np.zeros((12, 12, 128), np.float32)
    for r in range(12):
        gsel[r, r, :] = 1.0
    return {"ov1": ov1, "scoreA": A, "scoreB": B, "expand": ex.reshape(32, 2048), "gsel": gsel.reshape(12, 12 * 128),
            "ident": np.eye(128, dtype=np.float32)}


def stream(cx, specs, consume):
    pend = cx.fetch(*specs[0])
    for i in range(len(specs)):
        nxt = cx.fetch(*specs[i + 1]) if i + 1 < len(specs) else None
        consume(i, pend[0], pend[1])
        pend = nxt


def stage_merge(cx, h_d, aT_d, bT_d, out_d, gains, gcol_pre, gcol_post, wmg, wa, wb, wo, T, cbase=0):
    tk, nc = cx.tk, cx.nc
    mark(tk, 'merge')
    g_sb, g_b = gains
    nt = T // 512
    with ExitStack() as es:
        mT = es.enter_context(_sbt(nc, "mg_m", [128, 16, T], BF16)); m_b = tk.bufs(16)
        with ExitStack() as es2:
            uT = es2.enter_context(_sbt(nc, "mg_u", [128, 16, T], BF16)); u_b = tk.bufs(16)
            aS = es2.enter_context(_sbt(nc, "mg_a", [128, 8, T], BF16)); bS = es2.enter_context(_sbt(nc, "mg_b", [128, 8, T], BF16))
            ab_b = tk.buf()
            tk.dma("sp", aS[:, :, :], aT_d.rearrange("(k p) t -> p k t", p=128), writes=[ab_b])
            tk.dma("sp", bS[:, :, :], bT_d.rearrange("(k p) t -> p k t", p=128), writes=[ab_b], join=True)
            norm_from_dram(cx, h_d, g_sb[:, gcol_pre:gcol_pre + 16], g_b, uT, u_b, T, D, [0, 1, 2, 3])
            sg = [es2.enter_context(_sbt(nc, "mg_sg%d" % i, [128, 512], F32)) for i in range(2)]; sg_b = tk.bufs(2)
            t1 = [es2.enter_context(_sbt(nc, "mg_t%d" % i, [128, 512], F32)) for i in range(2)]; t1_b = tk.bufs(2)
            specs = []
            for o in range(16):
                specs += [(wmg, 0, 16, cbase + o * 128, 128), (wmg, 0, 16, cbase + 2048 + o * 128, 128), (wa, 0, 8, o * 128, 128), (wb, 0, 8, o * 128, 128)]

            def consume(i, view, wbuf):
                o, kind = i // 4, i % 4
                nk = 16 if kind < 2 else 8
                src, src_bufs = (uT, u_b) if kind < 2 else ((aS, [ab_b] * 8) if kind == 2 else (bS, [ab_b] * 8))
                for t in range(nt):
                    pid = kind * 2 + t
                    ps, pb = cx.ps[pid], cx.ps_b[pid]
                    for k in range(nk):
                        tk.op("pe", lambda e: e.matmul(ps[:, :], lhsT=view[:, k, :], rhs=src[:, k, t * 512:(t + 1) * 512],
                                                       start=(k == 0), stop=(k == nk - 1)),
                              reads=[wbuf, src_bufs[k]], writes=[pb], pe_acc=(k > 0))
                if kind == 3:
                    for t in range(nt):
                        sl = slice(t * 512, (t + 1) * 512)
                        for br in range(2):
                            gid, pid = br * 2 + t, 4 + br * 2 + t
                            tk.op("act", lambda e: e.activation(out=sg[br][:, :], in_=cx.ps[gid][:, :], func=AF.Sigmoid),
                                  reads=[cx.ps_b[gid]], writes=[sg_b[br]])
                            tk.op("dve", lambda e: e.tensor_tensor(out=t1[br][:, :], in0=cx.ps[pid][:, :], in1=sg[br][:, :], op=ALU.mult),
                                  reads=[cx.ps_b[pid], sg_b[br]], writes=[t1_b[br]])
                        tk.op("pool", lambda e: e.tensor_tensor(out=mT[:, o, sl], in0=t1[0][:, :], in1=t1[1][:, :], op=ALU.add),
                              reads=[t1_b[0], t1_b[1]], writes=[m_b[o]])
            stream(cx, specs, consume)
            tk.barrier()
        yT = es.enter_context(_sbt(nc, "mg_y", [128, 16, T], F32)); y_b = tk.bufs(16)

        def epi(ci, m, t, ps, pb):
            sl = slice(t * 512, (t + 1) * 512)
            if (ci + t) % 2:
                tk.op("act", lambda e: e.copy(out=yT[:, ci, sl], in_=ps[:, :]), reads=[pb], writes=[y_b[ci]])
            else:
                tk.op("dve", lambda e: e.tensor_copy(out=yT[:, ci, sl], in_=ps[:, :]), reads=[pb], writes=[y_b[ci]])
        gemm_fm(cx, wo, 16, col_slabs(0, D, 256), mT, m_b, T, [0, 1, 2, 3, 4, 5, 6, 7], epi)
        resid_tail(cx, yT, y_b, h_d, g_sb[:, gcol_post:gcol_post + 16], g_b, out_d, T, [0, 1, 2, 3])


def build_ca_prog(with_next, T=1024):
    nc = bass.Bass("TRN2", target_bir_lowering=False)
    din = lambda n, s, d=F32: nc.dram_tensor(n, s, d, kind="ExternalInput").ap()
    h_d = din("hT", [D, T])
    aT_d = din("aT", [1024, T], BF16)
    bT_d = din("bT", [1024, T], BF16)
    g_d = din("gains", [128, 96])
    wmg = din("wmg", [D, 4096]); wa = din("wa", [1024, D]); wb = din("wb", [1024, D]); wo = din("wo", [D, D])
    w2g = din("w2g", [D, FF]); w2u = din("w2u", [D, FF]); w2d = din("w2d", [FF, D])
    if with_next:
        w1g = din("w1g", [D, FF]); w1u = din("w1u", [D, FF]); w1d = din("w1d", [FF, D])
        hn_d = nc.dram_tensor("hnT", [D, T], F32, kind="ExternalOutput").ap()
    x_d = nc.dram_tensor("xT", [D, T], F32, kind="ExternalOutput").ap()
    h2_d = nc.dram_tensor("h2T", [D, T], F32, kind="Internal").ap()
    with ExitStack() as es:
        cx = Ctx(nc, es)
        gains = load_gains(cx, es, g_d, 96, half_cols=[(48, 64), (80, 96)])
        stage_merge(cx, h_d, aT_d, bT_d, h2_d, gains, 0, 16, wmg, wa, wb, wo, T)
        stage_ffn(cx, h2_d, x_d, gains, 32, 48, w2g, w2u, w2d, T)
        if with_next:
            stage_ffn(cx, x_d, hn_d, gains, 64, 80, w1g, w1u, w1d, T)
        cx.tk.barrier()
    return nc


N_LAUNCH_CORES = 4
GPL = 104


def build_fused_prog():
    nc = bass.Bass("TRN2", target_bir_lowering=False)
    din = lambda n, s, d=F32: nc.dram_tensor(n, s, d, kind="ExternalInput").ap()
    x_d = din("xT", [D, S])
    pos_d = din("pos", [1, S], I32)
    rb_d = din("rbT", [8, 32])
    rc_d = din("ropec", [64, 2])
    g_d = din("gains", [128, GPL * DEPTH])
    W = {}
    for nm, shp in (("f1g", [D, FF]), ("f1u", [D, FF]), ("f1d", [FF, D]), ("f2g", [D, FF]), ("f2u", [D, FF]), ("f2d", [FF, D]),
                    ("win", [D, 8664]), ("wq", [512, 1536]), ("wuk", [512, 1024]), ("wuv", [512, 1024]),
                    ("pekT", [192, 32]), ("w1k", [6144, 256]), ("w2k", [256, 192]), ("pevT", [128, 32]), ("w1v", [4096, 256]),
                    ("w2v", [256, 128]), ("wa", [1024, D]), ("wb", [1024, D]), ("wo", [D, D])):
        W[nm] = (din(nm, [DEPTH * shp[0], shp[1]]), shp[0])
    wl = lambda nm, l: W[nm][0][l * W[nm][1]:(l + 1) * W[nm][1], :]
    ov_d = din("ov1", [128, 33]); sa_d = din("scoreA", [128, 16, 32]); sbb_d = din("scoreB", [128, 16, 32])
    ex_d = din("expand", [32, 16 * 128]); sel_d = din("gsel", [12, 12 * 128]); id_d = din("ident", [128, 128])
    out_d = nc.dram_tensor("outT", [D, S], F32, kind="ExternalOutput").ap()
    di = lambda n, s, d=F32: nc.dram_tensor(n, s, d, kind="Internal")
    hA = di("hA", [D, S]).ap(); hB = di("hB", [D, S]).ap(); xb = [di("xb0", [D, S]).ap(), di("xb1", [D, S]).ap()]
    aT = di("aTi", [1024, S], BF16).ap(); bT = di("bTi", [1024, S], BF16).ap()
    z32 = di("z32", [NZ, S]).ap(); z16 = di("z16", [NZ, S], BF16).ap(); vtok = di("vtok", [S, 256], BF16).ap()
    gc_all = di("gc_all", [8, 128, GL]); gw_all = di("gw_all", [8, 128, GL])
    cos_i = di("cos_i", [64, S]).ap(); sin_i = di("sin_i", [64, S]).ap()
    T = 1024
    with ExitStack() as es:
        cx = Ctx(nc, es)
        halves = []
        for l in range(DEPTH):
            halves += [(l * GPL + 16, l * GPL + 32), (l * GPL + 80, l * GPL + 96)]
        gains = load_gains(cx, es, g_d, GPL * DEPTH, half_cols=halves)
        p0_tables(cx.tk, nc, rb_d, pos_d, rc_d,
                  [(h, bass.AP(gc_all, h * 128 * GL, [[GL, 128], [1, GL]]), bass.AP(gw_all, h * 128 * GL, [[GL, 128], [1, GL]]))
                   for h in range(8)], cos_i, sin_i)
        cur = x_d
        for l in range(DEPTH):
            gb = l * GPL
            for half in range(2):
                tsl = slice(half * T, (half + 1) * T)
                stage_ffn(cx, cur[:, tsl], hA[:, tsl], gains, gb + 0, gb + 16, wl("f1g", l), wl("f1u", l), wl("f1d", l), T)
            for hh in range(2):
                L = {"hT_d": hA, "g_d": None, "win_d": wl("win", l), "wq_d": wl("wq", l), "wuk_d": wl("wuk", l), "wuv_d": wl("wuv", l),
                     "pek_d": wl("pekT", l), "w1k_d": wl("w1k", l), "w2k_d": wl("w2k", l), "pev_d": wl("pevT", l),
                     "w1v_d": wl("w1v", l), "w2v_d": wl("w2v", l), "gc_d": gc_all, "gw_d": gw_all, "cos_d": cos_i, "sin_d": sin_i,
                     "ov_d": ov_d, "sa_d": sa_d, "sbb_d": sbb_d, "ex_d": ex_d, "sel_d": sel_d, "id_d": id_d, "aT_d": aT, "bT_d": bT,
                     "z32_d": z32, "z16_d": z16, "vtok_d": vtok, "hh": hh, "gcols": (gb + 32, gb + 96, gb + 100), "gains": gains}
                _attn_body(cx, es, nc, L)
                cx.tk.barrier()
            for half in range(2):
                tsl = slice(half * T, (half + 1) * T)
                stage_merge(cx, hA[:, tsl], aT[:, tsl], bT[:, tsl], hB[:, tsl], gains, gb + 32, gb + 48,
                            wl("win", l), wl("wa", l), wl("wb", l), wl("wo", l), T, cbase=4568)
            dst = out_d if l == DEPTH - 1 else xb[l % 2]
            for half in range(2):
                tsl = slice(half * T, (half + 1) * T)
                stage_ffn(cx, hB[:, tsl], dst[:, tsl], gains, gb + 64, gb + 80, wl("f2g", l), wl("f2u", l), wl("f2d", l), T)
            cur = dst
        cx.tk.barrier()
    return nc


_PROGS = {}


def kernel(x, positions, rel_bias,
           ffn1_pre_g, ffn1_post_g, ffn1_w_gate, ffn1_w_up, ffn1_w_down,
           mix_pre_g, mix_post_g, w_in,
           mla_q_norm_g, mla_w_q_up, mla_kv_norm_g, mla_w_uk, mla_w_uv,
           cmp_pe_k, cmp_w1_k, cmp_w2_k, cmp_pe_v, cmp_w1_v, cmp_w2_v,
           w_branch_mla, w_branch_nsa, w_out,
           ffn2_pre_g, ffn2_post_g, ffn2_w_gate, ffn2_w_up, ffn2_w_down):
    f = lambda a: np.ascontiguousarray(np.asarray(a, dtype=np.float32))
    st = lambda a: f(a).reshape(-1, np.asarray(a).shape[-1])
    x = f(x)
    positions = np.asarray(positions).astype(np.int32)
    gl = []
    for l in range(DEPTH):
        gl += [garr(ffn1_pre_g[l]), garr(ffn1_post_g[l]), garr(mix_pre_g[l]), garr(mix_post_g[l]), garr(ffn2_pre_g[l]),
               garr(ffn2_post_g[l]), garr(mla_q_norm_g[l]), garr(mla_kv_norm_g[l])]
    common = {"rbT": np.ascontiguousarray(f(rel_bias).T), "ropec": rope_consts(), "gains": np.concatenate(gl, axis=1),
              "f1g": st(ffn1_w_gate), "f1u": st(ffn1_w_up), "f1d": st(ffn1_w_down),
              "f2g": st(ffn2_w_gate), "f2u": st(ffn2_w_up), "f2d": st(ffn2_w_down),
              "win": st(w_in), "wq": st(mla_w_q_up), "wuk": st(mla_w_uk), "wuv": st(mla_w_uv),
              "pekT": np.ascontiguousarray(f(cmp_pe_k).transpose(0, 2, 1)).reshape(-1, 32), "w1k": st(cmp_w1_k), "w2k": st(cmp_w2_k),
              "pevT": np.ascontiguousarray(f(cmp_pe_v).transpose(0, 2, 1)).reshape(-1, 32), "w1v": st(cmp_w1_v), "w2v": st(cmp_w2_v),
              "wa": st(w_branch_mla), "wb": st(w_branch_nsa), "wo": st(w_out)}
    common.update(attn_consts())
    if "fused" not in _PROGS:
        _PROGS["fused"] = build_fused_prog()
    active = {0: 0, 1: 1, 2: 2, 3: 3} if N_LAUNCH_CORES == 4 else {0: 0, 1: 1, 4: 2, 5: 3}
    zeros = None
    maps = []
    for c in range(N_LAUNCH_CORES):
        if c in active:
            b = active[c]
            m = dict(common)
            m["xT"] = np.ascontiguousarray(x[b].T)
            m["pos"] = np.ascontiguousarray(positions[b][None, :])
        else:
            if zeros is None:
                zeros = {k: np.zeros_like(v) for k, v in common.items()}
                zeros["xT"] = np.zeros((D, S), np.float32)
                zeros["pos"] = np.zeros((1, S), np.int32)
            m = zeros
        maps.append(m)
    res = run_bass_kernel_spmd(_PROGS["fused"], maps, core_ids=list(range(N_LAUNCH_CORES))).results
    inv = {b: c for c, b in active.items()}
    out = np.stack([np.asarray(res[inv[b]]["outT"]).T for b in range(4)], axis=0)
    return np.ascontiguousarray(out.astype(np.float32))
```

```python
import math
import numpy as np
from contextlib import ExitStack
import concourse.bass as bass
import concourse.mybir as mybir
from concourse.bass_utils import run_bass_kernel_spmd

F32 = mybir.dt.float32
BF16 = mybir.dt.bfloat16
I32 = mybir.dt.int32
AF = mybir.ActivationFunctionType
ALU = mybir.AluOpType

D = 2048
FF = 5632
SEQ = 2048
DEPTH = 4
EPS = 1e-6
NEGM = -30000.0
GL = 4096
GOFF = 2048
DEAD = [False]
MARKS = []


def mark(tk, label):
    MARKS.append((label, tk.cnt['pe']))

_SBN = [0]


def _sbt(nc, name, shape, dt):
    _SBN[0] += 1
    return nc.sbuf_tensor("%s_u%d" % (name, _SBN[0]), shape, dt)


class Buf:
    __slots__ = ("name", "lw", "rd", "dsem")

    def __init__(self, name, dsem):
        self.name = name
        self.lw = None
        self.rd = {}
        self.dsem = dsem


class TK:
    ENG = ("pe", "act", "dve", "pool", "sp")

    def __init__(self, nc, es, n_dma_sems=48):
        self.nc = nc
        self.E = {"pe": nc.tensor, "act": nc.scalar, "dve": nc.vector,
                  "pool": nc.gpsimd, "sp": nc.sync}
        self.sem = {k: es.enter_context(nc.semaphore("s_" + k)) for k in ("pe", "act", "dve", "pool")}
        self.cnt = {k: 0 for k in self.sem}
        self.dsems = [es.enter_context(nc.semaphore("d%d" % i)) for i in range(n_dma_sems)]
        self.dtot = [0] * n_dma_sems
        self.seen = {k: {} for k in self.ENG}
        self._rr = 0
        self.nbuf = 0

    def buf(self, name=None):
        self.nbuf += 1
        b = Buf(name or ("b%d" % self.nbuf), self._rr)
        self._rr = (self._rr + 1) % len(self.dsems)
        return b

    def bufs(self, n):
        return [self.buf() for _ in range(n)]

    def _wait(self, eng, need):
        seen = self.seen[eng]
        for k2, val in need.items():
            kind, key = k2
            if kind == "dma":
                val = self.dtot[key]
            if seen.get(k2, 0) >= val:
                continue
            sem = self.sem[key] if kind == "eng" else self.dsems[key]
            self.E[eng].wait_ge(sem, val)
            seen[k2] = val

    @staticmethod
    def _add(need, ev):
        if ev is None:
            return
        k2 = (ev[0], ev[1])
        if need.get(k2, 0) < ev[2]:
            need[k2] = ev[2]

    def _deps(self, reads, writes):
        need = {}
        for b in reads:
            self._add(need, b.lw)
        for b in writes:
            self._add(need, b.lw)
            for k2, v in b.rd.items():
                if need.get(k2, 0) < v:
                    need[k2] = v
        return need

    def _record(self, ev, reads, writes):
        k2 = (ev[0], ev[1])
        for b in reads:
            if b.rd.get(k2, 0) < ev[2]:
                b.rd[k2] = ev[2]
        for b in writes:
            b.lw = ev
            b.rd = {}

    def op(self, eng, fn, reads=(), writes=(), pe_acc=False):
        if DEAD[0]:
            return None
        need = self._deps(reads, writes)
        if pe_acc:
            need.pop(("eng", "pe"), None)
        self._wait(eng, need)
        ins = fn(self.E[eng])
        self.cnt[eng] += 1
        ev = ("eng", eng, self.cnt[eng])
        ins.then_inc(self.sem[eng], 1)
        self._record(ev, reads, writes)
        return ev

    def dma(self, q, out, in_, reads=(), writes=(), join=False, anchor=None):
        if DEAD[0]:
            return None
        anchor = anchor or (list(writes) + list(reads))[0]
        si = anchor.dsem
        need = self._deps(reads, writes)
        if join:
            need.pop(("dma", si), None)
        self._wait(q, need)
        ins = self.E[q].dma_start(out=out, in_=in_)
        ins.then_inc(self.dsems[si], 16)
        self.dtot[si] += 16
        ev = ("dma", si, self.dtot[si])
        self._record(ev, reads, writes)
        return ev

    def barrier(self):
        need = {("eng", k): v for k, v in self.cnt.items() if v > 0}
        for i, v in enumerate(self.dtot):
            if v > 0:
                need[("dma", i)] = v
        for e in self.ENG:
            self._wait(e, dict(need))


class Ctx:
    def __init__(self, nc, es):
        self.nc = nc
        self.es = es
        self.tk = TK(nc, es)
        tk = self.tk
        self.ps = [es.enter_context(nc.psum_tensor("ps%d" % i, [128, 512], F32)) for i in range(8)]
        self.ps_b = tk.bufs(8)
        self.ones = es.enter_context(_sbt(nc, "ones_bf", [128, 128], BF16))
        self.ones_b = tk.buf()
        tk.op("dve", lambda e: e.memset(self.ones[:], 1.0), writes=[self.ones_b])
        self.stg = [es.enter_context(_sbt(nc, "wstg%d" % i, [128, 4096], F32)) for i in range(2)]
        self.stg_b = tk.bufs(2)
        self.slab = [es.enter_context(_sbt(nc, "wslab%d" % i, [128, 4096], BF16)) for i in range(2)]
        self.slab_b = tk.bufs(2)
        self.wi = 0
        self.cast_rr = 0

    def fetch(self, W, r0, nk, c0, M, pk=128, rstride=None):
        tk = self.tk
        assert nk * M <= 4096
        rstride = rstride or pk
        N = W.shape[1]
        j = self.wi % 2
        self.wi += 1
        stg, sb = self.stg[j], self.stg_b[j]
        src = bass.AP(W.tensor, W.offset + r0 * N + c0, [[N, pk], [rstride * N, nk], [1, M]])
        dst = stg[0:pk, 0:nk * M].rearrange("p (kc m) -> p kc m", m=M)
        step = max(1, 2048 // max(M, 1))
        first = True
        for k0 in range(0, nk, step):
            k1 = min(nk, k0 + step)
            tk.dma("sp", dst[:, k0:k1, :], src[:, k0:k1, :], writes=[sb], join=not first)
            first = False
        slab, lb = self.slab[j], self.slab_b[j]
        eng = ("pool", "dve", "act")[self.cast_rr % 3]
        self.cast_rr += 1
        if eng == "act":
            tk.op("act", lambda e: e.copy(out=slab[0:pk, 0:nk * M], in_=stg[0:pk, 0:nk * M]), reads=[sb], writes=[lb])
        else:
            tk.op(eng, lambda e: e.tensor_copy(out=slab[0:pk, 0:nk * M], in_=stg[0:pk, 0:nk * M]), reads=[sb], writes=[lb])
        return slab[0:pk, 0:nk * M].rearrange("p (kc m) -> p kc m", m=M), lb


def gemm_fm(cx, W, nk, slabs, inT, in_bufs, T, ps_ids, epi, r0=0, pend=None):
    tk = cx.tk
    if pend is None:
        pend = cx.fetch(W, r0, nk, slabs[0][0], sum(slabs[0][1]))
    ci = 0
    pi = 0
    for si, (c0, ms) in enumerate(slabs):
        nxt = cx.fetch(W, r0, nk, slabs[si + 1][0], sum(slabs[si + 1][1])) if si + 1 < len(slabs) else None
        view, wb = pend
        off = 0
        for m in ms:
            for t in range(T // 512):
                pid = ps_ids[pi % len(ps_ids)]
                pi += 1
                ps, pb = cx.ps[pid], cx.ps_b[pid]
                for k in range(nk):
                    tk.op("pe", lambda e: e.matmul(ps[0:m, :], lhsT=view[:, k, off:off + m],
                                                   rhs=inT[:, k, t * 512:(t + 1) * 512],
                                                   start=(k == 0), stop=(k == nk - 1)),
                          reads=[wb] + list(in_bufs), writes=[pb], pe_acc=(k > 0))
                epi(ci, m, t, ps, pb)
            off += m
            ci += 1
        pend = nxt


def col_slabs(c0, n, width=256):
    out = []
    c = c0
    end = c0 + n
    while c < end:
        w = min(width, end - c)
        ms = [min(128, w - i) for i in range(0, w, 128)]
        out.append((c, ms))
        c += w
    return out


def ssq_rstd(cx, es, chunk_src, nchunk, T, Kdim, rstd, rstd_b, ps_ids, pk=128):
    tk, nc = cx.tk, cx.nc
    sq = [es.enter_context(_sbt(nc, "sq%d_%d" % (i, tk.nbuf), [128, T], BF16)) for i in range(2)]
    sq_b = tk.bufs(2)
    nt = T // 512
    for k in range(nchunk):
        ap, b = chunk_src(k)
        s, sb_ = sq[k % 2], sq_b[k % 2]
        tk.op("act", lambda e: e.activation(out=s[0:pk, :], in_=ap, func=AF.Square), reads=[b], writes=[sb_])
        for t in range(nt):
            pid = ps_ids[t]
            tk.op("pe", lambda e: e.matmul(cx.ps[pid][:, :], lhsT=cx.ones[0:pk, :], rhs=s[0:pk, t * 512:(t + 1) * 512],
                                           start=(k == 0), stop=(k == nchunk - 1)),
                  reads=[sb_, cx.ones_b], writes=[cx.ps_b[pid]], pe_acc=(k > 0))
    for t in range(nt):
        pid = ps_ids[t]
        sl = slice(t * 512, (t + 1) * 512)
        tk.op("act", lambda e: e.activation(out=rstd[:, sl], in_=cx.ps[pid][:, :], func=AF.Sqrt,
                                            scale=1.0 / Kdim, bias=cx.eps_ap),
              reads=[cx.ps_b[pid]], writes=[rstd_b])
    tk.op("dve", lambda e: e.reciprocal(out=rstd[:, :], in_=rstd[:, :]), reads=[rstd_b], writes=[rstd_b])


def norm_from_dram(cx, src_d, g_sb, g_b, uT, u_bufs, T, K, ps_ids):
    tk, nc = cx.tk, cx.nc
    nk = K // 128
    with ExitStack() as es:
        NXB = 4 if T <= 1024 else 2
        xs = [es.enter_context(_sbt(nc, "nx%d_%d" % (i, tk.nbuf), [128, T], F32)) for i in range(NXB)]
        xs_b = tk.bufs(NXB)
        rstd = es.enter_context(_sbt(nc, "nrstd_%d" % tk.nbuf, [128, T], F32))
        rstd_b = tk.buf()

        def src(k):
            tk.dma("sp", xs[k % NXB][:, :], src_d[k * 128:(k + 1) * 128, :], writes=[xs_b[k % NXB]])
            return xs[k % NXB][:, :], xs_b[k % NXB]
        ssq_rstd(cx, es, src, nk, T, K, rstd, rstd_b, ps_ids)
        for k in range(nk):
            ap, b = src(k)
            tk.op("dve", lambda e: e.scalar_tensor_tensor(out=uT[:, k, :], in0=ap, scalar=g_sb[:, k:k + 1],
                                                          in1=rstd[:, :], op0=ALU.mult, op1=ALU.mult),
                  reads=[b, rstd_b, g_b], writes=[u_bufs[k]])
        tk.barrier()


def resid_tail(cx, yT, y_bufs, x_d, gf_sb, gf_b, out_d, T, ps_ids):
    tk, nc = cx.tk, cx.nc
    with ExitStack() as es:
        rstd = es.enter_context(_sbt(nc, "trstd_%d" % tk.nbuf, [128, T], F32))
        rstd_b = tk.buf()
        ssq_rstd(cx, es, lambda k: (yT[:, k, :], y_bufs[k]), 16, T, D, rstd, rstd_b, ps_ids)
        xs = [es.enter_context(_sbt(nc, "tx%d_%d" % (i, tk.nbuf), [128, T], F32)) for i in range(4)]
        xs_b = tk.bufs(4)
        for k in range(16):
            x, xb = xs[k % 4], xs_b[k % 4]
            tk.dma("sp", x[:, :], x_d[k * 128:(k + 1) * 128, :], writes=[xb])
            tk.op("dve", lambda e: e.scalar_tensor_tensor(out=yT[:, k, :], in0=yT[:, k, :], scalar=gf_sb[:, k:k + 1],
                                                          in1=rstd[:, :], op0=ALU.mult, op1=ALU.mult),
                  reads=[rstd_b, gf_b], writes=[y_bufs[k]])
            tk.op("pool" if k % 3 == 0 else "dve", lambda e: e.tensor_tensor(out=x[:, :], in0=x[:, :], in1=yT[:, k, :], op=ALU.add),
                  reads=[y_bufs[k]], writes=[xb])
            tk.dma("sp", out_d[k * 128:(k + 1) * 128, :], x[:, :], reads=[xb])
        tk.barrier()


def stage_ffn(cx, x_d, out_d, gains, gcol_pre, gcol_post, Wg, Wu, Wd, T):
    tk, nc = cx.tk, cx.nc
    mark(tk, 'ffn')
    g_sb, g_b = gains
    NG = 4
    CPG = 11
    nt = T // 512
    with ExitStack() as es:
        yT = es.enter_context(_sbt(nc, "ffn_y_%d" % tk.nbuf, [128, 16, T], F32))
        y_b = tk.bufs(16)
        uT = es.enter_context(_sbt(nc, "ffn_u_%d" % tk.nbuf, [128, 16, T], BF16))
        u_b = tk.bufs(16)
        hT = es.enter_context(_sbt(nc, "ffn_h_%d" % tk.nbuf, [128, CPG, T], BF16))
        h_b = tk.bufs(CPG)
        sg = [es.enter_context(_sbt(nc, "ffn_sg%d_%d" % (i, tk.nbuf), [128, 512], F32)) for i in range(2)]
        sg_b = tk.bufs(2)
        specs, tags = [], []
        for grp in range(NG):
            f0 = grp * CPG * 128
            for c in range(0, CPG * 128, 256):
                w = min(256, CPG * 128 - c)
                specs.append((Wg, 0, 16, f0 + c, w)); tags.append(("g", grp, c, w))
                specs.append((Wu, 0, 16, f0 + c, w)); tags.append(("u", grp, c, w))
            for (c0, ms) in col_slabs(0, D, 256):
                specs.append((Wd, f0, CPG, c0, sum(ms))); tags.append(("d", grp, c0, ms))
        pend = cx.fetch(*specs[0])
        norm_from_dram(cx, x_d, g_sb[:, gcol_pre:gcol_pre + 16], g_b, uT, u_b, T, D, [0, 1, 2, 3])
        cnt = [0]
        dpi = [0]
        for si in range(len(specs)):
            nxt = cx.fetch(*specs[si + 1]) if si + 1 < len(specs) else None
            view, wb = pend
            kind, grp, c, w = tags[si]
            if kind in ("g", "u"):
                nch = w // 128
                for ci in range(nch):
                    n_loc = c // 128 + ci
                    for t in range(nt):
                        pid = (0 if kind == "g" else 2) + t + 4 * (n_loc % 2)
                        ps, pb = cx.ps[pid], cx.ps_b[pid]
                        for k in range(16):
                            tk.op("pe", lambda e: e.matmul(ps[:, :], lhsT=view[:, k, ci * 128:(ci + 1) * 128],
                                                           rhs=uT[:, k, t * 512:(t + 1) * 512],
                                                           start=(k == 0), stop=(k == 15)),
                                  reads=[wb, u_b[k]], writes=[pb], pe_acc=(k > 0))
                        if kind == "u":
                            gid = t + 4 * (n_loc % 2)
                            s_, sb_ = sg[cnt[0] % 2], sg_b[cnt[0] % 2]
                            cnt[0] += 1
                            tk.op("act", lambda e: e.activation(out=s_[:, :], in_=cx.ps[gid][:, :], func=AF.Silu),
                                  reads=[cx.ps_b[gid]], writes=[sb_])
                            tk.op("dve", lambda e: e.tensor_tensor(out=hT[:, n_loc, t * 512:(t + 1) * 512], in0=s_[:, :],
                                                                   in1=ps[:, :], op=ALU.mult),
                                  reads=[sb_, pb], writes=[h_b[n_loc]])
            else:
                ms = w
                off = 0
                for m in ms:
                    ci = (c + off) // 128
                    for t in range(nt):
                        pid = dpi[0] % 8
                        dpi[0] += 1
                        ps, pb = cx.ps[pid], cx.ps_b[pid]
                        for k in range(CPG):
                            tk.op("pe", lambda e: e.matmul(ps[0:m, :], lhsT=view[:, k, off:off + m], rhs=hT[:, k, t * 512:(t + 1) * 512],
                                                           start=(k == 0), stop=(k == CPG - 1)),
                                  reads=[wb, h_b[k]], writes=[pb], pe_acc=(k > 0))
                        sl = slice(t * 512, (t + 1) * 512)
                        if grp == 0:
                            tk.op("act", lambda e: e.copy(out=yT[:, ci, sl], in_=ps[:, :]), reads=[pb], writes=[y_b[ci]])
                        else:
                            tk.op("dve", lambda e: e.tensor_tensor(out=yT[:, ci, sl], in0=ps[:, :], in1=yT[:, ci, sl], op=ALU.add),
                                  reads=[pb], writes=[y_b[ci]])
                    off += m
            pend = nxt
        resid_tail(cx, yT, y_b, x_d, g_sb[:, gcol_post:gcol_post + 16], g_b, out_d, T, [0, 1, 2, 3])


def load_gains(cx, es, g_d, ncol, half_cols=()):
    tk, nc = cx.tk, cx.nc
    g_sb = es.enter_context(_sbt(nc, "gains_sb", [128, ncol], F32))
    g_b = tk.buf()
    tk.dma("sp", g_sb[:, :], g_d[:, :], writes=[g_b])
    for (c0, c1) in half_cols:
        tk.op("dve", lambda e: e.tensor_scalar(out=g_sb[:, c0:c1], in0=g_sb[:, c0:c1], scalar1=0.5, scalar2=None,
                                               op0=ALU.mult), reads=[g_b], writes=[g_b])
    eps = es.enter_context(_sbt(nc, "eps_t", [128, 1], F32))
    tk.op("dve", lambda e: e.memset(eps[:], EPS), writes=[g_b])
    cx.eps_ap = eps[:, 0:1]
    return g_sb, g_b


def build_ffn_prog(T=1024):
    nc = bass.Bass("TRN2", target_bir_lowering=False)
    x_d = nc.dram_tensor("xT", [D, T], F32, kind="ExternalInput").ap()
    g_d = nc.dram_tensor("gains", [128, 32], F32, kind="ExternalInput").ap()
    Wg = nc.dram_tensor("wg", [D, FF], F32, kind="ExternalInput").ap()
    Wu = nc.dram_tensor("wu", [D, FF], F32, kind="ExternalInput").ap()
    Wd = nc.dram_tensor("wd", [FF, D], F32, kind="ExternalInput").ap()
    o_d = nc.dram_tensor("outT", [D, T], F32, kind="ExternalOutput").ap()
    with ExitStack() as es:
        cx = Ctx(nc, es)
        gains = load_gains(cx, es, g_d, 32, half_cols=[(16, 32)])
        stage_ffn(cx, x_d, o_d, gains, 0, 16, Wg, Wu, Wd, T)
        cx.tk.barrier()
    return nc


def garr(g):
    return np.ascontiguousarray(np.asarray(g, dtype=np.float32).reshape(-1, 128).T)


def rel_thresholds():
    n = np.arange(0, 256)
    large = 16 + (np.log(np.maximum(n, 1).astype(np.float32) / np.float32(16)) / np.float32(math.log(128 / 16))
                  * np.float32(16)).astype(np.int32)
    large = np.minimum(large, 31)
    bucket = np.where(n < 16, n, large)
    return [int(np.argmax(bucket >= b)) for b in range(1, 32)]


def stage_p0(tk, nc, rb_d, pos_d, rc_d, heads):
    pass


def p0_tables(tk, nc, rb_d, pos_d, rc_d, head_dsts, cos_d, sin_d):
    thr = rel_thresholds()
    with ExitStack() as es:
        sb = lambda n, s, d=F32: es.enter_context(_sbt(nc, n, s, d))
        rb = sb("rb_sb", [128, 32]); rb_b = tk.buf()
        dt = sb("dtab", [128, 32]); dt_b = tk.buf()
        dg = sb("dgrid", [128, 128]); dg_b = tk.buf()
        tk.op("pool", lambda e: e.iota(dg[:, :], [[1, 128]], base=0, channel_multiplier=0,
                                       allow_small_or_imprecise_dtypes=True), writes=[dg_b])
        band = sb("band", [128, 128]); band_b = tk.buf()
        tmp = sb("btmp", [128, 128]); tmp_b = tk.buf()
        G = sb("gtab", [128, GL]); G_b = tk.buf()
        for (hrow, gc_dst, gw_dst) in head_dsts:
            tk.dma("sp", rb[:, :], rb_d[hrow:hrow + 1, :].to_broadcast([128, 32]), writes=[rb_b])
            tk.op("dve", lambda e: e.tensor_tensor(out=dt[:, 1:32], in0=rb[:, 1:32], in1=rb[:, 0:31], op=ALU.subtract),
                  reads=[rb_b], writes=[dt_b])
            tk.op("dve", lambda e: e.tensor_scalar(out=band[:, :], in0=dg[:, :], scalar1=0.0, scalar2=rb[:, 0:1],
                                                   op0=ALU.mult, op1=ALU.add), reads=[dg_b, rb_b], writes=[band_b])
            for b in range(1, 32):
                tk.op("dve", lambda e: e.tensor_scalar(out=tmp[:, :], in0=dg[:, :], scalar1=float(thr[b - 1]),
                                                       scalar2=dt[:, b:b + 1], op0=ALU.is_ge, op1=ALU.mult),
                      reads=[dg_b, dt_b], writes=[tmp_b])
                tk.op("dve", lambda e: e.tensor_tensor(out=band[:, :], in0=band[:, :], in1=tmp[:, :], op=ALU.add),
                      reads=[tmp_b], writes=[band_b])
            for kind, dst in (("c", gc_dst), ("w", gw_dst)):
                hi = GL if kind == "c" else GOFF + 512
                tk.op("pool", lambda e: e.memset(G[:, :], NEGM), writes=[G_b])
                tk.op("dve", lambda e: e.tensor_copy(out=G[:, GOFF:GOFF + 128], in_=band[:, :]), reads=[band_b], writes=[G_b])
                tk.op("dve", lambda e: e.tensor_scalar(out=G[:, GOFF + 128:hi], in0=G[:, GOFF + 128:hi], scalar1=0.0,
                                                       scalar2=rb[:, 31:32], op0=ALU.mult, op1=ALU.add),
                      reads=[rb_b], writes=[G_b])
                tk.dma("sp", dst, G[:, :], reads=[G_b])
        rc = sb("rc_sb", [64, 2]); rc_b = tk.buf()
        tk.dma("sp", rc[:, :], rc_d[:, :], writes=[rc_b])
        pi_ = sb("pos_i", [64, SEQ], I32); pi_b = tk.buf()
        tk.dma("sp", pi_[:, :], pos_d[0:1, :].to_broadcast([64, SEQ]), writes=[pi_b])
        ang = sb("ang", [64, SEQ]); ang_b = tk.buf()
        tk.op("dve", lambda e: e.tensor_copy(out=ang[:, :], in_=pi_[:, :]), reads=[pi_b], writes=[ang_b])
        tk.op("dve", lambda e: e.tensor_scalar(out=ang[:, :], in0=ang[:, :], scalar1=rc[:, 0:1], scalar2=None,
                                               op0=ALU.mult), reads=[rc_b], writes=[ang_b])
        kf = sb("kf", [64, SEQ]); kf_b = tk.buf()
        ki = sb("ki", [64, SEQ], I32); ki_b = tk.buf()
        r = sb("rr", [64, SEQ]); r_b = tk.buf()
        res_t = sb("rope_res", [64, SEQ]); res_b = tk.buf()
        C1 = 6.28125
        C2 = 2.0 * math.pi - C1
        for which, shift, dst in (("sin", 0.0, sin_d), ("cos", math.pi / 2, cos_d)):
            tk.op("dve", lambda e: e.tensor_scalar(out=kf[:, :], in0=ang[:, :], scalar1=shift, scalar2=1.0 / (2 * math.pi),
                                                   op0=ALU.add, op1=ALU.mult), reads=[ang_b], writes=[kf_b])
            tk.op("dve", lambda e: e.tensor_copy(out=ki[:, :], in_=kf[:, :]), reads=[kf_b], writes=[ki_b])
            tk.op("dve", lambda e: e.tensor_copy(out=kf[:, :], in_=ki[:, :]), reads=[ki_b], writes=[kf_b])
            tk.op("dve", lambda e: e.scalar_tensor_tensor(out=r[:, :], in0=kf[:, :], scalar=-C1, in1=ang[:, :],
                                                          op0=ALU.mult, op1=ALU.add), reads=[kf_b, ang_b], writes=[r_b])
            tk.op("dve", lambda e: e.tensor_scalar(out=r[:, :], in0=r[:, :], scalar1=shift, scalar2=None, op0=ALU.add),
                  writes=[r_b])
            tk.op("dve", lambda e: e.scalar_tensor_tensor(out=r[:, :], in0=kf[:, :], scalar=-C2, in1=r[:, :],
                                                          op0=ALU.mult, op1=ALU.add), reads=[kf_b], writes=[r_b])
            tk.op("dve", lambda e: e.tensor_scalar(out=r[:, :], in0=r[:, :], scalar1=3.1415925, scalar2=-3.1415925,
                                                   op0=ALU.min, op1=ALU.max), writes=[r_b])
            tk.op("act", lambda e: e.activation(out=res_t[:, :], in_=r[:, :], func=AF.Sin), reads=[r_b], writes=[res_b])
            if which == "sin":
                tk.op("dve", lambda e: e.tensor_scalar(out=res_t[:, :], in0=res_t[:, :], scalar1=rc[:, 1:2], scalar2=None,
                                                       op0=ALU.mult), reads=[rc_b], writes=[res_b])
            tk.dma("sp", dst[:, :], res_t[:, :], reads=[res_b])
        tk.barrier()


def build_p0_prog():
    nc = bass.Bass("TRN2", target_bir_lowering=False)
    rb_d = nc.dram_tensor("rb", [1, 32], F32, kind="ExternalInput").ap()
    pos_d = nc.dram_tensor("pos", [1, SEQ], I32, kind="ExternalInput").ap()
    rc_d = nc.dram_tensor("ropec", [64, 2], F32, kind="ExternalInput").ap()
    gc_d = nc.dram_tensor("gc", [128, GL], F32, kind="ExternalOutput").ap()
    gw_d = nc.dram_tensor("gw", [128, GL], F32, kind="ExternalOutput").ap()
    cos_d = nc.dram_tensor("cos2", [64, SEQ], F32, kind="ExternalOutput").ap()
    sin_d = nc.dram_tensor("sins", [64, SEQ], F32, kind="ExternalOutput").ap()
    with ExitStack() as es:
        tk = TK(nc, es)
        p0_tables(tk, nc, rb_d, pos_d, rc_d, [(0, gc_d[:, :], gw_d[:, :])], cos_d, sin_d)
    return nc


def rope_consts():
    inv = (10000.0 ** (-np.arange(32, dtype=np.float32) * 2.0 / 64)).astype(np.float32)
    rc = np.zeros((64, 2), np.float32)
    rc[:, 0] = np.concatenate([inv, inv])
    rc[:, 1] = np.concatenate([-np.ones(32), np.ones(32)])
    return rc


CQ, CKV, KR, NQ, KC, VC, KS, KW, VS, VW, GT, NZ = 0, 512, 1024, 1088, 1856, 2048, 2176, 2368, 2560, 2688, 2816, 2828
DEBUG_STOP = 99
DEBUG_FLAGS = set()


class StopBuild(Exception):
    pass


def dbg(level):
    if DEBUG_STOP <= level:
        DEAD[0] = True
S = SEQ
NT = S // 512
MLA_SCALE = 192 ** -0.5
NSA_SCALE = 192 ** -0.5


def run_pipelined(items, phA, phB, depth=2):
    n = len(items)
    for i in range(n + depth):
        if i < n:
            phA(items[i])
        if i - depth >= 0:
            phB(items[i - depth])


def glob_col(c, hh):
    if hh is None:
        return c
    segs = [(CQ, 0), (CKV, 512), (KR, 1024), (NQ, 1088 + hh * 768), (KC, 2624 + hh * 192), (VC, 3008 + hh * 128),
            (KS, 3264 + hh * 192), (KW, 3904 + hh * 192), (VS, 3648 + hh * 128), (VW, 4288 + hh * 128), (GT, 4544 + hh * 12)]
    base = None
    for lo, g in segs:
        if c >= lo:
            base = (lo, g)
    return base[1] + (c - base[0])


def wq_c0(hh):
    return 0 if hh is None else hh * 768


def wkv_c0(hh):
    return 0 if hh is None else hh * 512


def orow(hh):
    return 0 if hh is None else hh * 512


def win_cols(hh):
    g = hh
    r = lambda a, n: list(range(a, a + n))
    cols = r(0, 512) + r(512, 512) + r(1024, 64) + r(1088 + g * 768, 768)
    cols += r(2624 + g * 192, 192) + r(3008 + g * 128, 128) + r(3264 + g * 192, 192) + r(3904 + g * 192, 192)
    cols += r(3648 + g * 128, 128) + r(4288 + g * 128, 128) + r(4544 + g * 12, 12)
    assert len(cols) == NZ
    return np.array(cols)


def build_attn_prog():
    nc = bass.Bass("TRN2", target_bir_lowering=False)
    din = lambda n, s, d=F32: nc.dram_tensor(n, s, d, kind="ExternalInput").ap()
    hT_d = din("hT", [D, S])
    g_d = din("gains", [128, 24])
    win_d = din("win", [D, NZ])
    wq_d = din("wqup", [512, 768])
    wuk_d = din("wuk", [512, 512])
    wuv_d = din("wuv", [512, 512])
    pek_d = din("pekT", [192, 32])
    w1k_d = din("w1k", [6144, 256])
    w2k_d = din("w2k", [256, 192])
    pev_d = din("pevT", [128, 32])
    w1v_d = din("w1v", [4096, 256])
    w2v_d = din("w2v", [256, 128])
    gc_d = nc.dram_tensor("gc", [4, 128, GL], F32, kind="ExternalInput")
    gw_d = nc.dram_tensor("gw", [4, 128, GL], F32, kind="ExternalInput")
    cos_d = din("cos2", [64, S])
    sin_d = din("sins", [64, S])
    ov_d = din("ov1", [128, 33])
    sa_d = din("scoreA", [128, 16, 32])
    sbb_d = din("scoreB", [128, 16, 32])
    ex_d = din("expand", [32, 16 * 128])
    sel_d = din("gsel", [12, 12 * 128])
    id_d = din("ident", [128, 128])
    aT_d = nc.dram_tensor("aT", [512, S], BF16, kind="ExternalOutput").ap()
    bT_d = nc.dram_tensor("bT", [512, S], BF16, kind="ExternalOutput").ap()
    z32_d = nc.dram_tensor("z32", [NZ, S], F32, kind="Internal").ap()
    z16_d = nc.dram_tensor("z16", [NZ, S], BF16, kind="Internal").ap()
    vtok_d = nc.dram_tensor("vtok", [S, 256], BF16, kind="Internal").ap()

    with ExitStack() as es0:
        cx = Ctx(nc, es0)
        tk = cx.tk
        DEAD[0] = False
        _attn_body(cx, es0, nc, locals())
        DEAD[0] = False
        tk.barrier()
    return nc


def _attn_body(cx, es0, nc, L):
    globals_ = L
    (hT_d, g_d, win_d, wq_d, wuk_d, wuv_d, pek_d, w1k_d, w2k_d, pev_d, w1v_d, w2v_d, gc_d, gw_d, cos_d, sin_d, ov_d, sa_d, sbb_d,
     ex_d, sel_d, id_d, aT_d, bT_d, z32_d, z16_d, vtok_d) = [L[k] for k in (
        'hT_d', 'g_d', 'win_d', 'wq_d', 'wuk_d', 'wuv_d', 'pek_d', 'w1k_d', 'w2k_d', 'pev_d', 'w1v_d', 'w2v_d', 'gc_d', 'gw_d',
        'cos_d', 'sin_d', 'ov_d', 'sa_d', 'sbb_d', 'ex_d', 'sel_d', 'id_d', 'aT_d', 'bT_d', 'z32_d', 'z16_d', 'vtok_d')]
    tk = cx.tk
    hh = L.get('hh', None)
    gm, gq, gkv = L.get('gcols', (0, 16, 20))
    if True:
        gains = L['gains'] if 'gains' in L else load_gains(cx, es0, g_d, 24)
        g_sb, g_b = gains
        z32_b, z16_b, vtok_b = tk.buf(), tk.buf(), tk.buf()

        mark(tk, 'attn.s1')
        with ExitStack() as es:
            uT = es.enter_context(_sbt(nc, "uT", [128, 16, S], BF16))
            u_b = tk.bufs(16)
            st32 = [es.enter_context(_sbt(nc, "st32_%d" % i, [128, 512], F32)) for i in range(2)]
            st16 = [es.enter_context(_sbt(nc, "st16_%d" % i, [128, 512], BF16)) for i in range(2)]
            st32_b, st16_b = tk.bufs(2), tk.bufs(2)
            chunks = []
            if hh != 1:
                for c in range(0, 1024, 128):
                    chunks.append((c, 128))
                chunks.append((KR, 64))
            for h in range(4):
                chunks += [(NQ + h * 192, 128), (NQ + h * 192 + 128, 64)]
            chunks += [(KC, 128), (KC + 128, 64), (VC, 128), (KS, 128), (KS + 128, 64), (KW, 128), (KW + 128, 64), (GT, 12)]
            slabs = []
            lastc = None
            for (c, m) in chunks:
                gcl = glob_col(c, hh)
                if slabs and lastc == c and slabs[-1][0] + sum(slabs[-1][1]) == gcl and sum(slabs[-1][1]) + m <= 256:
                    slabs[-1][1].append(m)
                else:
                    slabs.append((gcl, [m]))
                lastc = c + m
            pend_s1 = cx.fetch(win_d, 0, 16, slabs[0][0], sum(slabs[0][1]))
            norm_from_dram(cx, hT_d, g_sb[:, gm:gm + 16], g_b, uT, u_b, S, D, [0, 1, 2, 3])
            cnt = [0]

            def epi(ci, m, t, ps, pb):
                c0 = chunks[ci][0]
                i = cnt[0] % 2
                cnt[0] += 1
                sl = slice(t * 512, (t + 1) * 512)
                eng = "act" if cnt[0] % 2 else "dve"
                if c0 == GT:
                    tk.op("act", lambda e: e.activation(out=st32[i][0:m, :], in_=ps[0:m, :], func=AF.Sigmoid),
                          reads=[pb], writes=[st32_b[i]])
                    tk.dma("sp", z32_d[c0:c0 + m, sl], st32[i][0:m, :], reads=[st32_b[i]], writes=[z32_b], join=True, anchor=st32_b[i])
                elif c0 < NQ or KC <= c0 < KS:
                    if eng == "act":
                        tk.op("act", lambda e: e.copy(out=st32[i][0:m, :], in_=ps[0:m, :]), reads=[pb], writes=[st32_b[i]])
                    else:
                        tk.op("dve", lambda e: e.tensor_copy(out=st32[i][0:m, :], in_=ps[0:m, :]), reads=[pb], writes=[st32_b[i]])
                    tk.dma("sp", z32_d[c0:c0 + m, sl], st32[i][0:m, :], reads=[st32_b[i]], writes=[z32_b], join=True, anchor=st32_b[i])
                else:
                    if eng == "act":
                        tk.op("act", lambda e: e.copy(out=st16[i][0:m, :], in_=ps[0:m, :]), reads=[pb], writes=[st16_b[i]])
                    else:
                        tk.op("dve", lambda e: e.tensor_copy(out=st16[i][0:m, :], in_=ps[0:m, :]), reads=[pb], writes=[st16_b[i]])
                    tk.dma("sp", z16_d[c0:c0 + m, sl], st16[i][0:m, :], reads=[st16_b[i]], writes=[z16_b], join=True, anchor=st16_b[i])
            gemm_fm(cx, win_d, 16, slabs, uT, u_b, S, [0, 1, 2, 3, 4, 5, 6, 7], epi, pend=pend_s1)
            wv0, wvb0 = cx.fetch(win_d, 0, 16, glob_col(VS, hh), 128)
            wv1, wvb1 = cx.fetch(win_d, 0, 16, glob_col(VW, hh), 128)
            for tt in range(16):
                pid = tt % 4
                ps, pb = cx.ps[pid], cx.ps_b[pid]
                for (wv, wvb, co) in ((wv0, wvb0, 0), (wv1, wvb1, 128)):
                    for k in range(16):
                        tk.op("pe", lambda e: e.matmul(ps[:, co:co + 128], lhsT=uT[:, k, tt * 128:(tt + 1) * 128], rhs=wv[:, k, :],
                                                       start=(k == 0), stop=(k == 15)),
                              reads=[wvb, u_b[k]], writes=[pb], pe_acc=(k > 0 or co > 0))
                i = tt % 2
                tk.op("act", lambda e: e.copy(out=st16[i][:, 0:256], in_=ps[:, 0:256]), reads=[pb], writes=[st16_b[i]])
                tk.dma("sp", vtok_d[tt * 128:(tt + 1) * 128, :], st16[i][:, 0:256], reads=[st16_b[i]], writes=[vtok_b], join=True, anchor=st16_b[i])
            tk.barrier()

        mark(tk, 'mla.proj')
        with ExitStack() as es:
          dbg(1)
          if True:
              sbt = lambda n, s, d: es.enter_context(_sbt(nc, n, s, d))
              qa = sbt("m_qa", [128, 4, S], BF16); qa_b = tk.buf()
              qrr = sbt("m_qrr", [64, 4, S], BF16); qrr_b = tk.buf()
              ka = sbt("m_ka", [128, 4, S], BF16); ka_b = tk.buf()
              krr = sbt("m_krr", [64, S], BF16); krr_b = tk.buf()
              vt = sbt("m_v", [128, 16, 512], BF16); vt_b = tk.buf()
              cos_t = sbt("m_cos", [64, S], F32); sin_t = sbt("m_sin", [64, S], F32); rp_b = tk.buf()
              tk.dma("sp", cos_t[:, :], cos_d[:, :], writes=[rp_b])
              tk.dma("sp", sin_t[:, :], sin_d[:, :], writes=[rp_b], join=True)
              rt = [sbt("m_rt%d" % i, [64, 512], F32) for i in range(2)]
              rs = [sbt("m_rs%d" % i, [64, 512], F32) for i in range(2)]
              rt_b, rs_b = tk.bufs(2), tk.bufs(2)
              raw = [sbt("m_raw%d" % i, [64, 512], F32) for i in range(2)]; raw_b = tk.bufs(2)
              sinx = sbt("m_sinx", [64, S], F32)
              tk.op("dve", lambda e: e.tensor_scalar(out=sinx[:, :], in0=sin_t[:, :], scalar1=-1.0, scalar2=None, op0=ALU.mult),
                    reads=[rp_b], writes=[rp_b])
              rcnt = [0]

              def rope_epi(ps, pb, m_dst, dst_b, t):
                  if 'norope' in DEBUG_FLAGS:
                      tk.op("act", lambda e: e.copy(out=m_dst, in_=ps[0:64, :]), reads=[pb], writes=[dst_b])
                      return
                  i = rcnt[0] % 2
                  rcnt[0] += 1
                  sl = slice(t * 512, (t + 1) * 512)
                  tk.op("act", lambda e: e.copy(out=raw[i][:, :], in_=ps[0:64, :]), reads=[pb], writes=[raw_b[i]])
                  rope_math(raw[i][:, :], raw_b[i], i, sl, m_dst, dst_b)

              def rope_math(src, src_b, i, sl, m_dst, dst_b):
                  a, s_ = rt[i], rs[i]
                  tk.op("dve", lambda e: e.tensor_tensor(out=s_[0:32, :], in0=src[32:64, :], in1=sinx[32:64, sl], op=ALU.mult),
                        reads=[src_b, rp_b], writes=[rs_b[i]])
                  tk.op("dve", lambda e: e.tensor_tensor(out=s_[32:64, :], in0=src[0:32, :], in1=sinx[0:32, sl], op=ALU.mult),
                        reads=[src_b, rp_b], writes=[rs_b[i]])
                  tk.op("dve", lambda e: e.tensor_tensor(out=a[:, :], in0=src, in1=cos_t[:, sl], op=ALU.mult),
                        reads=[src_b, rp_b], writes=[rt_b[i]])
                  tk.op("dve", lambda e: e.tensor_tensor(out=m_dst, in0=a[:, :], in1=s_[:, :], op=ALU.add),
                        reads=[rt_b[i], rs_b[i]], writes=[dst_b])

              for which in ("q", "kv"):
                  with ExitStack() as es2:
                      cn = es2.enter_context(_sbt(nc, "m_cn" + which, [128, 4, S], BF16)); cn_b = tk.bufs(4)
                      base = CQ if which == "q" else CKV
                      gcol = gq if which == "q" else gkv
                      dbg(1.21)
                      norm_from_dram(cx, z32_d[base:base + 512, :], g_sb[:, gcol:gcol + 4], g_b, cn, cn_b, S, 512, [0, 1, 2, 3])
                      dbg(1.22)
                      if which == "q":
                          slabs = [(wq_c0(hh) + h * 192, [128, 64]) for h in range(4)]

                          def epi(ci, m, t, ps, pb):
                              h = ci // 2
                              sl = slice(t * 512, (t + 1) * 512)
                              if ci % 2 == 0:
                                  tk.op("act", lambda e: e.copy(out=qa[:, h, sl], in_=ps[:, :]), reads=[pb], writes=[qa_b])
                              else:
                                  rope_epi(ps, pb, qrr[:, h, sl], qrr_b, t)
                          gemm_fm(cx, wq_d, 4, slabs, cn, cn_b, S, [4, 5, 6, 7], epi)
                      else:
                          def epi(ci, m, t, ps, pb):
                              sl = slice(t * 512, (t + 1) * 512)
                              tk.op("act", lambda e: e.copy(out=ka[:, ci, sl], in_=ps[:, :]), reads=[pb], writes=[ka_b])
                          gemm_fm(cx, wuk_d, 4, col_slabs(wkv_c0(hh), 512, 256), cn, cn_b, S, [4, 5, 6, 7], epi)
                          wv, wvb = cx.fetch(wuv_d, 0, 4, wkv_c0(hh), 512)
                          for tt in range(16):
                              pid = 4 + tt % 4
                              ps, pb = cx.ps[pid], cx.ps_b[pid]
                              for k in range(4):
                                  tk.op("pe", lambda e: e.matmul(ps[:, :], lhsT=cn[:, k, tt * 128:(tt + 1) * 128], rhs=wv[:, k, :],
                                                                 start=(k == 0), stop=(k == 3)),
                                        reads=[wvb, cn_b[k]], writes=[pb], pe_acc=(k > 0))
                              tk.op("dve", lambda e: e.tensor_copy(out=vt[:, tt, :], in_=ps[:, :]), reads=[pb], writes=[vt_b])
                      tk.barrier()
              dbg(1.3)
              with ExitStack() as es2:
                  kr32 = es2.enter_context(_sbt(nc, "m_kr32", [64, S], F32)); kr32_b = tk.buf()
                  tk.dma("sp", kr32[:, :], z32_d[KR:KR + 64, :], reads=[z32_b], writes=[kr32_b])
                  for t in range(NT):
                      sl = slice(t * 512, (t + 1) * 512)
                      i = rcnt[0] % 2
                      rcnt[0] += 1
                      rope_math(kr32[:, sl], kr32_b, i, sl, krr[:, sl], krr_b)
                  tk.barrier()
              dbg(1.5)
              cm = sbt("m_cm", [128, 4, 512], F32); cm_b = tk.buf()
              tk.op("pool", lambda e: e.memset(cm[:, :, :], 0.0), writes=[cm_b])
              for di in range(4):
                  tk.op("pool", lambda e: e.affine_select(out=cm[:, di, :], in_=cm[:, di, :], pattern=[[1, 512]],
                                                          compare_op=ALU.is_ge, fill=NEGM, base=-128 * di,
                                                          channel_multiplier=-1), writes=[cm_b])
              dbg(1.7)
              mark(tk, 'mla.attn')
              NB = 4
              pt = [sbt("m_p%d" % i, [128, 512], BF16) for i in range(NB)]; pt_b = tk.bufs(NB)
              tm = [sbt("m_tm%d" % i, [128, 512], F32) for i in range(NB)]; tm_b = tk.bufs(NB)
              rl = sbt("m_rl", [128, 512], F32); rl_b = tk.buf()
              ot = [sbt("m_ot%d" % i, [128, 512], BF16) for i in range(2)]; ot_b = tk.bufs(2)
              items = []
              hq = 0
              for h in range(4):
                  for qt in range(NT):
                      nkt = 4 * qt + 4
                      for kt in range(nkt):
                          items.append((h, qt, kt, nkt, hq, len(items)))
                      hq += 1

              def phA(it):
                  h, qt, kt, nkt, g, n = it
                  qsl = slice(qt * 512, (qt + 1) * 512)
                  ksl = slice(kt * 128, (kt + 1) * 128)
                  i = n % NB
                  ps, pb = cx.ps[i], cx.ps_b[i]
                  tk.op("pe", lambda e: e.matmul(ps[:, :], lhsT=ka[:, h, ksl], rhs=qa[:, h, qsl], start=True, stop=False),
                        reads=[ka_b, qa_b], writes=[pb])
                  tk.op("pe", lambda e: e.matmul(ps[:, :], lhsT=krr[:, ksl], rhs=qrr[:, h, qsl], start=False, stop=True),
                        reads=[krr_b, qrr_b], writes=[pb], pe_acc=True)
                  di = kt - 4 * qt
                  if di >= 0:
                      tk.op("dve", lambda e: e.tensor_tensor(out=tm[i][:, :], in0=ps[:, :], in1=cm[:, di, :], op=ALU.add),
                            reads=[pb, cm_b], writes=[tm_b[i]])
                      tk.op("act", lambda e: e.activation(out=pt[i][:, :], in_=tm[i][:, :], func=AF.Exp, scale=MLA_SCALE),
                            reads=[tm_b[i]], writes=[pt_b[i]])
                  else:
                      tk.op("act", lambda e: e.activation(out=pt[i][:, :], in_=ps[:, :], func=AF.Exp, scale=MLA_SCALE),
                            reads=[pb], writes=[pt_b[i]])

              def phB(it):
                  h, qt, kt, nkt, g, n = it
                  qsl = slice(qt * 512, (qt + 1) * 512)
                  i = n % NB
                  O_id, L_id = 4 + (g % 2) * 2, 5 + (g % 2) * 2
                  tk.op("pe", lambda e: e.matmul(cx.ps[O_id][:, :], lhsT=vt[:, kt, h * 128:(h + 1) * 128], rhs=pt[i][:, :],
                                                 start=(kt == 0), stop=(kt == nkt - 1)),
                        reads=[vt_b, pt_b[i]], writes=[cx.ps_b[O_id]], pe_acc=(kt > 0))
                  tk.op("pe", lambda e: e.matmul(cx.ps[L_id][:, :], lhsT=cx.ones[:, :], rhs=pt[i][:, :],
                                                 start=(kt == 0), stop=(kt == nkt - 1)),
                        reads=[cx.ones_b, pt_b[i]], writes=[cx.ps_b[L_id]], pe_acc=(kt > 0))
                  if kt == nkt - 1:
                      oi = g % 2
                      tk.op("dve", lambda e: e.reciprocal(out=rl[:, :], in_=cx.ps[L_id][:, :]), reads=[cx.ps_b[L_id]], writes=[rl_b])
                      tk.op("dve", lambda e: e.tensor_tensor(out=ot[oi][:, :], in0=cx.ps[O_id][:, :], in1=rl[:, :], op=ALU.mult),
                            reads=[cx.ps_b[O_id], rl_b], writes=[ot_b[oi]])
                      tk.dma("sp", aT_d[orow(hh) + h * 128:orow(hh) + (h + 1) * 128, qsl], ot[oi][:, :], reads=[ot_b[oi]])
              run_pipelined(items, phA, phB, depth=2)
              tk.barrier()

        dbg(2)
        if True:
            nsa_stage(cx, es0, nc, gains, z32_d, z16_d, vtok_d, (z32_b, z16_b, vtok_b), pek_d, w1k_d, w2k_d, pev_d, w1v_d, w2v_d,
                  gc_d, gw_d, ov_d, sa_d, sbb_d, ex_d, sel_d, id_d, bT_d, hh)


def nsa_stage(cx, es0, nc, gains, z32_d, z16_d, vtok_d, zbufs, pek_d, w1k_d, w2k_d, pev_d, w1v_d, w2v_d,
              gc_d, gw_d, ov_d, sa_d, sbb_d, ex_d, sel_d, id_d, bT_d, hh=None):
    tk = cx.tk
    hb = 0 if hh is None else hh * 4
    with ExitStack() as es:
        sbt = lambda n, s, d: es.enter_context(_sbt(nc, n, s, d))
        kcTa = sbt("n_kcTa", [128, 128], BF16); kcTb = sbt("n_kcTb", [64, 128], BF16); vc = sbt("n_vc", [128, 128], BF16)
        kc_b = tk.buf()
        mark(tk, 'nsa.load')
        nqa = sbt("n_qa", [128, 4, S], BF16); nqb = sbt("n_qb", [64, 4, S], BF16)
        ksa = sbt("n_ksa", [128, S], BF16); ksb = sbt("n_ksb", [64, S], BF16)
        kwa = sbt("n_kwa", [128, S], BF16); kwb = sbt("n_kwb", [64, S], BF16)
        vsw = sbt("n_vsw", [128, 16, 256], BF16)
        gsig = sbt("n_gsig", [12, S], F32)
        ld_b = tk.buf()
        first = [True]

        def ld(dst, src):
            tk.dma("act", dst, src, writes=[ld_b], join=not first[0])
            first[0] = False
        for h in range(4):
            ld(nqa[:, h, :], z16_d[NQ + h * 192:NQ + h * 192 + 128, :])
            ld(nqb[:, h, :], z16_d[NQ + h * 192 + 128:NQ + h * 192 + 192, :])
        ld(ksa[:, :], z16_d[KS:KS + 128, :]); ld(ksb[:, :], z16_d[KS + 128:KS + 192, :])
        ld(kwa[:, :], z16_d[KW:KW + 128, :]); ld(kwb[:, :], z16_d[KW + 128:KW + 192, :])
        ld(vsw[:, :, :], vtok_d.rearrange("(t p) c -> p t c", p=128))
        ld(gsig[:, :], z32_d[GT:GT + 12, :])
        ov1 = sbt("n_ov1", [128, 33], BF16); expd = sbt("n_exp", [32, 16, 128], BF16)
        scA = sbt("n_scA", [128, 16, 32], F32); scB = sbt("n_scB", [128, 16, 32], F32)
        gsel = sbt("n_gsel", [12, 12, 128], F32); ident = sbt("n_ident", [128, 128], F32)
        ld(scA[:, :, :], sa_d[:, :, :]); ld(scB[:, :, :], sbb_d[:, :, :])
        ld(gsel[:, :, :], sel_d.rearrange("r (a p) -> r a p", p=128)); ld(ident[:, :], id_d[:, :])
        mark(tk, 'nsa.compress')
        with ExitStack() as es2:
            sb2 = lambda n, s, d: es2.enter_context(_sbt(nc, n, s, d))
            src32 = {"ka": sb2("c_ka", [128, S], F32), "kb": sb2("c_kb", [64, S], F32), "v": sb2("c_v", [128, S], F32)}
            src_b = tk.buf()
            tk.dma("sp", src32["ka"][:, :], z32_d[KC:KC + 128, :], writes=[src_b])
            tk.dma("sp", src32["kb"][:, :], z32_d[KC + 128:KC + 192, :], writes=[src_b], join=True)
            tk.dma("sp", src32["v"][:, :], z32_d[VC:VC + 128, :], writes=[src_b], join=True)
            pe = {"ka": sb2("c_pea", [128, 32], F32), "kb": sb2("c_peb", [64, 32], F32), "v": sb2("c_pev", [128, 32], F32)}
            pe_b = tk.buf()
            tk.dma("sp", pe["ka"][:, :], pek_d[0:128, :], writes=[pe_b])
            tk.dma("sp", pe["kb"][:, :], pek_d[128:192, :], writes=[pe_b], join=True)
            tk.dma("sp", pe["v"][:, :], pev_d[:, :], writes=[pe_b], join=True)
            zl = {"ka": sb2("c_zla", [128, 32, 127], BF16), "kb": sb2("c_zlb", [64, 32, 127], BF16),
                  "v": sb2("c_zlv", [128, 32, 127], BF16)}
            zlb = {key: tk.bufs(32) for key in ("ka", "kb", "v")}
            for key, pk in (("ka", 128), ("kb", 64), ("v", 128)):
                v3 = src32[key][:, :].rearrange("p (n s) -> p n s", s=16)
                for l in range(32):
                    a, r = l // 16, l % 16
                    tk.op("dve", lambda e: e.tensor_scalar(out=zl[key][:, l, :], in0=v3[:, a:a + 127, r],
                                                           scalar1=pe[key][:, l:l + 1], scalar2=None, op0=ALU.add),
                          reads=[src_b, pe_b], writes=[zlb[key][l]])
            hid = {"k": sb2("c_hk", [128, 2, 128], BF16), "v": sb2("c_hv", [128, 2, 128], BF16)}
            hid_b = tk.buf()
            for hc in range(2):
                sa_, sab = cx.fetch(w1k_d, 0, 32, hc * 128, 128, pk=128, rstride=192)
                sb_, sbb = cx.fetch(w1k_d, 128, 32, hc * 128, 128, pk=64, rstride=192)
                ps, pb = cx.ps[hc], cx.ps_b[hc]
                for l in range(32):
                    tk.op("pe", lambda e: e.matmul(ps[:, 0:127], lhsT=sa_[:, l, :], rhs=zl["ka"][:, l, :], start=(l == 0), stop=False),
                          reads=[sab, zlb["ka"][l]], writes=[pb], pe_acc=(l > 0))
                    tk.op("pe", lambda e: e.matmul(ps[:, 0:127], lhsT=sb_[:, l, :], rhs=zl["kb"][:, l, :], start=False, stop=(l == 31)),
                          reads=[sbb, zlb["kb"][l]], writes=[pb], pe_acc=True)
                tk.op("act", lambda e: e.activation(out=hid["k"][:, hc, 0:127], in_=ps[:, 0:127], func=AF.Silu),
                      reads=[pb], writes=[hid_b])
            for hc in range(2):
                sv_, svb = cx.fetch(w1v_d, 0, 32, hc * 128, 128, pk=128, rstride=128)
                ps, pb = cx.ps[2 + hc], cx.ps_b[2 + hc]
                for l in range(32):
                    tk.op("pe", lambda e: e.matmul(ps[:, 0:127], lhsT=sv_[:, l, :], rhs=zl["v"][:, l, :], start=(l == 0), stop=(l == 31)),
                          reads=[svb, zlb["v"][l]], writes=[pb], pe_acc=(l > 0))
                tk.op("act", lambda e: e.activation(out=hid["v"][:, hc, 0:127], in_=ps[:, 0:127], func=AF.Silu),
                      reads=[pb], writes=[hid_b])
            w2k, w2kb = cx.fetch(w2k_d, 0, 2, 0, 192)
            w2v, w2vb = cx.fetch(w2v_d, 0, 2, 0, 128)
            for hc in range(2):
                tk.op("pe", lambda e: e.matmul(cx.ps[4][:, 0:127], lhsT=w2k[:, hc, 0:128], rhs=hid["k"][:, hc, 0:127],
                                               start=(hc == 0), stop=(hc == 1)), reads=[w2kb, hid_b], writes=[cx.ps_b[4]], pe_acc=(hc > 0))
            for hc in range(2):
                tk.op("pe", lambda e: e.matmul(cx.ps[5][0:64, 0:127], lhsT=w2k[:, hc, 128:192], rhs=hid["k"][:, hc, 0:127],
                                               start=(hc == 0), stop=(hc == 1)), reads=[w2kb, hid_b], writes=[cx.ps_b[5]], pe_acc=(hc > 0))
            for hc in range(2):
                tk.op("pe", lambda e: e.matmul(cx.ps[6][0:127, 0:128], lhsT=hid["v"][:, hc, 0:127], rhs=w2v[:, hc, :],
                                               start=(hc == 0), stop=(hc == 1)), reads=[w2vb, hid_b], writes=[cx.ps_b[6]], pe_acc=(hc > 0))
            tk.op("dve", lambda e: e.tensor_copy(out=kcTa[:, 0:127], in_=cx.ps[4][:, 0:127]), reads=[cx.ps_b[4]], writes=[kc_b])
            tk.op("dve", lambda e: e.tensor_copy(out=kcTb[:, 0:127], in_=cx.ps[5][0:64, 0:127]), reads=[cx.ps_b[5]], writes=[kc_b])
            tk.op("dve", lambda e: e.tensor_copy(out=vc[0:127, :], in_=cx.ps[6][0:127, 0:128]), reads=[cx.ps_b[6]], writes=[kc_b])
            tk.barrier()

        acc = sbt("n_acc", [128, 4, S], F32); acc_b = tk.buf()
        negT = sbt("n_negT", [32, S], BF16); negT_b = tk.buf()
        with ExitStack() as es2:
            t_ov = es2.enter_context(_sbt(nc, "n_tov", [128, 33], F32))
            t_ex = es2.enter_context(_sbt(nc, "n_tex", [32, 16 * 128], F32))
            ld(t_ov[:, :], ov_d[:, :]); ld(t_ex[:, :], ex_d[:, :])
            tk.op("dve", lambda e: e.tensor_copy(out=ov1[:, :], in_=t_ov[:, :]), reads=[ld_b], writes=[ld_b])
            tk.op("dve", lambda e: e.tensor_copy(out=expd[:, :, :].rearrange("p a b -> p (a b)"), in_=t_ex[:, :]), reads=[ld_b], writes=[ld_b])
            tk.barrier()

        NBN = 3
        tm = [sbt("n_tm%d" % i, [128, 512], F32) for i in range(NBN)]; tm_b = tk.bufs(NBN)
        pt = [sbt("n_pt%d" % i, [128, 512], BF16) for i in range(NBN)]; pt_b = tk.bufs(NBN)
        rl = sbt("n_rl", [128, 512], F32); rl_b = tk.buf()
        rlg = sbt("n_rlg", [128, 512], F32); rlg_b = tk.buf()
        tmp = sbt("n_tmp", [128, 512], F32); tmp_b = tk.buf()
        cc = [0]

        def combine(br, j, qt, O_id, L_id, G_id=None):
            qsl = slice(qt * 512, (qt + 1) * 512)
            if G_id is None:
                G_id = 6 + cc[0] % 2
                cc[0] += 1
            tk.op("pe", lambda e: e.matmul(cx.ps[G_id][:, :], lhsT=gsel[:, j * 3 + br, :], rhs=gsig[:, qsl], start=True, stop=True),
                  reads=[ld_b], writes=[cx.ps_b[G_id]])
            tk.op("dve", lambda e: e.tensor_scalar(out=rl[:, :], in0=cx.ps[L_id][:, :], scalar1=1e-30, scalar2=None, op0=ALU.max),
                  reads=[cx.ps_b[L_id]], writes=[rl_b])
            tk.op("dve", lambda e: e.reciprocal(out=rl[:, :], in_=rl[:, :]), writes=[rl_b])
            tk.op("dve", lambda e: e.tensor_tensor(out=rlg[:, :], in0=cx.ps[G_id][:, :], in1=rl[:, :], op=ALU.mult),
                  reads=[cx.ps_b[G_id], rl_b], writes=[rlg_b])
            if br == 0:
                tk.op("dve", lambda e: e.tensor_tensor(out=acc[:, j, qsl], in0=cx.ps[O_id][:, :], in1=rlg[:, :], op=ALU.mult),
                      reads=[cx.ps_b[O_id], rlg_b], writes=[acc_b])
            else:
                tk.op("dve", lambda e: e.tensor_tensor(out=tmp[:, :], in0=cx.ps[O_id][:, :], in1=rlg[:, :], op=ALU.mult),
                      reads=[cx.ps_b[O_id], rlg_b], writes=[tmp_b])
                tk.op("pool", lambda e: e.tensor_tensor(out=acc[:, j, qsl], in0=acc[:, j, qsl], in1=tmp[:, :], op=ALU.add),
                      reads=[tmp_b], writes=[acc_b])

        mark(tk, 'nsa.cmp')
        with ExitStack() as es2:
            eT = es2.enter_context(_sbt(nc, "n_eT", [128, 4, S], BF16)); eT_b = tk.buf()
            with ExitStack() as es3:
                bmc = [es3.enter_context(_sbt(nc, "n_bmc%d" % i, [128, S // 2], F32)) for i in range(2)]; bmc_b = tk.bufs(2)
                eTb = [[tk.buf() for _ in range(NT)] for _ in range(4)]
                items = [(j, qt, j * NT + qt) for j in range(4) for qt in range(NT)]

                def strip(u):
                    j, qh = u // 2, u % 2
                    tk.dma("sp", bmc[u % 2][0:127, :], bass.AP(gc_d, (hb + j) * 128 * GL + 2017 + qh * (S // 2), [[GL - 16, 127], [1, S // 2]]),
                           writes=[bmc_b[u % 2]])
                strip(0)

                def cA(it):
                    j, qt, n = it
                    u = j * 2 + qt // 2
                    if qt % 2 == 0 and u + 1 < 8:
                        strip(u + 1)
                    qsl = slice(qt * 512, (qt + 1) * 512)
                    sid, i = n % 2, n % NBN
                    ps, pb = cx.ps[sid], cx.ps_b[sid]
                    tk.op("pe", lambda e: e.matmul(ps[0:127, :], lhsT=kcTa[:, 0:127], rhs=nqa[:, j, qsl], start=True, stop=False),
                          reads=[kc_b, ld_b], writes=[pb])
                    tk.op("pe", lambda e: e.matmul(ps[0:127, :], lhsT=kcTb[:, 0:127], rhs=nqb[:, j, qsl], start=False, stop=True),
                          reads=[kc_b, ld_b], writes=[pb], pe_acc=True)
                    tk.op("dve", lambda e: e.scalar_tensor_tensor(out=tm[i][0:127, :], in0=ps[0:127, :], scalar=NSA_SCALE,
                                                                  in1=bmc[u % 2][0:127, (qt % 2) * 512:(qt % 2 + 1) * 512], op0=ALU.mult, op1=ALU.add),
                          reads=[pb, bmc_b[u % 2]], writes=[tm_b[i]])
                    tk.op("act", lambda e: e.activation(out=eT[0:127, j, qsl], in_=tm[i][0:127, :], func=AF.Exp),
                          reads=[tm_b[i]], writes=[eTb[j][qt]])

                def cB(it):
                    j, qt, n = it
                    qsl = slice(qt * 512, (qt + 1) * 512)
                    O_id, L_id = 2 + n % 2, 4 + n % 2
                    tk.op("pe", lambda e: e.matmul(cx.ps[O_id][:, :], lhsT=vc[0:127, :], rhs=eT[0:127, j, qsl], start=True, stop=True),
                          reads=[kc_b, eTb[j][qt]], writes=[cx.ps_b[O_id]])
                    tk.op("pe", lambda e: e.matmul(cx.ps[L_id][:, :], lhsT=cx.ones[0:127, :], rhs=eT[0:127, j, qsl], start=True, stop=True),
                          reads=[cx.ones_b, eTb[j][qt]], writes=[cx.ps_b[L_id]])
                    combine(0, j, qt, O_id, L_id)
                run_pipelined(items, cA, cB, depth=1)
                tk.barrier()
            mark(tk, 'nsa.topk')
            NQQ = 16
            mk = lambda nm, shp: [es2.enter_context(_sbt(nc, "%s%d" % (nm, q), shp, F32)) for q in range(NQQ)]
            l4, imp, sc2, m8, thr, neg = mk("n_l4", [128, 4]), mk("n_imp", [128, 32]), mk("n_sc2", [128, 32]), mk("n_m8", [128, 16]), \
                mk("n_thr", [128, 1]), mk("n_neg", [128, 32])
            tb = tk.bufs(NQQ)
            psI = lambda qq: (cx.ps[qq // 3], cx.ps_b[qq // 3], (qq % 3) * 132)

            def s_mm(qq):
                ps, pb, c0 = psI(qq)
                for j in range(4):
                    tk.op("pe", lambda e: e.matmul(ps[:, c0 + j * 33:c0 + (j + 1) * 33], lhsT=eT[0:127, j, qq * 128:(qq + 1) * 128],
                                                   rhs=ov1[0:127, :], start=True, stop=True), reads=[eTb[j][qq // 4], ld_b], writes=[pb], pe_acc=True)

            def s_l4(qq):
                ps, pb, c0 = psI(qq)
                lv = ps[:, c0:c0 + 132].rearrange("p (j c) -> p j c", c=33)[:, :, 32]
                tk.op("dve", lambda e: e.tensor_scalar(out=l4[qq][:, :], in0=lv, scalar1=1e-30, scalar2=None, op0=ALU.max),
                      reads=[pb], writes=[tb[qq]])

            def s_rc(qq):
                tk.op("dve", lambda e: e.reciprocal(out=l4[qq][:, :], in_=l4[qq][:, :]), writes=[tb[qq]])

            def s_imp(j):
                def f(qq):
                    ps, pb, c0 = psI(qq)
                    if j == 0:
                        tk.op("dve", lambda e: e.tensor_scalar(out=imp[qq][:, :], in0=ps[:, c0:c0 + 32], scalar1=l4[qq][:, 0:1], scalar2=None,
                                                               op0=ALU.mult), reads=[pb], writes=[tb[qq]])
                    else:
                        tk.op("dve", lambda e: e.scalar_tensor_tensor(out=imp[qq][:, :], in0=ps[:, c0 + j * 33:c0 + j * 33 + 32],
                                                                      scalar=l4[qq][:, j:j + 1], in1=imp[qq][:, :], op0=ALU.mult, op1=ALU.add),
                              reads=[pb], writes=[tb[qq]])
                return f

            def s_sa(qq):
                tk.op("dve", lambda e: e.tensor_tensor(out=imp[qq][:, :], in0=imp[qq][:, :], in1=scA[:, qq, :], op=ALU.mult), reads=[ld_b], writes=[tb[qq]])

            def s_sb(qq):
                tk.op("dve", lambda e: e.tensor_tensor(out=imp[qq][:, :], in0=imp[qq][:, :], in1=scB[:, qq, :], op=ALU.add), reads=[ld_b], writes=[tb[qq]])

            def s_m1(qq):
                tk.op("dve", lambda e: e.max(out=m8[qq][:, 0:8], in_=imp[qq][:, :]), writes=[tb[qq]])

            def s_mr(qq):
                tk.op("dve", lambda e: e.match_replace(out=sc2[qq][:, :], in_to_replace=m8[qq][:, 0:8], in_values=imp[qq][:, :], imm_value=-1e30),
                      writes=[tb[qq]])

            def s_m2(qq):
                tk.op("dve", lambda e: e.max(out=m8[qq][:, 8:16], in_=sc2[qq][:, :]), writes=[tb[qq]])

            def s_th(qq):
                tk.op("dve", lambda e: e.tensor_scalar(out=thr[qq][:, :], in0=m8[qq][:, 15:16], scalar1=0.0, scalar2=None, op0=ALU.max), writes=[tb[qq]])

            def s_ng(qq):
                tk.op("dve", lambda e: e.tensor_scalar(out=neg[qq][:, :], in0=imp[qq][:, :], scalar1=thr[qq][:, 0:1], scalar2=None, op0=ALU.is_ge),
                      writes=[tb[qq]])

            def s_n2(qq):
                tk.op("dve", lambda e: e.tensor_scalar(out=neg[qq][:, :], in0=neg[qq][:, :], scalar1=-NEGM, scalar2=NEGM, op0=ALU.mult, op1=ALU.add),
                      writes=[tb[qq]])

            def s_tr(qq):
                tp, tpb = cx.ps[6 + qq % 2], cx.ps_b[6 + qq % 2]
                tk.op("pe", lambda e: e.transpose(out=tp[0:32, 0:128], in_=neg[qq][:, :], identity=ident[:, :]), reads=[tb[qq], ld_b], writes=[tpb])
                tk.op("act", lambda e: e.copy(out=negT[:, qq * 128:(qq + 1) * 128], in_=tp[0:32, 0:128]), reads=[tpb], writes=[negT_b])
            for step in (s_mm, s_l4, s_rc, s_imp(0), s_imp(1), s_imp(2), s_imp(3), s_sa, s_sb, s_m1, s_mr, s_m2, s_th, s_ng, s_n2, s_tr):
                for qq in range(NQQ):
                    step(qq)
            tk.barrier()

        for br in (1, 2):
            mark(tk, 'nsa.br%d' % br)
            with ExitStack() as es2:
                W_ = 1152 if br == 1 else 1408
                bm = [es2.enter_context(_sbt(nc, "n_bm%d_%d" % (br, i), [128, W_], F32)) for i in range(2)]
                bm_b = tk.bufs(2)
                g_tab = gc_d if br == 1 else gw_d
                ka_, kb_ = (ksa, ksb) if br == 1 else (kwa, kwb)
                voff = 0 if br == 1 else 128
                items = []
                g = 0
                for j in range(4):
                    for qt in range(NT):
                        kts = list(range(0, 4 * qt + 4)) if br == 1 else list(range(max(0, 4 * qt - 4), 4 * qt + 4))
                        for n_, kt in enumerate(kts):
                            items.append((j, qt, kt, n_, len(kts), g, len(items)))
                        g += 1

                def phA(it):
                    j, qt, kt, n_, nk_, g, n = it
                    if qt == 0 and n_ == 0:
                        tk.dma("sp", bm[j % 2][:, :], bass.AP(g_tab, (hb + j) * 128 * GL + 1664, [[GL - 1, 128], [1, W_]]), writes=[bm_b[j % 2]])
                    qsl = slice(qt * 512, (qt + 1) * 512)
                    ksl = slice(kt * 128, (kt + 1) * 128)
                    i = n % NBN
                    ps, pb = cx.ps[i], cx.ps_b[i]
                    tk.op("pe", lambda e: e.matmul(ps[:, :], lhsT=ka_[:, ksl], rhs=nqa[:, j, qsl], start=True, stop=False),
                          reads=[ld_b], writes=[pb])
                    tk.op("pe", lambda e: e.matmul(ps[:, :], lhsT=kb_[:, ksl], rhs=nqb[:, j, qsl], start=False, stop=(br == 2)),
                          reads=[ld_b], writes=[pb], pe_acc=True)
                    if br == 1:
                        tk.op("pe", lambda e: e.matmul(ps[:, :], lhsT=expd[:, kt, :], rhs=negT[:, qsl], start=False, stop=True),
                              reads=[ld_b, negT_b], writes=[pb], pe_acc=True)
                    delta = qt * 512 - kt * 128
                    off = min(delta, 256) + 384 if br == 1 else delta + 384
                    tk.op("dve", lambda e: e.scalar_tensor_tensor(out=tm[i][:, :], in0=ps[:, :], scalar=NSA_SCALE,
                                                                  in1=bm[j % 2][:, off:off + 512], op0=ALU.mult, op1=ALU.add),
                          reads=[pb, bm_b[j % 2]], writes=[tm_b[i]])
                    tk.op("act", lambda e: e.activation(out=pt[i][:, :], in_=tm[i][:, :], func=AF.Exp),
                          reads=[tm_b[i]], writes=[pt_b[i]])

                def phB(it):
                    j, qt, kt, n_, nk_, g, n = it
                    i = n % NBN
                    O_id, L_id = 3 + g % 2, 5 + g % 2
                    tk.op("pe", lambda e: e.matmul(cx.ps[O_id][:, :], lhsT=vsw[:, kt, voff:voff + 128], rhs=pt[i][:, :],
                                                   start=(n_ == 0), stop=(n_ == nk_ - 1)),
                          reads=[ld_b, pt_b[i]], writes=[cx.ps_b[O_id]], pe_acc=(n_ > 0))
                    tk.op("pe", lambda e: e.matmul(cx.ps[L_id][:, :], lhsT=cx.ones[:, :], rhs=pt[i][:, :],
                                                   start=(n_ == 0), stop=(n_ == nk_ - 1)),
                          reads=[cx.ones_b, pt_b[i]], writes=[cx.ps_b[L_id]], pe_acc=(n_ > 0))
                    if n_ == nk_ - 1:
                        combine(br, j, qt, O_id, L_id, G_id=7)
                run_pipelined(items, phA, phB, depth=2)
                tk.barrier()
        mark(tk, 'nsa.out')
        ob = [sbt("n_ob%d" % i, [128, S], BF16) for i in range(2)]; ob_b = tk.bufs(2)
        for j in range(4):
            tk.op("act", lambda e: e.copy(out=ob[j % 2][:, :], in_=acc[:, j, :]), reads=[acc_b], writes=[ob_b[j % 2]])
            tk.dma("sp", bT_d[orow(hh) + j * 128:orow(hh) + (j + 1) * 128, :], ob[j % 2][:, :], reads=[ob_b[j % 2]])
        tk.barrier()


def attn_consts():
    n_cmp, n_slc = 127, 32
    cs = np.arange(n_cmp) * 16
    ce = cs + 31
    bs = np.arange(n_slc) * 64
    be = bs + 63
    ov = ((cs[:, None] <= be[None, :]) & (ce[:, None] >= bs[None, :])).astype(np.float32)
    ov1 = np.zeros((128, 33), np.float32)
    ov1[:127, :32] = ov
    ov1[:127, 32] = 1.0
    t = np.arange(SEQ)
    cur = t // 64
    jb = np.arange(n_slc)
    valid = jb[None, :] <= cur[:, None]
    forced = valid & ((jb[None, :] == 0) | (jb[None, :] >= cur[:, None] - 1))
    A = (valid & ~forced).astype(np.float32)
    B = np.where(forced, 1e6, np.where(valid, 0.0, -1.0)).astype(np.float32)
    A = np.ascontiguousarray(A.reshape(16, 128, 32).transpose(1, 0, 2))
    B = np.ascontiguousarray(B.reshape(16, 128, 32).transpose(1, 0, 2))
    ex = np.zeros((32, 16, 128), np.float32)
    for kt in range(16):
        ex[2 * kt, kt, :64] = 1.0
        ex[2 * kt + 1, kt, 64:] = 1.0
    gsel = np.zeros((12, 12, 128), np.float32)
    for r in range(12):
        gsel[r, r, :] = 1.0
    return {"ov1": ov1, "scoreA": A, "scoreB": B, "expand": ex.reshape(32, 2048), "gsel": gsel.reshape(12, 12 * 128),
            "ident": np.eye(128, dtype=np.float32)}


def stream(cx, specs, consume, pend=None):
    if pend is None:
        pend = cx.fetch(*specs[0])
    for i in range(len(specs)):
        nxt = cx.fetch(*specs[i + 1]) if i + 1 < len(specs) else None
        consume(i, pend[0], pend[1])
        pend = nxt


def stage_merge(cx, h_d, aT_d, bT_d, out_d, gains, gcol_pre, gcol_post, wmg, wa, wb, wo, T, cbase=0):
    tk, nc = cx.tk, cx.nc
    mark(tk, 'merge')
    g_sb, g_b = gains
    nt = T // 512
    with ExitStack() as es:
        mT = es.enter_context(_sbt(nc, "mg_m", [128, 16, T], BF16)); m_b = tk.bufs(16)
        with ExitStack() as es2:
            uT = es2.enter_context(_sbt(nc, "mg_u", [128, 16, T], BF16)); u_b = tk.bufs(16)
            aS = es2.enter_context(_sbt(nc, "mg_a", [128, 8, T], BF16)); bS = es2.enter_context(_sbt(nc, "mg_b", [128, 8, T], BF16))
            ab_b = tk.buf()
            tk.dma("sp", aS[:, :, :], aT_d.rearrange("(k p) t -> p k t", p=128), writes=[ab_b])
            tk.dma("sp", bS[:, :, :], bT_d.rearrange("(k p) t -> p k t", p=128), writes=[ab_b], join=True)
            specs = []
            for o in range(16):
                specs += [(wmg, 0, 16, cbase + o * 128, 128), (wmg, 0, 16, cbase + 2048 + o * 128, 128), (wa, 0, 8, o * 128, 128), (wb, 0, 8, o * 128, 128)]
            pend0 = cx.fetch(*specs[0])
            norm_from_dram(cx, h_d, g_sb[:, gcol_pre:gcol_pre + 16], g_b, uT, u_b, T, D, [0, 1, 2, 3])
            sg = [es2.enter_context(_sbt(nc, "mg_sg%d" % i, [128, 512], F32)) for i in range(2)]; sg_b = tk.bufs(2)
            t1 = [es2.enter_context(_sbt(nc, "mg_t%d" % i, [128, 512], F32)) for i in range(2)]; t1_b = tk.bufs(2)

            def consume(i, view, wbuf):
                o, kind = i // 4, i % 4
                nk = 16 if kind < 2 else 8
                src, src_bufs = (uT, u_b) if kind < 2 else ((aS, [ab_b] * 8) if kind == 2 else (bS, [ab_b] * 8))
                for t in range(nt):
                    pid = kind * 2 + t
                    ps, pb = cx.ps[pid], cx.ps_b[pid]
                    for k in range(nk):
                        tk.op("pe", lambda e: e.matmul(ps[:, :], lhsT=view[:, k, :], rhs=src[:, k, t * 512:(t + 1) * 512],
                                                       start=(k == 0), stop=(k == nk - 1)),
                              reads=[wbuf, src_bufs[k]], writes=[pb], pe_acc=(k > 0))
                if kind == 3:
                    for t in range(nt):
                        sl = slice(t * 512, (t + 1) * 512)
                        for br in range(2):
                            gid, pid = br * 2 + t, 4 + br * 2 + t
                            tk.op("act", lambda e: e.activation(out=sg[br][:, :], in_=cx.ps[gid][:, :], func=AF.Sigmoid),
                                  reads=[cx.ps_b[gid]], writes=[sg_b[br]])
                            tk.op("dve", lambda e: e.tensor_tensor(out=t1[br][:, :], in0=cx.ps[pid][:, :], in1=sg[br][:, :], op=ALU.mult),
                                  reads=[cx.ps_b[pid], sg_b[br]], writes=[t1_b[br]])
                        tk.op("pool", lambda e: e.tensor_tensor(out=mT[:, o, sl], in0=t1[0][:, :], in1=t1[1][:, :], op=ALU.add),
                              reads=[t1_b[0], t1_b[1]], writes=[m_b[o]])
            stream(cx, specs, consume, pend=pend0)
            tk.barrier()
        yT = es.enter_context(_sbt(nc, "mg_y", [128, 16, T], F32)); y_b = tk.bufs(16)

        def epi(ci, m, t, ps, pb):
            sl = slice(t * 512, (t + 1) * 512)
            if (ci + t) % 2:
                tk.op("act", lambda e: e.copy(out=yT[:, ci, sl], in_=ps[:, :]), reads=[pb], writes=[y_b[ci]])
            else:
                tk.op("dve", lambda e: e.tensor_copy(out=yT[:, ci, sl], in_=ps[:, :]), reads=[pb], writes=[y_b[ci]])
        gemm_fm(cx, wo, 16, col_slabs(0, D, 256), mT, m_b, T, [0, 1, 2, 3, 4, 5, 6, 7], epi)
        resid_tail(cx, yT, y_b, h_d, g_sb[:, gcol_post:gcol_post + 16], g_b, out_d, T, [0, 1, 2, 3])


def build_ca_prog(with_next, T=1024):
    nc = bass.Bass("TRN2", target_bir_lowering=False)
    din = lambda n, s, d=F32: nc.dram_tensor(n, s, d, kind="ExternalInput").ap()
    h_d = din("hT", [D, T])
    aT_d = din("aT", [1024, T], BF16)
    bT_d = din("bT", [1024, T], BF16)
    g_d = din("gains", [128, 96])
    wmg = din("wmg", [D, 4096]); wa = din("wa", [1024, D]); wb = din("wb", [1024, D]); wo = din("wo", [D, D])
    w2g = din("w2g", [D, FF]); w2u = din("w2u", [D, FF]); w2d = din("w2d", [FF, D])
    if with_next:
        w1g = din("w1g", [D, FF]); w1u = din("w1u", [D, FF]); w1d = din("w1d", [FF, D])
        hn_d = nc.dram_tensor("hnT", [D, T], F32, kind="ExternalOutput").ap()
    x_d = nc.dram_tensor("xT", [D, T], F32, kind="ExternalOutput").ap()
    h2_d = nc.dram_tensor("h2T", [D, T], F32, kind="Internal").ap()
    with ExitStack() as es:
        cx = Ctx(nc, es)
        gains = load_gains(cx, es, g_d, 96, half_cols=[(48, 64), (80, 96)])
        stage_merge(cx, h_d, aT_d, bT_d, h2_d, gains, 0, 16, wmg, wa, wb, wo, T)
        stage_ffn(cx, h2_d, x_d, gains, 32, 48, w2g, w2u, w2d, T)
        if with_next:
            stage_ffn(cx, x_d, hn_d, gains, 64, 80, w1g, w1u, w1d, T)
        cx.tk.barrier()
    return nc


N_LAUNCH_CORES = 4
GPL = 104


def build_fused_prog():
    nc = bass.Bass("TRN2", target_bir_lowering=False)
    din = lambda n, s, d=F32: nc.dram_tensor(n, s, d, kind="ExternalInput").ap()
    x_d = din("xT", [D, S])
    pos_d = din("pos", [1, S], I32)
    rb_d = din("rbT", [8, 32])
    rc_d = din("ropec", [64, 2])
    g_d = din("gains", [128, GPL * DEPTH])
    W = {}
    for nm, shp in (("f1g", [D, FF]), ("f1u", [D, FF]), ("f1d", [FF, D]), ("f2g", [D, FF]), ("f2u", [D, FF]), ("f2d", [FF, D]),
                    ("win", [D, 8664]), ("wq", [512, 1536]), ("wuk", [512, 1024]), ("wuv", [512, 1024]),
                    ("pekT", [192, 32]), ("w1k", [6144, 256]), ("w2k", [256, 192]), ("pevT", [128, 32]), ("w1v", [4096, 256]),
                    ("w2v", [256, 128]), ("wa", [1024, D]), ("wb", [1024, D]), ("wo", [D, D])):
        W[nm] = (din(nm, [DEPTH * shp[0], shp[1]]), shp[0])
    wl = lambda nm, l: W[nm][0][l * W[nm][1]:(l + 1) * W[nm][1], :]
    ov_d = din("ov1", [128, 33]); sa_d = din("scoreA", [128, 16, 32]); sbb_d = din("scoreB", [128, 16, 32])
    ex_d = din("expand", [32, 16 * 128]); sel_d = din("gsel", [12, 12 * 128]); id_d = din("ident", [128, 128])
    out_d = nc.dram_tensor("outT", [D, S], F32, kind="ExternalOutput").ap()
    di = lambda n, s, d=F32: nc.dram_tensor(n, s, d, kind="Internal")
    hA = di("hA", [D, S]).ap(); hB = di("hB", [D, S]).ap(); xb = [di("xb0", [D, S]).ap(), di("xb1", [D, S]).ap()]
    aT = di("aTi", [1024, S], BF16).ap(); bT = di("bTi", [1024, S], BF16).ap()
    z32 = di("z32", [NZ, S]).ap(); z16 = di("z16", [NZ, S], BF16).ap(); vtok = di("vtok", [S, 256], BF16).ap()
    gc_all = di("gc_all", [8, 128, GL]); gw_all = di("gw_all", [8, 128, GL])
    cos_i = di("cos_i", [64, S]).ap(); sin_i = di("sin_i", [64, S]).ap()
    T = 1024
    with ExitStack() as es:
        cx = Ctx(nc, es)
        halves = []
        for l in range(DEPTH):
            halves += [(l * GPL + 16, l * GPL + 32), (l * GPL + 80, l * GPL + 96)]
        gains = load_gains(cx, es, g_d, GPL * DEPTH, half_cols=halves)
        p0_tables(cx.tk, nc, rb_d, pos_d, rc_d,
                  [(h, bass.AP(gc_all, h * 128 * GL, [[GL, 128], [1, GL]]), bass.AP(gw_all, h * 128 * GL, [[GL, 128], [1, GL]]))
                   for h in range(8)], cos_i, sin_i)
        cur = x_d
        for l in range(DEPTH):
            gb = l * GPL
            for half in range(2):
                tsl = slice(half * T, (half + 1) * T)
                stage_ffn(cx, cur[:, tsl], hA[:, tsl], gains, gb + 0, gb + 16, wl("f1g", l), wl("f1u", l), wl("f1d", l), T)
            for hh in range(2):
                L = {"hT_d": hA, "g_d": None, "win_d": wl("win", l), "wq_d": wl("wq", l), "wuk_d": wl("wuk", l), "wuv_d": wl("wuv", l),
                     "pek_d": wl("pekT", l), "w1k_d": wl("w1k", l), "w2k_d": wl("w2k", l), "pev_d": wl("pevT", l),
                     "w1v_d": wl("w1v", l), "w2v_d": wl("w2v", l), "gc_d": gc_all, "gw_d": gw_all, "cos_d": cos_i, "sin_d": sin_i,
                     "ov_d": ov_d, "sa_d": sa_d, "sbb_d": sbb_d, "ex_d": ex_d, "sel_d": sel_d, "id_d": id_d, "aT_d": aT, "bT_d": bT,
                     "z32_d": z32, "z16_d": z16, "vtok_d": vtok, "hh": hh, "gcols": (gb + 32, gb + 96, gb + 100), "gains": gains}
                _attn_body(cx, es, nc, L)
                cx.tk.barrier()
            for half in range(2):
                tsl = slice(half * T, (half + 1) * T)
                stage_merge(cx, hA[:, tsl], aT[:, tsl], bT[:, tsl], hB[:, tsl], gains, gb + 32, gb + 48,
                            wl("win", l), wl("wa", l), wl("wb", l), wl("wo", l), T, cbase=4568)
            dst = out_d if l == DEPTH - 1 else xb[l % 2]
            for half in range(2):
                tsl = slice(half * T, (half + 1) * T)
                stage_ffn(cx, hB[:, tsl], dst[:, tsl], gains, gb + 64, gb + 80, wl("f2g", l), wl("f2u", l), wl("f2d", l), T)
            cur = dst
        cx.tk.barrier()
    return nc


_PROGS = {}


def kernel(x, positions, rel_bias,
           ffn1_pre_g, ffn1_post_g, ffn1_w_gate, ffn1_w_up, ffn1_w_down,
           mix_pre_g, mix_post_g, w_in,
           mla_q_norm_g, mla_w_q_up, mla_kv_norm_g, mla_w_uk, mla_w_uv,
           cmp_pe_k, cmp_w1_k, cmp_w2_k, cmp_pe_v, cmp_w1_v, cmp_w2_v,
           w_branch_mla, w_branch_nsa, w_out,
           ffn2_pre_g, ffn2_post_g, ffn2_w_gate, ffn2_w_up, ffn2_w_down):
    f = lambda a: np.ascontiguousarray(np.asarray(a, dtype=np.float32))
    st = lambda a: f(a).reshape(-1, np.asarray(a).shape[-1])
    x = f(x)
    positions = np.asarray(positions).astype(np.int32)
    gl = []
    for l in range(DEPTH):
        gl += [garr(ffn1_pre_g[l]), garr(ffn1_post_g[l]), garr(mix_pre_g[l]), garr(mix_post_g[l]), garr(ffn2_pre_g[l]),
               garr(ffn2_post_g[l]), garr(mla_q_norm_g[l]), garr(mla_kv_norm_g[l])]
    common = {"rbT": np.ascontiguousarray(f(rel_bias).T), "ropec": rope_consts(), "gains": np.concatenate(gl, axis=1),
              "f1g": st(ffn1_w_gate), "f1u": st(ffn1_w_up), "f1d": st(ffn1_w_down),
              "f2g": st(ffn2_w_gate), "f2u": st(ffn2_w_up), "f2d": st(ffn2_w_down),
              "win": st(w_in), "wq": st(mla_w_q_up), "wuk": st(mla_w_uk), "wuv": st(mla_w_uv),
              "pekT": np.ascontiguousarray(f(cmp_pe_k).transpose(0, 2, 1)).reshape(-1, 32), "w1k": st(cmp_w1_k), "w2k": st(cmp_w2_k),
              "pevT": np.ascontiguousarray(f(cmp_pe_v).transpose(0, 2, 1)).reshape(-1, 32), "w1v": st(cmp_w1_v), "w2v": st(cmp_w2_v),
              "wa": st(w_branch_mla), "wb": st(w_branch_nsa), "wo": st(w_out)}
    common.update(attn_consts())
    if "fused" not in _PROGS:
        _PROGS["fused"] = build_fused_prog()
    active = {0: 0, 1: 1, 2: 2, 3: 3} if N_LAUNCH_CORES == 4 else {0: 0, 1: 1, 4: 2, 5: 3}
    zeros = None
    maps = []
    for c in range(N_LAUNCH_CORES):
        if c in active:
            b = active[c]
            m = dict(common)
            m["xT"] = np.ascontiguousarray(x[b].T)
            m["pos"] = np.ascontiguousarray(positions[b][None, :])
        else:
            if zeros is None:
                zeros = {k: np.zeros_like(v) for k, v in common.items()}
                zeros["xT"] = np.zeros((D, S), np.float32)
                zeros["pos"] = np.zeros((1, S), np.int32)
            m = zeros
        maps.append(m)
    res = run_bass_kernel_spmd(_PROGS["fused"], maps, core_ids=list(range(N_LAUNCH_CORES))).results
    inv = {b: c for c, b in active.items()}
    out = np.stack([np.asarray(res[inv[b]]["outT"]).T for b in range(4)], axis=0)
    return np.ascontiguousarray(out.astype(np.float32))
```

```python
import math
import numpy as np
from contextlib import ExitStack
import concourse.bass as bass
import concourse.mybir as mybir
from concourse.bass_utils import run_bass_kernel_spmd

F32 = mybir.dt.float32
BF16 = mybir.dt.bfloat16
I32 = mybir.dt.int32
AF = mybir.ActivationFunctionType
ALU = mybir.AluOpType

D = 2048
FF = 5632
SEQ = 2048
DEPTH = 4
EPS = 1e-6
NEGM = -30000.0
GL = 4096
GOFF = 2048
DEAD = [False]
MARKS = []


def mark(tk, label):
    MARKS.append((label, tk.cnt['pe']))

_SBN = [0]


def _sbt(nc, name, shape, dt):
    _SBN[0] += 1
    return nc.sbuf_tensor("%s_u%d" % (name, _SBN[0]), shape, dt)


class Buf:
    __slots__ = ("name", "lw", "rd", "dsem")

    def __init__(self, name, dsem):
        self.name = name
        self.lw = None
        self.rd = {}
        self.dsem = dsem


class TK:
    ENG = ("pe", "act", "dve", "pool", "sp")

    def __init__(self, nc, es, n_dma_sems=48):
        self.nc = nc
        self.E = {"pe": nc.tensor, "act": nc.scalar, "dve": nc.vector,
                  "pool": nc.gpsimd, "sp": nc.sync}
        self.sem = {k: es.enter_context(nc.semaphore("s_" + k)) for k in ("pe", "act", "dve", "pool")}
        self.cnt = {k: 0 for k in self.sem}
        self.dsems = [es.enter_context(nc.semaphore("d%d" % i)) for i in range(n_dma_sems)]
        self.dtot = [0] * n_dma_sems
        self.seen = {k: {} for k in self.ENG}
        self._rr = 0
        self.nbuf = 0

    def buf(self, name=None):
        self.nbuf += 1
        b = Buf(name or ("b%d" % self.nbuf), self._rr)
        self._rr = (self._rr + 1) % len(self.dsems)
        return b

    def bufs(self, n):
        return [self.buf() for _ in range(n)]

    def _wait(self, eng, need):
        seen = self.seen[eng]
        for k2, val in need.items():
            kind, key = k2
            if kind == "dma":
                val = self.dtot[key]
            if seen.get(k2, 0) >= val:
                continue
            sem = self.sem[key] if kind == "eng" else self.dsems[key]
            self.E[eng].wait_ge(sem, val)
            seen[k2] = val

    @staticmethod
    def _add(need, ev):
        if ev is None:
            return
        k2 = (ev[0], ev[1])
        if need.get(k2, 0) < ev[2]:
            need[k2] = ev[2]

    def _deps(self, reads, writes):
        need = {}
        for b in reads:
            self._add(need, b.lw)
        for b in writes:
            self._add(need, b.lw)
            for k2, v in b.rd.items():
                if need.get(k2, 0) < v:
                    need[k2] = v
        return need

    def _record(self, ev, reads, writes):
        k2 = (ev[0], ev[1])
        for b in reads:
            if b.rd.get(k2, 0) < ev[2]:
                b.rd[k2] = ev[2]
        for b in writes:
            b.lw = ev
            b.rd = {}

    def op(self, eng, fn, reads=(), writes=(), pe_acc=False):
        if DEAD[0]:
            return None
        need = self._deps(reads, writes)
        if pe_acc:
            need.pop(("eng", "pe"), None)
        self._wait(eng, need)
        ins = fn(self.E[eng])
        self.cnt[eng] += 1
        ev = ("eng", eng, self.cnt[eng])
        ins.then_inc(self.sem[eng], 1)
        self._record(ev, reads, writes)
        return ev

    def dma(self, q, out, in_, reads=(), writes=(), join=False, anchor=None):
        if DEAD[0]:
            return None
        anchor = anchor or (list(writes) + list(reads))[0]
        si = anchor.dsem
        need = self._deps(reads, writes)
        if join:
            need.pop(("dma", si), None)
        self._wait(q, need)
        ins = self.E[q].dma_start(out=out, in_=in_)
        ins.then_inc(self.dsems[si], 16)
        self.dtot[si] += 16
        ev = ("dma", si, self.dtot[si])
        self._record(ev, reads, writes)
        return ev

    def barrier(self):
        need = {("eng", k): v for k, v in self.cnt.items() if v > 0}
        for i, v in enumerate(self.dtot):
            if v > 0:
                need[("dma", i)] = v
        for e in self.ENG:
            self._wait(e, dict(need))


class Ctx:
    def __init__(self, nc, es):
        self.nc = nc
        self.es = es
        self.tk = TK(nc, es)
        tk = self.tk
        self.ps = [es.enter_context(nc.psum_tensor("ps%d" % i, [128, 512], F32)) for i in range(8)]
        self.ps_b = tk.bufs(8)
        self.ones = es.enter_context(_sbt(nc, "ones_bf", [128, 128], BF16))
        self.ones_b = tk.buf()
        tk.op("dve", lambda e: e.memset(self.ones[:], 1.0), writes=[self.ones_b])
        self.stg = [es.enter_context(_sbt(nc, "wstg%d" % i, [128, 4096], F32)) for i in range(2)]
        self.stg_b = tk.bufs(2)
        self.slab = [es.enter_context(_sbt(nc, "wslab%d" % i, [128, 4096], BF16)) for i in range(2)]
        self.slab_b = tk.bufs(2)
        self.wi = 0
        self.cast_rr = 0

    def fetch(self, W, r0, nk, c0, M, pk=128, rstride=None):
        tk = self.tk
        assert nk * M <= 4096
        rstride = rstride or pk
        N = W.shape[1]
        j = self.wi % 2
        self.wi += 1
        stg, sb = self.stg[j], self.stg_b[j]
        src = bass.AP(W.tensor, W.offset + r0 * N + c0, [[N, pk], [rstride * N, nk], [1, M]])
        dst = stg[0:pk, 0:nk * M].rearrange("p (kc m) -> p kc m", m=M)
        step = max(1, 2048 // max(M, 1))
        first = True
        for k0 in range(0, nk, step):
            k1 = min(nk, k0 + step)
            tk.dma("sp", dst[:, k0:k1, :], src[:, k0:k1, :], writes=[sb], join=not first)
            first = False
        slab, lb = self.slab[j], self.slab_b[j]
        eng = ("pool", "dve", "act")[self.cast_rr % 3]
        self.cast_rr += 1
        if eng == "act":
            tk.op("act", lambda e: e.copy(out=slab[0:pk, 0:nk * M], in_=stg[0:pk, 0:nk * M]), reads=[sb], writes=[lb])
        else:
            tk.op(eng, lambda e: e.tensor_copy(out=slab[0:pk, 0:nk * M], in_=stg[0:pk, 0:nk * M]), reads=[sb], writes=[lb])
        return slab[0:pk, 0:nk * M].rearrange("p (kc m) -> p kc m", m=M), lb


def gemm_fm(cx, W, nk, slabs, inT, in_bufs, T, ps_ids, epi, r0=0, pend=None):
    tk = cx.tk
    if pend is None:
        pend = cx.fetch(W, r0, nk, slabs[0][0], sum(slabs[0][1]))
    ci = 0
    pi = 0
    for si, (c0, ms) in enumerate(slabs):
        nxt = cx.fetch(W, r0, nk, slabs[si + 1][0], sum(slabs[si + 1][1])) if si + 1 < len(slabs) else None
        view, wb = pend
        off = 0
        for m in ms:
            for t in range(T // 512):
                pid = ps_ids[pi % len(ps_ids)]
                pi += 1
                ps, pb = cx.ps[pid], cx.ps_b[pid]
                for k in range(nk):
                    tk.op("pe", lambda e: e.matmul(ps[0:m, :], lhsT=view[:, k, off:off + m],
                                                   rhs=inT[:, k, t * 512:(t + 1) * 512],
                                                   start=(k == 0), stop=(k == nk - 1)),
                          reads=[wb] + list(in_bufs), writes=[pb], pe_acc=(k > 0))
                epi(ci, m, t, ps, pb)
            off += m
            ci += 1
        pend = nxt


def col_slabs(c0, n, width=256):
    out = []
    c = c0
    end = c0 + n
    while c < end:
        w = min(width, end - c)
        ms = [min(128, w - i) for i in range(0, w, 128)]
        out.append((c, ms))
        c += w
    return out


def ssq_rstd(cx, es, chunk_src, nchunk, T, Kdim, rstd, rstd_b, ps_ids, pk=128):
    tk, nc = cx.tk, cx.nc
    sq = [es.enter_context(_sbt(nc, "sq%d_%d" % (i, tk.nbuf), [128, T], BF16)) for i in range(2)]
    sq_b = tk.bufs(2)
    nt = T // 512
    for k in range(nchunk):
        ap, b = chunk_src(k)
        s, sb_ = sq[k % 2], sq_b[k % 2]
        tk.op("act", lambda e: e.activation(out=s[0:pk, :], in_=ap, func=AF.Square), reads=[b], writes=[sb_])
        for t in range(nt):
            pid = ps_ids[t]
            tk.op("pe", lambda e: e.matmul(cx.ps[pid][:, :], lhsT=cx.ones[0:pk, :], rhs=s[0:pk, t * 512:(t + 1) * 512],
                                           start=(k == 0), stop=(k == nchunk - 1)),
                  reads=[sb_, cx.ones_b], writes=[cx.ps_b[pid]], pe_acc=(k > 0))
    for t in range(nt):
        pid = ps_ids[t]
        sl = slice(t * 512, (t + 1) * 512)
        tk.op("act", lambda e: e.activation(out=rstd[:, sl], in_=cx.ps[pid][:, :], func=AF.Sqrt,
                                            scale=1.0 / Kdim, bias=cx.eps_ap),
              reads=[cx.ps_b[pid]], writes=[rstd_b])
    tk.op("dve", lambda e: e.reciprocal(out=rstd[:, :], in_=rstd[:, :]), reads=[rstd_b], writes=[rstd_b])


def norm_from_dram(cx, src_d, g_sb, g_b, uT, u_bufs, T, K, ps_ids):
    tk, nc = cx.tk, cx.nc
    nk = K // 128
    with ExitStack() as es:
        NXB = 4 if T <= 1024 else 2
        xs = [es.enter_context(_sbt(nc, "nx%d_%d" % (i, tk.nbuf), [128, T], F32)) for i in range(NXB)]
        xs_b = tk.bufs(NXB)
        rstd = es.enter_context(_sbt(nc, "nrstd_%d" % tk.nbuf, [128, T], F32))
        rstd_b = tk.buf()

        def src(k):
            tk.dma("sp", xs[k % NXB][:, :], src_d[k * 128:(k + 1) * 128, :], writes=[xs_b[k % NXB]])
            return xs[k % NXB][:, :], xs_b[k % NXB]
        ssq_rstd(cx, es, src, nk, T, K, rstd, rstd_b, ps_ids)
        for k in range(nk):
            ap, b = src(k)
            tk.op("dve", lambda e: e.scalar_tensor_tensor(out=uT[:, k, :], in0=ap, scalar=g_sb[:, k:k + 1],
                                                          in1=rstd[:, :], op0=ALU.mult, op1=ALU.mult),
                  reads=[b, rstd_b, g_b], writes=[u_bufs[k]])
        tk.barrier()


def resid_tail(cx, yT, y_bufs, x_d, gf_sb, gf_b, out_d, T, ps_ids):
    tk, nc = cx.tk, cx.nc
    with ExitStack() as es:
        rstd = es.enter_context(_sbt(nc, "trstd_%d" % tk.nbuf, [128, T], F32))
        rstd_b = tk.buf()
        ssq_rstd(cx, es, lambda k: (yT[:, k, :], y_bufs[k]), 16, T, D, rstd, rstd_b, ps_ids)
        xs = [es.enter_context(_sbt(nc, "tx%d_%d" % (i, tk.nbuf), [128, T], F32)) for i in range(4)]
        xs_b = tk.bufs(4)
        for k in range(16):
            x, xb = xs[k % 4], xs_b[k % 4]
            tk.dma("sp", x[:, :], x_d[k * 128:(k + 1) * 128, :], writes=[xb])
            tk.op("dve", lambda e: e.scalar_tensor_tensor(out=yT[:, k, :], in0=yT[:, k, :], scalar=gf_sb[:, k:k + 1],
                                                          in1=rstd[:, :], op0=ALU.mult, op1=ALU.mult),
                  reads=[rstd_b, gf_b], writes=[y_bufs[k]])
            tk.op("pool" if k % 3 == 0 else "dve", lambda e: e.tensor_tensor(out=x[:, :], in0=x[:, :], in1=yT[:, k, :], op=ALU.add),
                  reads=[y_bufs[k]], writes=[xb])
            tk.dma("sp", out_d[k * 128:(k + 1) * 128, :], x[:, :], reads=[xb])
        tk.barrier()


def stage_ffn(cx, x_d, out_d, gains, gcol_pre, gcol_post, Wg, Wu, Wd, T):
    tk, nc = cx.tk, cx.nc
    mark(tk, 'ffn')
    g_sb, g_b = gains
    NG = 4
    CPG = 11
    nt = T // 512
    with ExitStack() as es:
        yT = es.enter_context(_sbt(nc, "ffn_y_%d" % tk.nbuf, [128, 16, T], F32))
        y_b = tk.bufs(16)
        uT = es.enter_context(_sbt(nc, "ffn_u_%d" % tk.nbuf, [128, 16, T], BF16))
        u_b = tk.bufs(16)
        hT = es.enter_context(_sbt(nc, "ffn_h_%d" % tk.nbuf, [128, CPG, T], BF16))
        h_b = tk.bufs(CPG)
        sg = [es.enter_context(_sbt(nc, "ffn_sg%d_%d" % (i, tk.nbuf), [128, 512], F32)) for i in range(2)]
        sg_b = tk.bufs(2)
        specs, tags = [], []
        for grp in range(NG):
            f0 = grp * CPG * 128
            for c in range(0, CPG * 128, 256):
                w = min(256, CPG * 128 - c)
                specs.append((Wg, 0, 16, f0 + c, w)); tags.append(("g", grp, c, w))
                specs.append((Wu, 0, 16, f0 + c, w)); tags.append(("u", grp, c, w))
            for (c0, ms) in col_slabs(0, D, 256):
                specs.append((Wd, f0, CPG, c0, sum(ms))); tags.append(("d", grp, c0, ms))
        pend = cx.fetch(*specs[0])
        norm_from_dram(cx, x_d, g_sb[:, gcol_pre:gcol_pre + 16], g_b, uT, u_b, T, D, [0, 1, 2, 3])
        cnt = [0]
        dpi = [0]
        for si in range(len(specs)):
            nxt = cx.fetch(*specs[si + 1]) if si + 1 < len(specs) else None
            view, wb = pend
            kind, grp, c, w = tags[si]
            if kind in ("g", "u"):
                nch = w // 128
                for ci in range(nch):
                    n_loc = c // 128 + ci
                    for t in range(nt):
                        pid = (0 if kind == "g" else 2) + t + 4 * (n_loc % 2)
                        ps, pb = cx.ps[pid], cx.ps_b[pid]
                        for k in range(16):
                            tk.op("pe", lambda e: e.matmul(ps[:, :], lhsT=view[:, k, ci * 128:(ci + 1) * 128],
                                                           rhs=uT[:, k, t * 512:(t + 1) * 512],
                                                           start=(k == 0), stop=(k == 15)),
                                  reads=[wb, u_b[k]], writes=[pb], pe_acc=(k > 0))
                        if kind == "u":
                            gid = t + 4 * (n_loc % 2)
                            s_, sb_ = sg[cnt[0] % 2], sg_b[cnt[0] % 2]
                            cnt[0] += 1
                            tk.op("act", lambda e: e.activation(out=s_[:, :], in_=cx.ps[gid][:, :], func=AF.Silu),
                                  reads=[cx.ps_b[gid]], writes=[sb_])
                            tk.op("dve", lambda e: e.tensor_tensor(out=hT[:, n_loc, t * 512:(t + 1) * 512], in0=s_[:, :],
                                                                   in1=ps[:, :], op=ALU.mult),
                                  reads=[sb_, pb], writes=[h_b[n_loc]])
            else:
                ms = w
                off = 0
                for m in ms:
                    ci = (c + off) // 128
                    for t in range(nt):
                        pid = dpi[0] % 8
                        dpi[0] += 1
                        ps, pb = cx.ps[pid], cx.ps_b[pid]
                        for k in range(CPG):
                            tk.op("pe", lambda e: e.matmul(ps[0:m, :], lhsT=view[:, k, off:off + m], rhs=hT[:, k, t * 512:(t + 1) * 512],
                                                           start=(k == 0), stop=(k == CPG - 1)),
                                  reads=[wb, h_b[k]], writes=[pb], pe_acc=(k > 0))
                        sl = slice(t * 512, (t + 1) * 512)
                        if grp == 0:
                            tk.op("act", lambda e: e.copy(out=yT[:, ci, sl], in_=ps[:, :]), reads=[pb], writes=[y_b[ci]])
                        else:
                            tk.op("dve", lambda e: e.tensor_tensor(out=yT[:, ci, sl], in0=ps[:, :], in1=yT[:, ci, sl], op=ALU.add),
                                  reads=[pb], writes=[y_b[ci]])
                    off += m
            pend = nxt
        resid_tail(cx, yT, y_b, x_d, g_sb[:, gcol_post:gcol_post + 16], g_b, out_d, T, [0, 1, 2, 3])


def load_gains(cx, es, g_d, ncol, half_cols=()):
    tk, nc = cx.tk, cx.nc
    g_sb = es.enter_context(_sbt(nc, "gains_sb", [128, ncol], F32))
    g_b = tk.buf()
    tk.dma("sp", g_sb[:, :], g_d[:, :], writes=[g_b])
    for (c0, c1) in half_cols:
        tk.op("dve", lambda e: e.tensor_scalar(out=g_sb[:, c0:c1], in0=g_sb[:, c0:c1], scalar1=0.5, scalar2=None,
                                               op0=ALU.mult), reads=[g_b], writes=[g_b])
    eps = es.enter_context(_sbt(nc, "eps_t", [128, 1], F32))
    tk.op("dve", lambda e: e.memset(eps[:], EPS), writes=[g_b])
    cx.eps_ap = eps[:, 0:1]
    return g_sb, g_b


def build_ffn_prog(T=1024):
    nc = bass.Bass("TRN2", target_bir_lowering=False)
    x_d = nc.dram_tensor("xT", [D, T], F32, kind="ExternalInput").ap()
    g_d = nc.dram_tensor("gains", [128, 32], F32, kind="ExternalInput").ap()
    Wg = nc.dram_tensor("wg", [D, FF], F32, kind="ExternalInput").ap()
    Wu = nc.dram_tensor("wu", [D, FF], F32, kind="ExternalInput").ap()
    Wd = nc.dram_tensor("wd", [FF, D], F32, kind="ExternalInput").ap()
    o_d = nc.dram_tensor("outT", [D, T], F32, kind="ExternalOutput").ap()
    with ExitStack() as es:
        cx = Ctx(nc, es)
        gains = load_gains(cx, es, g_d, 32, half_cols=[(16, 32)])
        stage_ffn(cx, x_d, o_d, gains, 0, 16, Wg, Wu, Wd, T)
        cx.tk.barrier()
    return nc


def garr(g):
    return np.ascontiguousarray(np.asarray(g, dtype=np.float32).reshape(-1, 128).T)


def rel_thresholds():
    n = np.arange(0, 256)
    large = 16 + (np.log(np.maximum(n, 1).astype(np.float32) / np.float32(16)) / np.float32(math.log(128 / 16))
                  * np.float32(16)).astype(np.int32)
    large = np.minimum(large, 31)
    bucket = np.where(n < 16, n, large)
    return [int(np.argmax(bucket >= b)) for b in range(1, 32)]


def stage_p0(tk, nc, rb_d, pos_d, rc_d, heads):
    pass


def p0_tables(tk, nc, rb_d, pos_d, rc_d, head_dsts, cos_d, sin_d):
    thr = rel_thresholds()
    with ExitStack() as es:
        sb = lambda n, s, d=F32: es.enter_context(_sbt(nc, n, s, d))
        rb = sb("rb_sb", [128, 32]); rb_b = tk.buf()
        dt = sb("dtab", [128, 32]); dt_b = tk.buf()
        dg = sb("dgrid", [128, 128]); dg_b = tk.buf()
        tk.op("pool", lambda e: e.iota(dg[:, :], [[1, 128]], base=0, channel_multiplier=0,
                                       allow_small_or_imprecise_dtypes=True), writes=[dg_b])
        band = sb("band", [128, 128]); band_b = tk.buf()
        tmp = sb("btmp", [128, 128]); tmp_b = tk.buf()
        G = sb("gtab", [128, GL]); G_b = tk.buf()
        for (hrow, gc_dst, gw_dst) in head_dsts:
            tk.dma("sp", rb[:, :], rb_d[hrow:hrow + 1, :].to_broadcast([128, 32]), writes=[rb_b])
            tk.op("dve", lambda e: e.tensor_tensor(out=dt[:, 1:32], in0=rb[:, 1:32], in1=rb[:, 0:31], op=ALU.subtract),
                  reads=[rb_b], writes=[dt_b])
            tk.op("dve", lambda e: e.tensor_scalar(out=band[:, :], in0=dg[:, :], scalar1=0.0, scalar2=rb[:, 0:1],
                                                   op0=ALU.mult, op1=ALU.add), reads=[dg_b, rb_b], writes=[band_b])
            for b in range(1, 32):
                tk.op("dve", lambda e: e.tensor_scalar(out=tmp[:, :], in0=dg[:, :], scalar1=float(thr[b - 1]),
                                                       scalar2=dt[:, b:b + 1], op0=ALU.is_ge, op1=ALU.mult),
                      reads=[dg_b, dt_b], writes=[tmp_b])
                tk.op("dve", lambda e: e.tensor_tensor(out=band[:, :], in0=band[:, :], in1=tmp[:, :], op=ALU.add),
                      reads=[tmp_b], writes=[band_b])
            for kind, dst in (("c", gc_dst), ("w", gw_dst)):
                hi = GL if kind == "c" else GOFF + 512
                tk.op("pool", lambda e: e.memset(G[:, :], NEGM), writes=[G_b])
                tk.op("dve", lambda e: e.tensor_copy(out=G[:, GOFF:GOFF + 128], in_=band[:, :]), reads=[band_b], writes=[G_b])
                tk.op("dve", lambda e: e.tensor_scalar(out=G[:, GOFF + 128:hi], in0=G[:, GOFF + 128:hi], scalar1=0.0,
                                                       scalar2=rb[:, 31:32], op0=ALU.mult, op1=ALU.add),
                      reads=[rb_b], writes=[G_b])
                tk.dma("sp", dst, G[:, :], reads=[G_b])
        rc = sb("rc_sb", [64, 2]); rc_b = tk.buf()
        tk.dma("sp", rc[:, :], rc_d[:, :], writes=[rc_b])
        pi_ = sb("pos_i", [64, SEQ], I32); pi_b = tk.buf()
        tk.dma("sp", pi_[:, :], pos_d[0:1, :].to_broadcast([64, SEQ]), writes=[pi_b])
        ang = sb("ang", [64, SEQ]); ang_b = tk.buf()
        tk.op("dve", lambda e: e.tensor_copy(out=ang[:, :], in_=pi_[:, :]), reads=[pi_b], writes=[ang_b])
        tk.op("dve", lambda e: e.tensor_scalar(out=ang[:, :], in0=ang[:, :], scalar1=rc[:, 0:1], scalar2=None,
                                               op0=ALU.mult), reads=[rc_b], writes=[ang_b])
        kf = sb("kf", [64, SEQ]); kf_b = tk.buf()
        ki = sb("ki", [64, SEQ], I32); ki_b = tk.buf()
        r = sb("rr", [64, SEQ]); r_b = tk.buf()
        res_t = sb("rope_res", [64, SEQ]); res_b = tk.buf()
        C1 = 6.28125
        C2 = 2.0 * math.pi - C1
        for which, shift, dst in (("sin", 0.0, sin_d), ("cos", math.pi / 2, cos_d)):
            tk.op("dve", lambda e: e.tensor_scalar(out=kf[:, :], in0=ang[:, :], scalar1=shift, scalar2=1.0 / (2 * math.pi),
                                                   op0=ALU.add, op1=ALU.mult), reads=[ang_b], writes=[kf_b])
            tk.op("dve", lambda e: e.tensor_copy(out=ki[:, :], in_=kf[:, :]), reads=[kf_b], writes=[ki_b])
            tk.op("dve", lambda e: e.tensor_copy(out=kf[:, :], in_=ki[:, :]), reads=[ki_b], writes=[kf_b])
            tk.op("dve", lambda e: e.scalar_tensor_tensor(out=r[:, :], in0=kf[:, :], scalar=-C1, in1=ang[:, :],
                                                          op0=ALU.mult, op1=ALU.add), reads=[kf_b, ang_b], writes=[r_b])
            tk.op("dve", lambda e: e.tensor_scalar(out=r[:, :], in0=r[:, :], scalar1=shift, scalar2=None, op0=ALU.add),
                  writes=[r_b])
            tk.op("dve", lambda e: e.scalar_tensor_tensor(out=r[:, :], in0=kf[:, :], scalar=-C2, in1=r[:, :],
                                                          op0=ALU.mult, op1=ALU.add), reads=[kf_b], writes=[r_b])
            tk.op("dve", lambda e: e.tensor_scalar(out=r[:, :], in0=r[:, :], scalar1=3.1415925, scalar2=-3.1415925,
                                                   op0=ALU.min, op1=ALU.max), writes=[r_b])
            tk.op("act", lambda e: e.activation(out=res_t[:, :], in_=r[:, :], func=AF.Sin), reads=[r_b], writes=[res_b])
            if which == "sin":
                tk.op("dve", lambda e: e.tensor_scalar(out=res_t[:, :], in0=res_t[:, :], scalar1=rc[:, 1:2], scalar2=None,
                                                       op0=ALU.mult), reads=[rc_b], writes=[res_b])
            tk.dma("sp", dst[:, :], res_t[:, :], reads=[res_b])
        tk.barrier()


def build_p0_prog():
    nc = bass.Bass("TRN2", target_bir_lowering=False)
    rb_d = nc.dram_tensor("rb", [1, 32], F32, kind="ExternalInput").ap()
    pos_d = nc.dram_tensor("pos", [1, SEQ], I32, kind="ExternalInput").ap()
    rc_d = nc.dram_tensor("ropec", [64, 2], F32, kind="ExternalInput").ap()
    gc_d = nc.dram_tensor("gc", [128, GL], F32, kind="ExternalOutput").ap()
    gw_d = nc.dram_tensor("gw", [128, GL], F32, kind="ExternalOutput").ap()
    cos_d = nc.dram_tensor("cos2", [64, SEQ], F32, kind="ExternalOutput").ap()
    sin_d = nc.dram_tensor("sins", [64, SEQ], F32, kind="ExternalOutput").ap()
    with ExitStack() as es:
        tk = TK(nc, es)
        p0_tables(tk, nc, rb_d, pos_d, rc_d, [(0, gc_d[:, :], gw_d[:, :])], cos_d, sin_d)
    return nc


def rope_consts():
    inv = (10000.0 ** (-np.arange(32, dtype=np.float32) * 2.0 / 64)).astype(np.float32)
    rc = np.zeros((64, 2), np.float32)
    rc[:, 0] = np.concatenate([inv, inv])
    rc[:, 1] = np.concatenate([-np.ones(32), np.ones(32)])
    return rc


CQ, CKV, KR, NQ, KC, VC, KS, KW, VS, VW, GT, NZ = 0, 512, 1024, 1088, 1856, 2048, 2176, 2368, 2560, 2688, 2816, 2828
DEBUG_STOP = 99
DEBUG_FLAGS = set()


class StopBuild(Exception):
    pass


def dbg(level):
    if DEBUG_STOP <= level:
        DEAD[0] = True
S = SEQ
NT = S // 512
MLA_SCALE = 192 ** -0.5
NSA_SCALE = 192 ** -0.5


def run_pipelined(items, phA, phB, depth=2):
    n = len(items)
    for i in range(n + depth):
        if i < n:
            phA(items[i])
        if i - depth >= 0:
            phB(items[i - depth])


def glob_col(c, hh):
    if hh is None:
        return c
    segs = [(CQ, 0), (CKV, 512), (KR, 1024), (NQ, 1088 + hh * 768), (KC, 2624 + hh * 192), (VC, 3008 + hh * 128),
            (KS, 3264 + hh * 192), (KW, 3904 + hh * 192), (VS, 3648 + hh * 128), (VW, 4288 + hh * 128), (GT, 4544 + hh * 12)]
    base = None
    for lo, g in segs:
        if c >= lo:
            base = (lo, g)
    return base[1] + (c - base[0])


def wq_c0(hh):
    return 0 if hh is None else hh * 768


def wkv_c0(hh):
    return 0 if hh is None else hh * 512


def orow(hh):
    return 0 if hh is None else hh * 512


def win_cols(hh):
    g = hh
    r = lambda a, n: list(range(a, a + n))
    cols = r(0, 512) + r(512, 512) + r(1024, 64) + r(1088 + g * 768, 768)
    cols += r(2624 + g * 192, 192) + r(3008 + g * 128, 128) + r(3264 + g * 192, 192) + r(3904 + g * 192, 192)
    cols += r(3648 + g * 128, 128) + r(4288 + g * 128, 128) + r(4544 + g * 12, 12)
    assert len(cols) == NZ
    return np.array(cols)


def build_attn_prog():
    nc = bass.Bass("TRN2", target_bir_lowering=False)
    din = lambda n, s, d=F32: nc.dram_tensor(n, s, d, kind="ExternalInput").ap()
    hT_d = din("hT", [D, S])
    g_d = din("gains", [128, 24])
    win_d = din("win", [D, NZ])
    wq_d = din("wqup", [512, 768])
    wuk_d = din("wuk", [512, 512])
    wuv_d = din("wuv", [512, 512])
    pek_d = din("pekT", [192, 32])
    w1k_d = din("w1k", [6144, 256])
    w2k_d = din("w2k", [256, 192])
    pev_d = din("pevT", [128, 32])
    w1v_d = din("w1v", [4096, 256])
    w2v_d = din("w2v", [256, 128])
    gc_d = nc.dram_tensor("gc", [4, 128, GL], F32, kind="ExternalInput")
    gw_d = nc.dram_tensor("gw", [4, 128, GL], F32, kind="ExternalInput")
    cos_d = din("cos2", [64, S])
    sin_d = din("sins", [64, S])
    ov_d = din("ov1", [128, 33])
    sa_d = din("scoreA", [128, 16, 32])
    sbb_d = din("scoreB", [128, 16, 32])
    ex_d = din("expand", [32, 16 * 128])
    sel_d = din("gsel", [12, 12 * 128])
    id_d = din("ident", [128, 128])
    aT_d = nc.dram_tensor("aT", [512, S], BF16, kind="ExternalOutput").ap()
    bT_d = nc.dram_tensor("bT", [512, S], BF16, kind="ExternalOutput").ap()
    z32_d = nc.dram_tensor("z32", [NZ, S], F32, kind="Internal").ap()
    z16_d = nc.dram_tensor("z16", [NZ, S], BF16, kind="Internal").ap()
    vtok_d = nc.dram_tensor("vtok", [S, 256], BF16, kind="Internal").ap()

    with ExitStack() as es0:
        cx = Ctx(nc, es0)
        tk = cx.tk
        DEAD[0] = False
        _attn_body(cx, es0, nc, locals())
        DEAD[0] = False
        tk.barrier()
    return nc


def _attn_body(cx, es0, nc, L):
    globals_ = L
    (hT_d, g_d, win_d, wq_d, wuk_d, wuv_d, pek_d, w1k_d, w2k_d, pev_d, w1v_d, w2v_d, gc_d, gw_d, cos_d, sin_d, ov_d, sa_d, sbb_d,
     ex_d, sel_d, id_d, aT_d, bT_d, z32_d, z16_d, vtok_d) = [L[k] for k in (
        'hT_d', 'g_d', 'win_d', 'wq_d', 'wuk_d', 'wuv_d', 'pek_d', 'w1k_d', 'w2k_d', 'pev_d', 'w1v_d', 'w2v_d', 'gc_d', 'gw_d',
        'cos_d', 'sin_d', 'ov_d', 'sa_d', 'sbb_d', 'ex_d', 'sel_d', 'id_d', 'aT_d', 'bT_d', 'z32_d', 'z16_d', 'vtok_d')]
    tk = cx.tk
    hh = L.get('hh', None)
    gm, gq, gkv = L.get('gcols', (0, 16, 20))
    if True:
        gains = L['gains'] if 'gains' in L else load_gains(cx, es0, g_d, 24)
        g_sb, g_b = gains
        z32_b, z16_b, vtok_b = tk.buf(), tk.buf(), tk.buf()

        mark(tk, 'attn.s1')
        with ExitStack() as es:
            uT = es.enter_context(_sbt(nc, "uT", [128, 16, S], BF16))
            u_b = tk.bufs(16)
            st32 = [es.enter_context(_sbt(nc, "st32_%d" % i, [128, 512], F32)) for i in range(2)]
            st16 = [es.enter_context(_sbt(nc, "st16_%d" % i, [128, 512], BF16)) for i in range(2)]
            st32_b, st16_b = tk.bufs(2), tk.bufs(2)
            chunks = []
            if hh != 1:
                for c in range(0, 1024, 128):
                    chunks.append((c, 128))
                chunks.append((KR, 64))
            for h in range(4):
                chunks += [(NQ + h * 192, 128), (NQ + h * 192 + 128, 64)]
            chunks += [(KC, 128), (KC + 128, 64), (VC, 128), (KS, 128), (KS + 128, 64), (KW, 128), (KW + 128, 64), (GT, 12)]
            slabs = []
            lastc = None
            for (c, m) in chunks:
                gcl = glob_col(c, hh)
                if slabs and lastc == c and slabs[-1][0] + sum(slabs[-1][1]) == gcl and sum(slabs[-1][1]) + m <= 256:
                    slabs[-1][1].append(m)
                else:
                    slabs.append((gcl, [m]))
                lastc = c + m
            pend_s1 = cx.fetch(win_d, 0, 16, slabs[0][0], sum(slabs[0][1]))
            norm_from_dram(cx, hT_d, g_sb[:, gm:gm + 16], g_b, uT, u_b, S, D, [0, 1, 2, 3])
            cnt = [0]

            def epi(ci, m, t, ps, pb):
                c0 = chunks[ci][0]
                i = cnt[0] % 2
                cnt[0] += 1
                sl = slice(t * 512, (t + 1) * 512)
                eng = "act" if cnt[0] % 2 else "dve"
                if c0 == GT:
                    tk.op("act", lambda e: e.activation(out=st32[i][0:m, :], in_=ps[0:m, :], func=AF.Sigmoid),
                          reads=[pb], writes=[st32_b[i]])
                    tk.dma("sp", z32_d[c0:c0 + m, sl], st32[i][0:m, :], reads=[st32_b[i]], writes=[z32_b], join=True, anchor=st32_b[i])
                elif c0 < NQ or KC <= c0 < KS:
                    if eng == "act":
                        tk.op("act", lambda e: e.copy(out=st32[i][0:m, :], in_=ps[0:m, :]), reads=[pb], writes=[st32_b[i]])
                    else:
                        tk.op("dve", lambda e: e.tensor_copy(out=st32[i][0:m, :], in_=ps[0:m, :]), reads=[pb], writes=[st32_b[i]])
                    tk.dma("sp", z32_d[c0:c0 + m, sl], st32[i][0:m, :], reads=[st32_b[i]], writes=[z32_b], join=True, anchor=st32_b[i])
                else:
                    if eng == "act":
                        tk.op("act", lambda e: e.copy(out=st16[i][0:m, :], in_=ps[0:m, :]), reads=[pb], writes=[st16_b[i]])
                    else:
                        tk.op("dve", lambda e: e.tensor_copy(out=st16[i][0:m, :], in_=ps[0:m, :]), reads=[pb], writes=[st16_b[i]])
                    tk.dma("sp", z16_d[c0:c0 + m, sl], st16[i][0:m, :], reads=[st16_b[i]], writes=[z16_b], join=True, anchor=st16_b[i])
            gemm_fm(cx, win_d, 16, slabs, uT, u_b, S, [0, 1, 2, 3, 4, 5, 6, 7], epi, pend=pend_s1)
            wv0, wvb0 = cx.fetch(win_d, 0, 16, glob_col(VS, hh), 128)
            wv1, wvb1 = cx.fetch(win_d, 0, 16, glob_col(VW, hh), 128)
            for tt in range(16):
                pid = tt % 4
                ps, pb = cx.ps[pid], cx.ps_b[pid]
                for (wv, wvb, co) in ((wv0, wvb0, 0), (wv1, wvb1, 128)):
                    for k in range(16):
                        tk.op("pe", lambda e: e.matmul(ps[:, co:co + 128], lhsT=uT[:, k, tt * 128:(tt + 1) * 128], rhs=wv[:, k, :],
                                                       start=(k == 0), stop=(k == 15)),
                              reads=[wvb, u_b[k]], writes=[pb], pe_acc=(k > 0 or co > 0))
                i = tt % 2
                tk.op("act", lambda e: e.copy(out=st16[i][:, 0:256], in_=ps[:, 0:256]), reads=[pb], writes=[st16_b[i]])
                tk.dma("sp", vtok_d[tt * 128:(tt + 1) * 128, :], st16[i][:, 0:256], reads=[st16_b[i]], writes=[vtok_b], join=True, anchor=st16_b[i])
            tk.barrier()

        mark(tk, 'mla.proj')
        with ExitStack() as es:
          dbg(1)
          if True:
              sbt = lambda n, s, d: es.enter_context(_sbt(nc, n, s, d))
              qa = sbt("m_qa", [128, 4, S], BF16); qa_b = tk.buf()
              qrr = sbt("m_qrr", [64, 4, S], BF16); qrr_b = tk.buf()
              ka = sbt("m_ka", [128, 4, S], BF16); ka_b = tk.buf()
              krr = sbt("m_krr", [64, S], BF16); krr_b = tk.buf()
              vt = sbt("m_v", [128, 16, 512], BF16); vt_b = tk.buf()
              cos_t = sbt("m_cos", [64, S], F32); sin_t = sbt("m_sin", [64, S], F32); rp_b = tk.buf()
              tk.dma("sp", cos_t[:, :], cos_d[:, :], writes=[rp_b])
              tk.dma("sp", sin_t[:, :], sin_d[:, :], writes=[rp_b], join=True)
              rt = [sbt("m_rt%d" % i, [64, 512], F32) for i in range(2)]
              rs = [sbt("m_rs%d" % i, [64, 512], F32) for i in range(2)]
              rt_b, rs_b = tk.bufs(2), tk.bufs(2)
              raw = [sbt("m_raw%d" % i, [64, 512], F32) for i in range(2)]; raw_b = tk.bufs(2)
              sinx = sbt("m_sinx", [64, S], F32)
              tk.op("dve", lambda e: e.tensor_scalar(out=sinx[:, :], in0=sin_t[:, :], scalar1=-1.0, scalar2=None, op0=ALU.mult),
                    reads=[rp_b], writes=[rp_b])
              rcnt = [0]

              def rope_epi(ps, pb, m_dst, dst_b, t):
                  if 'norope' in DEBUG_FLAGS:
                      tk.op("act", lambda e: e.copy(out=m_dst, in_=ps[0:64, :]), reads=[pb], writes=[dst_b])
                      return
                  i = rcnt[0] % 2
                  rcnt[0] += 1
                  sl = slice(t * 512, (t + 1) * 512)
                  tk.op("act", lambda e: e.copy(out=raw[i][:, :], in_=ps[0:64, :]), reads=[pb], writes=[raw_b[i]])
                  rope_math(raw[i][:, :], raw_b[i], i, sl, m_dst, dst_b)

              def rope_math(src, src_b, i, sl, m_dst, dst_b):
                  a, s_ = rt[i], rs[i]
                  tk.op("dve", lambda e: e.tensor_tensor(out=s_[0:32, :], in0=src[32:64, :], in1=sinx[32:64, sl], op=ALU.mult),
                        reads=[src_b, rp_b], writes=[rs_b[i]])
                  tk.op("dve", lambda e: e.tensor_tensor(out=s_[32:64, :], in0=src[0:32, :], in1=sinx[0:32, sl], op=ALU.mult),
                        reads=[src_b, rp_b], writes=[rs_b[i]])
                  tk.op("dve", lambda e: e.tensor_tensor(out=a[:, :], in0=src, in1=cos_t[:, sl], op=ALU.mult),
                        reads=[src_b, rp_b], writes=[rt_b[i]])
                  tk.op("dve", lambda e: e.tensor_tensor(out=m_dst, in0=a[:, :], in1=s_[:, :], op=ALU.add),
                        reads=[rt_b[i], rs_b[i]], writes=[dst_b])

              for which in ("q", "kv"):
                  with ExitStack() as es2:
                      cn = es2.enter_context(_sbt(nc, "m_cn" + which, [128, 4, S], BF16)); cn_b = tk.bufs(4)
                      base = CQ if which == "q" else CKV
                      gcol = gq if which == "q" else gkv
                      dbg(1.21)
                      norm_from_dram(cx, z32_d[base:base + 512, :], g_sb[:, gcol:gcol + 4], g_b, cn, cn_b, S, 512, [0, 1, 2, 3])
                      dbg(1.22)
                      if which == "q":
                          slabs = [(wq_c0(hh) + h * 192, [128, 64]) for h in range(4)]

                          def epi(ci, m, t, ps, pb):
                              h = ci // 2
                              sl = slice(t * 512, (t + 1) * 512)
                              if ci % 2 == 0:
                                  tk.op("act", lambda e: e.copy(out=qa[:, h, sl], in_=ps[:, :]), reads=[pb], writes=[qa_b])
                              else:
                                  rope_epi(ps, pb, qrr[:, h, sl], qrr_b, t)
                          gemm_fm(cx, wq_d, 4, slabs, cn, cn_b, S, [4, 5, 6, 7], epi)
                      else:
                          def epi(ci, m, t, ps, pb):
                              sl = slice(t * 512, (t + 1) * 512)
                              tk.op("act", lambda e: e.copy(out=ka[:, ci, sl], in_=ps[:, :]), reads=[pb], writes=[ka_b])
                          gemm_fm(cx, wuk_d, 4, col_slabs(wkv_c0(hh), 512, 256), cn, cn_b, S, [4, 5, 6, 7], epi)
                          wv, wvb = cx.fetch(wuv_d, 0, 4, wkv_c0(hh), 512)
                          for tt in range(16):
                              pid = 4 + tt % 4
                              ps, pb = cx.ps[pid], cx.ps_b[pid]
                              for k in range(4):
                                  tk.op("pe", lambda e: e.matmul(ps[:, :], lhsT=cn[:, k, tt * 128:(tt + 1) * 128], rhs=wv[:, k, :],
                                                                 start=(k == 0), stop=(k == 3)),
                                        reads=[wvb, cn_b[k]], writes=[pb], pe_acc=(k > 0))
                              tk.op("dve", lambda e: e.tensor_copy(out=vt[:, tt, :], in_=ps[:, :]), reads=[pb], writes=[vt_b])
                      tk.barrier()
              dbg(1.3)
              with ExitStack() as es2:
                  kr32 = es2.enter_context(_sbt(nc, "m_kr32", [64, S], F32)); kr32_b = tk.buf()
                  tk.dma("sp", kr32[:, :], z32_d[KR:KR + 64, :], reads=[z32_b], writes=[kr32_b])
                  for t in range(NT):
                      sl = slice(t * 512, (t + 1) * 512)
                      i = rcnt[0] % 2
                      rcnt[0] += 1
                      rope_math(kr32[:, sl], kr32_b, i, sl, krr[:, sl], krr_b)
                  tk.barrier()
              dbg(1.5)
              cm = sbt("m_cm", [128, 4, 512], F32); cm_b = tk.buf()
              tk.op("pool", lambda e: e.memset(cm[:, :, :], 0.0), writes=[cm_b])
              for di in range(4):
                  tk.op("pool", lambda e: e.affine_select(out=cm[:, di, :], in_=cm[:, di, :], pattern=[[1, 512]],
                                                          compare_op=ALU.is_ge, fill=NEGM, base=-128 * di,
                                                          channel_multiplier=-1), writes=[cm_b])
              dbg(1.7)
              mark(tk, 'mla.attn')
              NB = 4
              pt = [sbt("m_p%d" % i, [128, 512], BF16) for i in range(NB)]; pt_b = tk.bufs(NB)
              tm = [sbt("m_tm%d" % i, [128, 512], F32) for i in range(NB)]; tm_b = tk.bufs(NB)
              rl = sbt("m_rl", [128, 512], F32); rl_b = tk.buf()
              ot = [sbt("m_ot%d" % i, [128, 512], BF16) for i in range(2)]; ot_b = tk.bufs(2)
              items = []
              hq = 0
              for h in range(4):
                  for qt in range(NT):
                      nkt = 4 * qt + 4
                      for kt in range(nkt):
                          items.append((h, qt, kt, nkt, hq, len(items)))
                      hq += 1

              def phA(it):
                  h, qt, kt, nkt, g, n = it
                  qsl = slice(qt * 512, (qt + 1) * 512)
                  ksl = slice(kt * 128, (kt + 1) * 128)
                  i = n % NB
                  ps, pb = cx.ps[i], cx.ps_b[i]
                  tk.op("pe", lambda e: e.matmul(ps[:, :], lhsT=ka[:, h, ksl], rhs=qa[:, h, qsl], start=True, stop=False),
                        reads=[ka_b, qa_b], writes=[pb])
                  tk.op("pe", lambda e: e.matmul(ps[:, :], lhsT=krr[:, ksl], rhs=qrr[:, h, qsl], start=False, stop=True),
                        reads=[krr_b, qrr_b], writes=[pb], pe_acc=True)
                  di = kt - 4 * qt
                  if di >= 0:
                      tk.op("dve", lambda e: e.tensor_tensor(out=tm[i][:, :], in0=ps[:, :], in1=cm[:, di, :], op=ALU.add),
                            reads=[pb, cm_b], writes=[tm_b[i]])
                      tk.op("act", lambda e: e.activation(out=pt[i][:, :], in_=tm[i][:, :], func=AF.Exp, scale=MLA_SCALE),
                            reads=[tm_b[i]], writes=[pt_b[i]])
                  else:
                      tk.op("act", lambda e: e.activation(out=pt[i][:, :], in_=ps[:, :], func=AF.Exp, scale=MLA_SCALE),
                            reads=[pb], writes=[pt_b[i]])

              def phB(it):
                  h, qt, kt, nkt, g, n = it
                  qsl = slice(qt * 512, (qt + 1) * 512)
                  i = n % NB
                  O_id, L_id = 4 + (g % 2) * 2, 5 + (g % 2) * 2
                  tk.op("pe", lambda e: e.matmul(cx.ps[O_id][:, :], lhsT=vt[:, kt, h * 128:(h + 1) * 128], rhs=pt[i][:, :],
                                                 start=(kt == 0), stop=(kt == nkt - 1)),
                        reads=[vt_b, pt_b[i]], writes=[cx.ps_b[O_id]], pe_acc=(kt > 0))
                  tk.op("pe", lambda e: e.matmul(cx.ps[L_id][:, :], lhsT=cx.ones[:, :], rhs=pt[i][:, :],
                                                 start=(kt == 0), stop=(kt == nkt - 1)),
                        reads=[cx.ones_b, pt_b[i]], writes=[cx.ps_b[L_id]], pe_acc=(kt > 0))
                  if kt == nkt - 1:
                      oi = g % 2
                      tk.op("dve", lambda e: e.reciprocal(out=rl[:, :], in_=cx.ps[L_id][:, :]), reads=[cx.ps_b[L_id]], writes=[rl_b])
                      tk.op("dve", lambda e: e.tensor_tensor(out=ot[oi][:, :], in0=cx.ps[O_id][:, :], in1=rl[:, :], op=ALU.mult),
                            reads=[cx.ps_b[O_id], rl_b], writes=[ot_b[oi]])
                      tk.dma("sp", aT_d[orow(hh) + h * 128:orow(hh) + (h + 1) * 128, qsl], ot[oi][:, :], reads=[ot_b[oi]])
              run_pipelined(items, phA, phB, depth=2)
              tk.barrier()

        dbg(2)
        if True:
            nsa_stage(cx, es0, nc, gains, z32_d, z16_d, vtok_d, (z32_b, z16_b, vtok_b), pek_d, w1k_d, w2k_d, pev_d, w1v_d, w2v_d,
                  gc_d, gw_d, ov_d, sa_d, sbb_d, ex_d, sel_d, id_d, bT_d, hh)


def nsa_stage(cx, es0, nc, gains, z32_d, z16_d, vtok_d, zbufs, pek_d, w1k_d, w2k_d, pev_d, w1v_d, w2v_d,
              gc_d, gw_d, ov_d, sa_d, sbb_d, ex_d, sel_d, id_d, bT_d, hh=None):
    tk = cx.tk
    hb = 0 if hh is None else hh * 4
    with ExitStack() as es:
        sbt = lambda n, s, d: es.enter_context(_sbt(nc, n, s, d))
        kcTa = sbt("n_kcTa", [128, 128], BF16); kcTb = sbt("n_kcTb", [64, 128], BF16); vc = sbt("n_vc", [128, 128], BF16)
        kc_b = tk.buf()
        mark(tk, 'nsa.load')
        nqa = sbt("n_qa", [128, 4, S], BF16); nqb = sbt("n_qb", [64, 4, S], BF16)
        ksa = sbt("n_ksa", [128, S], BF16); ksb = sbt("n_ksb", [64, S], BF16)
        kwa = sbt("n_kwa", [128, S], BF16); kwb = sbt("n_kwb", [64, S], BF16)
        vsw = sbt("n_vsw", [128, 16, 256], BF16)
        gsig = sbt("n_gsig", [12, S], F32)
        ld_b = tk.buf()
        first = [True]

        def ld(dst, src):
            tk.dma("act", dst, src, writes=[ld_b], join=not first[0])
            first[0] = False
        for h in range(4):
            ld(nqa[:, h, :], z16_d[NQ + h * 192:NQ + h * 192 + 128, :])
            ld(nqb[:, h, :], z16_d[NQ + h * 192 + 128:NQ + h * 192 + 192, :])
        ld(ksa[:, :], z16_d[KS:KS + 128, :]); ld(ksb[:, :], z16_d[KS + 128:KS + 192, :])
        ld(kwa[:, :], z16_d[KW:KW + 128, :]); ld(kwb[:, :], z16_d[KW + 128:KW + 192, :])
        ld(vsw[:, :, :], vtok_d.rearrange("(t p) c -> p t c", p=128))
        ld(gsig[:, :], z32_d[GT:GT + 12, :])
        ov1 = sbt("n_ov1", [128, 33], BF16); expd = sbt("n_exp", [32, 16, 128], BF16)
        scA = sbt("n_scA", [128, 16, 32], F32); scB = sbt("n_scB", [128, 16, 32], F32)
        gsel = sbt("n_gsel", [12, 12, 128], F32); ident = sbt("n_ident", [128, 128], F32)
        ld(scA[:, :, :], sa_d[:, :, :]); ld(scB[:, :, :], sbb_d[:, :, :])
        ld(gsel[:, :, :], sel_d.rearrange("r (a p) -> r a p", p=128)); ld(ident[:, :], id_d[:, :])
        mark(tk, 'nsa.compress')
        with ExitStack() as es2:
            sb2 = lambda n, s, d: es2.enter_context(_sbt(nc, n, s, d))
            src32 = {"ka": sb2("c_ka", [128, S], F32), "kb": sb2("c_kb", [64, S], F32), "v": sb2("c_v", [128, S], F32)}
            src_b = tk.buf()
            tk.dma("sp", src32["ka"][:, :], z32_d[KC:KC + 128, :], writes=[src_b])
            tk.dma("sp", src32["kb"][:, :], z32_d[KC + 128:KC + 192, :], writes=[src_b], join=True)
            tk.dma("sp", src32["v"][:, :], z32_d[VC:VC + 128, :], writes=[src_b], join=True)
            pe = {"ka": sb2("c_pea", [128, 32], F32), "kb": sb2("c_peb", [64, 32], F32), "v": sb2("c_pev", [128, 32], F32)}
            pe_b = tk.buf()
            tk.dma("sp", pe["ka"][:, :], pek_d[0:128, :], writes=[pe_b])
            tk.dma("sp", pe["kb"][:, :], pek_d[128:192, :], writes=[pe_b], join=True)
            tk.dma("sp", pe["v"][:, :], pev_d[:, :], writes=[pe_b], join=True)
            zl = {"ka": sb2("c_zla", [128, 32, 127], BF16), "kb": sb2("c_zlb", [64, 32, 127], BF16),
                  "v": sb2("c_zlv", [128, 32, 127], BF16)}
            zlb = {key: tk.bufs(32) for key in ("ka", "kb", "v")}
            for key, pk in (("ka", 128), ("kb", 64), ("v", 128)):
                v3 = src32[key][:, :].rearrange("p (n s) -> p n s", s=16)
                for l in range(32):
                    a, r = l // 16, l % 16
                    tk.op("dve", lambda e: e.tensor_scalar(out=zl[key][:, l, :], in0=v3[:, a:a + 127, r],
                                                           scalar1=pe[key][:, l:l + 1], scalar2=None, op0=ALU.add),
                          reads=[src_b, pe_b], writes=[zlb[key][l]])
            hid = {"k": sb2("c_hk", [128, 2, 128], BF16), "v": sb2("c_hv", [128, 2, 128], BF16)}
            hid_b = tk.buf()
            for hc in range(2):
                sa_, sab = cx.fetch(w1k_d, 0, 32, hc * 128, 128, pk=128, rstride=192)
                sb_, sbb = cx.fetch(w1k_d, 128, 32, hc * 128, 128, pk=64, rstride=192)
                ps, pb = cx.ps[hc], cx.ps_b[hc]
                for l in range(32):
                    tk.op("pe", lambda e: e.matmul(ps[:, 0:127], lhsT=sa_[:, l, :], rhs=zl["ka"][:, l, :], start=(l == 0), stop=False),
                          reads=[sab, zlb["ka"][l]], writes=[pb], pe_acc=(l > 0))
                    tk.op("pe", lambda e: e.matmul(ps[:, 0:127], lhsT=sb_[:, l, :], rhs=zl["kb"][:, l, :], start=False, stop=(l == 31)),
                          reads=[sbb, zlb["kb"][l]], writes=[pb], pe_acc=True)
                tk.op("act", lambda e: e.activation(out=hid["k"][:, hc, 0:127], in_=ps[:, 0:127], func=AF.Silu),
                      reads=[pb], writes=[hid_b])
            for hc in range(2):
                sv_, svb = cx.fetch(w1v_d, 0, 32, hc * 128, 128, pk=128, rstride=128)
                ps, pb = cx.ps[2 + hc], cx.ps_b[2 + hc]
                for l in range(32):
                    tk.op("pe", lambda e: e.matmul(ps[:, 0:127], lhsT=sv_[:, l, :], rhs=zl["v"][:, l, :], start=(l == 0), stop=(l == 31)),
                          reads=[svb, zlb["v"][l]], writes=[pb], pe_acc=(l > 0))
                tk.op("act", lambda e: e.activation(out=hid["v"][:, hc, 0:127], in_=ps[:, 0:127], func=AF.Silu),
                      reads=[pb], writes=[hid_b])
            w2k, w2kb = cx.fetch(w2k_d, 0, 2, 0, 192)
            w2v, w2vb = cx.fetch(w2v_d, 0, 2, 0, 128)
            for hc in range(2):
                tk.op("pe", lambda e: e.matmul(cx.ps[4][:, 0:127], lhsT=w2k[:, hc, 0:128], rhs=hid["k"][:, hc, 0:127],
                                               start=(hc == 0), stop=(hc == 1)), reads=[w2kb, hid_b], writes=[cx.ps_b[4]], pe_acc=(hc > 0))
            for hc in range(2):
                tk.op("pe", lambda e: e.matmul(cx.ps[5][0:64, 0:127], lhsT=w2k[:, hc, 128:192], rhs=hid["k"][:, hc, 0:127],
                                               start=(hc == 0), stop=(hc == 1)), reads=[w2kb, hid_b], writes=[cx.ps_b[5]], pe_acc=(hc > 0))
            for hc in range(2):
                tk.op("pe", lambda e: e.matmul(cx.ps[6][0:127, 0:128], lhsT=hid["v"][:, hc, 0:127], rhs=w2v[:, hc, :],
                                               start=(hc == 0), stop=(hc == 1)), reads=[w2vb, hid_b], writes=[cx.ps_b[6]], pe_acc=(hc > 0))
            tk.op("dve", lambda e: e.tensor_copy(out=kcTa[:, 0:127], in_=cx.ps[4][:, 0:127]), reads=[cx.ps_b[4]], writes=[kc_b])
            tk.op("dve", lambda e: e.tensor_copy(out=kcTb[:, 0:127], in_=cx.ps[5][0:64, 0:127]), reads=[cx.ps_b[5]], writes=[kc_b])
            tk.op("dve", lambda e: e.tensor_copy(out=vc[0:127, :], in_=cx.ps[6][0:127, 0:128]), reads=[cx.ps_b[6]], writes=[kc_b])
            tk.barrier()

        acc = sbt("n_acc", [128, 4, S], F32); acc_b = tk.buf()
        negT = sbt("n_negT", [32, S], BF16); negT_b = tk.buf()
        with ExitStack() as es2:
            t_ov = es2.enter_context(_sbt(nc, "n_tov", [128, 33], F32))
            t_ex = es2.enter_context(_sbt(nc, "n_tex", [32, 16 * 128], F32))
            ld(t_ov[:, :], ov_d[:, :]); ld(t_ex[:, :], ex_d[:, :])
            tk.op("dve", lambda e: e.tensor_copy(out=ov1[:, :], in_=t_ov[:, :]), reads=[ld_b], writes=[ld_b])
            tk.op("dve", lambda e: e.tensor_copy(out=expd[:, :, :].rearrange("p a b -> p (a b)"), in_=t_ex[:, :]), reads=[ld_b], writes=[ld_b])
            tk.barrier()

        NBN = 3
        tm = [sbt("n_tm%d" % i, [128, 512], F32) for i in range(NBN)]; tm_b = tk.bufs(NBN)
        pt = [sbt("n_pt%d" % i, [128, 512], BF16) for i in range(NBN)]; pt_b = tk.bufs(NBN)
        rl = sbt("n_rl", [128, 512], F32); rl_b = tk.buf()
        rlg = sbt("n_rlg", [128, 512], F32); rlg_b = tk.buf()
        tmp = sbt("n_tmp", [128, 512], F32); tmp_b = tk.buf()
        cc = [0]

        def combine(br, j, qt, O_id, L_id, G_id=None):
            qsl = slice(qt * 512, (qt + 1) * 512)
            if G_id is None:
                G_id = 6 + cc[0] % 2
                cc[0] += 1
            tk.op("pe", lambda e: e.matmul(cx.ps[G_id][:, :], lhsT=gsel[:, j * 3 + br, :], rhs=gsig[:, qsl], start=True, stop=True),
                  reads=[ld_b], writes=[cx.ps_b[G_id]])
            if br == 0:
                tk.op("dve", lambda e: e.tensor_scalar(out=rl[:, :], in0=cx.ps[L_id][:, :], scalar1=1e-30, scalar2=None, op0=ALU.max),
                      reads=[cx.ps_b[L_id]], writes=[rl_b])
                tk.op("dve", lambda e: e.reciprocal(out=rl[:, :], in_=rl[:, :]), writes=[rl_b])
            else:
                tk.op("dve", lambda e: e.reciprocal(out=rl[:, :], in_=cx.ps[L_id][:, :]), reads=[cx.ps_b[L_id]], writes=[rl_b])
            tk.op("dve", lambda e: e.tensor_tensor(out=rlg[:, :], in0=cx.ps[G_id][:, :], in1=rl[:, :], op=ALU.mult),
                  reads=[cx.ps_b[G_id], rl_b], writes=[rlg_b])
            if br == 0:
                tk.op("dve", lambda e: e.tensor_tensor(out=acc[:, j, qsl], in0=cx.ps[O_id][:, :], in1=rlg[:, :], op=ALU.mult),
                      reads=[cx.ps_b[O_id], rlg_b], writes=[acc_b])
            else:
                tk.op("dve", lambda e: e.tensor_tensor(out=tmp[:, :], in0=cx.ps[O_id][:, :], in1=rlg[:, :], op=ALU.mult),
                      reads=[cx.ps_b[O_id], rlg_b], writes=[tmp_b])
                tk.op("pool", lambda e: e.tensor_tensor(out=acc[:, j, qsl], in0=acc[:, j, qsl], in1=tmp[:, :], op=ALU.add),
                      reads=[tmp_b], writes=[acc_b])

        mark(tk, 'nsa.cmp')
        with ExitStack() as es2:
            eT = es2.enter_context(_sbt(nc, "n_eT", [128, 4, S], BF16)); eT_b = tk.buf()
            with ExitStack() as es3:
                bmc = [es3.enter_context(_sbt(nc, "n_bmc%d" % i, [128, S // 2], F32)) for i in range(2)]; bmc_b = tk.bufs(2)
                eTb = [[tk.buf() for _ in range(NT)] for _ in range(4)]
                items = [(j, qt, j * NT + qt) for j in range(4) for qt in range(NT)]

                def strip(u):
                    j, qh = u // 2, u % 2
                    tk.dma("sp", bmc[u % 2][0:127, :], bass.AP(gc_d, (hb + j) * 128 * GL + 2017 + qh * (S // 2), [[GL - 16, 127], [1, S // 2]]),
                           writes=[bmc_b[u % 2]])
                strip(0)

                def cA(it):
                    j, qt, n = it
                    u = j * 2 + qt // 2
                    if qt % 2 == 0 and u + 1 < 8:
                        strip(u + 1)
                    qsl = slice(qt * 512, (qt + 1) * 512)
                    sid, i = n % 2, n % NBN
                    ps, pb = cx.ps[sid], cx.ps_b[sid]
                    tk.op("pe", lambda e: e.matmul(ps[0:127, :], lhsT=kcTa[:, 0:127], rhs=nqa[:, j, qsl], start=True, stop=False),
                          reads=[kc_b, ld_b], writes=[pb])
                    tk.op("pe", lambda e: e.matmul(ps[0:127, :], lhsT=kcTb[:, 0:127], rhs=nqb[:, j, qsl], start=False, stop=True),
                          reads=[kc_b, ld_b], writes=[pb], pe_acc=True)
                    tk.op("dve", lambda e: e.scalar_tensor_tensor(out=tm[i][0:127, :], in0=ps[0:127, :], scalar=NSA_SCALE,
                                                                  in1=bmc[u % 2][0:127, (qt % 2) * 512:(qt % 2 + 1) * 512], op0=ALU.mult, op1=ALU.add),
                          reads=[pb, bmc_b[u % 2]], writes=[tm_b[i]])
                    tk.op("act", lambda e: e.activation(out=eT[0:127, j, qsl], in_=tm[i][0:127, :], func=AF.Exp),
                          reads=[tm_b[i]], writes=[eTb[j][qt]])

                def cB(it):
                    j, qt, n = it
                    qsl = slice(qt * 512, (qt + 1) * 512)
                    O_id, L_id = 2 + n % 2, 4 + n % 2
                    tk.op("pe", lambda e: e.matmul(cx.ps[O_id][:, :], lhsT=vc[0:127, :], rhs=eT[0:127, j, qsl], start=True, stop=True),
                          reads=[kc_b, eTb[j][qt]], writes=[cx.ps_b[O_id]])
                    tk.op("pe", lambda e: e.matmul(cx.ps[L_id][:, :], lhsT=cx.ones[0:127, :], rhs=eT[0:127, j, qsl], start=True, stop=True),
                          reads=[cx.ones_b, eTb[j][qt]], writes=[cx.ps_b[L_id]])
                    combine(0, j, qt, O_id, L_id)
                run_pipelined(items, cA, cB, depth=1)
                tk.barrier()
            mark(tk, 'nsa.topk')
            NQQ = 16
            mk = lambda nm, shp: [es2.enter_context(_sbt(nc, "%s%d" % (nm, q), shp, F32)) for q in range(NQQ)]
            l4, imp, sc2, m8, thr, neg = mk("n_l4", [128, 4]), mk("n_imp", [128, 32]), mk("n_sc2", [128, 32]), mk("n_m8", [128, 16]), \
                mk("n_thr", [128, 1]), mk("n_neg", [128, 32])
            tb = tk.bufs(NQQ)
            psI = lambda qq: (cx.ps[qq // 3], cx.ps_b[qq // 3], (qq % 3) * 132)

            def s_mm(qq):
                ps, pb, c0 = psI(qq)
                for j in range(4):
                    tk.op("pe", lambda e: e.matmul(ps[:, c0 + j * 33:c0 + (j + 1) * 33], lhsT=eT[0:127, j, qq * 128:(qq + 1) * 128],
                                                   rhs=ov1[0:127, :], start=True, stop=True), reads=[eTb[j][qq // 4], ld_b], writes=[pb], pe_acc=True)

            def s_l4(qq):
                ps, pb, c0 = psI(qq)
                lv = ps[:, c0:c0 + 132].rearrange("p (j c) -> p j c", c=33)[:, :, 32]
                tk.op("dve", lambda e: e.tensor_scalar(out=l4[qq][:, :], in0=lv, scalar1=1e-30, scalar2=None, op0=ALU.max),
                      reads=[pb], writes=[tb[qq]])

            def s_rc(qq):
                tk.op("dve", lambda e: e.reciprocal(out=l4[qq][:, :], in_=l4[qq][:, :]), writes=[tb[qq]])

            def s_imp(j):
                def f(qq):
                    ps, pb, c0 = psI(qq)
                    if j == 0:
                        tk.op("dve", lambda e: e.tensor_scalar(out=imp[qq][:, :], in0=ps[:, c0:c0 + 32], scalar1=l4[qq][:, 0:1], scalar2=None,
                                                               op0=ALU.mult), reads=[pb], writes=[tb[qq]])
                    else:
                        tk.op("dve", lambda e: e.scalar_tensor_tensor(out=imp[qq][:, :], in0=ps[:, c0 + j * 33:c0 + j * 33 + 32],
                                                                      scalar=l4[qq][:, j:j + 1], in1=imp[qq][:, :], op0=ALU.mult, op1=ALU.add),
                              reads=[pb], writes=[tb[qq]])
                return f

            def s_sa(qq):
                tk.op("dve", lambda e: e.tensor_tensor(out=imp[qq][:, :], in0=imp[qq][:, :], in1=scA[:, qq, :], op=ALU.mult), reads=[ld_b], writes=[tb[qq]])

            def s_sb(qq):
                tk.op("dve", lambda e: e.tensor_tensor(out=imp[qq][:, :], in0=imp[qq][:, :], in1=scB[:, qq, :], op=ALU.add), reads=[ld_b], writes=[tb[qq]])

            def s_m1(qq):
                tk.op("dve", lambda e: e.max(out=m8[qq][:, 0:8], in_=imp[qq][:, :]), writes=[tb[qq]])

            def s_mr(qq):
                tk.op("dve", lambda e: e.match_replace(out=sc2[qq][:, :], in_to_replace=m8[qq][:, 0:8], in_values=imp[qq][:, :], imm_value=-1e30),
                      writes=[tb[qq]])

            def s_m2(qq):
                tk.op("dve", lambda e: e.max(out=m8[qq][:, 8:16], in_=sc2[qq][:, :]), writes=[tb[qq]])

            def s_th(qq):
                tk.op("dve", lambda e: e.tensor_scalar(out=thr[qq][:, :], in0=m8[qq][:, 15:16], scalar1=0.0, scalar2=None, op0=ALU.max), writes=[tb[qq]])

            def s_ng(qq):
                tk.op("dve", lambda e: e.tensor_scalar(out=neg[qq][:, :], in0=imp[qq][:, :], scalar1=thr[qq][:, 0:1], scalar2=None, op0=ALU.is_ge),
                      writes=[tb[qq]])

            def s_n2(qq):
                tk.op("dve", lambda e: e.tensor_scalar(out=neg[qq][:, :], in0=neg[qq][:, :], scalar1=-NEGM, scalar2=NEGM, op0=ALU.mult, op1=ALU.add),
                      writes=[tb[qq]])

            def s_tr(qq):
                tp, tpb = cx.ps[6 + qq % 2], cx.ps_b[6 + qq % 2]
                tk.op("pe", lambda e: e.transpose(out=tp[0:32, 0:128], in_=neg[qq][:, :], identity=ident[:, :]), reads=[tb[qq], ld_b], writes=[tpb])
                tk.op("act", lambda e: e.copy(out=negT[:, qq * 128:(qq + 1) * 128], in_=tp[0:32, 0:128]), reads=[tpb], writes=[negT_b])
            for step in (s_mm, s_l4, s_rc, s_imp(0), s_imp(1), s_imp(2), s_imp(3), s_sa, s_sb, s_m1, s_mr, s_m2, s_th, s_ng, s_n2, s_tr):
                for qq in range(NQQ):
                    step(qq)
            tk.barrier()

        for br in (1, 2):
            mark(tk, 'nsa.br%d' % br)
            with ExitStack() as es2:
                W_ = 1152 if br == 1 else 1408
                bm = [es2.enter_context(_sbt(nc, "n_bm%d_%d" % (br, i), [128, W_], F32)) for i in range(2)]
                bm_b = tk.bufs(2)
                g_tab = gc_d if br == 1 else gw_d
                ka_, kb_ = (ksa, ksb) if br == 1 else (kwa, kwb)
                voff = 0 if br == 1 else 128
                items = []
                g = 0
                for j in range(4):
                    for qt in range(NT):
                        kts = list(range(0, 4 * qt + 4)) if br == 1 else list(range(max(0, 4 * qt - 4), 4 * qt + 4))
                        for n_, kt in enumerate(kts):
                            items.append((j, qt, kt, n_, len(kts), g, len(items)))
                        g += 1

                def phA(it):
                    j, qt, kt, n_, nk_, g, n = it
                    if qt == 0 and n_ == 0:
                        tk.dma("sp", bm[j % 2][:, :], bass.AP(g_tab, (hb + j) * 128 * GL + 1664, [[GL - 1, 128], [1, W_]]), writes=[bm_b[j % 2]])
                    qsl = slice(qt * 512, (qt + 1) * 512)
                    ksl = slice(kt * 128, (kt + 1) * 128)
                    i = n % NBN
                    ps, pb = cx.ps[i], cx.ps_b[i]
                    tk.op("pe", lambda e: e.matmul(ps[:, :], lhsT=ka_[:, ksl], rhs=nqa[:, j, qsl], start=True, stop=False),
                          reads=[ld_b], writes=[pb])
                    tk.op("pe", lambda e: e.matmul(ps[:, :], lhsT=kb_[:, ksl], rhs=nqb[:, j, qsl], start=False, stop=(br == 2)),
                          reads=[ld_b], writes=[pb], pe_acc=True)
                    if br == 1:
                        tk.op("pe", lambda e: e.matmul(ps[:, :], lhsT=expd[:, kt, :], rhs=negT[:, qsl], start=False, stop=True),
                              reads=[ld_b, negT_b], writes=[pb], pe_acc=True)
                    delta = qt * 512 - kt * 128
                    off = min(delta, 256) + 384 if br == 1 else delta + 384
                    tk.op("dve", lambda e: e.scalar_tensor_tensor(out=tm[i][:, :], in0=ps[:, :], scalar=NSA_SCALE,
                                                                  in1=bm[j % 2][:, off:off + 512], op0=ALU.mult, op1=ALU.add),
                          reads=[pb, bm_b[j % 2]], writes=[tm_b[i]])
                    tk.op("act", lambda e: e.activation(out=pt[i][:, :], in_=tm[i][:, :], func=AF.Exp),
                          reads=[tm_b[i]], writes=[pt_b[i]])

                def phB(it):
                    j, qt, kt, n_, nk_, g, n = it
                    i = n % NBN
                    O_id, L_id = 3 + g % 2, 5 + g % 2
                    tk.op("pe", lambda e: e.matmul(cx.ps[O_id][:, :], lhsT=vsw[:, kt, voff:voff + 128], rhs=pt[i][:, :],
                                                   start=(n_ == 0), stop=(n_ == nk_ - 1)),
                          reads=[ld_b, pt_b[i]], writes=[cx.ps_b[O_id]], pe_acc=(n_ > 0))
                    tk.op("pe", lambda e: e.matmul(cx.ps[L_id][:, :], lhsT=cx.ones[:, :], rhs=pt[i][:, :],
                                                   start=(n_ == 0), stop=(n_ == nk_ - 1)),
                          reads=[cx.ones_b, pt_b[i]], writes=[cx.ps_b[L_id]], pe_acc=(n_ > 0))
                    if n_ == nk_ - 1:
                        combine(br, j, qt, O_id, L_id, G_id=7)
                run_pipelined(items, phA, phB, depth=2)
                tk.barrier()
        mark(tk, 'nsa.out')
        ob = [sbt("n_ob%d" % i, [128, S], BF16) for i in range(2)]; ob_b = tk.bufs(2)
        for j in range(4):
            tk.op("act", lambda e: e.copy(out=ob[j % 2][:, :], in_=acc[:, j, :]), reads=[acc_b], writes=[ob_b[j % 2]])
            tk.dma("sp", bT_d[orow(hh) + j * 128:orow(hh) + (j + 1) * 128, :], ob[j % 2][:, :], reads=[ob_b[j % 2]])
        tk.barrier()


def attn_consts():
    n_cmp, n_slc = 127, 32
    cs = np.arange(n_cmp) * 16
    ce = cs + 31
    bs = np.arange(n_slc) * 64
    be = bs + 63
    ov = ((cs[:, None] <= be[None, :]) & (ce[:, None] >= bs[None, :])).astype(np.float32)
    ov1 = np.zeros((128, 33), np.float32)
    ov1[:127, :32] = ov
    ov1[:127, 32] = 1.0
    t = np.arange(SEQ)
    cur = t // 64
    jb = np.arange(n_slc)
    valid = jb[None, :] <= cur[:, None]
    forced = valid & ((jb[None, :] == 0) | (jb[None, :] >= cur[:, None] - 1))
    A = (valid & ~forced).astype(np.float32)
    B = np.where(forced, 1e6, np.where(valid, 0.0, -1.0)).astype(np.float32)
    A = np.ascontiguousarray(A.reshape(16, 128, 32).transpose(1, 0, 2))
    B = np.ascontiguousarray(B.reshape(16, 128, 32).transpose(1, 0, 2))
    ex = np.zeros((32, 16, 128), np.float32)
    for kt in range(16):
        ex[2 * kt, kt, :64] = 1.0
        ex[2 * kt + 1, kt, 64:] = 1.0
    gsel = np.zeros((12, 12, 128), np.float32)
    for r in range(12):
        gsel[r, r, :] = 1.0
    return {"ov1": ov1, "scoreA": A, "scoreB": B, "expand": ex.reshape(32, 2048), "gsel": gsel.reshape(12, 12 * 128),
            "ident": np.eye(128, dtype=np.float32)}


def stream(cx, specs, consume, pend=None):
    if pend is None:
        pend = cx.fetch(*specs[0])
    for i in range(len(specs)):
        nxt = cx.fetch(*specs[i + 1]) if i + 1 < len(specs) else None
        consume(i, pend[0], pend[1])
        pend = nxt


def stage_merge(cx, h_d, aT_d, bT_d, out_d, gains, gcol_pre, gcol_post, wmg, wa, wb, wo, T, cbase=0):
    tk, nc = cx.tk, cx.nc
    mark(tk, 'merge')
    g_sb, g_b = gains
    nt = T // 512
    with ExitStack() as es:
        mT = es.enter_context(_sbt(nc, "mg_m", [128, 16, T], BF16)); m_b = tk.bufs(16)
        with ExitStack() as es2:
            uT = es2.enter_context(_sbt(nc, "mg_u", [128, 16, T], BF16)); u_b = tk.bufs(16)
            aS = es2.enter_context(_sbt(nc, "mg_a", [128, 8, T], BF16)); bS = es2.enter_context(_sbt(nc, "mg_b", [128, 8, T], BF16))
            ab_b = tk.buf()
            tk.dma("sp", aS[:, :, :], aT_d.rearrange("(k p) t -> p k t", p=128), writes=[ab_b])
            tk.dma("sp", bS[:, :, :], bT_d.rearrange("(k p) t -> p k t", p=128), writes=[ab_b], join=True)
            specs = []
            for o in range(16):
                specs += [(wmg, 0, 16, cbase + o * 128, 128), (wmg, 0, 16, cbase + 2048 + o * 128, 128), (wa, 0, 8, o * 128, 128), (wb, 0, 8, o * 128, 128)]
            pend0 = cx.fetch(*specs[0])
            norm_from_dram(cx, h_d, g_sb[:, gcol_pre:gcol_pre + 16], g_b, uT, u_b, T, D, [0, 1, 2, 3])
            sg = [es2.enter_context(_sbt(nc, "mg_sg%d" % i, [128, 512], F32)) for i in range(2)]; sg_b = tk.bufs(2)
            t1 = [es2.enter_context(_sbt(nc, "mg_t%d" % i, [128, 512], F32)) for i in range(2)]; t1_b = tk.bufs(2)

            def consume(i, view, wbuf):
                o, kind = i // 4, i % 4
                nk = 16 if kind < 2 else 8
                src, src_bufs = (uT, u_b) if kind < 2 else ((aS, [ab_b] * 8) if kind == 2 else (bS, [ab_b] * 8))
                for t in range(nt):
                    pid = kind * 2 + t
                    ps, pb = cx.ps[pid], cx.ps_b[pid]
                    for k in range(nk):
                        tk.op("pe", lambda e: e.matmul(ps[:, :], lhsT=view[:, k, :], rhs=src[:, k, t * 512:(t + 1) * 512],
                                                       start=(k == 0), stop=(k == nk - 1)),
                              reads=[wbuf, src_bufs[k]], writes=[pb], pe_acc=(k > 0))
                if kind == 3:
                    for t in range(nt):
                        sl = slice(t * 512, (t + 1) * 512)
                        for br in range(2):
                            gid, pid = br * 2 + t, 4 + br * 2 + t
                            tk.op("act", lambda e: e.activation(out=sg[br][:, :], in_=cx.ps[gid][:, :], func=AF.Sigmoid),
                                  reads=[cx.ps_b[gid]], writes=[sg_b[br]])
                            tk.op("dve", lambda e: e.tensor_tensor(out=t1[br][:, :], in0=cx.ps[pid][:, :], in1=sg[br][:, :], op=ALU.mult),
                                  reads=[cx.ps_b[pid], sg_b[br]], writes=[t1_b[br]])
                        tk.op("pool", lambda e: e.tensor_tensor(out=mT[:, o, sl], in0=t1[0][:, :], in1=t1[1][:, :], op=ALU.add),
                              reads=[t1_b[0], t1_b[1]], writes=[m_b[o]])
            stream(cx, specs, consume, pend=pend0)
            tk.barrier()
        yT = es.enter_context(_sbt(nc, "mg_y", [128, 16, T], F32)); y_b = tk.bufs(16)

        def epi(ci, m, t, ps, pb):
            sl = slice(t * 512, (t + 1) * 512)
            if (ci + t) % 2:
                tk.op("act", lambda e: e.copy(out=yT[:, ci, sl], in_=ps[:, :]), reads=[pb], writes=[y_b[ci]])
            else:
                tk.op("dve", lambda e: e.tensor_copy(out=yT[:, ci, sl], in_=ps[:, :]), reads=[pb], writes=[y_b[ci]])
        gemm_fm(cx, wo, 16, col_slabs(0, D, 256), mT, m_b, T, [0, 1, 2, 3, 4, 5, 6, 7], epi)
        resid_tail(cx, yT, y_b, h_d, g_sb[:, gcol_post:gcol_post + 16], g_b, out_d, T, [0, 1, 2, 3])


def build_ca_prog(with_next, T=1024):
    nc = bass.Bass("TRN2", target_bir_lowering=False)
    din = lambda n, s, d=F32: nc.dram_tensor(n, s, d, kind="ExternalInput").ap()
    h_d = din("hT", [D, T])
    aT_d = din("aT", [1024, T], BF16)
    bT_d = din("bT", [1024, T], BF16)
    g_d = din("gains", [128, 96])
    wmg = din("wmg", [D, 4096]); wa = din("wa", [1024, D]); wb = din("wb", [1024, D]); wo = din("wo", [D, D])
    w2g = din("w2g", [D, FF]); w2u = din("w2u", [D, FF]); w2d = din("w2d", [FF, D])
    if with_next:
        w1g = din("w1g", [D, FF]); w1u = din("w1u", [D, FF]); w1d = din("w1d", [FF, D])
        hn_d = nc.dram_tensor("hnT", [D, T], F32, kind="ExternalOutput").ap()
    x_d = nc.dram_tensor("xT", [D, T], F32, kind="ExternalOutput").ap()
    h2_d = nc.dram_tensor("h2T", [D, T], F32, kind="Internal").ap()
    with ExitStack() as es:
        cx = Ctx(nc, es)
        gains = load_gains(cx, es, g_d, 96, half_cols=[(48, 64), (80, 96)])
        stage_merge(cx, h_d, aT_d, bT_d, h2_d, gains, 0, 16, wmg, wa, wb, wo, T)
        stage_ffn(cx, h2_d, x_d, gains, 32, 48, w2g, w2u, w2d, T)
        if with_next:
            stage_ffn(cx, x_d, hn_d, gains, 64, 80, w1g, w1u, w1d, T)
        cx.tk.barrier()
    return nc


N_LAUNCH_CORES = 8
GPL = 104


def build_fused_prog():
    nc = bass.Bass("TRN2", target_bir_lowering=False)
    din = lambda n, s, d=F32: nc.dram_tensor(n, s, d, kind="ExternalInput").ap()
    x_d = din("xT", [D, S])
    pos_d = din("pos", [1, S], I32)
    rb_d = din("rbT", [8, 32])
    rc_d = din("ropec", [64, 2])
    g_d = din("gains", [128, GPL * DEPTH])
    W = {}
    for nm, shp in (("f1g", [D, FF]), ("f1u", [D, FF]), ("f1d", [FF, D]), ("f2g", [D, FF]), ("f2u", [D, FF]), ("f2d", [FF, D]),
                    ("win", [D, 8664]), ("wq", [512, 1536]), ("wuk", [512, 1024]), ("wuv", [512, 1024]),
                    ("pekT", [192, 32]), ("w1k", [6144, 256]), ("w2k", [256, 192]), ("pevT", [128, 32]), ("w1v", [4096, 256]),
                    ("w2v", [256, 128]), ("wa", [1024, D]), ("wb", [1024, D]), ("wo", [D, D])):
        W[nm] = (din(nm, [DEPTH * shp[0], shp[1]]), shp[0])
    wl = lambda nm, l: W[nm][0][l * W[nm][1]:(l + 1) * W[nm][1], :]
    ov_d = din("ov1", [128, 33]); sa_d = din("scoreA", [128, 16, 32]); sbb_d = din("scoreB", [128, 16, 32])
    ex_d = din("expand", [32, 16 * 128]); sel_d = din("gsel", [12, 12 * 128]); id_d = din("ident", [128, 128])
    out_d = nc.dram_tensor("outT", [D, S], F32, kind="ExternalOutput").ap()
    di = lambda n, s, d=F32: nc.dram_tensor(n, s, d, kind="Internal")
    hA = di("hA", [D, S]).ap(); hB = di("hB", [D, S]).ap(); xb = [di("xb0", [D, S]).ap(), di("xb1", [D, S]).ap()]
    aT = di("aTi", [1024, S], BF16).ap(); bT = di("bTi", [1024, S], BF16).ap()
    z32 = di("z32", [NZ, S]).ap(); z16 = di("z16", [NZ, S], BF16).ap(); vtok = di("vtok", [S, 256], BF16).ap()
    gc_all = di("gc_all", [8, 128, GL]); gw_all = di("gw_all", [8, 128, GL])
    cos_i = di("cos_i", [64, S]).ap(); sin_i = di("sin_i", [64, S]).ap()
    T = 1024
    with ExitStack() as es:
        cx = Ctx(nc, es)
        halves = []
        for l in range(DEPTH):
            halves += [(l * GPL + 16, l * GPL + 32), (l * GPL + 80, l * GPL + 96)]
        gains = load_gains(cx, es, g_d, GPL * DEPTH, half_cols=halves)
        p0_tables(cx.tk, nc, rb_d, pos_d, rc_d,
                  [(h, bass.AP(gc_all, h * 128 * GL, [[GL, 128], [1, GL]]), bass.AP(gw_all, h * 128 * GL, [[GL, 128], [1, GL]]))
                   for h in range(8)], cos_i, sin_i)
        cur = x_d
        for l in range(DEPTH):
            gb = l * GPL
            for half in range(2):
                tsl = slice(half * T, (half + 1) * T)
                stage_ffn(cx, cur[:, tsl], hA[:, tsl], gains, gb + 0, gb + 16, wl("f1g", l), wl("f1u", l), wl("f1d", l), T)
            for hh in range(2):
                L = {"hT_d": hA, "g_d": None, "win_d": wl("win", l), "wq_d": wl("wq", l), "wuk_d": wl("wuk", l), "wuv_d": wl("wuv", l),
                     "pek_d": wl("pekT", l), "w1k_d": wl("w1k", l), "w2k_d": wl("w2k", l), "pev_d": wl("pevT", l),
                     "w1v_d": wl("w1v", l), "w2v_d": wl("w2v", l), "gc_d": gc_all, "gw_d": gw_all, "cos_d": cos_i, "sin_d": sin_i,
                     "ov_d": ov_d, "sa_d": sa_d, "sbb_d": sbb_d, "ex_d": ex_d, "sel_d": sel_d, "id_d": id_d, "aT_d": aT, "bT_d": bT,
                     "z32_d": z32, "z16_d": z16, "vtok_d": vtok, "hh": hh, "gcols": (gb + 32, gb + 96, gb + 100), "gains": gains}
                _attn_body(cx, es, nc, L)
                cx.tk.barrier()
            for half in range(2):
                tsl = slice(half * T, (half + 1) * T)
                stage_merge(cx, hA[:, tsl], aT[:, tsl], bT[:, tsl], hB[:, tsl], gains, gb + 32, gb + 48,
                            wl("win", l), wl("wa", l), wl("wb", l), wl("wo", l), T, cbase=4568)
            dst = out_d if l == DEPTH - 1 else xb[l % 2]
            for half in range(2):
                tsl = slice(half * T, (half + 1) * T)
                stage_ffn(cx, hB[:, tsl], dst[:, tsl], gains, gb + 64, gb + 80, wl("f2g", l), wl("f2u", l), wl("f2d", l), T)
            cur = dst
        cx.tk.barrier()
    return nc


_PROGS = {}


def kernel(x, positions, rel_bias,
           ffn1_pre_g, ffn1_post_g, ffn1_w_gate, ffn1_w_up, ffn1_w_down,
           mix_pre_g, mix_post_g, w_in,
           mla_q_norm_g, mla_w_q_up, mla_kv_norm_g, mla_w_uk, mla_w_uv,
           cmp_pe_k, cmp_w1_k, cmp_w2_k, cmp_pe_v, cmp_w1_v, cmp_w2_v,
           w_branch_mla, w_branch_nsa, w_out,
           ffn2_pre_g, ffn2_post_g, ffn2_w_gate, ffn2_w_up, ffn2_w_down):
    f = lambda a: np.ascontiguousarray(np.asarray(a, dtype=np.float32))
    st = lambda a: f(a).reshape(-1, np.asarray(a).shape[-1])
    x = f(x)
    positions = np.asarray(positions).astype(np.int32)
    gl = []
    for l in range(DEPTH):
        gl += [garr(ffn1_pre_g[l]), garr(ffn1_post_g[l]), garr(mix_pre_g[l]), garr(mix_post_g[l]), garr(ffn2_pre_g[l]),
               garr(ffn2_post_g[l]), garr(mla_q_norm_g[l]), garr(mla_kv_norm_g[l])]
    common = {"rbT": np.ascontiguousarray(f(rel_bias).T), "ropec": rope_consts(), "gains": np.concatenate(gl, axis=1),
              "f1g": st(ffn1_w_gate), "f1u": st(ffn1_w_up), "f1d": st(ffn1_w_down),
              "f2g": st(ffn2_w_gate), "f2u": st(ffn2_w_up), "f2d": st(ffn2_w_down),
              "win": st(w_in), "wq": st(mla_w_q_up), "wuk": st(mla_w_uk), "wuv": st(mla_w_uv),
              "pekT": np.ascontiguousarray(f(cmp_pe_k).transpose(0, 2, 1)).reshape(-1, 32), "w1k": st(cmp_w1_k), "w2k": st(cmp_w2_k),
              "pevT": np.ascontiguousarray(f(cmp_pe_v).transpose(0, 2, 1)).reshape(-1, 32), "w1v": st(cmp_w1_v), "w2v": st(cmp_w2_v),
              "wa": st(w_branch_mla), "wb": st(w_branch_nsa), "wo": st(w_out)}
    common.update(attn_consts())
    if "fused" not in _PROGS:
        _PROGS["fused"] = build_fused_prog()
    active = {0: 0, 1: 1, 2: 2, 3: 3} if N_LAUNCH_CORES == 4 else {0: 0, 1: 1, 4: 2, 5: 3}
    zeros = None
    maps = []
    for c in range(N_LAUNCH_CORES):
        if c in active:
            b = active[c]
            m = dict(common)
            m["xT"] = np.ascontiguousarray(x[b].T)
            m["pos"] = np.ascontiguousarray(positions[b][None, :])
        else:
            if zeros is None:
                zeros = {k: np.zeros_like(v) for k, v in common.items()}
                zeros["xT"] = np.zeros((D, S), np.float32)
                zeros["pos"] = np.zeros((1, S), np.int32)
            m = zeros
        maps.append(m)
    res = run_bass_kernel_spmd(_PROGS["fused"], maps, core_ids=list(range(N_LAUNCH_CORES))).results
    inv = {b: c for c, b in active.items()}
    out = np.stack([np.asarray(res[inv[b]]["outT"]).T for b in range(4)], axis=0)
    return np.ascontiguousarray(out.astype(np.float32))
```
